# Optimizing a Trainium2 kernel written in Bass

```python
import jax, jax.numpy as jnp
from jax import lax
import numpy as np

D_MODEL = 1024
BATCH = 4
SEQ = 4096
DEPTH = 1

EPS = 1e-6
ROPE_THETA = 10000.0
BLOCK = 128
NEG = -1e30

SWA_HEADS = 8
SWA_KV_HEADS = 2
SWA_HEAD_DIM = D_MODEL // 16
SWA_WINDOW = 128
SWA_WIDTH = SWA_HEADS * SWA_HEAD_DIM

MLA_HEADS = 8
MLA_NOPE_DIM = D_MODEL // 16
MLA_ROPE_DIM = D_MODEL // 32
MLA_V_DIM = D_MODEL // 16
MLA_Q_RANK = 3 * D_MODEL // 16
MLA_KV_RANK = D_MODEL // 8
MLA_WIDTH = MLA_HEADS * MLA_V_DIM

MIX_WIDTH = SWA_WIDTH + MLA_WIDTH
IN_SIZES = (SWA_WIDTH, SWA_KV_HEADS * SWA_HEAD_DIM, SWA_KV_HEADS * SWA_HEAD_DIM,
            MLA_Q_RANK, MLA_KV_RANK, MLA_ROPE_DIM)
IN_WIDTH = sum(IN_SIZES)
IN_SPLITS = [int(v) for v in np.cumsum(IN_SIZES)[:-1]]

N_GROUPS = 4
EXPERTS_PER_GROUP = 8
N_EXPERTS = N_GROUPS * EXPERTS_PER_GROUP
TOP_K = 2
D_EXPERT = D_MODEL // 4

N_MOD = 6

kernel_name = "hymba_swa_mla_hiermoe_adaln_encoder"


def rmsnorm(x, g):
    xf = x.astype(jnp.float32)
    y = xf * lax.rsqrt(jnp.mean(xf * xf, axis=-1, keepdims=True) + EPS)
    return (y * g.astype(jnp.float32)).astype(x.dtype)


def rope_tables(seq, dim):
    inv = 1.0 / (ROPE_THETA ** (jnp.arange(0, dim, 2, dtype=jnp.float32) / dim))
    ang = jnp.arange(seq, dtype=jnp.float32)[:, None] * inv[None, :]
    return jnp.cos(ang), jnp.sin(ang)


def apply_rope(x, cos, sin):
    half = x.shape[-1] // 2
    xf = x.astype(jnp.float32)
    x1, x2 = xf[..., :half], xf[..., half:]
    return jnp.concatenate([x1 * cos - x2 * sin, x2 * cos + x1 * sin], axis=-1).astype(x.dtype)


def windowed_gqa(q, k, v, sink, cos, sin):
    B, S = q.shape[0], q.shape[1]
    nb = S // BLOCK
    G = SWA_HEADS // SWA_KV_HEADS
    q = apply_rope(q, cos[:, None, :], sin[:, None, :])
    k = apply_rope(k, cos[:, None, :], sin[:, None, :])
    qb = q.reshape(B, nb, BLOCK, SWA_KV_HEADS, G, SWA_HEAD_DIM)
    pad = ((0, 0), (BLOCK, BLOCK), (0, 0), (0, 0))
    kp = jnp.pad(k, pad).reshape(B, nb + 2, BLOCK, SWA_KV_HEADS, SWA_HEAD_DIM)
    vp = jnp.pad(v, pad).reshape(B, nb + 2, BLOCK, SWA_KV_HEADS, SWA_HEAD_DIM)
    kb = jnp.concatenate([kp[:, :-2], kp[:, 1:-1], kp[:, 2:]], axis=2)
    vb = jnp.concatenate([vp[:, :-2], vp[:, 1:-1], vp[:, 2:]], axis=2)
    r = jnp.arange(BLOCK)[:, None]
    j = jnp.arange(3 * BLOCK)[None, :]
    in_win = jnp.abs(j - BLOCK - r) <= SWA_WINDOW
    kpos = jnp.arange(nb)[:, None] * BLOCK - BLOCK + jnp.arange(3 * BLOCK)[None, :]
    valid = (kpos >= 0) & (kpos < S)
    mask = in_win[None, :, :] & valid[:, None, :]
    scale = SWA_HEAD_DIM ** -0.5
    s = jnp.einsum('bnqkgd,bnjkd->bnkgqj', qb, kb).astype(jnp.float32) * scale
    s = jnp.where(mask[None, :, None, None, :, :], s, NEG)
    sink_f = sink.astype(jnp.float32).reshape(SWA_KV_HEADS, G)[None, None, :, :, None]
    m = jnp.maximum(jnp.max(s, axis=-1), sink_f)
    p = jnp.exp(s - m[..., None])
    denom = jnp.sum(p, axis=-1) + jnp.exp(sink_f - m)
    p = (p / denom[..., None]).astype(v.dtype)
    o = jnp.einsum('bnkgqj,bnjkd->bnqkgd', p, vb)
    return o.reshape(B, S, SWA_HEADS * SWA_HEAD_DIM)


def latent_attention(c_q, c_kv, k_rope, g_q, w_uq, g_kv, w_ukv, cos, sin):
    B, S = c_q.shape[0], c_q.shape[1]
    nb = S // BLOCK
    q = (rmsnorm(c_q, g_q) @ w_uq).reshape(B, S, MLA_HEADS, MLA_NOPE_DIM + MLA_ROPE_DIM)
    qn, qr = q[..., :MLA_NOPE_DIM], q[..., MLA_NOPE_DIM:]
    qr = apply_rope(qr, cos[:, None, :], sin[:, None, :])
    kr = apply_rope(k_rope, cos, sin)
    kv = (rmsnorm(c_kv, g_kv) @ w_ukv).reshape(B, S, MLA_HEADS, MLA_NOPE_DIM + MLA_V_DIM)
    kn, v = kv[..., :MLA_NOPE_DIM], kv[..., MLA_NOPE_DIM:]
    scale = (MLA_NOPE_DIM + MLA_ROPE_DIM) ** -0.5
    qn_b = qn.reshape(B, nb, BLOCK, MLA_HEADS, MLA_NOPE_DIM).transpose(1, 0, 2, 3, 4)
    qr_b = qr.reshape(B, nb, BLOCK, MLA_HEADS, MLA_ROPE_DIM).transpose(1, 0, 2, 3, 4)

    def attend(blk):
        qn_i, qr_i = blk
        s = (jnp.einsum('bqhd,bkhd->bhqk', qn_i, kn)
             + jnp.einsum('bqhr,bkr->bhqk', qr_i, kr)).astype(jnp.float32) * scale
        p = jax.nn.softmax(s, axis=-1).astype(v.dtype)
        return jnp.einsum('bhqk,bkhd->bqhd', p, v)

    o = lax.map(attend, (qn_b, qr_b))
    return o.transpose(1, 0, 2, 3, 4).reshape(B, S, MLA_HEADS * MLA_V_DIM)


def hierarchical_moe(h, w_rg, b_rg, w_re, b_re, w_gate, w_up, w_down):
    B, S, Dm = h.shape
    t = h.reshape(B * S, Dm)
    g_prob = jax.nn.softmax((t @ w_rg + b_rg).astype(jnp.float32), axis=-1)
    g_w, g_idx = lax.top_k(g_prob, 1)
    e_logit = (t @ w_re + b_re).astype(jnp.float32).reshape(-1, N_GROUPS, EXPERTS_PER_GROUP)
    g_onehot = jax.nn.one_hot(g_idx[:, 0], N_GROUPS, dtype=jnp.float32)
    e_logit = jnp.einsum('tge,tg->te', e_logit, g_onehot)
    e_prob = jax.nn.softmax(e_logit, axis=-1)
    e_w, e_idx = lax.top_k(e_prob, TOP_K)
    e_w = e_w / jnp.sum(e_w, axis=-1, keepdims=True)
    weights = g_w * e_w
    expert_id = g_idx * EXPERTS_PER_GROUP + e_idx
    combine = jnp.sum(jax.nn.one_hot(expert_id, N_EXPERTS, dtype=jnp.float32)
                      * weights[..., None], axis=1)
    hidden = (jax.nn.silu(jnp.einsum('td,edf->tef', t, w_gate))
              * jnp.einsum('td,edf->tef', t, w_up) * combine[..., None].astype(t.dtype))
    out = jnp.einsum('tef,efd->td', hidden, w_down)
    return out.reshape(B, S, Dm)


def setup_inputs(seed: int = 0) -> dict:
    key = jax.random.key(seed)
    ks = jax.random.split(key, 24)
    f32 = jnp.float32
    L = DEPTH

    def nrm(k, shape, fan_in, mult=1.0):
        return jax.random.normal(k, shape, f32) * (mult * fan_in ** -0.5)

    def gain(k, shape):
        return 1.0 + 0.02 * jax.random.normal(k, shape, f32)

    return {
        "x": jax.random.normal(ks[0], (BATCH, SEQ, D_MODEL), f32),
        "c": jax.random.normal(ks[1], (BATCH, D_MODEL), f32),
        "w_ada": nrm(ks[2], (L, D_MODEL, N_MOD * D_MODEL), D_MODEL, 0.5),
        "b_ada": 0.01 * jax.random.normal(ks[3], (L, N_MOD * D_MODEL), f32),
        "g_norm1": gain(ks[4], (L, D_MODEL)),
        "w_in": nrm(ks[5], (L, D_MODEL, IN_WIDTH), D_MODEL),
        "g_q_lora": gain(ks[6], (L, MLA_Q_RANK)),
        "w_uq": nrm(ks[7], (L, MLA_Q_RANK, MLA_HEADS * (MLA_NOPE_DIM + MLA_ROPE_DIM)), MLA_Q_RANK),
        "g_kv_lora": gain(ks[8], (L, MLA_KV_RANK)),
        "w_ukv": nrm(ks[9], (L, MLA_KV_RANK, MLA_HEADS * (MLA_NOPE_DIM + MLA_V_DIM)), MLA_KV_RANK),
        "sink": jax.random.normal(ks[10], (L, SWA_HEADS), f32),
        "g_out_swa": gain(ks[11], (L, SWA_WIDTH)),
        "g_out_mla": gain(ks[12], (L, MLA_WIDTH)),
        "w_out": nrm(ks[13], (L, MIX_WIDTH, D_MODEL), MIX_WIDTH),
        "g_norm2": gain(ks[14], (L, D_MODEL)),
        "w_router_group": nrm(ks[15], (L, D_MODEL, N_GROUPS), D_MODEL),
        "b_router_group": 0.01 * jax.random.normal(ks[16], (L, N_GROUPS), f32),
        "w_router_expert": nrm(ks[17], (L, D_MODEL, N_EXPERTS), D_MODEL),
        "b_router_expert": 0.01 * jax.random.normal(ks[18], (L, N_EXPERTS), f32),
        "w_exp_gate": nrm(ks[19], (L, N_EXPERTS, D_MODEL, D_EXPERT), D_MODEL),
        "w_exp_up": nrm(ks[20], (L, N_EXPERTS, D_MODEL, D_EXPERT), D_MODEL),
        "w_exp_down": nrm(ks[21], (L, N_EXPERTS, D_EXPERT, D_MODEL), D_EXPERT),
        "g_final": gain(ks[22], (D_MODEL,)),
    }


def reference(x, c, w_ada, b_ada, g_norm1, w_in, g_q_lora, w_uq, g_kv_lora, w_ukv, sink,
              g_out_swa, g_out_mla, w_out, g_norm2, w_router_group, b_router_group,
              w_router_expert, b_router_expert, w_exp_gate, w_exp_up, w_exp_down, g_final):
    B, S, _ = x.shape
    cos_a, sin_a = rope_tables(S, SWA_HEAD_DIM)
    cos_b, sin_b = rope_tables(S, MLA_ROPE_DIM)
    for l in range(DEPTH):
        mod = jax.nn.silu(c) @ w_ada[l] + b_ada[l]
        sh1, sc1, gt1, sh2, sc2, gt2 = [m[:, None, :] for m in jnp.split(mod, N_MOD, axis=-1)]

        h = rmsnorm(x, g_norm1[l]) * (1.0 + sc1) + sh1
        proj = h @ w_in[l]
        qa, ka, va, cq, ckv, kr = jnp.split(proj, IN_SPLITS, axis=-1)
        qa = qa.reshape(B, S, SWA_HEADS, SWA_HEAD_DIM)
        ka = ka.reshape(B, S, SWA_KV_HEADS, SWA_HEAD_DIM)
        va = va.reshape(B, S, SWA_KV_HEADS, SWA_HEAD_DIM)
        o_a = windowed_gqa(qa, ka, va, sink[l], cos_a, sin_a)
        o_b = latent_attention(cq, ckv, kr, g_q_lora[l], w_uq[l], g_kv_lora[l], w_ukv[l],
                               cos_b, sin_b)
        mix = jnp.concatenate([rmsnorm(o_a, g_out_swa[l]), rmsnorm(o_b, g_out_mla[l])], axis=-1)
        x = x + gt1 * (mix @ w_out[l])

        h = rmsnorm(x, g_norm2[l]) * (1.0 + sc2) + sh2
        x = x + gt2 * hierarchical_moe(h, w_router_group[l], b_router_group[l],
                                       w_router_expert[l], b_router_expert[l],
                                       w_exp_gate[l], w_exp_up[l], w_exp_down[l])
    return rmsnorm(x, g_final)
```

```python
import numpy as np
from contextlib import ExitStack

import concourse.bass as bass
import concourse.mybir as mybir
from concourse.bass_utils import run_bass_kernel_spmd

F32 = mybir.dt.float32
BF16 = mybir.dt.bfloat16
U8 = mybir.dt.uint8
U32 = mybir.dt.uint32
AF = mybir.ActivationFunctionType
ALU = mybir.AluOpType
AX = mybir.AxisListType

D = 1024
S = 4096
NT = 32
NO = 16
EPS = 1e-6
NE = 32
KIB = 1024


class Region:
    __slots__ = ("name", "lw", "rd")

    def __init__(self, name):
        self.name = name
        self.lw = None
        self.rd = []


class Eng:
    def __init__(self, K, name, h):
        self.name = name
        self.h = h
        self.sem = K.es.enter_context(K.nc.semaphore("tl_" + name))
        self.cnt = 0
        self.waited = {}


class DSem:
    def __init__(self, K, name):
        self.sem = K.es.enter_context(K.nc.semaphore("d_" + name))
        self.cnt = 0


class KB:
    def __init__(self, nc, es):
        self.nc = nc
        self.es = es
        self.pe = Eng(self, "pe", nc.tensor)
        self.dve = Eng(self, "dve", nc.vector)
        self.act = Eng(self, "act", nc.scalar)
        self.pool = Eng(self, "pool", nc.gpsimd)
        self.sp = Eng(self, "sp", nc.sync)
        self.engs = [self.pe, self.dve, self.act, self.pool, self.sp]
        self.dsems = []

    def dsem(self, name):
        d = DSem(self, name)
        self.dsems.append(d)
        return d

    def _waits(self, eng, reads, writes):
        need = {}

        def add(t):
            if t is None:
                return
            s, v = t
            k = id(s)
            if k not in need or need[k][1] < v:
                need[k] = (s, v)

        for r in reads:
            add(r.lw)
        for w in writes:
            add(w.lw)
            for t in w.rd:
                add(t)
        for k, (s, v) in need.items():
            if eng.waited.get(k, 0) >= v:
                continue
            if eng.name == "pe" and s is eng.sem:
                continue
            eng.h.wait_ge(s, v)
            eng.waited[k] = v

    def _commit(self, ticket, reads, writes):
        for r in reads:
            r.rd.append(ticket)
            if len(r.rd) > 48:
                best = {}
                for (s, v) in r.rd:
                    if id(s) not in best or best[id(s)][1] < v:
                        best[id(s)] = (s, v)
                r.rd = list(best.values())
        for w in writes:
            w.lw = ticket
            w.rd = []

    def op(self, eng, fn, reads=(), writes=()):
        self._waits(eng, reads, writes)
        ins = fn()
        eng.cnt += 1
        ins.then_inc(eng.sem, 1)
        self._commit((eng.sem, eng.cnt), reads, writes)
        return ins

    def dma(self, q, pairs, dsem, reads=(), writes=(), **kw):
        self._waits(q, reads, writes)
        for (o, i) in pairs:
            q.h.dma_start(out=o, in_=i, **kw).then_inc(dsem.sem, 16)
            dsem.cnt += 16
        self._commit((dsem.sem, dsem.cnt), reads, writes)

    def barrier(self):
        for e in self.engs:
            for o in self.engs:
                if o is e or o.cnt == 0:
                    continue
                if e.waited.get(id(o.sem), 0) < o.cnt:
                    e.h.wait_ge(o.sem, o.cnt)
                    e.waited[id(o.sem)] = o.cnt
            for d in self.dsems:
                if d.cnt and e.waited.get(id(d.sem), 0) < d.cnt:
                    e.h.wait_ge(d.sem, d.cnt)
                    e.waited[id(d.sem)] = d.cnt


_DT_SIZE = {F32: 4, BF16: 2, U32: 4}


def build_program():
    nc = bass.Bass("TRN2", target_bir_lowering=False)
    es = ExitStack()
    K = KB(nc, es)
    V, A, P, G = nc.vector, nc.scalar, nc.gpsimd, nc.tensor

    def din(name, shape):
        return nc.dram_tensor(name, shape, F32, kind="ExternalInput").ap()

    x_d = din("x", [S, D])
    prm_d = din("prm", [128, 64])
    g1p_d = din("g1p", [128, 8])
    gB_d = din("gB", [128, 3 * D])
    rope_d = din("rope", [S, 192])
    msk_d = din("msk", [128, 512])
    wada_d = din("w_ada", [D, 6 * D])
    bada_d = din("b_ada", [1, 6 * D])
    win_d = din("w_in", [D, 1120])
    wuq_d = din("w_uq", [192, 768])
    wukv_d = din("w_ukv", [128, 1024])
    wout_d = din("w_out", [D, D])
    wr_d = din("w_r", [D, 36])
    wg_d = din("w_g", [NE * 128, 2048])
    wu_d = din("w_u", [NE * 128, 2048])
    wd_d = din("w_d", [NE * 128, 2048])
    y_d = nc.dram_tensor("y", [NO * 128, D], F32, kind="ExternalOutput").ap()
    mod_d = nc.dram_tensor("mod_scratch", [1, 6 * D], F32, kind="Internal").ap()
    NTL = 48
    xs_d = nc.dram_tensor("xs_scratch", [NTL * 256, D], BF16, kind="Internal").ap()
    ys_d = nc.dram_tensor("ys_scratch", [NTL * 256, D], F32, kind="Internal").ap()
    wo_d = nc.dram_tensor("wo_scratch", [D, D], BF16, kind="Internal").ap()
    wbf_d = nc.dram_tensor("wbf_all", [NE * 128, 3 * 2048], BF16, kind="Internal").ap()

    arena = nc.alloc_sbuf_tensor("arena", [128, 206 * KIB], U8)
    ps = nc.alloc_psum_tensor("ps", [128, 4096], F32)
    LIM = 206 * KIB

    def view(off, shape, dt, p0=0):
        off = int(off)
        n = 1
        for s_ in shape[1:]:
            n *= s_
        assert off + n * _DT_SIZE[dt] <= LIM, (off, shape)
        ap = arena[p0:p0 + shape[0], off:off + n * _DT_SIZE[dt]].bitcast(dt)
        if len(shape) == 3:
            ap = ap.rearrange("p (a b) -> p a b", a=shape[1])
        elif len(shape) == 4:
            ap = ap.rearrange("p (a b c) -> p a b c", a=shape[1], b=shape[2])
        return ap

    class Alloc:
        def __init__(self, start, end):
            self.o = start
            self.end = end

        def __call__(self, shape, dt, p0=0):
            n = 1
            for s_ in shape[1:]:
                n *= s_
            nb = (n * _DT_SIZE[dt] + 31) // 32 * 32
            v = view(self.o, shape, dt, p0)
            self.o += nb
            assert self.o <= self.end, (self.o, self.end, shape)
            return v

    def bank(b, n=1):
        return ps[:, b * 512:(b + n) * 512]

    def bankb(b):
        return ps[:, b * 512:(b + 1) * 512].bitcast(BF16)

    def bc(ap, shape):
        return ap.to_broadcast(shape)

    def modB(i):
        return mod_d[0, i * D:(i + 1) * D].partition_broadcast(128)

    Rg = {}

    def R(*key):
        if key not in Rg:
            Rg[key] = Region(str(key))
        return Rg[key]

    def RL(name, idxs):
        return [R(name, i) for i in idxs]

    def h3(ap, h):
        return ap.rearrange("p (h d) -> p h d", h=h)

    prm = view(0, [128, 64], F32)
    ident = view(256, [128, 128], BF16)
    ones_b = view(512, [128, 4], BF16)
    sT = view(528, [128, 8], F32)
    eps_t = view(560, [128, 1], F32)
    es8 = view(576, [1, 8], F32)
    st = view(640, [128, 160], F32)
    vsink = view(1280, [1, 96], BF16)
    g1P = view(1472, [128, 8], F32)
    a1P = view(1504, [128, 8], F32)
    sh1P = view(1536, [128, 8], F32)
    sh1Pb = view(1568, [128, 8], BF16)
    onesrow = view(1600, [1, 128], BF16)
    L2 = 2 * KIB
    L3 = 26 * KIB
    L4 = 86 * KIB
    PA = 118 * KIB

    dPrm = K.dsem("prm")
    K.dma(K.sp, [(prm, prm_d[:, :])], dPrm, writes=[R("prm")])
    K.op(K.pool, lambda: P.memset(ident, 1.0), writes=[R("ident")])
    K.op(K.pool, lambda: P.affine_select(out=ident, in_=ident, pattern=[[-1, 128]],
                                         compare_op=ALU.is_equal, fill=0.0, base=0,
                                         channel_multiplier=1),
         reads=[R("ident")], writes=[R("ident")])
    K.op(K.dve, lambda: V.memset(ones_b, 1.0), writes=[R("ones")])
    K.op(K.dve, lambda: V.memset(st, 0.0), writes=[R("st")])
    K.op(K.dve, lambda: V.memset(eps_t, EPS), writes=[R("eps")])
    K.op(K.dve, lambda: V.memset(vsink[0:1, 0:64], 0.0), writes=[R("vsink")])
    K.op(K.dve, lambda: V.memset(vsink[0:1, 64:96], 1.0), writes=[R("vsink")])
    K.op(K.act, lambda: A.activation(out=sT, in_=prm[:, 0:8], func=AF.Silu),
         reads=[R("prm")], writes=[R("sT")])

    def rstd_cols(src, dst, n_inv, rd, wr):
        K.op(K.act, lambda: A.activation(out=dst, in_=src, func=AF.Ln, scale=n_inv, bias=eps_t[:, 0:1]),
             reads=list(rd) + [R("eps")], writes=list(wr))
        K.op(K.act, lambda: A.activation(out=dst, in_=dst, func=AF.Exp, scale=-0.5),
             reads=list(wr), writes=list(wr))

    al = Alloc(4 * KIB, LIM)
    wa = [al([128, 8, 512], F32) for i in range(3)]
    bada = al([1, 6 * D], F32)
    modrow = al([1, 6 * D], F32)
    dWa = [K.dsem("wa%d" % i) for i in range(3)]
    dWin = K.dsem("win")
    win0 = view(PA, [128, 8, 1120], BF16)
    K.dma(K.pool, [(win0, win_d.rearrange("(kc p) n -> p kc n", p=128))], dWin, writes=[R("win")])
    dBa = K.dsem("bada")
    wada_v = wada_d.rearrange("(kc p) n -> p kc n", p=128)
    K.dma(K.sp, [(bada, bada_d[:, :])], dBa, writes=[R("bada")])
    for j in range(12):
        sl = j % 3
        K.dma(K.sp, [(wa[sl], wada_v[:, :, j * 512:(j + 1) * 512])], dWa[sl], writes=[R("wa", sl)])
        pb = bank(j % 2)

        def mm(sl=sl, pb=pb):
            ins = None
            for kc in range(8):
                ins = G.matmul(pb[0:1, :], lhsT=sT[:, kc:kc + 1], rhs=wa[sl][:, kc, :],
                               start=(kc == 0), stop=(kc == 7))
            return ins
        K.op(K.pe, mm, reads=[R("wa", sl), R("sT")], writes=[R("ps", j % 2)])
        K.op(K.dve, lambda j=j, pb=pb: V.tensor_tensor(out=modrow[0:1, j * 512:(j + 1) * 512], in0=pb[0:1, :],
                                                       in1=bada[0:1, j * 512:(j + 1) * 512], op=ALU.add),
             reads=[R("ps", j % 2), R("bada")], writes=[R("modrow")])
    dM = K.dsem("mod")
    K.dma(K.sp, [(mod_d[:, :], modrow)], dM, reads=[R("modrow")], writes=[R("mod_d")])
    K.barrier()

    ckvnT = view(L2, [128, S], BF16)
    krT = view(L2 + 8 * KIB, [96, S], BF16)
    cqnT = view(L2 + 16 * KIB, [96, 2, NO * 128], BF16)
    qTa = view(L3, [64, 8, NO * 128], BF16)
    kTa = view(L3 + 32 * KIB, [64, 2, S], BF16)
    va = view(L3 + 48 * KIB, [128, NT, 2, 96], BF16)

    al = Alloc(PA, LIM)
    win = al([128, 8, 1120], BF16)
    b1row = al([1, 1120], BF16)
    xt = [al([128, D], F32) for i in range(2)]
    hb = [al([128, D], BF16) for i in range(3)]
    hT = [al([128, 8, 128], BF16) for i in range(3)]
    projS = [al([128, 1120], F32) for i in range(3)]
    rt = [al([128, 192], F32) for i in range(4)]
    t1 = [al([128, 640], F32) for i in range(2)]
    t2 = [al([128, 640], F32) for i in range(2)]
    t1r = [al([128, 32], F32) for i in range(2)]
    t2r = [al([128, 32], F32) for i in range(2)]
    qkr = [al([128, 640], BF16) for i in range(2)]
    ckvn = [al([128, 128], BF16) for i in range(2)]
    krs = [al([128, 96], BF16) for i in range(2)]
    cqn = [al([128, 192], BF16) for i in range(2)]
    junk = al([128, D], BF16)
    junk2 = al([128, 192], BF16)
    assert al.o <= 198 * KIB, al.o
    dW = K.dsem("wA")
    K.dma(K.sp, [(g1P, g1p_d[:, :]),
                 (a1P, mod_d[0, D:2 * D].rearrange("(kc p) -> p kc", p=128)),
                 (sh1P, mod_d[0, 0:D].rearrange("(kc p) -> p kc", p=128))], dW,
          reads=[R("mod_d")], writes=[R("a1P"), R("sh1P")], allow_slow_non_contiguous=True)
    K.op(K.dve, lambda: V.scalar_tensor_tensor(out=a1P, in0=a1P, scalar=1.0, in1=g1P, op0=ALU.add, op1=ALU.mult),
         reads=[R("a1P")], writes=[R("a1P")])
    K.op(K.dve, lambda: V.tensor_copy(out=sh1Pb, in_=sh1P), reads=[R("sh1P")], writes=[R("sh1Pb")])
    K.op(K.dve, lambda: V.memset(onesrow, 1.0), writes=[R("onesrow")])

    def b1mm():
        ins = None
        for (b, c0, n) in [(2, 0, 512), (3, 512, 512), (4, 1024, 96)]:
            for kc in range(8):
                ins = G.matmul(bank(b)[0:1, 0:n], lhsT=sh1Pb[:, kc:kc + 1], rhs=win[:, kc, c0:c0 + n],
                               start=(kc == 0), stop=(kc == 7))
        return ins
    K.op(K.pe, b1mm, reads=[R("sh1Pb"), R("win")], writes=[R("ps", 2), R("ps", 3), R("ps", 4)])
    for (b, c0, n) in [(2, 0, 512), (3, 512, 512), (4, 1024, 96)]:
        K.op(K.dve, lambda b=b, c0=c0, n=n: V.tensor_copy(out=b1row[0:1, c0:c0 + n], in_=bank(b)[0:1, 0:n]),
             reads=[R("ps", b)], writes=[R("b1row")])
    for kc in range(8):
        K.op(K.dve, lambda kc=kc: V.tensor_scalar(out=win[:, kc, :], in0=win[:, kc, :], scalar1=a1P[:, kc:kc + 1], scalar2=None,
                                                  op0=ALU.mult),
             reads=[R("win"), R("a1P")], writes=[R("win")])
    K.op(K.pool, lambda: P.memset(va[:, :, :, 64:96], 1.0), writes=RL("va", range(NT)))
    for i in range(2):
        K.op(K.pool, lambda i=i: P.memset(krs[i], 0.0), writes=[R("krs", i)])

    dX = [K.dsem("x%d" % i) for i in range(2)]
    x_t = x_d.rearrange("(t p) d -> t p d", p=128)
    rope_t = rope_d.rearrange("(t p) d -> t p d", p=128)
    CQS = (128.0 / 192.0) ** 0.5

    def rope(src3, cs, sn, half, o1, o2, dst, rd, r1, r2, wr_dst, nh):
        w = 2 * half
        K.op(K.dve, lambda: V.tensor_tensor(out=o1, in0=src3, in1=bc(cs.unsqueeze(1), [128, nh, w]), op=ALU.mult),
             reads=rd, writes=[r1])
        K.op(K.dve, lambda: V.tensor_tensor(out=o2[:, :, 0:half], in0=src3[:, :, half:w],
                                            in1=bc(sn[:, 0:half].unsqueeze(1), [128, nh, half]), op=ALU.mult),
             reads=rd, writes=[r2])
        K.op(K.dve, lambda: V.tensor_tensor(out=o2[:, :, half:w], in0=src3[:, :, 0:half],
                                            in1=bc(sn[:, half:w].unsqueeze(1), [128, nh, half]), op=ALU.mult),
             reads=rd, writes=[r2])
        K.op(K.pool, lambda: P.tensor_tensor(out=dst, in0=o1, in1=o2, op=ALU.add),
             reads=[r1, r2], writes=wr_dst)

    def stA1(t):
        own = t < NO
        sl = t % 2
        K.dma(K.sp, [(xt[sl], x_t[t]), (rt[t % 4], rope_t[t])], dX[sl], writes=[R("xt", sl), R("rt", t % 4)])
        K.op(K.act, lambda sl=sl, t=t: A.activation(out=junk, in_=xt[sl], func=AF.Square, accum_out=st[:, t:t + 1]),
             reads=[R("xt", sl)], writes=[R("junk"), R("stx", t)])
        rstd_cols(st[:, t:t + 1], st[:, 32 + t:33 + t], 1.0 / D, [R("stx", t)], [R("strx", t)])
        K.op(K.act, lambda sl=sl, t=t: A.activation(out=hb[t % 3], in_=xt[sl], func=AF.Copy, scale=st[:, 32 + t:33 + t]),
             reads=[R("xt", sl), R("strx", t)], writes=[R("hb", t % 3)])

    def stA2(t):
        own = t < NO
        sl = t % 2

        def tr(sl=sl):
            ins = None
            for kc in range(8):
                ins = G.transpose(out=bankb(sl)[:, kc * 128:(kc + 1) * 128], in_=hb[t % 3][:, kc * 128:(kc + 1) * 128],
                                  identity=ident)
            return ins
        K.op(K.pe, tr, reads=[R("hb", t % 3), R("ident")], writes=[R("ps", sl)])
        K.op(K.act, lambda sl=sl: A.copy(out=hT[t % 3], in_=bankb(sl).rearrange("p (a b) -> p a b", a=8)),
             reads=[R("ps", sl)], writes=[R("hT", t % 3)])

    def stA2b(t):
        own = t < NO
        sl = t % 2
        chunks = [(2, 0, 512), (3, 512, 416), (4, 928, 192)] if own else [(3, 512, 416)]

        def proj(sl=sl, chunks=chunks):
            ins = None
            for (b, c0, n) in chunks:
                for kc in range(8):
                    G.matmul(bank(b)[:, 0:n], lhsT=hT[t % 3][:, kc, :], rhs=win[:, kc, c0:c0 + n],
                             start=(kc == 0), stop=False)
                ins = G.matmul(bank(b)[:, 0:n], lhsT=onesrow[0:1, :], rhs=b1row[0:1, c0:c0 + n], start=False, stop=True)
            return ins
        K.op(K.pe, proj, reads=[R("hT", t % 3), R("win"), R("b1row"), R("onesrow")], writes=[R("ps", b) for (b, _, _) in chunks])
        for (b, c0, n) in chunks:
            if b == 2:
                K.op(K.dve, lambda b=b, c0=c0, n=n, sl=sl: V.tensor_copy(out=projS[t % 3][:, c0:c0 + n], in_=bank(b)[:, 0:n]),
                     reads=[R("ps", b)], writes=[R("projS", t % 3, b)])
            else:
                K.op(K.act, lambda b=b, c0=c0, n=n, sl=sl: A.copy(out=projS[t % 3][:, c0:c0 + n], in_=bank(b)[:, 0:n]),
                     reads=[R("ps", b)], writes=[R("projS", t % 3, b)])

    def stA3(t):
        own = t < NO
        sl = t % 2
        pS = projS[t % 3]
        if own:
            rope(h3(pS[:, 0:640], 10), rt[t % 4][:, 0:64], rt[t % 4][:, 64:128], 32, h3(t1[sl], 10), h3(t2[sl], 10),
                 h3(qkr[sl], 10), [R("projS", t % 3, 2), R("projS", t % 3, 3), R("rt", t % 4)], R("t1", sl), R("t2", sl),
                 [R("qkr", sl)], 10)
        else:
            rope(h3(pS[:, 512:640], 2), rt[t % 4][:, 0:64], rt[t % 4][:, 64:128], 32, h3(t1[sl][:, 512:640], 2),
                 h3(t2[sl][:, 512:640], 2), h3(qkr[sl][:, 512:640], 2), [R("projS", t % 3, 3), R("rt", t % 4)],
                 R("t1", sl), R("t2", sl), [R("qkr", sl)], 2)
        rope(h3(pS[:, 896:928], 1), rt[t % 4][:, 128:160], rt[t % 4][:, 160:192], 16, h3(t1r[sl], 1), h3(t2r[sl], 1),
             h3(krs[sl][:, 64:96], 1), [R("projS", t % 3, 3), R("rt", t % 4)], R("t1r", sl), R("t2r", sl), [R("krs", sl)], 1)
        K.op(K.pool, lambda t=t, pS=pS: P.tensor_copy(out=va[:, t, :, 0:64], in_=h3(pS[:, 640:768], 2)),
             reads=[R("projS", t % 3, 3)], writes=[R("va", t)])
        K.op(K.act, lambda pS=pS, t=t: A.activation(out=junk2[:, 0:128], in_=pS[:, 768:896], func=AF.Square,
                                                    accum_out=st[:, 64 + 2 * t:65 + 2 * t]),
             reads=[R("projS", t % 3, 3)], writes=[R("junk2"), R("stk", t)])
        if own:
            K.op(K.act, lambda pS=pS, t=t: A.activation(out=junk2, in_=pS[:, 928:1120], func=AF.Square, scale=CQS,
                                                        accum_out=st[:, 65 + 2 * t:66 + 2 * t]),
                 reads=[R("projS", t % 3, 4)], writes=[R("junk2"), R("stk", t)])
        nsc = 2 if own else 1
        rc = 128 + 2 * (t % 16)
        rstd_cols(st[:, 64 + 2 * t:64 + 2 * t + nsc], st[:, rc:rc + nsc], 1.0 / 128, [R("stk", t)], [R("strk", t % 16)])
        K.op(K.dve, lambda pS=pS, sl=sl, rc=rc: V.tensor_scalar(out=ckvn[sl], in0=pS[:, 768:896], scalar1=st[:, rc:rc + 1],
                                                                scalar2=None, op0=ALU.mult),
             reads=[R("projS", t % 3, 3), R("strk", t % 16)], writes=[R("ckvn", sl)])
        if own:
            K.op(K.dve, lambda pS=pS, sl=sl, rc=rc: V.tensor_scalar(out=cqn[sl], in0=pS[:, 928:1120],
                                                                    scalar1=st[:, rc + 1:rc + 2], scalar2=None, op0=ALU.mult),
                 reads=[R("projS", t % 3, 4), R("strk", t % 16)], writes=[R("cqn", sl)])
        bk = 6 + sl
        b6 = bankb(bk)

        def trB(own=own, sl=sl, b6=b6):
            G.transpose(out=b6[0:64, 0:128], in_=qkr[sl][:, 512:576], identity=ident)
            G.transpose(out=b6[0:64, 128:256], in_=qkr[sl][:, 576:640], identity=ident)
            G.transpose(out=b6[:, 256:384], in_=ckvn[sl], identity=ident)
            ins = G.transpose(out=b6[0:96, 384:512], in_=krs[sl], identity=ident)
            if own:
                G.transpose(out=b6[0:96, 512:640], in_=cqn[sl][:, 0:96], identity=ident)
                ins = G.transpose(out=b6[0:96, 640:768], in_=cqn[sl][:, 96:192], identity=ident)
            return ins
        K.op(K.pe, trB, reads=[R("qkr", sl), R("ckvn", sl), R("krs", sl), R("ident")] + ([R("cqn", sl)] if own else []),
             writes=[R("ps", bk)])
        K.op(K.dve, lambda t=t, b6=b6: V.tensor_copy(out=kTa[:, :, t * 128:(t + 1) * 128],
                                                     in_=b6[0:64, 0:256].rearrange("p (a b) -> p a b", a=2)),
             reads=[R("ps", bk)], writes=[R("kTa", t)])
        K.op(K.dve, lambda t=t, b6=b6: V.tensor_copy(out=ckvnT[:, t * 128:(t + 1) * 128], in_=b6[:, 256:384]),
             reads=[R("ps", bk)], writes=[R("ckvnT", t)])
        K.op(K.dve, lambda t=t, b6=b6: V.tensor_copy(out=krT[64:96, t * 128:(t + 1) * 128], in_=b6[64:96, 384:512]),
             reads=[R("ps", bk)], writes=[R("krT", t)])
        if own:
            K.op(K.dve, lambda t=t, b6=b6: V.tensor_copy(out=cqnT[:, :, t * 128:(t + 1) * 128],
                                                         in_=b6[0:96, 512:768].rearrange("p (a b) -> p a b", a=2)),
                 reads=[R("ps", bk)], writes=[R("cqnT", t)])
            b5 = bankb(5)

            def trQ(sl=sl, b5=b5):
                ins = None
                for h in range(8):
                    ins = G.transpose(out=b5[0:64, h * 128:(h + 1) * 128], in_=qkr[sl][:, h * 64:(h + 1) * 64],
                                      identity=ident)
                return ins
            K.op(K.pe, trQ, reads=[R("qkr", sl), R("ident")], writes=[R("ps", 5)])
            K.op(K.act, lambda t=t, b5=b5: A.copy(out=qTa[:, :, t * 128:(t + 1) * 128],
                                                  in_=b5[0:64, :].rearrange("p (a b) -> p a b", a=8)),
                 reads=[R("ps", 5)], writes=[R("qTa", t)])

    gt1Ba = view(86 * KIB, [128, D], F32)
    wstg = [view(90 * KIB + i * 4 * KIB, [128, D], F32) for i in range(2)]
    wobf = [view(98 * KIB + i * 2 * KIB, [128, D], BF16) for i in range(2)]
    dWo = [K.dsem("wo%d" % i) for i in range(2)]
    dWos = [K.dsem("wos%d" % i) for i in range(2)]
    dGt = K.dsem("gt1a")
    wout_v = wout_d.rearrange("(kc p) n -> kc p n", p=128)
    wo_dv = wo_d.rearrange("(kc p) n -> kc p n", p=128)
    K.dma(K.sp, [(gt1Ba, modB(2))], dGt, reads=[R("mod_d")], writes=[R("gt1Ba")])

    def wo_load(kc):
        K.dma(K.sp, [(wstg[kc % 2], wout_v[kc])], dWo[kc % 2], writes=[R("wstg", kc % 2)])

    def wo_fold(kc):
        K.op(K.dve, lambda: V.scalar_tensor_tensor(out=wobf[kc % 2], in0=wstg[kc % 2], scalar=prm[:, 11 + kc:12 + kc],
                                                   in1=gt1Ba, op0=ALU.mult, op1=ALU.mult),
             reads=[R("wstg", kc % 2), R("prm"), R("gt1Ba")], writes=[R("wobf", kc % 2)])

    def wo_store(kc):
        K.dma(K.sp, [(wo_dv[kc], wobf[kc % 2])], dWos[kc % 2], reads=[R("wobf", kc % 2)], writes=[R("wo_d", kc)])

    zsrc = view(198 * KIB, [128, 2048], F32)
    K.op(K.pool, lambda: P.memset(zsrc, 0.0), writes=[R("zsrc")])
    dZf = K.dsem("zf")
    xs_z = xs_d.rearrange("(p r) d -> p r d", p=128)
    ys_z = ys_d.rearrange("(p r) d -> p r d", p=128)
    zjobs = [(xs_z[:, 4 * c:4 * c + 4, :], zsrc.bitcast(BF16).rearrange("p (a b) -> p a b", a=4)) for c in range(24)]
    zjobs += [(ys_z[:, 2 * c:2 * c + 2, :], zsrc.rearrange("p (a b) -> p a b", a=2)) for c in range(48)]

    for i in range(NT + 3):
        for zi in range(3 * i, min(3 * i + 3, len(zjobs))):
            K.dma(K.pool, [zjobs[zi]], dZf, reads=[R("zsrc")], writes=[R("zf", zi)])
        if i >= 6 and (i - 6) % 2 == 0 and (i - 6) // 2 < 8:
            wo_load((i - 6) // 2)
        if i >= 7 and (i - 7) % 2 == 0 and (i - 7) // 2 < 8:
            wo_fold((i - 7) // 2)
        if i >= 8 and (i - 8) % 2 == 0 and (i - 8) // 2 < 8:
            wo_store((i - 8) // 2)
        if i < NT:
            stA1(i)
        if 0 <= i - 1 < NT:
            stA2(i - 1)
        if 0 <= i - 2 < NT:
            stA2b(i - 2)
        if 0 <= i - 3 < NT:
            stA3(i - 3)
    K.barrier()

    mixTa = view(L4, [128, 4, NO * 128], BF16)
    mixTb = view(L4 + 16 * KIB, [128, 4, NO * 128], BF16)
    al = Alloc(PA, LIM)
    pT = [al([128, 3, 512], BF16) for i in range(2)]
    rden = [al([64, 512], F32) for i in range(2)]
    lnt = [al([32, 512], F32) for i in range(2)]
    msk = al([128, 4, 128], BF16)
    esrow = al([1, 8, 128], BF16)
    dMsk = K.dsem("msk")
    K.dma(K.pool, [(msk, msk_d.rearrange("p (a b) -> p a b", a=4))], dMsk, writes=[R("msk")])
    K.op(K.act, lambda: A.activation(out=es8, in_=prm[0:1, 20:28], func=AF.Exp), reads=[R("prm")], writes=[R("es8")])
    K.op(K.dve, lambda: V.tensor_copy(out=esrow, in_=bc(es8.unsqueeze(2), [1, 8, 128])), reads=[R("es8")],
         writes=[R("esrow")])

    def finish(ob, rd_sl, writers, use_act):
        oT = bank(ob)
        rd = rden[rd_sl]
        if use_act:
            K.op(K.act, lambda: A.activation(out=lnt[rd_sl], in_=oT[64:96, :], func=AF.Ln),
                 reads=[R("ps", ob)], writes=[R("lnt", rd_sl)])
            K.op(K.act, lambda: A.activation(out=rd[0:32, :], in_=lnt[rd_sl], func=AF.Exp, scale=-1.0),
                 reads=[R("lnt", rd_sl)], writes=[R("rden", rd_sl)])
            K.op(K.dve, lambda: V.tensor_copy(out=rd[32:64, :], in_=rd[0:32, :]),
                 reads=[R("rden", rd_sl)], writes=[R("rden", rd_sl)])
        else:
            K.op(K.dve, lambda: V.reciprocal(out=rd[0:32, :], in_=oT[64:96, :]),
                 reads=[R("ps", ob)], writes=[R("rden", rd_sl)])
            K.op(K.dve, lambda: V.tensor_copy(out=rd[32:64, :], in_=rd[0:32, :]),
                 reads=[R("rden", rd_sl)], writes=[R("rden", rd_sl)])
        for (out_ap, in_sl, wr) in writers:
            K.op(K.dve, lambda out_ap=out_ap, in_sl=in_sl: V.tensor_tensor(out=out_ap, in0=in_sl(oT[0:64, :]),
                                                                           in1=in_sl(rd), op=ALU.mult),
                 reads=[R("ps", ob), R("rden", rd_sl)], writes=[wr])

    def swa_ctx(it):
        n, kvh = divmod(it, 2)
        kts = [(31 if n == 0 else n - 1, 2 if n == 0 else 0), (n, None), (16 if n == NO - 1 else n + 1, 3 if n == NO - 1 else 1)]
        sl = it % 2
        return n, kvh, kts, sl, 3 * sl, 6 + sl

    def stW1(it):
        n, kvh, kts, sl, b0, ob = swa_ctx(it)

        def sc():
            ins = None
            for i, (kt, _) in enumerate(kts):
                ins = G.matmul(bank(b0 + i).rearrange("p (a b) -> p a b", a=4),
                               lhsT=kTa[:, kvh, kt * 128:(kt + 1) * 128],
                               rhs=qTa[:, kvh * 4:(kvh + 1) * 4, n * 128:(n + 1) * 128], start=True, stop=True)
            return ins
        K.op(K.pe, sc, reads=[], writes=[R("ps", b0 + i) for i in range(3)])
        K.op(K.act, lambda: A.activation(out=pT[sl].rearrange("p a b -> p (a b)"), in_=bank(b0, 3),
                                         func=AF.Exp, scale=0.125),
             reads=[R("ps", b0 + i) for i in range(3)], writes=[R("pT", sl)])
        for i, (kt, m) in enumerate(kts):
            if m is None:
                continue
            K.op(K.pool, lambda i=i, m=m: P.tensor_tensor(
                out=pT[sl][:, i, :].rearrange("p (a b) -> p a b", a=4),
                in0=pT[sl][:, i, :].rearrange("p (a b) -> p a b", a=4),
                in1=bc(msk[:, m, :].unsqueeze(1), [128, 4, 128]), op=ALU.mult),
                reads=[R("pT", sl), R("msk")], writes=[R("pT", sl)])

    def stW2(it):
        n, kvh, kts, sl, b0, ob = swa_ctx(it)

        def pv():
            for i, (kt, _) in enumerate(kts):
                G.matmul(bank(ob)[0:96, :], lhsT=va[:, kt, kvh, :], rhs=pT[sl][:, i, :],
                         start=(i == 0), stop=False)
            return G.matmul(bank(ob)[0:96, :], lhsT=vsink[0:1, :],
                            rhs=esrow[0:1, kvh * 4:(kvh + 1) * 4, :].rearrange("p a b -> p (a b)"),
                            start=False, stop=True)
        K.op(K.pe, pv, reads=[R("pT", sl), R("vsink"), R("esrow")], writes=[R("ps", ob)])
        writers = []
        for par in range(2):
            def in_sl(ap, par=par):
                return ap.rearrange("p (i two b) -> p i two b", two=2, b=128)[:, :, par, :]
            writers.append((mixTa[par * 64:par * 64 + 64, 2 * kvh:2 * kvh + 2, n * 128:(n + 1) * 128], in_sl,
                            R("mixTa", n, kvh, par)))
        finish(ob, sl, writers, True)

    for i in range(2 * NO + 1):
        if i < 2 * NO:
            stW1(i)
        if i >= 1:
            stW2(i - 1)
    K.barrier()

    qTb = view(L3, [96, 8, NO * 128], BF16)
    kTb = [view(L3 + 32 * KIB + i * 8 * KIB, [96, S], BF16) for i in range(2)]
    pTm = [view(L3 + 48 * KIB + i * 3072, [128, 3, 512], BF16) for i in range(4)]
    al = Alloc(PA, LIM)
    vb = al([128, NT, 8, 96], BF16)
    rden = [al([64, 512], F32) for i in range(2)]
    wuqs = al([96, 2, 768], F32)
    wuq = al([96, 2, 768], BF16)
    wukvs = al([128, 1024], F32)
    wukv = al([128, 8, 128], BF16)
    rt2 = [al([128, 64], F32) for i in range(2)]
    qbS = [al([128, 8, 96], F32) for i in range(2)]
    t1b = [al([128, 8, 32], F32) for i in range(2)]
    t2b = [al([128, 8, 32], F32) for i in range(2)]
    qbr = [al([128, 8, 96], BF16) for i in range(2)]

    dW2 = K.dsem("wB")
    K.dma(K.sp, [(wuqs, wuq_d.rearrange("(kc p) n -> p kc n", p=96)), (wukvs, wukv_d[:, :])], dW2,
          writes=[R("wuqs"), R("wukvs")])
    for kc in range(2):
        K.op(K.dve, lambda kc=kc: V.tensor_scalar(out=wuq[:, kc, :], in0=wuqs[:, kc, :], scalar1=prm[0:96, 8 + kc:9 + kc],
                                                  scalar2=None, op0=ALU.mult),
             reads=[R("wuqs"), R("prm")], writes=[R("wuq")])
    K.op(K.dve, lambda: V.tensor_scalar(out=wukv.rearrange("p a b -> p (a b)"), in0=wukvs, scalar1=prm[:, 10:11],
                                        scalar2=None, op0=ALU.mult),
         reads=[R("wukvs"), R("prm")], writes=[R("wukv")])
    K.op(K.pool, lambda: P.memset(vb[:, :, :, 64:96], 1.0), writes=RL("vb", range(NT)))
    for kt in range(NT):
        b = kt % 2
        K.op(K.pe, lambda kt=kt, b=b: G.matmul(bank(b).rearrange("p (a b) -> p a b", a=8),
                                               lhsT=ckvnT[:, kt * 128:(kt + 1) * 128], rhs=wukv[:, :, 64:128],
                                               start=True, stop=True),
             reads=[R("wukv")], writes=[R("ps", b)])
        if kt % 2:
            K.op(K.dve, lambda kt=kt, b=b: V.tensor_copy(out=vb[:, kt, :, 0:64], in_=bank(b).rearrange("p (a b) -> p a b", a=8)),
                 reads=[R("ps", b)], writes=[R("vb", kt)])
        else:
            K.op(K.act, lambda kt=kt, b=b: A.copy(out=vb[:, kt, :, 0:64], in_=bank(b).rearrange("p (a b) -> p a b", a=8)),
                 reads=[R("ps", b)], writes=[R("vb", kt)])
    dX2 = [K.dsem("r%d" % i) for i in range(2)]
    def stQ1(t):
        sl = t % 2
        K.dma(K.sp, [(rt2[sl], rope_t[t][:, 128:192])], dX2[sl], writes=[R("rt2", sl)])
        bq = 2 + 2 * sl

        def qp(t=t, bq=bq):
            ins = None
            for (b, c0, n) in [(bq, 0, 512), (bq + 1, 512, 256)]:
                for kc in range(2):
                    ins = G.matmul(bank(b)[:, 0:n], lhsT=cqnT[:, kc, t * 128:(t + 1) * 128], rhs=wuq[:, kc, c0:c0 + n],
                                   start=(kc == 0), stop=(kc == 1))
            return ins
        K.op(K.pe, qp, reads=[R("wuq")], writes=[R("ps", bq), R("ps", bq + 1)])
        qf = qbS[sl].rearrange("p a b -> p (a b)")
        K.op(K.act, lambda qf=qf, bq=bq: A.copy(out=qf[:, 0:512], in_=bank(bq)), reads=[R("ps", bq)], writes=[R("qbS", sl)])
        K.op(K.act, lambda qf=qf, bq=bq: A.copy(out=qf[:, 512:768], in_=bank(bq + 1)[:, 0:256]), reads=[R("ps", bq + 1)],
             writes=[R("qbS", sl)])

    def stQ2(t):
        sl = t % 2
        K.op(K.pool, lambda sl=sl: P.tensor_copy(out=qbr[sl][:, :, 0:64], in_=qbS[sl][:, :, 0:64]), reads=[R("qbS", sl)],
             writes=[R("qbr", sl)])
        rope(qbS[sl][:, :, 64:96], rt2[sl][:, 0:32], rt2[sl][:, 32:64], 16, t1b[sl], t2b[sl], qbr[sl][:, :, 64:96],
             [R("qbS", sl), R("rt2", sl)], R("t1b", sl), R("t2b", sl), [R("qbr", sl)], 8)
        b4 = bankb(6 + sl)

        def trq(sl=sl, b4=b4):
            ins = None
            for h in range(8):
                ins = G.transpose(out=b4[0:96, h * 128:(h + 1) * 128], in_=qbr[sl][:, h, :], identity=ident)
            return ins
        K.op(K.pe, trq, reads=[R("qbr", sl), R("ident")], writes=[R("ps", 6 + sl)])
        K.op(K.dve, lambda t=t, b4=b4: V.tensor_copy(out=qTb[:, :, t * 128:(t + 1) * 128],
                                                     in_=b4[0:96, :].rearrange("p (a b) -> p a b", a=8)),
             reads=[R("ps", 6 + sl)], writes=[R("qTb", t)])

    for i in range(NO + 1):
        if i < NO:
            stQ1(i)
        if i >= 1:
            stQ2(i - 1)

    ktg = [list(range(k, min(k + 3, NT))) for k in range(0, NT, 3)]
    cgs = [[0, 1, 2], [3, 4, 5], [6, 7]]

    def setup_steps(h):
        return [("setup", h, cg) for cg in cgs]

    dWbf = K.dsem("wbf")
    K.dma(K.pool, [(wbf_d[c * 512:(c + 1) * 512, m * 2048:(m + 1) * 2048], src[c * 512:(c + 1) * 512, :])
                   for c in range(8) for m, src in enumerate((wg_d, wu_d, wd_d))], dWbf, writes=[R("wbf")])
    steps = setup_steps(0)
    for h in range(8):
        for qg in range(4):
            steps += [("attn", h, qg, gi) for gi in range(len(ktg))]
            if qg == 0 and h < 7:
                steps += setup_steps(h + 1)
    SK = 2
    sc_scale = 96.0 ** -0.5
    for i in range(len(steps) + SK):
        if i < len(steps):
            stp = steps[i]
            b0 = 3 * (i % 2)
            if stp[0] == "setup":
                _, h, cg = stp
                sl = h % 2
                if cg[0] == 0:
                    K.op(K.pool, lambda sl=sl: P.tensor_copy(out=kTb[sl][64:96, :], in_=krT[64:96, :]),
                         writes=[R("kTbr", sl)])

                def su(h=h, cg=cg, b0=b0):
                    ins = None
                    for j, c in enumerate(cg):
                        ins = G.matmul(bank(b0 + j)[0:64, :], lhsT=wukv[:, h, 0:64], rhs=ckvnT[:, c * 512:(c + 1) * 512],
                                       start=True, stop=True)
                    return ins
                K.op(K.pe, su, reads=[R("wukv")], writes=[R("ps", b0 + j) for j in range(3)])
                K.op(K.dve, lambda sl=sl, cg=cg, b0=b0: V.tensor_copy(
                    out=kTb[sl][0:64, cg[0] * 512:(cg[-1] + 1) * 512], in_=bank(b0, len(cg))[0:64, :]),
                    reads=[R("ps", b0 + j) for j in range(3)], writes=[R("kTb", sl)])
            else:
                _, h, qg, gi = stp
                kl = ktg[gi]

                def sc(h=h, qg=qg, kl=kl, b0=b0):
                    ins = None
                    for j, kt in enumerate(kl):
                        ins = G.matmul(bank(b0 + j), lhsT=kTb[h % 2][:, kt * 128:(kt + 1) * 128],
                                       rhs=qTb[:, h, qg * 512:(qg + 1) * 512], start=True, stop=True)
                    return ins
                K.op(K.pe, sc, reads=[R("kTb", h % 2), R("kTbr", h % 2)] + RL("qTb", range(qg * 4, qg * 4 + 4)),
                     writes=[R("ps", b0 + j) for j in range(3)])
                s4 = i % 4
                nk = len(kl)
                K.op(K.act, lambda s4=s4, b0=b0, nk=nk: A.activation(out=pTm[s4].rearrange("p a b -> p (a b)")[:, 0:nk * 512],
                                                                     in_=bank(b0, nk), func=AF.Exp, scale=sc_scale),
                     reads=[R("ps", b0 + j) for j in range(3)], writes=[R("pTm", s4)])
        if i >= SK and steps[i - SK][0] == "attn":
            _, h, qg, gi = steps[i - SK]
            kl = ktg[gi]
            s4 = (i - SK) % 4
            gsl = (h * 4 + qg) % 2
            ob = 6 + gsl

            def pv(h=h, kl=kl, s4=s4, ob=ob):
                ins = None
                for j, kt in enumerate(kl):
                    ins = G.matmul(bank(ob)[0:96, :], lhsT=vb[:, kt, h, :], rhs=pTm[s4][:, j, :],
                                   start=(kt == 0), stop=(kt == NT - 1))
                return ins
            K.op(K.pe, pv, reads=[R("pTm", s4)] + [R("vb", kt) for kt in kl], writes=[R("ps", ob)])
            if kl[-1] == NT - 1:
                finish(ob, gsl, [(mixTb[(h % 2) * 64:(h % 2) * 64 + 64, h // 2, qg * 512:(qg + 1) * 512],
                                 (lambda ap: ap), R("mixTb", h, qg))], False)
    K.barrier()

    PC = 120 * KIB
    acc = view(2 * KIB, [128, NO, D], F32)
    posu = view(66 * KIB, [128, 2, NO], U32)
    w12 = view(66 * KIB + 128, [128, 2, NO], F32)
    widx = view(66 * KIB + 256, [128, 48], U32)
    sidx = view(66 * KIB + 448, [128, 48], U32)
    wo = view(68 * KIB, [128, 8, D], BF16)
    h2tok = view(PC, [128, NO, D], BF16)
    al = Alloc(PC + 32 * KIB, LIM)
    LGa = al([128, NO, 36], F32)
    r_m4 = al([128, NO], F32)
    r_d4 = al([128, NO, 4], F32)
    r_e4 = al([128, NO, 4], F32)
    r_s4 = al([128, NO], F32)
    r_oh = al([128, NO, 4], F32)
    r_t32 = al([128, NO, 32], F32)
    r_el = al([128, NO, 8], F32)
    r_el2 = al([128, NO, 8], F32)
    r_v1 = al([128, NO], F32)
    r_d8 = al([128, NO, 8], F32)
    r_eq = al([128, NO, 8], F32)
    r_v2 = al([128, NO], F32)
    r_mk = al([128, NO, 8], F32)
    r_ex = al([128, NO, 8], F32)
    r_cw = al([128, NO, 8], F32)
    r_den = al([128, NO], F32)
    SORT0 = al.o
    xt = [al([128, D], F32) for i in range(2)]
    tmpB = [al([128, D], F32) for i in range(2)]
    a2B = al([128, D], F32)
    sh2B = al([128, D], F32)
    h2Th = [al([128, 8, 128], BF16) for i in range(2)]
    lo_t = [al([128, D], BF16) for i in range(2)]
    h2Tl = [al([128, 8, 128], BF16) for i in range(2)]
    wrs = al([128, 8, 36], F32)
    wrh = al([128, 8, 36], BF16)
    wrl = al([128, 8, 36], BF16)
    rab = al([128, 32], F32)
    sqs = [al([128, 8, 128], BF16) for i in range(2)]
    junk = al([128, D], BF16)
    gt1B = tmpB[0]
    g2B = tmpB[1]

    K.op(K.dve, lambda: V.memset(st, 0.0), writes=[R("st")])
    dW3 = K.dsem("wC")
    K.dma(K.sp, [(a2B, modB(4)), (sh2B, modB(3)), (gt1B, modB(2)), (g2B, gB_d[:, D:2 * D]),
                 (wrs, wr_d.rearrange("(kc p) n -> p kc n", p=128))], dW3,
          writes=[R("a2B"), R("sh2B"), R("tmpB", 0), R("tmpB", 1), R("wrs")])
    K.op(K.dve, lambda: V.scalar_tensor_tensor(out=a2B, in0=a2B, scalar=1.0, in1=g2B, op0=ALU.add, op1=ALU.mult),
         reads=[R("a2B"), R("tmpB", 1)], writes=[R("a2B"), R("tmpB", 1)])
    K.op(K.act, lambda: A.copy(out=wrh, in_=wrs), reads=[R("wrs")], writes=[R("wrh")])
    K.op(K.pool, lambda: P.tensor_tensor(out=wrl, in0=wrs, in1=wrh, op=ALU.subtract), reads=[R("wrs"), R("wrh")],
         writes=[R("wrl")])
    dWoL = K.dsem("woL")
    K.dma(K.sp, [(wo, wo_d.rearrange("(kc p) n -> p kc n", p=128))], dWoL, writes=[R("wo")])
    for t in range(NO):
        sl = t % 2
        K.op(K.pool, lambda t=t, sl=sl: P.tensor_tensor(out=sqs[sl][:, 0:4, :], in0=mixTa[:, :, t * 128:(t + 1) * 128],
                                                        in1=mixTa[:, :, t * 128:(t + 1) * 128], op=ALU.mult),
             writes=[R("sqs", sl)])
        K.op(K.act, lambda t=t, sl=sl: A.activation(out=sqs[sl][:, 4:8, :], in_=mixTb[:, :, t * 128:(t + 1) * 128], func=AF.Square),
             writes=[R("sqsb", sl)])

        def ssq(t=t, sl=sl):
            ins = None
            for g2 in range(2):
                for j in range(4):
                    ins = G.matmul(bank(7)[:, 2 * t + g2:2 * t + g2 + 1], lhsT=sqs[sl][:, 4 * g2 + j, :], rhs=ones_b[:, 0:1],
                                   start=(j == 0), stop=(j == 3))
            return ins
        K.op(K.pe, ssq, reads=[R("sqs", sl), R("sqsb", sl), R("ones")], writes=[R("ps", 7)])
    K.op(K.act, lambda: A.activation(out=rab, in_=bank(7)[:, 0:32], func=AF.Ln, scale=1.0 / 512, bias=eps_t[:, 0:1]),
         reads=[R("ps", 7), R("eps")], writes=[R("rab")])
    K.op(K.act, lambda: A.activation(out=rab, in_=rab, func=AF.Exp, scale=-0.5), reads=[R("rab")], writes=[R("rab")])

    brB = prm[:, 28:64]
    def stC1(t):
        sl = t % 2
        K.dma(K.sp, [(xt[sl], x_t[t])], dX[sl], writes=[R("xt", sl)])

        def op_(t=t):
            ins = None
            for (mt, b0, k0) in [(mixTa, 0, 0), (mixTb, 2, 4)]:
                for dh in range(2):
                    for kc in range(4):
                        ins = G.matmul(bank(b0 + dh), lhsT=mt[:, kc, t * 128:(t + 1) * 128],
                                       rhs=wo[:, k0 + kc, dh * 512:(dh + 1) * 512], start=(kc == 0), stop=(kc == 3))
            return ins
        K.op(K.pe, op_, reads=[R("wo")], writes=[R("ps", b) for b in range(4)])
        K.op(K.dve, lambda t=t, sl=sl: V.scalar_tensor_tensor(out=acc[:, t, :], in0=bank(0, 2), scalar=rab[:, 2 * t:2 * t + 1],
                                                              in1=xt[sl], op0=ALU.mult, op1=ALU.add),
             reads=[R("ps", 0), R("ps", 1), R("rab"), R("xt", sl)], writes=[R("acc", t)])
        K.op(K.dve, lambda t=t: V.scalar_tensor_tensor(out=acc[:, t, :], in0=bank(2, 2), scalar=rab[:, 2 * t + 1:2 * t + 2],
                                                       in1=acc[:, t, :], op0=ALU.mult, op1=ALU.add),
             reads=[R("ps", 2), R("ps", 3), R("rab"), R("acc", t)], writes=[R("acc", t)])
        K.op(K.act, lambda t=t: A.activation(out=junk, in_=acc[:, t, :], func=AF.Square, accum_out=st[:, t:t + 1]),
             reads=[R("acc", t), R("st")], writes=[R("junk"), R("stx", t)])
        rstd_cols(st[:, t:t + 1], st[:, 32 + t:33 + t], 1.0 / D, [R("stx", t)], [R("strx", t)])

    def stC2(t):
        sl = t % 2
        K.op(K.act, lambda t=t, sl=sl: A.activation(out=tmpB[sl], in_=acc[:, t, :], func=AF.Copy, scale=st[:, 32 + t:33 + t]),
             reads=[R("acc", t), R("strx", t)], writes=[R("tmpB", sl)])
        K.op(K.dve, lambda sl=sl: V.tensor_tensor(out=tmpB[sl], in0=tmpB[sl], in1=a2B, op=ALU.mult),
             reads=[R("tmpB", sl), R("a2B")], writes=[R("tmpB", sl)])
        K.op(K.dve, lambda sl=sl: V.tensor_tensor(out=tmpB[sl], in0=tmpB[sl], in1=sh2B, op=ALU.add),
             reads=[R("tmpB", sl), R("sh2B")], writes=[R("tmpB", sl)])
        K.op(K.act, lambda sl=sl, t=t: A.copy(out=h2tok[:, t, :], in_=tmpB[sl]), reads=[R("tmpB", sl)], writes=[R("h2tok", t)])
        K.op(K.pool, lambda sl=sl, t=t: P.tensor_tensor(out=lo_t[sl], in0=tmpB[sl], in1=h2tok[:, t, :], op=ALU.subtract),
             reads=[R("tmpB", sl), R("h2tok", t)], writes=[R("lo_t", sl)])

    def stC2b(t):
        sl = t % 2

        def tr2(sl=sl, t=t):
            ins = None
            for kc in range(8):
                G.transpose(out=bankb(4)[:, kc * 128:(kc + 1) * 128], in_=h2tok[:, t, kc * 128:(kc + 1) * 128], identity=ident)
                ins = G.transpose(out=bankb(5)[:, kc * 128:(kc + 1) * 128], in_=lo_t[sl][:, kc * 128:(kc + 1) * 128],
                                  identity=ident)
            return ins
        K.op(K.pe, tr2, reads=[R("h2tok", t), R("lo_t", sl), R("ident")], writes=[R("ps", 4), R("ps", 5)])
        K.op(K.act, lambda sl=sl: A.copy(out=h2Th[sl], in_=bankb(4).rearrange("p (a b) -> p a b", a=8)),
             reads=[R("ps", 4)], writes=[R("h2Th", sl)])
        K.op(K.dve, lambda sl=sl: V.tensor_copy(out=h2Tl[sl], in_=bankb(5).rearrange("p (a b) -> p a b", a=8)),
             reads=[R("ps", 5)], writes=[R("h2Tl", sl)])

    def stC3(t):
        sl = t % 2

        def lg(t=t, sl=sl):
            ins = None
            combos = [(h2Th[sl], wrh), (h2Tl[sl], wrh), (h2Th[sl], wrl)]
            for ci, (a_, w_) in enumerate(combos):
                for kc in range(8):
                    ins = G.matmul(bank(6)[:, 0:36], lhsT=a_[:, kc, :], rhs=w_[:, kc, :],
                                   start=(ci == 0 and kc == 0), stop=(ci == 2 and kc == 7))
            return ins
        K.op(K.pe, lg, reads=[R("h2Th", sl), R("h2Tl", sl), R("wrh"), R("wrl")], writes=[R("ps", 6)])
        K.op(K.dve, lambda t=t: V.tensor_tensor(out=LGa[:, t, :], in0=bank(6)[:, 0:36], in1=brB, op=ALU.add),
             reads=[R("ps", 6), R("prm")], writes=[R("rw")])

    for i in range(NO + 3):
        if i < NO:
            stC1(i)
        if 0 <= i - 1 < NO:
            stC2(i - 1)
        if 0 <= i - 2 < NO:
            stC2b(i - 2)
        if 0 <= i - 3 < NO:
            stC3(i - 3)
    RW = [R("rw")]

    def rop(eng, fn):
        K.op(eng, fn, reads=RW, writes=RW)
    T = NO
    Lg = LGa[:, :, 0:4]
    rop(K.dve, lambda: V.tensor_reduce(out=r_m4, in_=Lg, axis=AX.X, op=ALU.max))
    rop(K.dve, lambda: V.tensor_tensor(out=r_d4, in0=Lg, in1=bc(r_m4.unsqueeze(2), [128, T, 4]), op=ALU.subtract))
    rop(K.act, lambda: A.activation(out=r_e4, in_=r_d4, func=AF.Exp))
    rop(K.dve, lambda: V.tensor_reduce(out=r_s4, in_=r_e4, axis=AX.X, op=ALU.add))
    rop(K.dve, lambda: V.tensor_scalar(out=r_oh, in0=r_d4, scalar1=0.0, scalar2=None, op0=ALU.is_ge))
    for g in range(4):
        rop(K.dve, lambda g=g: V.tensor_tensor(out=r_t32[:, :, g * 8:(g + 1) * 8], in0=LGa[:, :, 4 + g * 8:12 + g * 8],
                                               in1=bc(r_oh[:, :, g:g + 1], [128, T, 8]), op=ALU.mult))
    rop(K.dve, lambda: V.tensor_tensor(out=r_el, in0=r_t32[:, :, 0:8], in1=r_t32[:, :, 8:16], op=ALU.add))
    rop(K.dve, lambda: V.tensor_tensor(out=r_el2, in0=r_t32[:, :, 16:24], in1=r_t32[:, :, 24:32], op=ALU.add))
    rop(K.dve, lambda: V.tensor_tensor(out=r_el, in0=r_el, in1=r_el2, op=ALU.add))
    rop(K.dve, lambda: V.tensor_reduce(out=r_v1, in_=r_el, axis=AX.X, op=ALU.max))
    rop(K.dve, lambda: V.tensor_tensor(out=r_d8, in0=r_el, in1=bc(r_v1.unsqueeze(2), [128, T, 8]), op=ALU.subtract))
    rop(K.dve, lambda: V.tensor_scalar(out=r_eq, in0=r_d8, scalar1=0.0, scalar2=None, op0=ALU.is_ge))
    rop(K.dve, lambda: V.scalar_tensor_tensor(out=r_el2, in0=r_eq, scalar=-1e30, in1=r_d8, op0=ALU.mult, op1=ALU.add))
    rop(K.dve, lambda: V.tensor_reduce(out=r_v2, in_=r_el2, axis=AX.X, op=ALU.max))
    rop(K.dve, lambda: V.tensor_tensor(out=r_mk, in0=r_d8, in1=bc(r_v2.unsqueeze(2), [128, T, 8]), op=ALU.is_ge))
    rop(K.act, lambda: A.activation(out=r_ex, in_=r_d8, func=AF.Exp))
    rop(K.dve, lambda: V.tensor_tensor(out=r_cw, in0=r_mk, in1=r_ex, op=ALU.mult))
    rop(K.dve, lambda: V.tensor_reduce(out=r_den, in_=r_cw, axis=AX.X, op=ALU.add))
    rop(K.dve, lambda: V.tensor_tensor(out=r_den, in0=r_den, in1=r_s4, op=ALU.mult))
    rop(K.dve, lambda: V.reciprocal(out=r_den, in_=r_den))
    rop(K.dve, lambda: V.tensor_tensor(out=r_cw, in0=r_cw, in1=bc(r_den.unsqueeze(2), [128, T, 8]), op=ALU.mult))
    K.barrier()

    al = Alloc(SORT0, LIM)
    m2 = al([128, NO, 8], F32)
    A1 = al([128, NO, NE], F32)
    A2 = al([128, NO, NE], F32)
    Mb = al([128, NO, NE], BF16)
    utri = al([128, 128], BF16)
    onesq = al([128, 128], BF16)
    cnt = al([128, NE], F32)
    thr = al([128, 8], F32)
    cmp8 = al([128, NE, 8], F32)
    Tt = al([128, NE], F32)
    sca = al([128, NE], F32)
    scb = al([128, NE], F32)
    off256 = al([128, NE], F32)
    offT = al([128, NE], F32)
    posf = al([128, NO, NE], F32)
    ptmp = al([128, NO, NE], F32)
    posk = al([128, 2, NO], F32)
    jv = al([128, 48], F32)
    ev = al([128, NE], F32)
    pidx = al([128, 1], F32)
    indg = al([128, NE, 48], F32)
    indl = al([128, NE, 48], F32)
    eidf = al([128, 48], F32)
    anyf = al([128, 48], F32)
    real1 = al([128, 48], F32)
    jv256 = al([128, 48], F32)
    sidf = al([128, 48], F32)
    p2 = al([128, 1], F32)
    SR = [R("sort")]

    def sop(eng, fn, extra_r=(), extra_w=()):
        K.op(eng, fn, reads=SR + list(extra_r), writes=SR + list(extra_w))
    T = NO
    sop(K.pool, lambda: P.memset(utri, 1.0))
    sop(K.pool, lambda: P.affine_select(out=utri, in_=utri, pattern=[[1, 128]], compare_op=ALU.is_ge, fill=0.0, base=-1,
                                        channel_multiplier=-1))
    sop(K.pool, lambda: P.memset(onesq, 1.0))
    for m in range(8):
        sop(K.dve, lambda m=m: V.memset(thr[:, m:m + 1], 256.0 * m))
    sop(K.pool, lambda: P.iota(jv, pattern=[[1, 48]], base=0, channel_multiplier=0, allow_small_or_imprecise_dtypes=True))
    sop(K.pool, lambda: P.iota(ev, pattern=[[1, NE]], base=0, channel_multiplier=0, allow_small_or_imprecise_dtypes=True))
    sop(K.pool, lambda: P.iota(pidx, pattern=[[0, 1]], base=0, channel_multiplier=1, allow_small_or_imprecise_dtypes=True))
    sop(K.dve, lambda: V.tensor_tensor(out=m2, in0=r_mk, in1=r_eq, op=ALU.subtract), extra_r=RW)
    for g in range(4):
        sop(K.dve, lambda g=g: V.tensor_tensor(out=A1[:, :, g * 8:(g + 1) * 8], in0=r_eq,
                                               in1=bc(r_oh[:, :, g:g + 1], [128, T, 8]), op=ALU.mult), extra_r=RW)
        sop(K.dve, lambda g=g: V.tensor_tensor(out=A2[:, :, g * 8:(g + 1) * 8], in0=m2,
                                               in1=bc(r_oh[:, :, g:g + 1], [128, T, 8]), op=ALU.mult), extra_r=RW)
    sop(K.dve, lambda: V.tensor_tensor(out=r_ex, in0=r_cw, in1=r_eq, op=ALU.mult), extra_r=RW, extra_w=RW)
    sop(K.dve, lambda: V.tensor_reduce(out=w12[:, 0, :], in_=r_ex, axis=AX.X, op=ALU.add), extra_r=RW, extra_w=[R("w12")])
    sop(K.dve, lambda: V.tensor_tensor(out=r_ex, in0=r_cw, in1=m2, op=ALU.mult), extra_r=RW, extra_w=RW)
    sop(K.dve, lambda: V.tensor_reduce(out=w12[:, 1, :], in_=r_ex, axis=AX.X, op=ALU.add), extra_r=RW, extra_w=[R("w12")])
    sop(K.dve, lambda: V.tensor_tensor(out=Mb, in0=A1, in1=A2, op=ALU.add))
    def rk():
        ins = None
        for i in range(NO):
            for i2 in range(i):
                G.matmul(bank(0)[:, i * 32:(i + 1) * 32], lhsT=onesq, rhs=Mb[:, i2, :], start=(i2 == 0), stop=False)
            ins = G.matmul(bank(0)[:, i * 32:(i + 1) * 32], lhsT=utri, rhs=Mb[:, i, :], start=(i == 0), stop=True)
        for i in range(NO):
            ins = G.matmul(bank(1)[:, 0:32], lhsT=onesq, rhs=Mb[:, i, :], start=(i == 0), stop=(i == NO - 1))
        return ins
    K.op(K.pe, rk, reads=SR, writes=[R("ps", 0), R("ps", 1)])
    sop(K.dve, lambda: V.tensor_copy(out=cnt, in_=bank(1)[:, 0:32]), extra_r=[R("ps", 1)])
    sop(K.dve, lambda: V.tensor_tensor(out=cmp8, in0=bc(cnt.unsqueeze(2), [128, NE, 8]), in1=bc(thr.unsqueeze(1), [128, NE, 8]),
                                       op=ALU.is_gt))
    sop(K.dve, lambda: V.tensor_reduce(out=Tt, in_=cmp8, axis=AX.X, op=ALU.add))
    sop(K.dve, lambda: V.tensor_copy(out=sca, in_=Tt))
    cur, oth = sca, scb
    for sft in (1, 2, 4, 8, 16):
        sop(K.dve, lambda cur=cur, oth=oth: V.tensor_copy(out=oth, in_=cur))
        sop(K.dve, lambda cur=cur, oth=oth, sft=sft: V.tensor_tensor(out=oth[:, sft:NE], in0=cur[:, sft:NE], in1=cur[:, 0:NE - sft],
                                                                      op=ALU.add))
        cur, oth = oth, cur
    incl = cur
    sop(K.dve, lambda: V.tensor_tensor(out=offT, in0=incl, in1=Tt, op=ALU.subtract))
    sop(K.dve, lambda: V.tensor_scalar(out=off256, in0=offT, scalar1=256.0, scalar2=None, op0=ALU.mult))
    sop(K.dve, lambda: V.tensor_tensor(out=posf, in0=bank(0).rearrange("p (a b) -> p a b", a=NO),
                                       in1=bc(off256.unsqueeze(1), [128, T, NE]), op=ALU.add), extra_r=[R("ps", 0)])
    for k, Ak in enumerate((A1, A2)):
        sop(K.dve, lambda Ak=Ak: V.tensor_tensor(out=ptmp, in0=posf, in1=Ak, op=ALU.mult))
        sop(K.dve, lambda k=k: V.tensor_reduce(out=posk[:, k, :], in_=ptmp, axis=AX.X, op=ALU.add))
    sop(K.dve, lambda: V.tensor_copy(out=posu, in_=posk), extra_w=[R("posu")])
    dSc = K.dsem("scat")
    for i in range(NO):
        for k in range(2):
            K._waits(K.pool, [R("posu"), R("h2tok", i)], [R("xs_d")])
            P.indirect_dma_start(out=xs_d[:, :], out_offset=bass.IndirectOffsetOnAxis(posu[:, k, i:i + 1], 0),
                                 in_=h2tok[:, i, :], in_offset=None).then_inc(dSc.sem, 16)
            dSc.cnt += 16
    K._commit((dSc.sem, dSc.cnt), [R("posu")] + RL("h2tok", range(NO)), [R("xs_d")])
    sop(K.dve, lambda: V.tensor_tensor(out=indg, in0=bc(jv.unsqueeze(1), [128, NE, 48]), in1=bc(offT.unsqueeze(2), [128, NE, 48]),
                                       op=ALU.is_ge))
    sop(K.dve, lambda: V.tensor_tensor(out=indl, in0=bc(jv.unsqueeze(1), [128, NE, 48]), in1=bc(incl.unsqueeze(2), [128, NE, 48]),
                                       op=ALU.is_lt))
    sop(K.dve, lambda: V.tensor_tensor(out=indg, in0=indg, in1=indl, op=ALU.mult))
    sop(K.dve, lambda: V.tensor_reduce(out=anyf, in_=indg.rearrange("p e j -> p j e"), axis=AX.X, op=ALU.add))
    sop(K.dve, lambda: V.tensor_tensor(out=indl, in0=indg, in1=bc(ev.unsqueeze(2), [128, NE, 48]), op=ALU.mult))
    sop(K.dve, lambda: V.tensor_reduce(out=eidf, in_=indl.rearrange("p e j -> p j e"), axis=AX.X, op=ALU.add))
    sop(K.dve, lambda: V.tensor_tensor(out=sca, in0=cnt, in1=off256, op=ALU.add))
    sop(K.dve, lambda: V.tensor_scalar(out=jv256, in0=jv, scalar1=256.0, scalar2=None, op0=ALU.mult))
    sop(K.dve, lambda: V.tensor_tensor(out=indl, in0=bc(sca.unsqueeze(2), [128, NE, 48]), in1=bc(jv256.unsqueeze(1), [128, NE, 48]),
                                       op=ALU.subtract))
    sop(K.dve, lambda: V.tensor_tensor(out=indl, in0=indl, in1=indg, op=ALU.mult))
    sop(K.dve, lambda: V.tensor_reduce(out=real1, in_=indl.rearrange("p e j -> p j e"), axis=AX.X, op=ALU.add))
    sop(K.dve, lambda: V.tensor_scalar(out=p2, in0=pidx, scalar1=2.0, scalar2=None, op0=ALU.mult))
    sop(K.dve, lambda: V.tensor_scalar(out=sidf, in0=real1, scalar1=p2[:, 0:1], scalar2=None, op0=ALU.is_gt))
    sop(K.dve, lambda: V.tensor_scalar(out=sidf, in0=sidf, scalar1=-1.0e6, scalar2=1.0e6, op0=ALU.mult, op1=ALU.add))
    sop(K.dve, lambda: V.tensor_scalar(out=jv256, in0=jv, scalar1=128.0, scalar2=pidx[:, 0:1], op0=ALU.mult, op1=ALU.add))
    sop(K.dve, lambda: V.tensor_tensor(out=sidf, in0=sidf, in1=jv256, op=ALU.add))
    sop(K.dve, lambda: V.tensor_copy(out=sidx, in_=sidf), extra_w=[R("sidx")])
    sop(K.dve, lambda: V.tensor_scalar(out=anyf, in0=anyf, scalar1=-32.0, scalar2=32.0, op0=ALU.mult, op1=ALU.add))
    sop(K.dve, lambda: V.tensor_tensor(out=eidf, in0=eidf, in1=anyf, op=ALU.add))
    sop(K.dve, lambda: V.tensor_scalar(out=eidf, in0=eidf, scalar1=128.0, scalar2=pidx[:, 0:1], op0=ALU.mult, op1=ALU.add))
    sop(K.dve, lambda: V.tensor_copy(out=widx, in_=eidf), extra_w=[R("widx")])
    alw = Alloc(68 * KIB, 116 * KIB)
    wall = [alw([128, 3 * 2048], BF16) for i in range(4)]
    wgb = [w[:, 0:2048].rearrange("p (a b) -> p a b", a=8) for w in wall]
    wub = [w[:, 2048:4096].rearrange("p (a b) -> p a b", a=8) for w in wall]
    wdb = [w[:, 4096:6144].rearrange("p (a b) -> p a b", a=2) for w in wall]
    dG = [K.dsem("g%d" % i) for i in range(4)]
    breg = P.to_reg(NE * 128 - 1)
    for i in range(4):
        K.op(K.act, lambda i=i: A.memzero(wall[i]), writes=[R("wall", i)])

    def gather_w(q, j):
        s4 = q % 4
        K._waits(K.pool, [R("widx"), R("wbf")], [R("wall", s4)])
        P.indirect_dma_start(out=wall[s4], out_offset=None, in_=wbf_d[:, :],
                             in_offset=bass.IndirectOffsetOnAxis(widx[:, j:j + 1], 0),
                             bounds_check=breg, oob_is_err=False).then_inc(dG[s4].sem, 16)
        dG[s4].cnt += 16
        K._commit((dG[s4].sem, dG[s4].cnt), [R("widx"), R("wbf")], [R("wall", s4)])
    order = []
    for g3 in range(16):
        order += [2 * g3, 2 * g3 + 1, 32 + g3]
    assert sorted(order) == list(range(NTL))

    for q in range(4):
        gather_w(q, order[q])
    K.barrier()

    al = Alloc(116 * KIB, LIM)
    xs = [al([128, 2, D], BF16) for i in range(4)]
    xT = [al([128, 8, 256], BF16) for i in range(2)]
    ssb = [al([128, 512], F32) for i in range(2)]
    hid = [al([128, 2, 256], BF16) for i in range(2)]
    ysb = [al([128, 2, D], F32) for i in range(2)]
    gt2B = al([128, D], F32)
    gfB = al([128, D], F32)
    yg = [al([128, 2, D], F32) for i in range(2)]
    yt = [al([128, D], F32) for i in range(2)]
    junk = al([128, D], BF16)

    dXs = [K.dsem("xs%d" % i) for i in range(4)]
    dYs = [K.dsem("ys%d" % i) for i in range(2)]
    dF = K.dsem("fin")
    K.op(K.dve, lambda: V.memset(st, 0.0), writes=[R("st")])
    K.dma(K.sp, [(gt2B, modB(5)), (gfB, gB_d[:, 2 * D:3 * D])], dF, writes=[R("gt2B"), R("gfB")])
    sreg = P.to_reg(NTL * 128 - 1)
    for i in range(4):
        K.op(K.act, lambda i=i: A.memzero(xs[i].rearrange("p a b -> p (a b)")), writes=[R("xs", i)])
    xs_p = xs_d.rearrange("(n r) d -> n (r d)", r=2)
    ys_p = ys_d.rearrange("(n r) d -> n (r d)", r=2)

    def stM1(q, j):
        K._waits(K.pool, [R("sidx"), R("xs_d")], [R("xs", q % 4)])
        P.indirect_dma_start(out=xs[q % 4].rearrange("p a b -> p (a b)"), out_offset=None, in_=xs_p,
                             in_offset=bass.IndirectOffsetOnAxis(sidx[:, j:j + 1], 0),
                             bounds_check=sreg, oob_is_err=False).then_inc(dXs[q % 4].sem, 16)
        dXs[q % 4].cnt += 16
        K._commit((dXs[q % 4].sem, dXs[q % 4].cnt), [R("sidx"), R("xs_d")], [R("xs", q % 4)])

    def stM2(q, j):
        s3, s2 = q % 4, q % 2

        def tr():
            ins = None
            for s_ in range(2):
                for kc in range(8):
                    ins = G.transpose(out=bankb(s_)[:, kc * 128:(kc + 1) * 128], in_=xs[s3][:, s_, kc * 128:(kc + 1) * 128],
                                      identity=ident)
            return ins
        K.op(K.pe, tr, reads=[R("xs", s3), R("ident")], writes=[R("ps", 0), R("ps", 1)])
        K.op(K.act, lambda: A.copy(out=xT[s2][:, :, 0:128], in_=bankb(0).rearrange("p (a b) -> p a b", a=8)),
             reads=[R("ps", 0)], writes=[R("xT", s2)])
        K.op(K.dve, lambda: V.tensor_copy(out=xT[s2][:, :, 128:256], in_=bankb(1).rearrange("p (a b) -> p a b", a=8)),
             reads=[R("ps", 1)], writes=[R("xT", s2)])

    def stM3(q, j):
        s4, s2 = q % 4, q % 2

        def mm():
            ins = None
            for (wb, b) in ((wgb[s4], 2), (wub[s4], 3)):
                for fc in range(2):
                    for kc in range(8):
                        ins = G.matmul(bank(b)[:, fc * 256:(fc + 1) * 256], lhsT=wb[:, kc, fc * 128:(fc + 1) * 128],
                                       rhs=xT[s2][:, kc, :], start=(kc == 0), stop=(kc == 7))
            return ins
        K.op(K.pe, mm, reads=[R("wall", s4), R("xT", s2)], writes=[R("ps", 2), R("ps", 3)])
        K.op(K.act, lambda: A.activation(out=ssb[s2], in_=bank(2), func=AF.Silu), reads=[R("ps", 2)], writes=[R("ssb", s2)])
        K.op(K.dve, lambda: V.tensor_tensor(out=hid[s2].rearrange("p a b -> p (a b)"), in0=ssb[s2], in1=bank(3), op=ALU.mult),
             reads=[R("ssb", s2), R("ps", 3)], writes=[R("hid", s2)])

    def stM4(q, j):
        s4, s2 = q % 4, q % 2
        for sh in range(2):
            b0 = 4 + 2 * sh

            def mm(sh=sh, b0=b0):
                ins = None
                for dh in range(2):
                    for fc in range(2):
                        ins = G.matmul(bank(b0 + dh), lhsT=hid[s2][:, fc, sh * 128:(sh + 1) * 128],
                                       rhs=wdb[s4][:, fc, dh * 512:(dh + 1) * 512], start=(fc == 0), stop=(fc == 1))
                return ins
            K.op(K.pe, mm, reads=[R("hid", s2), R("wall", s4)], writes=[R("ps", b0), R("ps", b0 + 1)])
            K.op(K.dve, lambda sh=sh, b0=b0: V.tensor_tensor(out=ysb[s2][:, sh, :], in0=bank(b0, 2), in1=gt2B, op=ALU.mult),
                 reads=[R("ps", b0), R("ps", b0 + 1), R("gt2B")], writes=[R("ysb", s2)])

    def stM4b(q, j):
        s2 = q % 2
        K._waits(K.pool, [R("sidx"), R("ysb", s2)], [R("ys_d", j)])
        P.indirect_dma_start(out=ys_p, out_offset=bass.IndirectOffsetOnAxis(sidx[:, j:j + 1], 0),
                             in_=ysb[s2].rearrange("p a b -> p (a b)"), in_offset=None, bounds_check=sreg,
                             oob_is_err=False).then_inc(dYs[s2].sem, 16)
        dYs[s2].cnt += 16
        K._commit((dYs[s2].sem, dYs[s2].cnt), [R("sidx"), R("ysb", s2)], [R("ys_d", j)])

    stM1(0, order[0])
    stM1(1, order[1])
    for i in range(NTL + 3):
        if 0 <= i - 3 < NTL:
            stM4(i - 3, order[i - 3])
            if i + 1 < NTL:
                gather_w(i + 1, order[i + 1])
        if i + 2 < NTL:
            stM1(i + 2, order[i + 2])
        if 0 <= i - 1 < NTL:
            stM2(i - 1, order[i - 1])
        if 0 <= i - 2 < NTL:
            stM3(i - 2, order[i - 2])
        if 0 <= i - 3 < NTL:
            stM4b(i - 3, order[i - 3])

    dO = [K.dsem("o%d" % i) for i in range(2)]
    dYg = [K.dsem("yg%d" % i) for i in range(2)]
    y_t = y_d.rearrange("(t p) d -> t p d", p=128)
    for t in range(NO):
        sl = t % 2
        ysr = [R("ys_d", j) for j in range(NTL)]
        K._waits(K.pool, ysr + [R("posu")], [R("yg", sl)])
        for k in range(2):
            P.indirect_dma_start(out=yg[sl][:, k, :], out_offset=None, in_=ys_d[:, :],
                                 in_offset=bass.IndirectOffsetOnAxis(posu[:, k, t:t + 1], 0)).then_inc(dYg[sl].sem, 16)
            dYg[sl].cnt += 16
        K._commit((dYg[sl].sem, dYg[sl].cnt), [R("posu")], [R("yg", sl)])
        for k in range(2):
            K.op(K.dve, lambda t=t, k=k, sl=sl: V.scalar_tensor_tensor(out=acc[:, t, :], in0=yg[sl][:, k, :],
                                                                       scalar=w12[:, k, t:t + 1], in1=acc[:, t, :],
                                                                       op0=ALU.mult, op1=ALU.add),
                 reads=[R("yg", sl), R("w12"), R("acc", t)], writes=[R("acc", t)])
        K.op(K.act, lambda t=t: A.activation(out=junk, in_=acc[:, t, :], func=AF.Square, accum_out=st[:, t:t + 1]),
             reads=[R("acc", t), R("st")], writes=[R("junk"), R("stx", t)])
        rstd_cols(st[:, t:t + 1], st[:, 32 + t:33 + t], 1.0 / D, [R("stx", t)], [R("strx", t)])
        K.op(K.dve, lambda t=t, sl=sl: V.scalar_tensor_tensor(out=yt[sl], in0=acc[:, t, :], scalar=st[:, 32 + t:33 + t], in1=gfB,
                                                              op0=ALU.mult, op1=ALU.mult),
             reads=[R("acc", t), R("strx", t), R("gfB")], writes=[R("yt", sl)])
        K.dma(K.sp, [(y_t[t], yt[sl])], dO[sl], reads=[R("yt", sl)])
    for d in dO:
        K.sp.h.wait_ge(d.sem, d.cnt)
    es.close()
    return nc


_ROPE_THETA = 10000.0


def _rope_table():
    out = np.zeros((S, 192), np.float32)
    pos = np.arange(S, dtype=np.float32)[:, None]
    for (dim, o) in ((64, 0), (32, 128)):
        inv = (1.0 / (_ROPE_THETA ** (np.arange(0, dim, 2, dtype=np.float32) / dim))).astype(np.float32)
        ang = (pos * inv[None, :]).astype(np.float32)
        c, s_ = np.cos(ang).astype(np.float32), np.sin(ang).astype(np.float32)
        out[:, o:o + dim] = np.concatenate([c, c], axis=1)
        out[:, o + dim:o + 2 * dim] = np.concatenate([-s_, s_], axis=1)
    return out


_NC_CACHE = {}


def kernel(x, c, w_ada, b_ada, g_norm1, w_in, g_q_lora, w_uq, g_kv_lora, w_ukv, sink,
           g_out_swa, g_out_mla, w_out, g_norm2, w_router_group, b_router_group,
           w_router_expert, b_router_expert, w_exp_gate, w_exp_up, w_exp_down, g_final):
    f = lambda a: np.ascontiguousarray(np.asarray(a, dtype=np.float32))
    x = f(x); c = f(c)
    if "nc" not in _NC_CACHE:
        _NC_CACHE["nc"] = build_program()
    nc = _NC_CACHE["nc"]
    rope = _rope_table()
    w_in0 = f(w_in)[0]
    perm = np.concatenate([np.arange(0, 768), np.arange(960, 1120), np.arange(768, 960)])
    w_in_p = np.ascontiguousarray(w_in0[:, perm])
    gB = np.ascontiguousarray(np.broadcast_to(
        np.concatenate([f(g_norm1)[0], f(g_norm2)[0], f(g_final)])[None, :], (128, 3 * D)))
    w_r = np.ascontiguousarray(np.concatenate([f(w_router_group)[0], f(w_router_expert)[0]], axis=1))
    b_r = np.concatenate([f(b_router_group)[0], f(b_router_expert)[0]])
    gcat = np.concatenate([f(g_out_swa)[0], f(g_out_mla)[0]])
    jj = np.arange(128)[:, None]
    rr = np.arange(128)[None, :]
    mprev = (jj >= rr).astype(np.float32)
    mnext = (jj <= rr).astype(np.float32)
    shared = {
        "w_ada": f(w_ada)[0], "b_ada": f(b_ada), "w_in": w_in_p, "w_uq": f(w_uq)[0], "w_ukv": f(w_ukv)[0],
        "w_out": f(w_out)[0], "w_r": w_r, "w_g": np.ascontiguousarray(f(w_exp_gate)[0].reshape(NE, 8, 128, 256).transpose(0, 2, 1, 3).reshape(NE * 128, 2048)),
        "w_u": np.ascontiguousarray(f(w_exp_up)[0].reshape(NE, 8, 128, 256).transpose(0, 2, 1, 3).reshape(NE * 128, 2048)),
        "w_d": np.ascontiguousarray(f(w_exp_down)[0].reshape(NE, 2, 128, D).transpose(0, 2, 1, 3).reshape(NE * 128, 2048)),
        "gB": gB,
    }
    in_maps = []
    for core in range(8):
        b, hf = core // 2, core % 2
        own = slice(hf * 2048, (hf + 1) * 2048)
        oth = slice((1 - hf) * 2048, (2 - hf) * 2048)
        prm = np.zeros((128, 64), np.float32)
        prm[:, 0:8] = c[b].reshape(8, 128).T
        prm[0:96, 8:10] = f(g_q_lora)[0].reshape(2, 96).T
        prm[:, 10] = f(g_kv_lora)[0]
        prm[:, 11:19] = gcat.reshape(8, 128).T
        prm[:, 20:28] = f(sink)[0][None, :]
        prm[:, 28:64] = b_r[None, :]
        msk = np.stack([mprev, mnext, mprev * float(hf == 1), mnext * float(hf == 0)], axis=1).reshape(128, 512)
        m = dict(shared)
        m["x"] = np.ascontiguousarray(np.concatenate([x[b, own], x[b, oth]], axis=0))
        m["rope"] = np.ascontiguousarray(np.concatenate([rope[own], rope[oth]], axis=0))
        m["prm"] = prm
        m["g1p"] = np.ascontiguousarray(f(g_norm1)[0].reshape(8, 128).T)
        m["msk"] = np.ascontiguousarray(msk.astype(np.float32))
        in_maps.append(m)
    res = run_bass_kernel_spmd(nc, in_maps, core_ids=list(range(8)))
    out = np.zeros((4, S, D), np.float32)
    for core in range(8):
        b, hf = core // 2, core % 2
        out[b, hf * 2048:(hf + 1) * 2048] = res.results[core]["y"]
    return out
```

```python
import numpy as np
from contextlib import ExitStack

import concourse.bass as bass
import concourse.mybir as mybir
from concourse.bass_utils import run_bass_kernel_spmd

F32 = mybir.dt.float32
BF16 = mybir.dt.bfloat16
U8 = mybir.dt.uint8
U32 = mybir.dt.uint32
AF = mybir.ActivationFunctionType
ALU = mybir.AluOpType
AX = mybir.AxisListType

D = 1024
S = 4096
NT = 32
NO = 16
EPS = 1e-6
NE = 32
KIB = 1024


class Region:
    __slots__ = ("name", "lw", "rd")

    def __init__(self, name):
        self.name = name
        self.lw = None
        self.rd = []


class Eng:
    def __init__(self, K, name, h):
        self.name = name
        self.h = h
        self.sem = K.es.enter_context(K.nc.semaphore("tl_" + name))
        self.cnt = 0
        self.waited = {}


class DSem:
    def __init__(self, K, name):
        self.sem = K.es.enter_context(K.nc.semaphore("d_" + name))
        self.cnt = 0


class KB:
    def __init__(self, nc, es):
        self.nc = nc
        self.es = es
        self.pe = Eng(self, "pe", nc.tensor)
        self.dve = Eng(self, "dve", nc.vector)
        self.act = Eng(self, "act", nc.scalar)
        self.pool = Eng(self, "pool", nc.gpsimd)
        self.sp = Eng(self, "sp", nc.sync)
        self.engs = [self.pe, self.dve, self.act, self.pool, self.sp]
        self.dsems = []

    def dsem(self, name):
        d = DSem(self, name)
        self.dsems.append(d)
        return d

    def _waits(self, eng, reads, writes):
        need = {}

        def add(t):
            if t is None:
                return
            s, v = t
            k = id(s)
            if k not in need or need[k][1] < v:
                need[k] = (s, v)

        for r in reads:
            add(r.lw)
        for w in writes:
            add(w.lw)
            for t in w.rd:
                add(t)
        for k, (s, v) in need.items():
            if eng.waited.get(k, 0) >= v:
                continue
            if eng.name == "pe" and s is eng.sem:
                continue
            eng.h.wait_ge(s, v)
            eng.waited[k] = v

    def _commit(self, ticket, reads, writes):
        for r in reads:
            r.rd.append(ticket)
            if len(r.rd) > 48:
                best = {}
                for (s, v) in r.rd:
                    if id(s) not in best or best[id(s)][1] < v:
                        best[id(s)] = (s, v)
                r.rd = list(best.values())
        for w in writes:
            w.lw = ticket
            w.rd = []

    def op(self, eng, fn, reads=(), writes=()):
        self._waits(eng, reads, writes)
        ins = fn()
        eng.cnt += 1
        ins.then_inc(eng.sem, 1)
        self._commit((eng.sem, eng.cnt), reads, writes)
        return ins

    def dma(self, q, pairs, dsem, reads=(), writes=(), **kw):
        self._waits(q, reads, writes)
        for (o, i) in pairs:
            q.h.dma_start(out=o, in_=i, **kw).then_inc(dsem.sem, 16)
            dsem.cnt += 16
        self._commit((dsem.sem, dsem.cnt), reads, writes)

    def barrier(self):
        for e in self.engs:
            for o in self.engs:
                if o is e or o.cnt == 0:
                    continue
                if e.waited.get(id(o.sem), 0) < o.cnt:
                    e.h.wait_ge(o.sem, o.cnt)
                    e.waited[id(o.sem)] = o.cnt
            for d in self.dsems:
                if d.cnt and e.waited.get(id(d.sem), 0) < d.cnt:
                    e.h.wait_ge(d.sem, d.cnt)
                    e.waited[id(d.sem)] = d.cnt


_DT_SIZE = {F32: 4, BF16: 2, U32: 4}


def build_program():
    nc = bass.Bass("TRN2", target_bir_lowering=False)
    es = ExitStack()
    K = KB(nc, es)
    V, A, P, G = nc.vector, nc.scalar, nc.gpsimd, nc.tensor

    def din(name, shape):
        return nc.dram_tensor(name, shape, F32, kind="ExternalInput").ap()

    x_d = din("x", [S, D])
    prm_d = din("prm", [128, 64])
    g1p_d = din("g1p", [128, 8])
    gB_d = din("gB", [128, 3 * D])
    rope_d = din("rope", [S, 192])
    msk_d = din("msk", [128, 512])
    wada_d = din("w_ada", [D, 6 * D])
    bada_d = din("b_ada", [1, 6 * D])
    win_d = din("w_in", [D, 1120])
    wuq_d = din("w_uq", [192, 768])
    wukv_d = din("w_ukv", [128, 1024])
    wout_d = din("w_out", [D, D])
    wr_d = din("w_r", [D, 36])
    wg_d = din("w_g", [NE * 128, 2048])
    wu_d = din("w_u", [NE * 128, 2048])
    wd_d = din("w_d", [NE * 128, 2048])
    y_d = nc.dram_tensor("y", [NO * 128, D], F32, kind="ExternalOutput").ap()
    mod_d = nc.dram_tensor("mod_scratch", [1, 6 * D], F32, kind="Internal").ap()
    NTL = 48
    xs_d = nc.dram_tensor("xs_scratch", [NTL * 256, D], BF16, kind="Internal").ap()
    ys_d = nc.dram_tensor("ys_scratch", [NTL * 256, D], F32, kind="Internal").ap()
    wo_d = nc.dram_tensor("wo_scratch", [D, D], BF16, kind="Internal").ap()
    wbf_d = nc.dram_tensor("wbf_all", [NE * 128, 3 * 2048], BF16, kind="Internal").ap()

    arena = nc.alloc_sbuf_tensor("arena", [128, 206 * KIB], U8)
    ps = nc.alloc_psum_tensor("ps", [128, 4096], F32)
    LIM = 206 * KIB

    def view(off, shape, dt, p0=0):
        off = int(off)
        n = 1
        for s_ in shape[1:]:
            n *= s_
        assert off + n * _DT_SIZE[dt] <= LIM, (off, shape)
        ap = arena[p0:p0 + shape[0], off:off + n * _DT_SIZE[dt]].bitcast(dt)
        if len(shape) == 3:
            ap = ap.rearrange("p (a b) -> p a b", a=shape[1])
        elif len(shape) == 4:
            ap = ap.rearrange("p (a b c) -> p a b c", a=shape[1], b=shape[2])
        return ap

    class Alloc:
        def __init__(self, start, end):
            self.o = start
            self.end = end

        def __call__(self, shape, dt, p0=0):
            n = 1
            for s_ in shape[1:]:
                n *= s_
            nb = (n * _DT_SIZE[dt] + 31) // 32 * 32
            v = view(self.o, shape, dt, p0)
            self.o += nb
            assert self.o <= self.end, (self.o, self.end, shape)
            return v

    def bank(b, n=1):
        return ps[:, b * 512:(b + n) * 512]

    def bankb(b):
        return ps[:, b * 512:(b + 1) * 512].bitcast(BF16)

    def bc(ap, shape):
        return ap.to_broadcast(shape)

    def modB(i):
        return mod_d[0, i * D:(i + 1) * D].partition_broadcast(128)

    Rg = {}

    def R(*key):
        if key not in Rg:
            Rg[key] = Region(str(key))
        return Rg[key]

    def RL(name, idxs):
        return [R(name, i) for i in idxs]

    def h3(ap, h):
        return ap.rearrange("p (h d) -> p h d", h=h)

    prm = view(0, [128, 64], F32)
    ident = view(256, [128, 128], BF16)
    ones_b = view(512, [128, 4], BF16)
    sT = view(528, [128, 8], F32)
    eps_t = view(560, [128, 1], F32)
    es8 = view(576, [1, 8], F32)
    st = view(640, [128, 160], F32)
    vsink = view(1280, [1, 96], BF16)
    g1P = view(1472, [128, 8], F32)
    a1P = view(1504, [128, 8], F32)
    sh1P = view(1536, [128, 8], F32)
    sh1Pb = view(1568, [128, 8], BF16)
    onesrow = view(1600, [1, 128], BF16)
    L2 = 2 * KIB
    L3 = 26 * KIB
    L4 = 86 * KIB
    PA = 118 * KIB

    dPrm = K.dsem("prm")
    K.dma(K.sp, [(prm, prm_d[:, :])], dPrm, writes=[R("prm")])
    K.op(K.pool, lambda: P.memset(ident, 1.0), writes=[R("ident")])
    K.op(K.pool, lambda: P.affine_select(out=ident, in_=ident, pattern=[[-1, 128]],
                                         compare_op=ALU.is_equal, fill=0.0, base=0,
                                         channel_multiplier=1),
         reads=[R("ident")], writes=[R("ident")])
    K.op(K.dve, lambda: V.memset(ones_b, 1.0), writes=[R("ones")])
    K.op(K.dve, lambda: V.memset(st, 0.0), writes=[R("st")])
    K.op(K.dve, lambda: V.memset(eps_t, EPS), writes=[R("eps")])
    K.op(K.dve, lambda: V.memset(vsink[0:1, 0:64], 0.0), writes=[R("vsink")])
    K.op(K.dve, lambda: V.memset(vsink[0:1, 64:96], 1.0), writes=[R("vsink")])
    K.op(K.act, lambda: A.activation(out=sT, in_=prm[:, 0:8], func=AF.Silu),
         reads=[R("prm")], writes=[R("sT")])

    def rstd_cols(src, dst, n_inv, rd, wr):
        K.op(K.act, lambda: A.activation(out=dst, in_=src, func=AF.Ln, scale=n_inv, bias=eps_t[:, 0:1]),
             reads=list(rd) + [R("eps")], writes=list(wr))
        K.op(K.act, lambda: A.activation(out=dst, in_=dst, func=AF.Exp, scale=-0.5),
             reads=list(wr), writes=list(wr))

    al = Alloc(4 * KIB, LIM)
    wa = [al([128, 8, 512], F32) for i in range(3)]
    bada = al([1, 6 * D], F32)
    modrow = al([1, 6 * D], F32)
    dWa = [K.dsem("wa%d" % i) for i in range(3)]
    dWin = K.dsem("win")
    win0 = view(PA, [128, 8, 1120], BF16)
    K.dma(K.pool, [(win0, win_d.rearrange("(kc p) n -> p kc n", p=128))], dWin, writes=[R("win")])
    dBa = K.dsem("bada")
    wada_v = wada_d.rearrange("(kc p) n -> p kc n", p=128)
    K.dma(K.sp, [(bada, bada_d[:, :])], dBa, writes=[R("bada")])
    for j in range(12):
        sl = j % 3
        K.dma(K.sp, [(wa[sl], wada_v[:, :, j * 512:(j + 1) * 512])], dWa[sl], writes=[R("wa", sl)])
        pb = bank(j % 2)

        def mm(sl=sl, pb=pb):
            ins = None
            for kc in range(8):
                ins = G.matmul(pb[0:1, :], lhsT=sT[:, kc:kc + 1], rhs=wa[sl][:, kc, :],
                               start=(kc == 0), stop=(kc == 7))
            return ins
        K.op(K.pe, mm, reads=[R("wa", sl), R("sT")], writes=[R("ps", j % 2)])
        K.op(K.dve, lambda j=j, pb=pb: V.tensor_tensor(out=modrow[0:1, j * 512:(j + 1) * 512], in0=pb[0:1, :],
                                                       in1=bada[0:1, j * 512:(j + 1) * 512], op=ALU.add),
             reads=[R("ps", j % 2), R("bada")], writes=[R("modrow")])
    dM = K.dsem("mod")
    K.dma(K.sp, [(mod_d[:, :], modrow)], dM, reads=[R("modrow")], writes=[R("mod_d")])
    K.barrier()

    ckvnT = view(L2, [128, S], BF16)
    krT = view(L2 + 8 * KIB, [96, S], BF16)
    cqnT = view(L2 + 16 * KIB, [96, 2, NO * 128], BF16)
    qTa = view(L3, [64, 8, NO * 128], BF16)
    kTa = view(L3 + 32 * KIB, [64, 2, S], BF16)
    va = view(L3 + 48 * KIB, [128, NT, 2, 96], BF16)

    al = Alloc(PA, LIM)
    win = al([128, 8, 1120], BF16)
    b1row = al([1, 1120], BF16)
    xt = [al([128, D], F32) for i in range(2)]
    hb = [al([128, D], BF16) for i in range(3)]
    hT = [al([128, 8, 128], BF16) for i in range(3)]
    projS = [al([128, 1120], F32) for i in range(3)]
    rt = [al([128, 192], F32) for i in range(4)]
    t1 = [al([128, 640], F32) for i in range(2)]
    t2 = [al([128, 640], F32) for i in range(2)]
    t1r = [al([128, 32], F32) for i in range(2)]
    t2r = [al([128, 32], F32) for i in range(2)]
    qkr = [al([128, 640], BF16) for i in range(2)]
    ckvn = [al([128, 128], BF16) for i in range(2)]
    krs = [al([128, 96], BF16) for i in range(2)]
    cqn = [al([128, 192], BF16) for i in range(2)]
    junk = al([128, D], BF16)
    junk2 = al([128, 192], BF16)
    assert al.o <= 198 * KIB, al.o
    dW = K.dsem("wA")
    K.dma(K.sp, [(g1P, g1p_d[:, :]),
                 (a1P, mod_d[0, D:2 * D].rearrange("(kc p) -> p kc", p=128)),
                 (sh1P, mod_d[0, 0:D].rearrange("(kc p) -> p kc", p=128))], dW,
          reads=[R("mod_d")], writes=[R("a1P"), R("sh1P")], allow_slow_non_contiguous=True)
    K.op(K.dve, lambda: V.scalar_tensor_tensor(out=a1P, in0=a1P, scalar=1.0, in1=g1P, op0=ALU.add, op1=ALU.mult),
         reads=[R("a1P")], writes=[R("a1P")])
    K.op(K.dve, lambda: V.tensor_copy(out=sh1Pb, in_=sh1P), reads=[R("sh1P")], writes=[R("sh1Pb")])
    K.op(K.dve, lambda: V.memset(onesrow, 1.0), writes=[R("onesrow")])

    def b1mm():
        ins = None
        for (b, c0, n) in [(2, 0, 512), (3, 512, 512), (4, 1024, 96)]:
            for kc in range(8):
                ins = G.matmul(bank(b)[0:1, 0:n], lhsT=sh1Pb[:, kc:kc + 1], rhs=win[:, kc, c0:c0 + n],
                               start=(kc == 0), stop=(kc == 7))
        return ins
    K.op(K.pe, b1mm, reads=[R("sh1Pb"), R("win")], writes=[R("ps", 2), R("ps", 3), R("ps", 4)])
    for (b, c0, n) in [(2, 0, 512), (3, 512, 512), (4, 1024, 96)]:
        K.op(K.dve, lambda b=b, c0=c0, n=n: V.tensor_copy(out=b1row[0:1, c0:c0 + n], in_=bank(b)[0:1, 0:n]),
             reads=[R("ps", b)], writes=[R("b1row")])
    for kc in range(8):
        K.op(K.dve, lambda kc=kc: V.tensor_scalar(out=win[:, kc, :], in0=win[:, kc, :], scalar1=a1P[:, kc:kc + 1], scalar2=None,
                                                  op0=ALU.mult),
             reads=[R("win"), R("a1P")], writes=[R("win")])
    K.op(K.pool, lambda: P.memset(va[:, :, :, 64:96], 1.0), writes=RL("va", range(NT)))
    for i in range(2):
        K.op(K.pool, lambda i=i: P.memset(krs[i], 0.0), writes=[R("krs", i)])

    dX = [K.dsem("x%d" % i) for i in range(2)]
    x_t = x_d.rearrange("(t p) d -> t p d", p=128)
    rope_t = rope_d.rearrange("(t p) d -> t p d", p=128)
    CQS = (128.0 / 192.0) ** 0.5

    def rope(src3, cs, sn, half, o1, o2, dst, rd, r1, r2, wr_dst, nh):
        w = 2 * half
        K.op(K.dve, lambda: V.tensor_tensor(out=o1, in0=src3, in1=bc(cs.unsqueeze(1), [128, nh, w]), op=ALU.mult),
             reads=rd, writes=[r1])
        K.op(K.dve, lambda: V.tensor_tensor(out=o2[:, :, 0:half], in0=src3[:, :, half:w],
                                            in1=bc(sn[:, 0:half].unsqueeze(1), [128, nh, half]), op=ALU.mult),
             reads=rd, writes=[r2])
        K.op(K.dve, lambda: V.tensor_tensor(out=o2[:, :, half:w], in0=src3[:, :, 0:half],
                                            in1=bc(sn[:, half:w].unsqueeze(1), [128, nh, half]), op=ALU.mult),
             reads=rd, writes=[r2])
        K.op(K.pool, lambda: P.tensor_tensor(out=dst, in0=o1, in1=o2, op=ALU.add),
             reads=[r1, r2], writes=wr_dst)

    def stA1(t):
        own = t < NO
        sl = t % 2
        K.dma(K.sp, [(xt[sl], x_t[t]), (rt[t % 4], rope_t[t])], dX[sl], writes=[R("xt", sl), R("rt", t % 4)])
        K.op(K.act, lambda sl=sl, t=t: A.activation(out=junk, in_=xt[sl], func=AF.Square, accum_out=st[:, t:t + 1]),
             reads=[R("xt", sl)], writes=[R("junk"), R("stx", t)])
        rstd_cols(st[:, t:t + 1], st[:, 32 + t:33 + t], 1.0 / D, [R("stx", t)], [R("strx", t)])
        K.op(K.act, lambda sl=sl, t=t: A.activation(out=hb[t % 3], in_=xt[sl], func=AF.Copy, scale=st[:, 32 + t:33 + t]),
             reads=[R("xt", sl), R("strx", t)], writes=[R("hb", t % 3)])

    def stA2(t):
        own = t < NO
        sl = t % 2

        def tr(sl=sl):
            ins = None
            for kc in range(8):
                ins = G.transpose(out=bankb(sl)[:, kc * 128:(kc + 1) * 128], in_=hb[t % 3][:, kc * 128:(kc + 1) * 128],
                                  identity=ident)
            return ins
        K.op(K.pe, tr, reads=[R("hb", t % 3), R("ident")], writes=[R("ps", sl)])
        K.op(K.act, lambda sl=sl: A.copy(out=hT[t % 3], in_=bankb(sl).rearrange("p (a b) -> p a b", a=8)),
             reads=[R("ps", sl)], writes=[R("hT", t % 3)])

    def stA2b(t):
        own = t < NO
        sl = t % 2
        chunks = [(2, 0, 512), (3, 512, 416), (4, 928, 192)] if own else [(3, 512, 416)]

        def proj(sl=sl, chunks=chunks):
            ins = None
            for (b, c0, n) in chunks:
                for kc in range(8):
                    G.matmul(bank(b)[:, 0:n], lhsT=hT[t % 3][:, kc, :], rhs=win[:, kc, c0:c0 + n],
                             start=(kc == 0), stop=False)
                ins = G.matmul(bank(b)[:, 0:n], lhsT=onesrow[0:1, :], rhs=b1row[0:1, c0:c0 + n], start=False, stop=True)
            return ins
        K.op(K.pe, proj, reads=[R("hT", t % 3), R("win"), R("b1row"), R("onesrow")], writes=[R("ps", b) for (b, _, _) in chunks])
        for (b, c0, n) in chunks:
            if b == 2:
                K.op(K.dve, lambda b=b, c0=c0, n=n, sl=sl: V.tensor_copy(out=projS[t % 3][:, c0:c0 + n], in_=bank(b)[:, 0:n]),
                     reads=[R("ps", b)], writes=[R("projS", t % 3, b)])
            else:
                K.op(K.act, lambda b=b, c0=c0, n=n, sl=sl: A.copy(out=projS[t % 3][:, c0:c0 + n], in_=bank(b)[:, 0:n]),
                     reads=[R("ps", b)], writes=[R("projS", t % 3, b)])

    def stA3(t):
        own = t < NO
        sl = t % 2
        pS = projS[t % 3]
        if own:
            rope(h3(pS[:, 0:640], 10), rt[t % 4][:, 0:64], rt[t % 4][:, 64:128], 32, h3(t1[sl], 10), h3(t2[sl], 10),
                 h3(qkr[sl], 10), [R("projS", t % 3, 2), R("projS", t % 3, 3), R("rt", t % 4)], R("t1", sl), R("t2", sl),
                 [R("qkr", sl)], 10)
        else:
            rope(h3(pS[:, 512:640], 2), rt[t % 4][:, 0:64], rt[t % 4][:, 64:128], 32, h3(t1[sl][:, 512:640], 2),
                 h3(t2[sl][:, 512:640], 2), h3(qkr[sl][:, 512:640], 2), [R("projS", t % 3, 3), R("rt", t % 4)],
                 R("t1", sl), R("t2", sl), [R("qkr", sl)], 2)
        rope(h3(pS[:, 896:928], 1), rt[t % 4][:, 128:160], rt[t % 4][:, 160:192], 16, h3(t1r[sl], 1), h3(t2r[sl], 1),
             h3(krs[sl][:, 64:96], 1), [R("projS", t % 3, 3), R("rt", t % 4)], R("t1r", sl), R("t2r", sl), [R("krs", sl)], 1)
        K.op(K.pool, lambda t=t, pS=pS: P.tensor_copy(out=va[:, t, :, 0:64], in_=h3(pS[:, 640:768], 2)),
             reads=[R("projS", t % 3, 3)], writes=[R("va", t)])
        K.op(K.act, lambda pS=pS, t=t: A.activation(out=junk2[:, 0:128], in_=pS[:, 768:896], func=AF.Square,
                                                    accum_out=st[:, 64 + 2 * t:65 + 2 * t]),
             reads=[R("projS", t % 3, 3)], writes=[R("junk2"), R("stk", t)])
        if own:
            K.op(K.act, lambda pS=pS, t=t: A.activation(out=junk2, in_=pS[:, 928:1120], func=AF.Square, scale=CQS,
                                                        accum_out=st[:, 65 + 2 * t:66 + 2 * t]),
                 reads=[R("projS", t % 3, 4)], writes=[R("junk2"), R("stk", t)])
        nsc = 2 if own else 1
        rc = 128 + 2 * (t % 16)
        rstd_cols(st[:, 64 + 2 * t:64 + 2 * t + nsc], st[:, rc:rc + nsc], 1.0 / 128, [R("stk", t)], [R("strk", t % 16)])
        K.op(K.dve, lambda pS=pS, sl=sl, rc=rc: V.tensor_scalar(out=ckvn[sl], in0=pS[:, 768:896], scalar1=st[:, rc:rc + 1],
                                                                scalar2=None, op0=ALU.mult),
             reads=[R("projS", t % 3, 3), R("strk", t % 16)], writes=[R("ckvn", sl)])
        if own:
            K.op(K.dve, lambda pS=pS, sl=sl, rc=rc: V.tensor_scalar(out=cqn[sl], in0=pS[:, 928:1120],
                                                                    scalar1=st[:, rc + 1:rc + 2], scalar2=None, op0=ALU.mult),
                 reads=[R("projS", t % 3, 4), R("strk", t % 16)], writes=[R("cqn", sl)])
        bk = 6 + sl
        b6 = bankb(bk)

        def trB(own=own, sl=sl, b6=b6):
            G.transpose(out=b6[0:64, 0:128], in_=qkr[sl][:, 512:576], identity=ident)
            G.transpose(out=b6[0:64, 128:256], in_=qkr[sl][:, 576:640], identity=ident)
            G.transpose(out=b6[:, 256:384], in_=ckvn[sl], identity=ident)
            ins = G.transpose(out=b6[0:96, 384:512], in_=krs[sl], identity=ident)
            if own:
                G.transpose(out=b6[0:96, 512:640], in_=cqn[sl][:, 0:96], identity=ident)
                ins = G.transpose(out=b6[0:96, 640:768], in_=cqn[sl][:, 96:192], identity=ident)
            return ins
        K.op(K.pe, trB, reads=[R("qkr", sl), R("ckvn", sl), R("krs", sl), R("ident")] + ([R("cqn", sl)] if own else []),
             writes=[R("ps", bk)])
        K.op(K.dve, lambda t=t, b6=b6: V.tensor_copy(out=kTa[:, :, t * 128:(t + 1) * 128],
                                                     in_=b6[0:64, 0:256].rearrange("p (a b) -> p a b", a=2)),
             reads=[R("ps", bk)], writes=[R("kTa", t)])
        K.op(K.dve, lambda t=t, b6=b6: V.tensor_copy(out=ckvnT[:, t * 128:(t + 1) * 128], in_=b6[:, 256:384]),
             reads=[R("ps", bk)], writes=[R("ckvnT", t)])
        K.op(K.dve, lambda t=t, b6=b6: V.tensor_copy(out=krT[64:96, t * 128:(t + 1) * 128], in_=b6[64:96, 384:512]),
             reads=[R("ps", bk)], writes=[R("krT", t)])
        if own:
            K.op(K.dve, lambda t=t, b6=b6: V.tensor_copy(out=cqnT[:, :, t * 128:(t + 1) * 128],
                                                         in_=b6[0:96, 512:768].rearrange("p (a b) -> p a b", a=2)),
                 reads=[R("ps", bk)], writes=[R("cqnT", t)])
            b5 = bankb(5)

            def trQ(sl=sl, b5=b5):
                ins = None
                for h in range(8):
                    ins = G.transpose(out=b5[0:64, h * 128:(h + 1) * 128], in_=qkr[sl][:, h * 64:(h + 1) * 64],
                                      identity=ident)
                return ins
            K.op(K.pe, trQ, reads=[R("qkr", sl), R("ident")], writes=[R("ps", 5)])
            K.op(K.act, lambda t=t, b5=b5: A.copy(out=qTa[:, :, t * 128:(t + 1) * 128],
                                                  in_=b5[0:64, :].rearrange("p (a b) -> p a b", a=8)),
                 reads=[R("ps", 5)], writes=[R("qTa", t)])

    gt1Ba = view(86 * KIB, [128, D], F32)
    wstg = [view(90 * KIB + i * 4 * KIB, [128, D], F32) for i in range(2)]
    wobf = [view(98 * KIB + i * 2 * KIB, [128, D], BF16) for i in range(2)]
    dWo = [K.dsem("wo%d" % i) for i in range(2)]
    dWos = [K.dsem("wos%d" % i) for i in range(2)]
    dGt = K.dsem("gt1a")
    wout_v = wout_d.rearrange("(kc p) n -> kc p n", p=128)
    wo_dv = wo_d.rearrange("(kc p) n -> kc p n", p=128)
    K.dma(K.sp, [(gt1Ba, modB(2))], dGt, reads=[R("mod_d")], writes=[R("gt1Ba")])

    def wo_load(kc):
        K.dma(K.sp, [(wstg[kc % 2], wout_v[kc])], dWo[kc % 2], writes=[R("wstg", kc % 2)])

    def wo_fold(kc):
        K.op(K.dve, lambda: V.scalar_tensor_tensor(out=wobf[kc % 2], in0=wstg[kc % 2], scalar=prm[:, 11 + kc:12 + kc],
                                                   in1=gt1Ba, op0=ALU.mult, op1=ALU.mult),
             reads=[R("wstg", kc % 2), R("prm"), R("gt1Ba")], writes=[R("wobf", kc % 2)])

    def wo_store(kc):
        K.dma(K.sp, [(wo_dv[kc], wobf[kc % 2])], dWos[kc % 2], reads=[R("wobf", kc % 2)], writes=[R("wo_d", kc)])

    zsrc = view(198 * KIB, [128, 2048], F32)
    K.op(K.pool, lambda: P.memset(zsrc, 0.0), writes=[R("zsrc")])
    dZf = K.dsem("zf")
    xs_z = xs_d.rearrange("(p r) d -> p r d", p=128)
    ys_z = ys_d.rearrange("(p r) d -> p r d", p=128)
    zjobs = [(xs_z[:, 4 * c:4 * c + 4, :], zsrc.bitcast(BF16).rearrange("p (a b) -> p a b", a=4)) for c in range(24)]
    zjobs += [(ys_z[:, 2 * c:2 * c + 2, :], zsrc.rearrange("p (a b) -> p a b", a=2)) for c in range(48)]

    for i in range(NT + 3):
        for zi in range(3 * i, min(3 * i + 3, len(zjobs))):
            K.dma(K.sp, [zjobs[zi]], dZf, reads=[R("zsrc")], writes=[R("zf", zi)])
        if i >= 6 and (i - 6) % 2 == 0 and (i - 6) // 2 < 8:
            wo_load((i - 6) // 2)
        if i >= 7 and (i - 7) % 2 == 0 and (i - 7) // 2 < 8:
            wo_fold((i - 7) // 2)
        if i >= 8 and (i - 8) % 2 == 0 and (i - 8) // 2 < 8:
            wo_store((i - 8) // 2)
        if i < NT:
            stA1(i)
        if 0 <= i - 1 < NT:
            stA2(i - 1)
        if 0 <= i - 2 < NT:
            stA2b(i - 2)
        if 0 <= i - 3 < NT:
            stA3(i - 3)
    K.barrier()

    mixTa = view(L4, [128, 4, NO * 128], BF16)
    mixTb = view(L4 + 16 * KIB, [128, 4, NO * 128], BF16)
    al = Alloc(PA, LIM)
    pT = [al([128, 3, 512], BF16) for i in range(2)]
    rden = [al([64, 512], F32) for i in range(2)]
    lnt = [al([32, 512], F32) for i in range(2)]
    msk = al([128, 4, 128], BF16)
    esrow = al([1, 8, 128], BF16)
    dMsk = K.dsem("msk")
    K.dma(K.pool, [(msk, msk_d.rearrange("p (a b) -> p a b", a=4))], dMsk, writes=[R("msk")])
    K.op(K.act, lambda: A.activation(out=es8, in_=prm[0:1, 20:28], func=AF.Exp), reads=[R("prm")], writes=[R("es8")])
    K.op(K.dve, lambda: V.tensor_copy(out=esrow, in_=bc(es8.unsqueeze(2), [1, 8, 128])), reads=[R("es8")],
         writes=[R("esrow")])

    def finish(ob, rd_sl, writers, use_act):
        oT = bank(ob)
        rd = rden[rd_sl]
        if use_act:
            K.op(K.act, lambda: A.activation(out=lnt[rd_sl], in_=oT[64:96, :], func=AF.Ln),
                 reads=[R("ps", ob)], writes=[R("lnt", rd_sl)])
            K.op(K.act, lambda: A.activation(out=rd[0:32, :], in_=lnt[rd_sl], func=AF.Exp, scale=-1.0),
                 reads=[R("lnt", rd_sl)], writes=[R("rden", rd_sl)])
            K.op(K.dve, lambda: V.tensor_copy(out=rd[32:64, :], in_=rd[0:32, :]),
                 reads=[R("rden", rd_sl)], writes=[R("rden", rd_sl)])
        else:
            K.op(K.dve, lambda: V.reciprocal(out=rd[0:32, :], in_=oT[64:96, :]),
                 reads=[R("ps", ob)], writes=[R("rden", rd_sl)])
            K.op(K.dve, lambda: V.tensor_copy(out=rd[32:64, :], in_=rd[0:32, :]),
                 reads=[R("rden", rd_sl)], writes=[R("rden", rd_sl)])
        for (out_ap, in_sl, wr) in writers:
            K.op(K.dve, lambda out_ap=out_ap, in_sl=in_sl: V.tensor_tensor(out=out_ap, in0=in_sl(oT[0:64, :]),
                                                                           in1=in_sl(rd), op=ALU.mult),
                 reads=[R("ps", ob), R("rden", rd_sl)], writes=[wr])

    def swa_ctx(it):
        n, kvh = divmod(it, 2)
        kts = [(31 if n == 0 else n - 1, 2 if n == 0 else 0), (n, None), (16 if n == NO - 1 else n + 1, 3 if n == NO - 1 else 1)]
        sl = it % 2
        return n, kvh, kts, sl, 3 * sl, 6 + sl

    def stW1(it):
        n, kvh, kts, sl, b0, ob = swa_ctx(it)

        def sc():
            ins = None
            for i, (kt, _) in enumerate(kts):
                ins = G.matmul(bank(b0 + i).rearrange("p (a b) -> p a b", a=4),
                               lhsT=kTa[:, kvh, kt * 128:(kt + 1) * 128],
                               rhs=qTa[:, kvh * 4:(kvh + 1) * 4, n * 128:(n + 1) * 128], start=True, stop=True)
            return ins
        K.op(K.pe, sc, reads=[], writes=[R("ps", b0 + i) for i in range(3)])
        K.op(K.act, lambda: A.activation(out=pT[sl].rearrange("p a b -> p (a b)"), in_=bank(b0, 3),
                                         func=AF.Exp, scale=0.125),
             reads=[R("ps", b0 + i) for i in range(3)], writes=[R("pT", sl)])
        for i, (kt, m) in enumerate(kts):
            if m is None:
                continue
            K.op(K.pool, lambda i=i, m=m: P.tensor_tensor(
                out=pT[sl][:, i, :].rearrange("p (a b) -> p a b", a=4),
                in0=pT[sl][:, i, :].rearrange("p (a b) -> p a b", a=4),
                in1=bc(msk[:, m, :].unsqueeze(1), [128, 4, 128]), op=ALU.mult),
                reads=[R("pT", sl), R("msk")], writes=[R("pT", sl)])

    def stW2(it):
        n, kvh, kts, sl, b0, ob = swa_ctx(it)

        def pv():
            for i, (kt, _) in enumerate(kts):
                G.matmul(bank(ob)[0:96, :], lhsT=va[:, kt, kvh, :], rhs=pT[sl][:, i, :],
                         start=(i == 0), stop=False)
            return G.matmul(bank(ob)[0:96, :], lhsT=vsink[0:1, :],
                            rhs=esrow[0:1, kvh * 4:(kvh + 1) * 4, :].rearrange("p a b -> p (a b)"),
                            start=False, stop=True)
        K.op(K.pe, pv, reads=[R("pT", sl), R("vsink"), R("esrow")], writes=[R("ps", ob)])
        writers = []
        for par in range(2):
            def in_sl(ap, par=par):
                return ap.rearrange("p (i two b) -> p i two b", two=2, b=128)[:, :, par, :]
            writers.append((mixTa[par * 64:par * 64 + 64, 2 * kvh:2 * kvh + 2, n * 128:(n + 1) * 128], in_sl,
                            R("mixTa", n, kvh, par)))
        finish(ob, sl, writers, True)

    for i in range(2 * NO + 1):
        if i < 2 * NO:
            stW1(i)
        if i >= 1:
            stW2(i - 1)
    K.barrier()

    qTb = view(L3, [96, 8, NO * 128], BF16)
    kTb = [view(L3 + 32 * KIB + i * 8 * KIB, [96, S], BF16) for i in range(2)]
    pTm = [view(L3 + 48 * KIB + i * 3072, [128, 3, 512], BF16) for i in range(4)]
    al = Alloc(PA, LIM)
    vb = al([128, NT, 8, 96], BF16)
    rden = [al([64, 512], F32) for i in range(2)]
    wuqs = al([96, 2, 768], F32)
    wuq = al([96, 2, 768], BF16)
    wukvs = al([128, 1024], F32)
    wukv = al([128, 8, 128], BF16)
    rt2 = [al([128, 64], F32) for i in range(2)]
    qbS = [al([128, 8, 96], F32) for i in range(2)]
    t1b = [al([128, 8, 32], F32) for i in range(2)]
    t2b = [al([128, 8, 32], F32) for i in range(2)]
    qbr = [al([128, 8, 96], BF16) for i in range(2)]

    dW2 = K.dsem("wB")
    K.dma(K.sp, [(wuqs, wuq_d.rearrange("(kc p) n -> p kc n", p=96)), (wukvs, wukv_d[:, :])], dW2,
          writes=[R("wuqs"), R("wukvs")])
    for kc in range(2):
        K.op(K.dve, lambda kc=kc: V.tensor_scalar(out=wuq[:, kc, :], in0=wuqs[:, kc, :], scalar1=prm[0:96, 8 + kc:9 + kc],
                                                  scalar2=None, op0=ALU.mult),
             reads=[R("wuqs"), R("prm")], writes=[R("wuq")])
    K.op(K.dve, lambda: V.tensor_scalar(out=wukv.rearrange("p a b -> p (a b)"), in0=wukvs, scalar1=prm[:, 10:11],
                                        scalar2=None, op0=ALU.mult),
         reads=[R("wukvs"), R("prm")], writes=[R("wukv")])
    K.op(K.pool, lambda: P.memset(vb[:, :, :, 64:96], 1.0), writes=RL("vb", range(NT)))
    for kt in range(NT):
        b = kt % 2
        K.op(K.pe, lambda kt=kt, b=b: G.matmul(bank(b).rearrange("p (a b) -> p a b", a=8),
                                               lhsT=ckvnT[:, kt * 128:(kt + 1) * 128], rhs=wukv[:, :, 64:128],
                                               start=True, stop=True),
             reads=[R("wukv")], writes=[R("ps", b)])
        if kt % 2:
            K.op(K.dve, lambda kt=kt, b=b: V.tensor_copy(out=vb[:, kt, :, 0:64], in_=bank(b).rearrange("p (a b) -> p a b", a=8)),
                 reads=[R("ps", b)], writes=[R("vb", kt)])
        else:
            K.op(K.act, lambda kt=kt, b=b: A.copy(out=vb[:, kt, :, 0:64], in_=bank(b).rearrange("p (a b) -> p a b", a=8)),
                 reads=[R("ps", b)], writes=[R("vb", kt)])
    dX2 = [K.dsem("r%d" % i) for i in range(2)]
    def stQ1(t):
        sl = t % 2
        K.dma(K.sp, [(rt2[sl], rope_t[t][:, 128:192])], dX2[sl], writes=[R("rt2", sl)])
        bq = 2 + 2 * sl

        def qp(t=t, bq=bq):
            ins = None
            for (b, c0, n) in [(bq, 0, 512), (bq + 1, 512, 256)]:
                for kc in range(2):
                    ins = G.matmul(bank(b)[:, 0:n], lhsT=cqnT[:, kc, t * 128:(t + 1) * 128], rhs=wuq[:, kc, c0:c0 + n],
                                   start=(kc == 0), stop=(kc == 1))
            return ins
        K.op(K.pe, qp, reads=[R("wuq")], writes=[R("ps", bq), R("ps", bq + 1)])
        qf = qbS[sl].rearrange("p a b -> p (a b)")
        K.op(K.act, lambda qf=qf, bq=bq: A.copy(out=qf[:, 0:512], in_=bank(bq)), reads=[R("ps", bq)], writes=[R("qbS", sl)])
        K.op(K.act, lambda qf=qf, bq=bq: A.copy(out=qf[:, 512:768], in_=bank(bq + 1)[:, 0:256]), reads=[R("ps", bq + 1)],
             writes=[R("qbS", sl)])

    def stQ2(t):
        sl = t % 2
        K.op(K.pool, lambda sl=sl: P.tensor_copy(out=qbr[sl][:, :, 0:64], in_=qbS[sl][:, :, 0:64]), reads=[R("qbS", sl)],
             writes=[R("qbr", sl)])
        rope(qbS[sl][:, :, 64:96], rt2[sl][:, 0:32], rt2[sl][:, 32:64], 16, t1b[sl], t2b[sl], qbr[sl][:, :, 64:96],
             [R("qbS", sl), R("rt2", sl)], R("t1b", sl), R("t2b", sl), [R("qbr", sl)], 8)
        b4 = bankb(6 + sl)

        def trq(sl=sl, b4=b4):
            ins = None
            for h in range(8):
                ins = G.transpose(out=b4[0:96, h * 128:(h + 1) * 128], in_=qbr[sl][:, h, :], identity=ident)
            return ins
        K.op(K.pe, trq, reads=[R("qbr", sl), R("ident")], writes=[R("ps", 6 + sl)])
        K.op(K.dve, lambda t=t, b4=b4: V.tensor_copy(out=qTb[:, :, t * 128:(t + 1) * 128],
                                                     in_=b4[0:96, :].rearrange("p (a b) -> p a b", a=8)),
             reads=[R("ps", 6 + sl)], writes=[R("qTb", t)])

    for i in range(NO + 1):
        if i < NO:
            stQ1(i)
        if i >= 1:
            stQ2(i - 1)

    ktg = [list(range(k, min(k + 3, NT))) for k in range(0, NT, 3)]
    cgs = [[0, 1, 2], [3, 4, 5], [6, 7]]

    def setup_steps(h):
        return [("setup", h, cg) for cg in cgs]

    dWbf = K.dsem("wbf")
    K.dma(K.pool, [(wbf_d[c * 512:(c + 1) * 512, m * 2048:(m + 1) * 2048], src[c * 512:(c + 1) * 512, :])
                   for c in range(8) for m, src in enumerate((wg_d, wu_d, wd_d))], dWbf, writes=[R("wbf")])
    steps = setup_steps(0)
    for h in range(8):
        for qg in range(4):
            steps += [("attn", h, qg, gi) for gi in range(len(ktg))]
            if qg == 0 and h < 7:
                steps += setup_steps(h + 1)
    SK = 2
    sc_scale = 96.0 ** -0.5
    for i in range(len(steps) + SK):
        if i < len(steps):
            stp = steps[i]
            b0 = 3 * (i % 2)
            if stp[0] == "setup":
                _, h, cg = stp
                sl = h % 2
                if cg[0] == 0:
                    K.op(K.pool, lambda sl=sl: P.tensor_copy(out=kTb[sl][64:96, :], in_=krT[64:96, :]),
                         writes=[R("kTbr", sl)])

                def su(h=h, cg=cg, b0=b0):
                    ins = None
                    for j, c in enumerate(cg):
                        ins = G.matmul(bank(b0 + j)[0:64, :], lhsT=wukv[:, h, 0:64], rhs=ckvnT[:, c * 512:(c + 1) * 512],
                                       start=True, stop=True)
                    return ins
                K.op(K.pe, su, reads=[R("wukv")], writes=[R("ps", b0 + j) for j in range(3)])
                K.op(K.dve, lambda sl=sl, cg=cg, b0=b0: V.tensor_copy(
                    out=kTb[sl][0:64, cg[0] * 512:(cg[-1] + 1) * 512], in_=bank(b0, len(cg))[0:64, :]),
                    reads=[R("ps", b0 + j) for j in range(3)], writes=[R("kTb", sl)])
            else:
                _, h, qg, gi = stp
                kl = ktg[gi]

                def sc(h=h, qg=qg, kl=kl, b0=b0):
                    ins = None
                    for j, kt in enumerate(kl):
                        ins = G.matmul(bank(b0 + j), lhsT=kTb[h % 2][:, kt * 128:(kt + 1) * 128],
                                       rhs=qTb[:, h, qg * 512:(qg + 1) * 512], start=True, stop=True)
                    return ins
                K.op(K.pe, sc, reads=[R("kTb", h % 2), R("kTbr", h % 2)] + RL("qTb", range(qg * 4, qg * 4 + 4)),
                     writes=[R("ps", b0 + j) for j in range(3)])
                s4 = i % 4
                nk = len(kl)
                K.op(K.act, lambda s4=s4, b0=b0, nk=nk: A.activation(out=pTm[s4].rearrange("p a b -> p (a b)")[:, 0:nk * 512],
                                                                     in_=bank(b0, nk), func=AF.Exp, scale=sc_scale),
                     reads=[R("ps", b0 + j) for j in range(3)], writes=[R("pTm", s4)])
        if i >= SK and steps[i - SK][0] == "attn":
            _, h, qg, gi = steps[i - SK]
            kl = ktg[gi]
            s4 = (i - SK) % 4
            gsl = (h * 4 + qg) % 2
            ob = 6 + gsl

            def pv(h=h, kl=kl, s4=s4, ob=ob):
                ins = None
                for j, kt in enumerate(kl):
                    ins = G.matmul(bank(ob)[0:96, :], lhsT=vb[:, kt, h, :], rhs=pTm[s4][:, j, :],
                                   start=(kt == 0), stop=(kt == NT - 1))
                return ins
            K.op(K.pe, pv, reads=[R("pTm", s4)] + [R("vb", kt) for kt in kl], writes=[R("ps", ob)])
            if kl[-1] == NT - 1:
                finish(ob, gsl, [(mixTb[(h % 2) * 64:(h % 2) * 64 + 64, h // 2, qg * 512:(qg + 1) * 512],
                                 (lambda ap: ap), R("mixTb", h, qg))], False)
    K.barrier()

    PC = 120 * KIB
    acc = view(2 * KIB, [128, NO, D], F32)
    posu = view(66 * KIB, [128, 2, NO], U32)
    w12 = view(66 * KIB + 128, [128, 2, NO], F32)
    widx = view(66 * KIB + 256, [128, 48], U32)
    sidx = view(66 * KIB + 448, [128, 48], U32)
    wo = view(68 * KIB, [128, 8, D], BF16)
    h2tok = view(PC, [128, NO, D], BF16)
    al = Alloc(PC + 32 * KIB, LIM)
    LGa = al([128, NO, 36], F32)
    r_m4 = al([128, NO], F32)
    r_d4 = al([128, NO, 4], F32)
    r_e4 = al([128, NO, 4], F32)
    r_s4 = al([128, NO], F32)
    r_oh = al([128, NO, 4], F32)
    r_t32 = al([128, NO, 32], F32)
    r_el = al([128, NO, 8], F32)
    r_el2 = al([128, NO, 8], F32)
    r_v1 = al([128, NO], F32)
    r_d8 = al([128, NO, 8], F32)
    r_eq = al([128, NO, 8], F32)
    r_v2 = al([128, NO], F32)
    r_mk = al([128, NO, 8], F32)
    r_ex = al([128, NO, 8], F32)
    r_cw = al([128, NO, 8], F32)
    r_den = al([128, NO], F32)
    SORT0 = al.o
    xt = [al([128, D], F32) for i in range(2)]
    tmpB = [al([128, D], F32) for i in range(2)]
    a2B = al([128, D], F32)
    sh2B = al([128, D], F32)
    h2Th = [al([128, 8, 128], BF16) for i in range(2)]
    lo_t = [al([128, D], BF16) for i in range(2)]
    h2Tl = [al([128, 8, 128], BF16) for i in range(2)]
    wrs = al([128, 8, 36], F32)
    wrh = al([128, 8, 36], BF16)
    wrl = al([128, 8, 36], BF16)
    rab = al([128, 32], F32)
    sqs = [al([128, 8, 128], BF16) for i in range(2)]
    junk = al([128, D], BF16)
    gt1B = tmpB[0]
    g2B = tmpB[1]

    K.op(K.dve, lambda: V.memset(st, 0.0), writes=[R("st")])
    dW3 = K.dsem("wC")
    K.dma(K.sp, [(a2B, modB(4)), (sh2B, modB(3)), (gt1B, modB(2)), (g2B, gB_d[:, D:2 * D]),
                 (wrs, wr_d.rearrange("(kc p) n -> p kc n", p=128))], dW3,
          writes=[R("a2B"), R("sh2B"), R("tmpB", 0), R("tmpB", 1), R("wrs")])
    K.op(K.dve, lambda: V.scalar_tensor_tensor(out=a2B, in0=a2B, scalar=1.0, in1=g2B, op0=ALU.add, op1=ALU.mult),
         reads=[R("a2B"), R("tmpB", 1)], writes=[R("a2B"), R("tmpB", 1)])
    K.op(K.act, lambda: A.copy(out=wrh, in_=wrs), reads=[R("wrs")], writes=[R("wrh")])
    K.op(K.pool, lambda: P.tensor_tensor(out=wrl, in0=wrs, in1=wrh, op=ALU.subtract), reads=[R("wrs"), R("wrh")],
         writes=[R("wrl")])
    dWoL = K.dsem("woL")
    K.dma(K.sp, [(wo, wo_d.rearrange("(kc p) n -> p kc n", p=128))], dWoL, writes=[R("wo")])
    for t in range(NO):
        sl = t % 2
        K.op(K.pool, lambda t=t, sl=sl: P.tensor_tensor(out=sqs[sl][:, 0:4, :], in0=mixTa[:, :, t * 128:(t + 1) * 128],
                                                        in1=mixTa[:, :, t * 128:(t + 1) * 128], op=ALU.mult),
             writes=[R("sqs", sl)])
        K.op(K.act, lambda t=t, sl=sl: A.activation(out=sqs[sl][:, 4:8, :], in_=mixTb[:, :, t * 128:(t + 1) * 128], func=AF.Square),
             writes=[R("sqsb", sl)])

        def ssq(t=t, sl=sl):
            ins = None
            for g2 in range(2):
                for j in range(4):
                    ins = G.matmul(bank(7)[:, 2 * t + g2:2 * t + g2 + 1], lhsT=sqs[sl][:, 4 * g2 + j, :], rhs=ones_b[:, 0:1],
                                   start=(j == 0), stop=(j == 3))
            return ins
        K.op(K.pe, ssq, reads=[R("sqs", sl), R("sqsb", sl), R("ones")], writes=[R("ps", 7)])
    K.op(K.act, lambda: A.activation(out=rab, in_=bank(7)[:, 0:32], func=AF.Ln, scale=1.0 / 512, bias=eps_t[:, 0:1]),
         reads=[R("ps", 7), R("eps")], writes=[R("rab")])
    K.op(K.act, lambda: A.activation(out=rab, in_=rab, func=AF.Exp, scale=-0.5), reads=[R("rab")], writes=[R("rab")])

    brB = prm[:, 28:64]
    def stC1(t):
        sl = t % 2
        K.dma(K.sp, [(xt[sl], x_t[t])], dX[sl], writes=[R("xt", sl)])

        def op_(t=t):
            ins = None
            for (mt, b0, k0) in [(mixTa, 0, 0), (mixTb, 2, 4)]:
                for dh in range(2):
                    for kc in range(4):
                        ins = G.matmul(bank(b0 + dh), lhsT=mt[:, kc, t * 128:(t + 1) * 128],
                                       rhs=wo[:, k0 + kc, dh * 512:(dh + 1) * 512], start=(kc == 0), stop=(kc == 3))
            return ins
        K.op(K.pe, op_, reads=[R("wo")], writes=[R("ps", b) for b in range(4)])
        K.op(K.dve, lambda t=t, sl=sl: V.scalar_tensor_tensor(out=acc[:, t, :], in0=bank(0, 2), scalar=rab[:, 2 * t:2 * t + 1],
                                                              in1=xt[sl], op0=ALU.mult, op1=ALU.add),
             reads=[R("ps", 0), R("ps", 1), R("rab"), R("xt", sl)], writes=[R("acc", t)])
        K.op(K.dve, lambda t=t: V.scalar_tensor_tensor(out=acc[:, t, :], in0=bank(2, 2), scalar=rab[:, 2 * t + 1:2 * t + 2],
                                                       in1=acc[:, t, :], op0=ALU.mult, op1=ALU.add),
             reads=[R("ps", 2), R("ps", 3), R("rab"), R("acc", t)], writes=[R("acc", t)])
        K.op(K.act, lambda t=t: A.activation(out=junk, in_=acc[:, t, :], func=AF.Square, accum_out=st[:, t:t + 1]),
             reads=[R("acc", t), R("st")], writes=[R("junk"), R("stx", t)])
        rstd_cols(st[:, t:t + 1], st[:, 32 + t:33 + t], 1.0 / D, [R("stx", t)], [R("strx", t)])

    def stC2(t):
        sl = t % 2
        K.op(K.act, lambda t=t, sl=sl: A.activation(out=tmpB[sl], in_=acc[:, t, :], func=AF.Copy, scale=st[:, 32 + t:33 + t]),
             reads=[R("acc", t), R("strx", t)], writes=[R("tmpB", sl)])
        K.op(K.dve, lambda sl=sl: V.tensor_tensor(out=tmpB[sl], in0=tmpB[sl], in1=a2B, op=ALU.mult),
             reads=[R("tmpB", sl), R("a2B")], writes=[R("tmpB", sl)])
        K.op(K.dve, lambda sl=sl: V.tensor_tensor(out=tmpB[sl], in0=tmpB[sl], in1=sh2B, op=ALU.add),
             reads=[R("tmpB", sl), R("sh2B")], writes=[R("tmpB", sl)])
        K.op(K.act, lambda sl=sl, t=t: A.copy(out=h2tok[:, t, :], in_=tmpB[sl]), reads=[R("tmpB", sl)], writes=[R("h2tok", t)])
        K.op(K.pool, lambda sl=sl, t=t: P.tensor_tensor(out=lo_t[sl], in0=tmpB[sl], in1=h2tok[:, t, :], op=ALU.subtract),
             reads=[R("tmpB", sl), R("h2tok", t)], writes=[R("lo_t", sl)])

    def stC2b(t):
        sl = t % 2

        def tr2(sl=sl, t=t):
            ins = None
            for kc in range(8):
                G.transpose(out=bankb(4)[:, kc * 128:(kc + 1) * 128], in_=h2tok[:, t, kc * 128:(kc + 1) * 128], identity=ident)
                ins = G.transpose(out=bankb(5)[:, kc * 128:(kc + 1) * 128], in_=lo_t[sl][:, kc * 128:(kc + 1) * 128],
                                  identity=ident)
            return ins
        K.op(K.pe, tr2, reads=[R("h2tok", t), R("lo_t", sl), R("ident")], writes=[R("ps", 4), R("ps", 5)])
        K.op(K.act, lambda sl=sl: A.copy(out=h2Th[sl], in_=bankb(4).rearrange("p (a b) -> p a b", a=8)),
             reads=[R("ps", 4)], writes=[R("h2Th", sl)])
        K.op(K.dve, lambda sl=sl: V.tensor_copy(out=h2Tl[sl], in_=bankb(5).rearrange("p (a b) -> p a b", a=8)),
             reads=[R("ps", 5)], writes=[R("h2Tl", sl)])

    def stC3(t):
        sl = t % 2

        def lg(t=t, sl=sl):
            ins = None
            combos = [(h2Th[sl], wrh), (h2Tl[sl], wrh), (h2Th[sl], wrl)]
            for ci, (a_, w_) in enumerate(combos):
                for kc in range(8):
                    ins = G.matmul(bank(6)[:, 0:36], lhsT=a_[:, kc, :], rhs=w_[:, kc, :],
                                   start=(ci == 0 and kc == 0), stop=(ci == 2 and kc == 7))
            return ins
        K.op(K.pe, lg, reads=[R("h2Th", sl), R("h2Tl", sl), R("wrh"), R("wrl")], writes=[R("ps", 6)])
        K.op(K.dve, lambda t=t: V.tensor_tensor(out=LGa[:, t, :], in0=bank(6)[:, 0:36], in1=brB, op=ALU.add),
             reads=[R("ps", 6), R("prm")], writes=[R("rw")])

    for i in range(NO + 3):
        if i < NO:
            stC1(i)
        if 0 <= i - 1 < NO:
            stC2(i - 1)
        if 0 <= i - 2 < NO:
            stC2b(i - 2)
        if 0 <= i - 3 < NO:
            stC3(i - 3)
    RW = [R("rw")]

    def rop(eng, fn):
        K.op(eng, fn, reads=RW, writes=RW)
    T = NO
    Lg = LGa[:, :, 0:4]
    rop(K.dve, lambda: V.tensor_reduce(out=r_m4, in_=Lg, axis=AX.X, op=ALU.max))
    rop(K.dve, lambda: V.tensor_tensor(out=r_d4, in0=Lg, in1=bc(r_m4.unsqueeze(2), [128, T, 4]), op=ALU.subtract))
    rop(K.act, lambda: A.activation(out=r_e4, in_=r_d4, func=AF.Exp))
    rop(K.dve, lambda: V.tensor_reduce(out=r_s4, in_=r_e4, axis=AX.X, op=ALU.add))
    rop(K.dve, lambda: V.tensor_scalar(out=r_oh, in0=r_d4, scalar1=0.0, scalar2=None, op0=ALU.is_ge))
    for g in range(4):
        rop(K.dve, lambda g=g: V.tensor_tensor(out=r_t32[:, :, g * 8:(g + 1) * 8], in0=LGa[:, :, 4 + g * 8:12 + g * 8],
                                               in1=bc(r_oh[:, :, g:g + 1], [128, T, 8]), op=ALU.mult))
    rop(K.dve, lambda: V.tensor_tensor(out=r_el, in0=r_t32[:, :, 0:8], in1=r_t32[:, :, 8:16], op=ALU.add))
    rop(K.dve, lambda: V.tensor_tensor(out=r_el2, in0=r_t32[:, :, 16:24], in1=r_t32[:, :, 24:32], op=ALU.add))
    rop(K.dve, lambda: V.tensor_tensor(out=r_el, in0=r_el, in1=r_el2, op=ALU.add))
    rop(K.dve, lambda: V.tensor_reduce(out=r_v1, in_=r_el, axis=AX.X, op=ALU.max))
    rop(K.dve, lambda: V.tensor_tensor(out=r_d8, in0=r_el, in1=bc(r_v1.unsqueeze(2), [128, T, 8]), op=ALU.subtract))
    rop(K.dve, lambda: V.tensor_scalar(out=r_eq, in0=r_d8, scalar1=0.0, scalar2=None, op0=ALU.is_ge))
    rop(K.dve, lambda: V.scalar_tensor_tensor(out=r_el2, in0=r_eq, scalar=-1e30, in1=r_d8, op0=ALU.mult, op1=ALU.add))
    rop(K.dve, lambda: V.tensor_reduce(out=r_v2, in_=r_el2, axis=AX.X, op=ALU.max))
    rop(K.dve, lambda: V.tensor_tensor(out=r_mk, in0=r_d8, in1=bc(r_v2.unsqueeze(2), [128, T, 8]), op=ALU.is_ge))
    rop(K.act, lambda: A.activation(out=r_ex, in_=r_d8, func=AF.Exp))
    rop(K.dve, lambda: V.tensor_tensor(out=r_cw, in0=r_mk, in1=r_ex, op=ALU.mult))
    rop(K.dve, lambda: V.tensor_reduce(out=r_den, in_=r_cw, axis=AX.X, op=ALU.add))
    rop(K.dve, lambda: V.tensor_tensor(out=r_den, in0=r_den, in1=r_s4, op=ALU.mult))
    rop(K.dve, lambda: V.reciprocal(out=r_den, in_=r_den))
    rop(K.dve, lambda: V.tensor_tensor(out=r_cw, in0=r_cw, in1=bc(r_den.unsqueeze(2), [128, T, 8]), op=ALU.mult))
    K.barrier()

    al = Alloc(SORT0, LIM)
    m2 = al([128, NO, 8], F32)
    A1 = al([128, NO, NE], F32)
    A2 = al([128, NO, NE], F32)
    Mb = al([128, NO, NE], BF16)
    utri = al([128, 128], BF16)
    onesq = al([128, 128], BF16)
    cnt = al([128, NE], F32)
    thr = al([128, 8], F32)
    cmp8 = al([128, NE, 8], F32)
    Tt = al([128, NE], F32)
    sca = al([128, NE], F32)
    scb = al([128, NE], F32)
    off256 = al([128, NE], F32)
    offT = al([128, NE], F32)
    posf = al([128, NO, NE], F32)
    ptmp = al([128, NO, NE], F32)
    posk = al([128, 2, NO], F32)
    jv = al([128, 48], F32)
    ev = al([128, NE], F32)
    pidx = al([128, 1], F32)
    indg = al([128, NE, 48], F32)
    indl = al([128, NE, 48], F32)
    eidf = al([128, 48], F32)
    anyf = al([128, 48], F32)
    real1 = al([128, 48], F32)
    jv256 = al([128, 48], F32)
    sidf = al([128, 48], F32)
    p2 = al([128, 1], F32)
    SR = [R("sort")]

    def sop(eng, fn, extra_r=(), extra_w=()):
        K.op(eng, fn, reads=SR + list(extra_r), writes=SR + list(extra_w))
    T = NO
    sop(K.pool, lambda: P.memset(utri, 1.0))
    sop(K.pool, lambda: P.affine_select(out=utri, in_=utri, pattern=[[1, 128]], compare_op=ALU.is_ge, fill=0.0, base=-1,
                                        channel_multiplier=-1))
    sop(K.pool, lambda: P.memset(onesq, 1.0))
    for m in range(8):
        sop(K.dve, lambda m=m: V.memset(thr[:, m:m + 1], 256.0 * m))
    sop(K.pool, lambda: P.iota(jv, pattern=[[1, 48]], base=0, channel_multiplier=0, allow_small_or_imprecise_dtypes=True))
    sop(K.pool, lambda: P.iota(ev, pattern=[[1, NE]], base=0, channel_multiplier=0, allow_small_or_imprecise_dtypes=True))
    sop(K.pool, lambda: P.iota(pidx, pattern=[[0, 1]], base=0, channel_multiplier=1, allow_small_or_imprecise_dtypes=True))
    sop(K.dve, lambda: V.tensor_tensor(out=m2, in0=r_mk, in1=r_eq, op=ALU.subtract), extra_r=RW)
    for g in range(4):
        sop(K.dve, lambda g=g: V.tensor_tensor(out=A1[:, :, g * 8:(g + 1) * 8], in0=r_eq,
                                               in1=bc(r_oh[:, :, g:g + 1], [128, T, 8]), op=ALU.mult), extra_r=RW)
        sop(K.dve, lambda g=g: V.tensor_tensor(out=A2[:, :, g * 8:(g + 1) * 8], in0=m2,
                                               in1=bc(r_oh[:, :, g:g + 1], [128, T, 8]), op=ALU.mult), extra_r=RW)
    sop(K.dve, lambda: V.tensor_tensor(out=r_ex, in0=r_cw, in1=r_eq, op=ALU.mult), extra_r=RW, extra_w=RW)
    sop(K.dve, lambda: V.tensor_reduce(out=w12[:, 0, :], in_=r_ex, axis=AX.X, op=ALU.add), extra_r=RW, extra_w=[R("w12")])
    sop(K.dve, lambda: V.tensor_tensor(out=r_ex, in0=r_cw, in1=m2, op=ALU.mult), extra_r=RW, extra_w=RW)
    sop(K.dve, lambda: V.tensor_reduce(out=w12[:, 1, :], in_=r_ex, axis=AX.X, op=ALU.add), extra_r=RW, extra_w=[R("w12")])
    sop(K.dve, lambda: V.tensor_tensor(out=Mb, in0=A1, in1=A2, op=ALU.add))
    def rk():
        ins = None
        for i in range(NO):
            for i2 in range(i):
                G.matmul(bank(0)[:, i * 32:(i + 1) * 32], lhsT=onesq, rhs=Mb[:, i2, :], start=(i2 == 0), stop=False)
            ins = G.matmul(bank(0)[:, i * 32:(i + 1) * 32], lhsT=utri, rhs=Mb[:, i, :], start=(i == 0), stop=True)
        for i in range(NO):
            ins = G.matmul(bank(1)[:, 0:32], lhsT=onesq, rhs=Mb[:, i, :], start=(i == 0), stop=(i == NO - 1))
        return ins
    K.op(K.pe, rk, reads=SR, writes=[R("ps", 0), R("ps", 1)])
    sop(K.dve, lambda: V.tensor_copy(out=cnt, in_=bank(1)[:, 0:32]), extra_r=[R("ps", 1)])
    sop(K.dve, lambda: V.tensor_tensor(out=cmp8, in0=bc(cnt.unsqueeze(2), [128, NE, 8]), in1=bc(thr.unsqueeze(1), [128, NE, 8]),
                                       op=ALU.is_gt))
    sop(K.dve, lambda: V.tensor_reduce(out=Tt, in_=cmp8, axis=AX.X, op=ALU.add))
    sop(K.dve, lambda: V.tensor_copy(out=sca, in_=Tt))
    cur, oth = sca, scb
    for sft in (1, 2, 4, 8, 16):
        sop(K.dve, lambda cur=cur, oth=oth: V.tensor_copy(out=oth, in_=cur))
        sop(K.dve, lambda cur=cur, oth=oth, sft=sft: V.tensor_tensor(out=oth[:, sft:NE], in0=cur[:, sft:NE], in1=cur[:, 0:NE - sft],
                                                                      op=ALU.add))
        cur, oth = oth, cur
    incl = cur
    sop(K.dve, lambda: V.tensor_tensor(out=offT, in0=incl, in1=Tt, op=ALU.subtract))
    sop(K.dve, lambda: V.tensor_scalar(out=off256, in0=offT, scalar1=256.0, scalar2=None, op0=ALU.mult))
    sop(K.dve, lambda: V.tensor_tensor(out=posf, in0=bank(0).rearrange("p (a b) -> p a b", a=NO),
                                       in1=bc(off256.unsqueeze(1), [128, T, NE]), op=ALU.add), extra_r=[R("ps", 0)])
    for k, Ak in enumerate((A1, A2)):
        sop(K.dve, lambda Ak=Ak: V.tensor_tensor(out=ptmp, in0=posf, in1=Ak, op=ALU.mult))
        sop(K.dve, lambda k=k: V.tensor_reduce(out=posk[:, k, :], in_=ptmp, axis=AX.X, op=ALU.add))
    sop(K.dve, lambda: V.tensor_copy(out=posu, in_=posk), extra_w=[R("posu")])
    dSc = K.dsem("scat")
    for i in range(NO):
        for k in range(2):
            K._waits(K.pool, [R("posu"), R("h2tok", i)], [R("xs_d")])
            P.indirect_dma_start(out=xs_d[:, :], out_offset=bass.IndirectOffsetOnAxis(posu[:, k, i:i + 1], 0),
                                 in_=h2tok[:, i, :], in_offset=None).then_inc(dSc.sem, 16)
            dSc.cnt += 16
    K._commit((dSc.sem, dSc.cnt), [R("posu")] + RL("h2tok", range(NO)), [R("xs_d")])
    sop(K.dve, lambda: V.tensor_tensor(out=indg, in0=bc(jv.unsqueeze(1), [128, NE, 48]), in1=bc(offT.unsqueeze(2), [128, NE, 48]),
                                       op=ALU.is_ge))
    sop(K.dve, lambda: V.tensor_tensor(out=indl, in0=bc(jv.unsqueeze(1), [128, NE, 48]), in1=bc(incl.unsqueeze(2), [128, NE, 48]),
                                       op=ALU.is_lt))
    sop(K.dve, lambda: V.tensor_tensor(out=indg, in0=indg, in1=indl, op=ALU.mult))
    sop(K.dve, lambda: V.tensor_reduce(out=anyf, in_=indg.rearrange("p e j -> p j e"), axis=AX.X, op=ALU.add))
    sop(K.dve, lambda: V.tensor_tensor(out=indl, in0=indg, in1=bc(ev.unsqueeze(2), [128, NE, 48]), op=ALU.mult))
    sop(K.dve, lambda: V.tensor_reduce(out=eidf, in_=indl.rearrange("p e j -> p j e"), axis=AX.X, op=ALU.add))
    sop(K.dve, lambda: V.tensor_tensor(out=sca, in0=cnt, in1=off256, op=ALU.add))
    sop(K.dve, lambda: V.tensor_scalar(out=jv256, in0=jv, scalar1=256.0, scalar2=None, op0=ALU.mult))
    sop(K.dve, lambda: V.tensor_tensor(out=indl, in0=bc(sca.unsqueeze(2), [128, NE, 48]), in1=bc(jv256.unsqueeze(1), [128, NE, 48]),
                                       op=ALU.subtract))
    sop(K.dve, lambda: V.tensor_tensor(out=indl, in0=indl, in1=indg, op=ALU.mult))
    sop(K.dve, lambda: V.tensor_reduce(out=real1, in_=indl.rearrange("p e j -> p j e"), axis=AX.X, op=ALU.add))
    sop(K.dve, lambda: V.tensor_scalar(out=p2, in0=pidx, scalar1=2.0, scalar2=None, op0=ALU.mult))
    sop(K.dve, lambda: V.tensor_scalar(out=sidf, in0=real1, scalar1=p2[:, 0:1], scalar2=None, op0=ALU.is_gt))
    sop(K.dve, lambda: V.tensor_scalar(out=sidf, in0=sidf, scalar1=-1.0e6, scalar2=1.0e6, op0=ALU.mult, op1=ALU.add))
    sop(K.dve, lambda: V.tensor_scalar(out=jv256, in0=jv, scalar1=128.0, scalar2=pidx[:, 0:1], op0=ALU.mult, op1=ALU.add))
    sop(K.dve, lambda: V.tensor_tensor(out=sidf, in0=sidf, in1=jv256, op=ALU.add))
    sop(K.dve, lambda: V.tensor_copy(out=sidx, in_=sidf), extra_w=[R("sidx")])
    sop(K.dve, lambda: V.tensor_scalar(out=anyf, in0=anyf, scalar1=-32.0, scalar2=32.0, op0=ALU.mult, op1=ALU.add))
    sop(K.dve, lambda: V.tensor_tensor(out=eidf, in0=eidf, in1=anyf, op=ALU.add))
    sop(K.dve, lambda: V.tensor_scalar(out=eidf, in0=eidf, scalar1=128.0, scalar2=pidx[:, 0:1], op0=ALU.mult, op1=ALU.add))
    sop(K.dve, lambda: V.tensor_copy(out=widx, in_=eidf), extra_w=[R("widx")])
    alw = Alloc(68 * KIB, 116 * KIB)
    wall = [alw([128, 3 * 2048], BF16) for i in range(4)]
    wgb = [w[:, 0:2048].rearrange("p (a b) -> p a b", a=8) for w in wall]
    wub = [w[:, 2048:4096].rearrange("p (a b) -> p a b", a=8) for w in wall]
    wdb = [w[:, 4096:6144].rearrange("p (a b) -> p a b", a=2) for w in wall]
    dG = [K.dsem("g%d" % i) for i in range(4)]
    breg = P.to_reg(NE * 128 - 1)
    for i in range(4):
        K.op(K.act, lambda i=i: A.memzero(wall[i]), writes=[R("wall", i)])

    def gather_w(q, j):
        s4 = q % 4
        K._waits(K.pool, [R("widx"), R("wbf")], [R("wall", s4)])
        P.indirect_dma_start(out=wall[s4], out_offset=None, in_=wbf_d[:, :],
                             in_offset=bass.IndirectOffsetOnAxis(widx[:, j:j + 1], 0),
                             bounds_check=breg, oob_is_err=False).then_inc(dG[s4].sem, 16)
        dG[s4].cnt += 16
        K._commit((dG[s4].sem, dG[s4].cnt), [R("widx"), R("wbf")], [R("wall", s4)])
    order = []
    for g3 in range(16):
        order += [2 * g3, 2 * g3 + 1, 32 + g3]
    assert sorted(order) == list(range(NTL))

    for q in range(4):
        gather_w(q, order[q])
    K.barrier()

    al = Alloc(116 * KIB, LIM)
    xs = [al([128, 2, D], BF16) for i in range(4)]
    xT = [al([128, 8, 256], BF16) for i in range(2)]
    ssb = [al([128, 512], F32) for i in range(2)]
    hid = [al([128, 2, 256], BF16) for i in range(2)]
    ysb = [al([128, 2, D], F32) for i in range(2)]
    gt2B = al([128, D], F32)
    gfB = al([128, D], F32)
    yg = [al([128, 2, D], F32) for i in range(2)]
    yt = [al([128, D], F32) for i in range(2)]
    junk = al([128, D], BF16)

    dXs = [K.dsem("xs%d" % i) for i in range(4)]
    dYs = [K.dsem("ys%d" % i) for i in range(2)]
    dF = K.dsem("fin")
    K.op(K.dve, lambda: V.memset(st, 0.0), writes=[R("st")])
    K.dma(K.sp, [(gt2B, modB(5)), (gfB, gB_d[:, 2 * D:3 * D])], dF, writes=[R("gt2B"), R("gfB")])
    sreg = P.to_reg(NTL * 128 - 1)
    for i in range(4):
        K.op(K.act, lambda i=i: A.memzero(xs[i].rearrange("p a b -> p (a b)")), writes=[R("xs", i)])
    xs_p = xs_d.rearrange("(n r) d -> n (r d)", r=2)
    ys_p = ys_d.rearrange("(n r) d -> n (r d)", r=2)

    def stM1(q, j):
        K._waits(K.pool, [R("sidx"), R("xs_d")], [R("xs", q % 4)])
        P.indirect_dma_start(out=xs[q % 4].rearrange("p a b -> p (a b)"), out_offset=None, in_=xs_p,
                             in_offset=bass.IndirectOffsetOnAxis(sidx[:, j:j + 1], 0),
                             bounds_check=sreg, oob_is_err=False).then_inc(dXs[q % 4].sem, 16)
        dXs[q % 4].cnt += 16
        K._commit((dXs[q % 4].sem, dXs[q % 4].cnt), [R("sidx"), R("xs_d")], [R("xs", q % 4)])

    def stM2(q, j):
        s3, s2 = q % 4, q % 2

        def tr():
            ins = None
            for s_ in range(2):
                for kc in range(8):
                    ins = G.transpose(out=bankb(s_)[:, kc * 128:(kc + 1) * 128], in_=xs[s3][:, s_, kc * 128:(kc + 1) * 128],
                                      identity=ident)
            return ins
        K.op(K.pe, tr, reads=[R("xs", s3), R("ident")], writes=[R("ps", 0), R("ps", 1)])
        K.op(K.act, lambda: A.copy(out=xT[s2][:, :, 0:128], in_=bankb(0).rearrange("p (a b) -> p a b", a=8)),
             reads=[R("ps", 0)], writes=[R("xT", s2)])
        K.op(K.dve, lambda: V.tensor_copy(out=xT[s2][:, :, 128:256], in_=bankb(1).rearrange("p (a b) -> p a b", a=8)),
             reads=[R("ps", 1)], writes=[R("xT", s2)])

    def stM3(q, j):
        s4, s2 = q % 4, q % 2

        def mm():
            ins = None
            for (wb, b) in ((wgb[s4], 2), (wub[s4], 3)):
                for fc in range(2):
                    for kc in range(8):
                        ins = G.matmul(bank(b)[:, fc * 256:(fc + 1) * 256], lhsT=wb[:, kc, fc * 128:(fc + 1) * 128],
                                       rhs=xT[s2][:, kc, :], start=(kc == 0), stop=(kc == 7))
            return ins
        K.op(K.pe, mm, reads=[R("wall", s4), R("xT", s2)], writes=[R("ps", 2), R("ps", 3)])
        K.op(K.act, lambda: A.activation(out=ssb[s2], in_=bank(2), func=AF.Silu), reads=[R("ps", 2)], writes=[R("ssb", s2)])
        K.op(K.dve, lambda: V.tensor_tensor(out=hid[s2].rearrange("p a b -> p (a b)"), in0=ssb[s2], in1=bank(3), op=ALU.mult),
             reads=[R("ssb", s2), R("ps", 3)], writes=[R("hid", s2)])

    def stM4(q, j):
        s4, s2 = q % 4, q % 2
        for sh in range(2):
            b0 = 4 + 2 * sh

            def mm(sh=sh, b0=b0):
                ins = None
                for dh in range(2):
                    for fc in range(2):
                        ins = G.matmul(bank(b0 + dh), lhsT=hid[s2][:, fc, sh * 128:(sh + 1) * 128],
                                       rhs=wdb[s4][:, fc, dh * 512:(dh + 1) * 512], start=(fc == 0), stop=(fc == 1))
                return ins
            K.op(K.pe, mm, reads=[R("hid", s2), R("wall", s4)], writes=[R("ps", b0), R("ps", b0 + 1)])
            K.op(K.dve, lambda sh=sh, b0=b0: V.tensor_tensor(out=ysb[s2][:, sh, :], in0=bank(b0, 2), in1=gt2B, op=ALU.mult),
                 reads=[R("ps", b0), R("ps", b0 + 1), R("gt2B")], writes=[R("ysb", s2)])

    def stM4b(q, j):
        s2 = q % 2
        K._waits(K.pool, [R("sidx"), R("ysb", s2)], [R("ys_d", j)])
        P.indirect_dma_start(out=ys_p, out_offset=bass.IndirectOffsetOnAxis(sidx[:, j:j + 1], 0),
                             in_=ysb[s2].rearrange("p a b -> p (a b)"), in_offset=None, bounds_check=sreg,
                             oob_is_err=False).then_inc(dYs[s2].sem, 16)
        dYs[s2].cnt += 16
        K._commit((dYs[s2].sem, dYs[s2].cnt), [R("sidx"), R("ysb", s2)], [R("ys_d", j)])

    stM1(0, order[0])
    stM1(1, order[1])
    for i in range(NTL + 3):
        if 0 <= i - 3 < NTL:
            stM4(i - 3, order[i - 3])
            if i + 1 < NTL:
                gather_w(i + 1, order[i + 1])
        if i + 2 < NTL:
            stM1(i + 2, order[i + 2])
        if 0 <= i - 1 < NTL:
            stM2(i - 1, order[i - 1])
        if 0 <= i - 2 < NTL:
            stM3(i - 2, order[i - 2])
        if 0 <= i - 3 < NTL:
            stM4b(i - 3, order[i - 3])

    dO = [K.dsem("o%d" % i) for i in range(2)]
    dYg = [K.dsem("yg%d" % i) for i in range(2)]
    y_t = y_d.rearrange("(t p) d -> t p d", p=128)
    for t in range(NO):
        sl = t % 2
        ysr = [R("ys_d", j) for j in range(NTL)]
        K._waits(K.pool, ysr + [R("posu")], [R("yg", sl)])
        for k in range(2):
            P.indirect_dma_start(out=yg[sl][:, k, :], out_offset=None, in_=ys_d[:, :],
                                 in_offset=bass.IndirectOffsetOnAxis(posu[:, k, t:t + 1], 0)).then_inc(dYg[sl].sem, 16)
            dYg[sl].cnt += 16
        K._commit((dYg[sl].sem, dYg[sl].cnt), [R("posu")], [R("yg", sl)])
        for k in range(2):
            K.op(K.dve, lambda t=t, k=k, sl=sl: V.scalar_tensor_tensor(out=acc[:, t, :], in0=yg[sl][:, k, :],
                                                                       scalar=w12[:, k, t:t + 1], in1=acc[:, t, :],
                                                                       op0=ALU.mult, op1=ALU.add),
                 reads=[R("yg", sl), R("w12"), R("acc", t)], writes=[R("acc", t)])
        K.op(K.act, lambda t=t: A.activation(out=junk, in_=acc[:, t, :], func=AF.Square, accum_out=st[:, t:t + 1]),
             reads=[R("acc", t), R("st")], writes=[R("junk"), R("stx", t)])
        rstd_cols(st[:, t:t + 1], st[:, 32 + t:33 + t], 1.0 / D, [R("stx", t)], [R("strx", t)])
        K.op(K.dve, lambda t=t, sl=sl: V.scalar_tensor_tensor(out=yt[sl], in0=acc[:, t, :], scalar=st[:, 32 + t:33 + t], in1=gfB,
                                                              op0=ALU.mult, op1=ALU.mult),
             reads=[R("acc", t), R("strx", t), R("gfB")], writes=[R("yt", sl)])
        K.dma(K.sp, [(y_t[t], yt[sl])], dO[sl], reads=[R("yt", sl)])
    for d in dO:
        K.sp.h.wait_ge(d.sem, d.cnt)
    es.close()
    return nc


_ROPE_THETA = 10000.0


def _rope_table():
    out = np.zeros((S, 192), np.float32)
    pos = np.arange(S, dtype=np.float32)[:, None]
    for (dim, o) in ((64, 0), (32, 128)):
        inv = (1.0 / (_ROPE_THETA ** (np.arange(0, dim, 2, dtype=np.float32) / dim))).astype(np.float32)
        ang = (pos * inv[None, :]).astype(np.float32)
        c, s_ = np.cos(ang).astype(np.float32), np.sin(ang).astype(np.float32)
        out[:, o:o + dim] = np.concatenate([c, c], axis=1)
        out[:, o + dim:o + 2 * dim] = np.concatenate([-s_, s_], axis=1)
    return out


_NC_CACHE = {}


def kernel(x, c, w_ada, b_ada, g_norm1, w_in, g_q_lora, w_uq, g_kv_lora, w_ukv, sink,
           g_out_swa, g_out_mla, w_out, g_norm2, w_router_group, b_router_group,
           w_router_expert, b_router_expert, w_exp_gate, w_exp_up, w_exp_down, g_final):
    f = lambda a: np.ascontiguousarray(np.asarray(a, dtype=np.float32))
    x = f(x); c = f(c)
    if "nc" not in _NC_CACHE:
        _NC_CACHE["nc"] = build_program()
    nc = _NC_CACHE["nc"]
    rope = _rope_table()
    w_in0 = f(w_in)[0]
    perm = np.concatenate([np.arange(0, 768), np.arange(960, 1120), np.arange(768, 960)])
    w_in_p = np.ascontiguousarray(w_in0[:, perm])
    gB = np.ascontiguousarray(np.broadcast_to(
        np.concatenate([f(g_norm1)[0], f(g_norm2)[0], f(g_final)])[None, :], (128, 3 * D)))
    w_r = np.ascontiguousarray(np.concatenate([f(w_router_group)[0], f(w_router_expert)[0]], axis=1))
    b_r = np.concatenate([f(b_router_group)[0], f(b_router_expert)[0]])
    gcat = np.concatenate([f(g_out_swa)[0], f(g_out_mla)[0]])
    jj = np.arange(128)[:, None]
    rr = np.arange(128)[None, :]
    mprev = (jj >= rr).astype(np.float32)
    mnext = (jj <= rr).astype(np.float32)
    shared = {
        "w_ada": f(w_ada)[0], "b_ada": f(b_ada), "w_in": w_in_p, "w_uq": f(w_uq)[0], "w_ukv": f(w_ukv)[0],
        "w_out": f(w_out)[0], "w_r": w_r, "w_g": np.ascontiguousarray(f(w_exp_gate)[0].reshape(NE, 8, 128, 256).transpose(0, 2, 1, 3).reshape(NE * 128, 2048)),
        "w_u": np.ascontiguousarray(f(w_exp_up)[0].reshape(NE, 8, 128, 256).transpose(0, 2, 1, 3).reshape(NE * 128, 2048)),
        "w_d": np.ascontiguousarray(f(w_exp_down)[0].reshape(NE, 2, 128, D).transpose(0, 2, 1, 3).reshape(NE * 128, 2048)),
        "gB": gB,
    }
    in_maps = []
    for core in range(8):
        b, hf = core // 2, core % 2
        own = slice(hf * 2048, (hf + 1) * 2048)
        oth = slice((1 - hf) * 2048, (2 - hf) * 2048)
        prm = np.zeros((128, 64), np.float32)
        prm[:, 0:8] = c[b].reshape(8, 128).T
        prm[0:96, 8:10] = f(g_q_lora)[0].reshape(2, 96).T
        prm[:, 10] = f(g_kv_lora)[0]
        prm[:, 11:19] = gcat.reshape(8, 128).T
        prm[:, 20:28] = f(sink)[0][None, :]
        prm[:, 28:64] = b_r[None, :]
        msk = np.stack([mprev, mnext, mprev * float(hf == 1), mnext * float(hf == 0)], axis=1).reshape(128, 512)
        m = dict(shared)
        m["x"] = np.ascontiguousarray(np.concatenate([x[b, own], x[b, oth]], axis=0))
        m["rope"] = np.ascontiguousarray(np.concatenate([rope[own], rope[oth]], axis=0))
        m["prm"] = prm
        m["g1p"] = np.ascontiguousarray(f(g_norm1)[0].reshape(8, 128).T)
        m["msk"] = np.ascontiguousarray(msk.astype(np.float32))
        in_maps.append(m)
    res = run_bass_kernel_spmd(nc, in_maps, core_ids=list(range(8)))
    out = np.zeros((4, S, D), np.float32)
    for core in range(8):
        b, hf = core // 2, core % 2
        out[b, hf * 2048:(hf + 1) * 2048] = res.results[core]["y"]
    return out
```

```python
import numpy as np
from contextlib import ExitStack

import concourse.bass as bass
import concourse.mybir as mybir
from concourse.bass_utils import run_bass_kernel_spmd

F32 = mybir.dt.float32
BF16 = mybir.dt.bfloat16
U8 = mybir.dt.uint8
U32 = mybir.dt.uint32
AF = mybir.ActivationFunctionType
ALU = mybir.AluOpType
AX = mybir.AxisListType

D = 1024
S = 4096
NT = 32
NO = 16
EPS = 1e-6
NE = 32
KIB = 1024


class Region:
    __slots__ = ("name", "lw", "rd")

    def __init__(self, name):
        self.name = name
        self.lw = None
        self.rd = []


class Eng:
    def __init__(self, K, name, h):
        self.name = name
        self.h = h
        self.sem = K.es.enter_context(K.nc.semaphore("tl_" + name))
        self.cnt = 0
        self.waited = {}


class DSem:
    def __init__(self, K, name):
        self.sem = K.es.enter_context(K.nc.semaphore("d_" + name))
        self.cnt = 0


class KB:
    def __init__(self, nc, es):
        self.nc = nc
        self.es = es
        self.pe = Eng(self, "pe", nc.tensor)
        self.dve = Eng(self, "dve", nc.vector)
        self.act = Eng(self, "act", nc.scalar)
        self.pool = Eng(self, "pool", nc.gpsimd)
        self.sp = Eng(self, "sp", nc.sync)
        self.engs = [self.pe, self.dve, self.act, self.pool, self.sp]
        self.dsems = []

    def dsem(self, name):
        d = DSem(self, name)
        self.dsems.append(d)
        return d

    def _waits(self, eng, reads, writes):
        need = {}

        def add(t):
            if t is None:
                return
            s, v = t
            k = id(s)
            if k not in need or need[k][1] < v:
                need[k] = (s, v)

        for r in reads:
            add(r.lw)
        for w in writes:
            add(w.lw)
            for t in w.rd:
                add(t)
        for k, (s, v) in need.items():
            if eng.waited.get(k, 0) >= v:
                continue
            if eng.name == "pe" and s is eng.sem:
                continue
            eng.h.wait_ge(s, v)
            eng.waited[k] = v

    def _commit(self, ticket, reads, writes):
        for r in reads:
            r.rd.append(ticket)
            if len(r.rd) > 48:
                best = {}
                for (s, v) in r.rd:
                    if id(s) not in best or best[id(s)][1] < v:
                        best[id(s)] = (s, v)
                r.rd = list(best.values())
        for w in writes:
            w.lw = ticket
            w.rd = []

    def op(self, eng, fn, reads=(), writes=()):
        self._waits(eng, reads, writes)
        ins = fn()
        eng.cnt += 1
        ins.then_inc(eng.sem, 1)
        self._commit((eng.sem, eng.cnt), reads, writes)
        return ins

    def dma(self, q, pairs, dsem, reads=(), writes=(), **kw):
        self._waits(q, reads, writes)
        for (o, i) in pairs:
            q.h.dma_start(out=o, in_=i, **kw).then_inc(dsem.sem, 16)
            dsem.cnt += 16
        self._commit((dsem.sem, dsem.cnt), reads, writes)

    def barrier(self):
        for e in self.engs:
            for o in self.engs:
                if o is e or o.cnt == 0:
                    continue
                if e.waited.get(id(o.sem), 0) < o.cnt:
                    e.h.wait_ge(o.sem, o.cnt)
                    e.waited[id(o.sem)] = o.cnt
            for d in self.dsems:
                if d.cnt and e.waited.get(id(d.sem), 0) < d.cnt:
                    e.h.wait_ge(d.sem, d.cnt)
                    e.waited[id(d.sem)] = d.cnt


_DT_SIZE = {F32: 4, BF16: 2, U32: 4}


def build_program():
    nc = bass.Bass("TRN2", target_bir_lowering=False)
    es = ExitStack()
    K = KB(nc, es)
    V, A, P, G = nc.vector, nc.scalar, nc.gpsimd, nc.tensor

    def din(name, shape):
        return nc.dram_tensor(name, shape, F32, kind="ExternalInput").ap()

    x_d = din("x", [S, D])
    prm_d = din("prm", [128, 64])
    g1p_d = din("g1p", [128, 8])
    gB_d = din("gB", [128, 3 * D])
    rope_d = din("rope", [S, 192])
    msk_d = din("msk", [128, 512])
    wada_d = din("w_ada", [D, 6 * D])
    bada_d = din("b_ada", [1, 6 * D])
    win_d = din("w_in", [D, 1120])
    wuq_d = din("w_uq", [192, 768])
    wukv_d = din("w_ukv", [128, 1024])
    wout_d = din("w_out", [D, D])
    wr_d = din("w_r", [D, 36])
    wg_d = din("w_g", [NE * 128, 2048])
    wu_d = din("w_u", [NE * 128, 2048])
    wd_d = din("w_d", [NE * 128, 2048])
    y_d = nc.dram_tensor("y", [NO * 128, D], F32, kind="ExternalOutput").ap()
    mod_d = nc.dram_tensor("mod_scratch", [1, 6 * D], F32, kind="Internal").ap()
    NTL = 48
    xs_d = nc.dram_tensor("xs_scratch", [NTL * 256, D], BF16, kind="Internal").ap()
    ys_d = nc.dram_tensor("ys_scratch", [NTL * 256, D], F32, kind="Internal").ap()
    wo_d = nc.dram_tensor("wo_scratch", [D, D], BF16, kind="Internal").ap()
    wbf_d = nc.dram_tensor("wbf_all", [NE * 128, 3 * 2048], BF16, kind="Internal").ap()

    arena = nc.alloc_sbuf_tensor("arena", [128, 206 * KIB], U8)
    ps = nc.alloc_psum_tensor("ps", [128, 4096], F32)
    LIM = 206 * KIB

    def view(off, shape, dt, p0=0):
        off = int(off)
        n = 1
        for s_ in shape[1:]:
            n *= s_
        assert off + n * _DT_SIZE[dt] <= LIM, (off, shape)
        ap = arena[p0:p0 + shape[0], off:off + n * _DT_SIZE[dt]].bitcast(dt)
        if len(shape) == 3:
            ap = ap.rearrange("p (a b) -> p a b", a=shape[1])
        elif len(shape) == 4:
            ap = ap.rearrange("p (a b c) -> p a b c", a=shape[1], b=shape[2])
        return ap

    class Alloc:
        def __init__(self, start, end):
            self.o = start
            self.end = end

        def __call__(self, shape, dt, p0=0):
            n = 1
            for s_ in shape[1:]:
                n *= s_
            nb = (n * _DT_SIZE[dt] + 31) // 32 * 32
            v = view(self.o, shape, dt, p0)
            self.o += nb
            assert self.o <= self.end, (self.o, self.end, shape)
            return v

    def bank(b, n=1):
        return ps[:, b * 512:(b + n) * 512]

    def bankb(b):
        return ps[:, b * 512:(b + 1) * 512].bitcast(BF16)

    def bc(ap, shape):
        return ap.to_broadcast(shape)

    def modB(i):
        return mod_d[0, i * D:(i + 1) * D].partition_broadcast(128)

    Rg = {}

    def R(*key):
        if key not in Rg:
            Rg[key] = Region(str(key))
        return Rg[key]

    def RL(name, idxs):
        return [R(name, i) for i in idxs]

    def h3(ap, h):
        return ap.rearrange("p (h d) -> p h d", h=h)

    prm = view(0, [128, 64], F32)
    ident = view(256, [128, 128], BF16)
    ones_b = view(512, [128, 4], BF16)
    sT = view(528, [128, 8], F32)
    eps_t = view(560, [128, 1], F32)
    es8 = view(576, [1, 8], F32)
    st = view(640, [128, 160], F32)
    vsink = view(1280, [1, 96], BF16)
    g1P = view(1472, [128, 8], F32)
    a1P = view(1504, [128, 8], F32)
    sh1P = view(1536, [128, 8], F32)
    sh1Pb = view(1568, [128, 8], BF16)
    onesrow = view(1600, [1, 128], BF16)
    L2 = 2 * KIB
    L3 = 26 * KIB
    L4 = 86 * KIB
    PA = 118 * KIB

    dPrm = K.dsem("prm")
    K.dma(K.sp, [(prm, prm_d[:, :])], dPrm, writes=[R("prm")])
    K.op(K.pool, lambda: P.memset(ident, 1.0), writes=[R("ident")])
    K.op(K.pool, lambda: P.affine_select(out=ident, in_=ident, pattern=[[-1, 128]],
                                         compare_op=ALU.is_equal, fill=0.0, base=0,
                                         channel_multiplier=1),
         reads=[R("ident")], writes=[R("ident")])
    K.op(K.dve, lambda: V.memset(ones_b, 1.0), writes=[R("ones")])
    K.op(K.dve, lambda: V.memset(st, 0.0), writes=[R("st")])
    K.op(K.dve, lambda: V.memset(eps_t, EPS), writes=[R("eps")])
    K.op(K.dve, lambda: V.memset(vsink[0:1, 0:64], 0.0), writes=[R("vsink")])
    K.op(K.dve, lambda: V.memset(vsink[0:1, 64:96], 1.0), writes=[R("vsink")])
    K.op(K.act, lambda: A.activation(out=sT, in_=prm[:, 0:8], func=AF.Silu),
         reads=[R("prm")], writes=[R("sT")])

    def rstd_cols(src, dst, n_inv, rd, wr):
        K.op(K.act, lambda: A.activation(out=dst, in_=src, func=AF.Ln, scale=n_inv, bias=eps_t[:, 0:1]),
             reads=list(rd) + [R("eps")], writes=list(wr))
        K.op(K.act, lambda: A.activation(out=dst, in_=dst, func=AF.Exp, scale=-0.5),
             reads=list(wr), writes=list(wr))

    al = Alloc(4 * KIB, LIM)
    wa = [al([128, 8, 512], F32) for i in range(3)]
    bada = al([1, 6 * D], F32)
    modrow = al([1, 6 * D], F32)
    dWa = [K.dsem("wa%d" % i) for i in range(3)]
    dWin = K.dsem("win")
    win0 = view(PA, [128, 8, 1120], BF16)
    K.dma(K.pool, [(win0, win_d.rearrange("(kc p) n -> p kc n", p=128))], dWin, writes=[R("win")])
    dBa = K.dsem("bada")
    wada_v = wada_d.rearrange("(kc p) n -> p kc n", p=128)
    K.dma(K.sp, [(bada, bada_d[:, :])], dBa, writes=[R("bada")])
    for j in range(12):
        sl = j % 3
        K.dma(K.sp, [(wa[sl], wada_v[:, :, j * 512:(j + 1) * 512])], dWa[sl], writes=[R("wa", sl)])
        pb = bank(j % 2)

        def mm(sl=sl, pb=pb):
            ins = None
            for kc in range(8):
                ins = G.matmul(pb[0:1, :], lhsT=sT[:, kc:kc + 1], rhs=wa[sl][:, kc, :],
                               start=(kc == 0), stop=(kc == 7))
            return ins
        K.op(K.pe, mm, reads=[R("wa", sl), R("sT")], writes=[R("ps", j % 2)])
        K.op(K.dve, lambda j=j, pb=pb: V.tensor_tensor(out=modrow[0:1, j * 512:(j + 1) * 512], in0=pb[0:1, :],
                                                       in1=bada[0:1, j * 512:(j + 1) * 512], op=ALU.add),
             reads=[R("ps", j % 2), R("bada")], writes=[R("modrow")])
    dM = K.dsem("mod")
    K.dma(K.sp, [(mod_d[:, :], modrow)], dM, reads=[R("modrow")], writes=[R("mod_d")])
    K.barrier()

    ckvnT = view(L2, [128, S], BF16)
    krT = view(L2 + 8 * KIB, [96, S], BF16)
    cqnT = view(L2 + 16 * KIB, [96, 2, NO * 128], BF16)
    qTa = view(L3, [64, 8, NO * 128], BF16)
    kTa = view(L3 + 32 * KIB, [64, 2, S], BF16)
    va = view(L3 + 48 * KIB, [128, NT, 2, 96], BF16)

    al = Alloc(PA, LIM)
    win = al([128, 8, 1120], BF16)
    b1row = al([1, 1120], BF16)
    xt = [al([128, D], F32) for i in range(2)]
    hb = [al([128, D], BF16) for i in range(3)]
    hT = [al([128, 8, 128], BF16) for i in range(3)]
    projS = [al([128, 1120], F32) for i in range(3)]
    rt = [al([128, 192], F32) for i in range(4)]
    t1 = [al([128, 640], F32) for i in range(2)]
    t2 = [al([128, 640], F32) for i in range(2)]
    t1r = [al([128, 32], F32) for i in range(2)]
    t2r = [al([128, 32], F32) for i in range(2)]
    qkr = [al([128, 640], BF16) for i in range(2)]
    ckvn = [al([128, 128], BF16) for i in range(2)]
    krs = [al([128, 96], BF16) for i in range(2)]
    cqn = [al([128, 192], BF16) for i in range(2)]
    junk = al([128, D], BF16)
    junk2 = al([128, 192], BF16)
    assert al.o <= 198 * KIB, al.o
    dW = K.dsem("wA")
    K.dma(K.sp, [(g1P, g1p_d[:, :]),
                 (a1P, mod_d[0, D:2 * D].rearrange("(kc p) -> p kc", p=128)),
                 (sh1P, mod_d[0, 0:D].rearrange("(kc p) -> p kc", p=128))], dW,
          reads=[R("mod_d")], writes=[R("a1P"), R("sh1P")], allow_slow_non_contiguous=True)
    K.op(K.dve, lambda: V.scalar_tensor_tensor(out=a1P, in0=a1P, scalar=1.0, in1=g1P, op0=ALU.add, op1=ALU.mult),
         reads=[R("a1P")], writes=[R("a1P")])
    K.op(K.dve, lambda: V.tensor_copy(out=sh1Pb, in_=sh1P), reads=[R("sh1P")], writes=[R("sh1Pb")])
    K.op(K.dve, lambda: V.memset(onesrow, 1.0), writes=[R("onesrow")])

    def b1mm():
        ins = None
        for (b, c0, n) in [(2, 0, 512), (3, 512, 512), (4, 1024, 96)]:
            for kc in range(8):
                ins = G.matmul(bank(b)[0:1, 0:n], lhsT=sh1Pb[:, kc:kc + 1], rhs=win[:, kc, c0:c0 + n],
                               start=(kc == 0), stop=(kc == 7))
        return ins
    K.op(K.pe, b1mm, reads=[R("sh1Pb"), R("win")], writes=[R("ps", 2), R("ps", 3), R("ps", 4)])
    for (b, c0, n) in [(2, 0, 512), (3, 512, 512), (4, 1024, 96)]:
        K.op(K.dve, lambda b=b, c0=c0, n=n: V.tensor_copy(out=b1row[0:1, c0:c0 + n], in_=bank(b)[0:1, 0:n]),
             reads=[R("ps", b)], writes=[R("b1row")])
    for kc in range(8):
        K.op(K.dve, lambda kc=kc: V.tensor_scalar(out=win[:, kc, :], in0=win[:, kc, :], scalar1=a1P[:, kc:kc + 1], scalar2=None,
                                                  op0=ALU.mult),
             reads=[R("win"), R("a1P")], writes=[R("win")])
    K.op(K.pool, lambda: P.memset(va[:, :, :, 64:96], 1.0), writes=RL("va", range(NT)))
    for i in range(2):
        K.op(K.pool, lambda i=i: P.memset(krs[i], 0.0), writes=[R("krs", i)])

    dX = [K.dsem("x%d" % i) for i in range(2)]
    x_t = x_d.rearrange("(t p) d -> t p d", p=128)
    rope_t = rope_d.rearrange("(t p) d -> t p d", p=128)
    CQS = (128.0 / 192.0) ** 0.5

    def rope(src3, cs, sn, half, o1, o2, dst, rd, r1, r2, wr_dst, nh):
        w = 2 * half
        K.op(K.dve, lambda: V.tensor_tensor(out=o1, in0=src3, in1=bc(cs.unsqueeze(1), [128, nh, w]), op=ALU.mult),
             reads=rd, writes=[r1])
        K.op(K.dve, lambda: V.tensor_tensor(out=o2[:, :, 0:half], in0=src3[:, :, half:w],
                                            in1=bc(sn[:, 0:half].unsqueeze(1), [128, nh, half]), op=ALU.mult),
             reads=rd, writes=[r2])
        K.op(K.dve, lambda: V.tensor_tensor(out=o2[:, :, half:w], in0=src3[:, :, 0:half],
                                            in1=bc(sn[:, half:w].unsqueeze(1), [128, nh, half]), op=ALU.mult),
             reads=rd, writes=[r2])
        K.op(K.pool, lambda: P.tensor_tensor(out=dst, in0=o1, in1=o2, op=ALU.add),
             reads=[r1, r2], writes=wr_dst)

    def stA1(t):
        own = t < NO
        sl = t % 2
        K.dma(K.sp, [(xt[sl], x_t[t]), (rt[t % 4], rope_t[t])], dX[sl], writes=[R("xt", sl), R("rt", t % 4)])
        K.op(K.act, lambda sl=sl, t=t: A.activation(out=junk, in_=xt[sl], func=AF.Square, accum_out=st[:, t:t + 1]),
             reads=[R("xt", sl)], writes=[R("junk"), R("stx", t)])
        rstd_cols(st[:, t:t + 1], st[:, 32 + t:33 + t], 1.0 / D, [R("stx", t)], [R("strx", t)])
        K.op(K.act, lambda sl=sl, t=t: A.activation(out=hb[t % 3], in_=xt[sl], func=AF.Copy, scale=st[:, 32 + t:33 + t]),
             reads=[R("xt", sl), R("strx", t)], writes=[R("hb", t % 3)])

    def stA2(t):
        own = t < NO
        sl = t % 2

        def tr(sl=sl):
            ins = None
            for kc in range(8):
                ins = G.transpose(out=bankb(sl)[:, kc * 128:(kc + 1) * 128], in_=hb[t % 3][:, kc * 128:(kc + 1) * 128],
                                  identity=ident)
            return ins
        K.op(K.pe, tr, reads=[R("hb", t % 3), R("ident")], writes=[R("ps", sl)])
        K.op(K.act, lambda sl=sl: A.copy(out=hT[t % 3], in_=bankb(sl).rearrange("p (a b) -> p a b", a=8)),
             reads=[R("ps", sl)], writes=[R("hT", t % 3)])

    def stA2b(t):
        own = t < NO
        sl = t % 2
        chunks = [(2, 0, 512), (3, 512, 416), (4, 928, 192)] if own else [(3, 512, 416)]

        def proj(sl=sl, chunks=chunks):
            ins = None
            for (b, c0, n) in chunks:
                for kc in range(8):
                    G.matmul(bank(b)[:, 0:n], lhsT=hT[t % 3][:, kc, :], rhs=win[:, kc, c0:c0 + n],
                             start=(kc == 0), stop=False)
                ins = G.matmul(bank(b)[:, 0:n], lhsT=onesrow[0:1, :], rhs=b1row[0:1, c0:c0 + n], start=False, stop=True)
            return ins
        K.op(K.pe, proj, reads=[R("hT", t % 3), R("win"), R("b1row"), R("onesrow")], writes=[R("ps", b) for (b, _, _) in chunks])
        for (b, c0, n) in chunks:
            if b == 2:
                K.op(K.dve, lambda b=b, c0=c0, n=n, sl=sl: V.tensor_copy(out=projS[t % 3][:, c0:c0 + n], in_=bank(b)[:, 0:n]),
                     reads=[R("ps", b)], writes=[R("projS", t % 3, b)])
            else:
                K.op(K.act, lambda b=b, c0=c0, n=n, sl=sl: A.copy(out=projS[t % 3][:, c0:c0 + n], in_=bank(b)[:, 0:n]),
                     reads=[R("ps", b)], writes=[R("projS", t % 3, b)])

    def stA3(t):
        own = t < NO
        sl = t % 2
        pS = projS[t % 3]
        if own:
            rope(h3(pS[:, 0:640], 10), rt[t % 4][:, 0:64], rt[t % 4][:, 64:128], 32, h3(t1[sl], 10), h3(t2[sl], 10),
                 h3(qkr[sl], 10), [R("projS", t % 3, 2), R("projS", t % 3, 3), R("rt", t % 4)], R("t1", sl), R("t2", sl),
                 [R("qkr", sl)], 10)
        else:
            rope(h3(pS[:, 512:640], 2), rt[t % 4][:, 0:64], rt[t % 4][:, 64:128], 32, h3(t1[sl][:, 512:640], 2),
                 h3(t2[sl][:, 512:640], 2), h3(qkr[sl][:, 512:640], 2), [R("projS", t % 3, 3), R("rt", t % 4)],
                 R("t1", sl), R("t2", sl), [R("qkr", sl)], 2)
        rope(h3(pS[:, 896:928], 1), rt[t % 4][:, 128:160], rt[t % 4][:, 160:192], 16, h3(t1r[sl], 1), h3(t2r[sl], 1),
             h3(krs[sl][:, 64:96], 1), [R("projS", t % 3, 3), R("rt", t % 4)], R("t1r", sl), R("t2r", sl), [R("krs", sl)], 1)
        K.op(K.pool, lambda t=t, pS=pS: P.tensor_copy(out=va[:, t, :, 0:64], in_=h3(pS[:, 640:768], 2)),
             reads=[R("projS", t % 3, 3)], writes=[R("va", t)])
        K.op(K.act, lambda pS=pS, t=t: A.activation(out=junk2[:, 0:128], in_=pS[:, 768:896], func=AF.Square,
                                                    accum_out=st[:, 64 + 2 * t:65 + 2 * t]),
             reads=[R("projS", t % 3, 3)], writes=[R("junk2"), R("stk", t)])
        if own:
            K.op(K.act, lambda pS=pS, t=t: A.activation(out=junk2, in_=pS[:, 928:1120], func=AF.Square, scale=CQS,
                                                        accum_out=st[:, 65 + 2 * t:66 + 2 * t]),
                 reads=[R("projS", t % 3, 4)], writes=[R("junk2"), R("stk", t)])
        nsc = 2 if own else 1
        rc = 128 + 2 * (t % 16)
        rstd_cols(st[:, 64 + 2 * t:64 + 2 * t + nsc], st[:, rc:rc + nsc], 1.0 / 128, [R("stk", t)], [R("strk", t % 16)])
        K.op(K.dve, lambda pS=pS, sl=sl, rc=rc: V.tensor_scalar(out=ckvn[sl], in0=pS[:, 768:896], scalar1=st[:, rc:rc + 1],
                                                                scalar2=None, op0=ALU.mult),
             reads=[R("projS", t % 3, 3), R("strk", t % 16)], writes=[R("ckvn", sl)])
        if own:
            K.op(K.dve, lambda pS=pS, sl=sl, rc=rc: V.tensor_scalar(out=cqn[sl], in0=pS[:, 928:1120],
                                                                    scalar1=st[:, rc + 1:rc + 2], scalar2=None, op0=ALU.mult),
                 reads=[R("projS", t % 3, 4), R("strk", t % 16)], writes=[R("cqn", sl)])
        bk = 6 + sl
        b6 = bankb(bk)

        def trB(own=own, sl=sl, b6=b6):
            G.transpose(out=b6[0:64, 0:128], in_=qkr[sl][:, 512:576], identity=ident)
            G.transpose(out=b6[0:64, 128:256], in_=qkr[sl][:, 576:640], identity=ident)
            G.transpose(out=b6[:, 256:384], in_=ckvn[sl], identity=ident)
            ins = G.transpose(out=b6[0:96, 384:512], in_=krs[sl], identity=ident)
            if own:
                G.transpose(out=b6[0:96, 512:640], in_=cqn[sl][:, 0:96], identity=ident)
                ins = G.transpose(out=b6[0:96, 640:768], in_=cqn[sl][:, 96:192], identity=ident)
            return ins
        K.op(K.pe, trB, reads=[R("qkr", sl), R("ckvn", sl), R("krs", sl), R("ident")] + ([R("cqn", sl)] if own else []),
             writes=[R("ps", bk)])
        K.op(K.dve, lambda t=t, b6=b6: V.tensor_copy(out=kTa[:, :, t * 128:(t + 1) * 128],
                                                     in_=b6[0:64, 0:256].rearrange("p (a b) -> p a b", a=2)),
             reads=[R("ps", bk)], writes=[R("kTa", t)])
        K.op(K.dve, lambda t=t, b6=b6: V.tensor_copy(out=ckvnT[:, t * 128:(t + 1) * 128], in_=b6[:, 256:384]),
             reads=[R("ps", bk)], writes=[R("ckvnT", t)])
        K.op(K.dve, lambda t=t, b6=b6: V.tensor_copy(out=krT[64:96, t * 128:(t + 1) * 128], in_=b6[64:96, 384:512]),
             reads=[R("ps", bk)], writes=[R("krT", t)])
        if own:
            K.op(K.dve, lambda t=t, b6=b6: V.tensor_copy(out=cqnT[:, :, t * 128:(t + 1) * 128],
                                                         in_=b6[0:96, 512:768].rearrange("p (a b) -> p a b", a=2)),
                 reads=[R("ps", bk)], writes=[R("cqnT", t)])
            b5 = bankb(5)

            def trQ(sl=sl, b5=b5):
                ins = None
                for h in range(8):
                    ins = G.transpose(out=b5[0:64, h * 128:(h + 1) * 128], in_=qkr[sl][:, h * 64:(h + 1) * 64],
                                      identity=ident)
                return ins
            K.op(K.pe, trQ, reads=[R("qkr", sl), R("ident")], writes=[R("ps", 5)])
            K.op(K.act, lambda t=t, b5=b5: A.copy(out=qTa[:, :, t * 128:(t + 1) * 128],
                                                  in_=b5[0:64, :].rearrange("p (a b) -> p a b", a=8)),
                 reads=[R("ps", 5)], writes=[R("qTa", t)])

    gt1Ba = view(86 * KIB, [128, D], F32)
    wstg = [view(90 * KIB + i * 4 * KIB, [128, D], F32) for i in range(2)]
    wobf = [view(98 * KIB + i * 2 * KIB, [128, D], BF16) for i in range(2)]
    dWo = [K.dsem("wo%d" % i) for i in range(2)]
    dWos = [K.dsem("wos%d" % i) for i in range(2)]
    dGt = K.dsem("gt1a")
    wout_v = wout_d.rearrange("(kc p) n -> kc p n", p=128)
    wo_dv = wo_d.rearrange("(kc p) n -> kc p n", p=128)
    K.dma(K.sp, [(gt1Ba, modB(2))], dGt, reads=[R("mod_d")], writes=[R("gt1Ba")])

    def wo_load(kc):
        K.dma(K.sp, [(wstg[kc % 2], wout_v[kc])], dWo[kc % 2], writes=[R("wstg", kc % 2)])

    def wo_fold(kc):
        K.op(K.dve, lambda: V.scalar_tensor_tensor(out=wobf[kc % 2], in0=wstg[kc % 2], scalar=prm[:, 11 + kc:12 + kc],
                                                   in1=gt1Ba, op0=ALU.mult, op1=ALU.mult),
             reads=[R("wstg", kc % 2), R("prm"), R("gt1Ba")], writes=[R("wobf", kc % 2)])

    def wo_store(kc):
        K.dma(K.sp, [(wo_dv[kc], wobf[kc % 2])], dWos[kc % 2], reads=[R("wobf", kc % 2)], writes=[R("wo_d", kc)])

    zsrc = view(198 * KIB, [128, 2048], F32)
    K.op(K.pool, lambda: P.memset(zsrc, 0.0), writes=[R("zsrc")])
    dZf = K.dsem("zf")
    xs_z = xs_d.rearrange("(p r) d -> p r d", p=128)
    ys_z = ys_d.rearrange("(p r) d -> p r d", p=128)
    zjobs = [(xs_z[:, 4 * c:4 * c + 4, :], zsrc.bitcast(BF16).rearrange("p (a b) -> p a b", a=4)) for c in range(24)]
    zjobs += [(ys_z[:, 2 * c:2 * c + 2, :], zsrc.rearrange("p (a b) -> p a b", a=2)) for c in range(48)]

    for i in range(NT + 3):
        for zi in range(3 * i, min(3 * i + 3, len(zjobs))):
            K.dma(K.sp, [zjobs[zi]], dZf, reads=[R("zsrc")], writes=[R("zf", zi)])
        if i >= 6 and (i - 6) % 2 == 0 and (i - 6) // 2 < 8:
            wo_load((i - 6) // 2)
        if i >= 7 and (i - 7) % 2 == 0 and (i - 7) // 2 < 8:
            wo_fold((i - 7) // 2)
        if i >= 8 and (i - 8) % 2 == 0 and (i - 8) // 2 < 8:
            wo_store((i - 8) // 2)
        if i < NT:
            stA1(i)
        if 0 <= i - 1 < NT:
            stA2(i - 1)
        if 0 <= i - 2 < NT:
            stA2b(i - 2)
        if 0 <= i - 3 < NT:
            stA3(i - 3)
    K.barrier()

    mixTa = view(L4, [128, 4, NO * 128], BF16)
    mixTb = view(L4 + 16 * KIB, [128, 4, NO * 128], BF16)
    al = Alloc(PA, LIM)
    pT = [al([128, 3, 512], BF16) for i in range(2)]
    rden = [al([64, 512], F32) for i in range(2)]
    lnt = [al([32, 512], F32) for i in range(2)]
    msk = al([128, 4, 128], BF16)
    esrow = al([1, 8, 128], BF16)
    dMsk = K.dsem("msk")
    K.dma(K.pool, [(msk, msk_d.rearrange("p (a b) -> p a b", a=4))], dMsk, writes=[R("msk")])
    K.op(K.act, lambda: A.activation(out=es8, in_=prm[0:1, 20:28], func=AF.Exp), reads=[R("prm")], writes=[R("es8")])
    K.op(K.dve, lambda: V.tensor_copy(out=esrow, in_=bc(es8.unsqueeze(2), [1, 8, 128])), reads=[R("es8")],
         writes=[R("esrow")])

    def finish(ob, rd_sl, writers, use_act):
        oT = bank(ob)
        rd = rden[rd_sl]
        if use_act:
            K.op(K.act, lambda: A.activation(out=lnt[rd_sl], in_=oT[64:96, :], func=AF.Ln),
                 reads=[R("ps", ob)], writes=[R("lnt", rd_sl)])
            K.op(K.act, lambda: A.activation(out=rd[0:32, :], in_=lnt[rd_sl], func=AF.Exp, scale=-1.0),
                 reads=[R("lnt", rd_sl)], writes=[R("rden", rd_sl)])
            K.op(K.dve, lambda: V.tensor_copy(out=rd[32:64, :], in_=rd[0:32, :]),
                 reads=[R("rden", rd_sl)], writes=[R("rden", rd_sl)])
        else:
            K.op(K.dve, lambda: V.reciprocal(out=rd[0:32, :], in_=oT[64:96, :]),
                 reads=[R("ps", ob)], writes=[R("rden", rd_sl)])
            K.op(K.dve, lambda: V.tensor_copy(out=rd[32:64, :], in_=rd[0:32, :]),
                 reads=[R("rden", rd_sl)], writes=[R("rden", rd_sl)])
        for (out_ap, in_sl, wr) in writers:
            K.op(K.dve, lambda out_ap=out_ap, in_sl=in_sl: V.tensor_tensor(out=out_ap, in0=in_sl(oT[0:64, :]),
                                                                           in1=in_sl(rd), op=ALU.mult),
                 reads=[R("ps", ob), R("rden", rd_sl)], writes=[wr])

    def swa_ctx(it):
        n, kvh = divmod(it, 2)
        kts = [(31 if n == 0 else n - 1, 2 if n == 0 else 0), (n, None), (16 if n == NO - 1 else n + 1, 3 if n == NO - 1 else 1)]
        sl = it % 2
        return n, kvh, kts, sl, 3 * sl, 6 + sl

    def stW1(it):
        n, kvh, kts, sl, b0, ob = swa_ctx(it)

        def sc():
            ins = None
            for i, (kt, _) in enumerate(kts):
                ins = G.matmul(bank(b0 + i).rearrange("p (a b) -> p a b", a=4),
                               lhsT=kTa[:, kvh, kt * 128:(kt + 1) * 128],
                               rhs=qTa[:, kvh * 4:(kvh + 1) * 4, n * 128:(n + 1) * 128], start=True, stop=True)
            return ins
        K.op(K.pe, sc, reads=[], writes=[R("ps", b0 + i) for i in range(3)])
        K.op(K.act, lambda: A.activation(out=pT[sl].rearrange("p a b -> p (a b)"), in_=bank(b0, 3),
                                         func=AF.Exp, scale=0.125),
             reads=[R("ps", b0 + i) for i in range(3)], writes=[R("pT", sl)])
        for i, (kt, m) in enumerate(kts):
            if m is None:
                continue
            K.op(K.pool, lambda i=i, m=m: P.tensor_tensor(
                out=pT[sl][:, i, :].rearrange("p (a b) -> p a b", a=4),
                in0=pT[sl][:, i, :].rearrange("p (a b) -> p a b", a=4),
                in1=bc(msk[:, m, :].unsqueeze(1), [128, 4, 128]), op=ALU.mult),
                reads=[R("pT", sl), R("msk")], writes=[R("pT", sl)])

    def stW2(it):
        n, kvh, kts, sl, b0, ob = swa_ctx(it)

        def pv():
            for i, (kt, _) in enumerate(kts):
                G.matmul(bank(ob)[0:96, :], lhsT=va[:, kt, kvh, :], rhs=pT[sl][:, i, :],
                         start=(i == 0), stop=False)
            return G.matmul(bank(ob)[0:96, :], lhsT=vsink[0:1, :],
                            rhs=esrow[0:1, kvh * 4:(kvh + 1) * 4, :].rearrange("p a b -> p (a b)"),
                            start=False, stop=True)
        K.op(K.pe, pv, reads=[R("pT", sl), R("vsink"), R("esrow")], writes=[R("ps", ob)])
        writers = []
        for par in range(2):
            def in_sl(ap, par=par):
                return ap.rearrange("p (i two b) -> p i two b", two=2, b=128)[:, :, par, :]
            writers.append((mixTa[par * 64:par * 64 + 64, 2 * kvh:2 * kvh + 2, n * 128:(n + 1) * 128], in_sl,
                            R("mixTa", n, kvh, par)))
        finish(ob, sl, writers, True)

    for i in range(2 * NO + 1):
        if i < 2 * NO:
            stW1(i)
        if i >= 1:
            stW2(i - 1)
    K.barrier()

    qTb = view(L3, [96, 8, NO * 128], BF16)
    kTb = [view(L3 + 32 * KIB + i * 8 * KIB, [96, S], BF16) for i in range(2)]
    pTm = [view(L3 + 48 * KIB + i * 3072, [128, 3, 512], BF16) for i in range(4)]
    al = Alloc(PA, LIM)
    vb = al([128, NT, 8, 96], BF16)
    rden = [al([64, 512], F32) for i in range(2)]
    wuqs = al([96, 2, 768], F32)
    wuq = al([96, 2, 768], BF16)
    wukvs = al([128, 1024], F32)
    wukv = al([128, 8, 128], BF16)
    rt2 = [al([128, 64], F32) for i in range(2)]
    qbS = [al([128, 8, 96], F32) for i in range(2)]
    t1b = [al([128, 8, 32], F32) for i in range(2)]
    t2b = [al([128, 8, 32], F32) for i in range(2)]
    qbr = [al([128, 8, 96], BF16) for i in range(2)]

    dW2 = K.dsem("wB")
    K.dma(K.sp, [(wuqs, wuq_d.rearrange("(kc p) n -> p kc n", p=96)), (wukvs, wukv_d[:, :])], dW2,
          writes=[R("wuqs"), R("wukvs")])
    for kc in range(2):
        K.op(K.dve, lambda kc=kc: V.tensor_scalar(out=wuq[:, kc, :], in0=wuqs[:, kc, :], scalar1=prm[0:96, 8 + kc:9 + kc],
                                                  scalar2=None, op0=ALU.mult),
             reads=[R("wuqs"), R("prm")], writes=[R("wuq")])
    K.op(K.dve, lambda: V.tensor_scalar(out=wukv.rearrange("p a b -> p (a b)"), in0=wukvs, scalar1=prm[:, 10:11],
                                        scalar2=None, op0=ALU.mult),
         reads=[R("wukvs"), R("prm")], writes=[R("wukv")])
    K.op(K.pool, lambda: P.memset(vb[:, :, :, 64:96], 1.0), writes=RL("vb", range(NT)))
    for kt in range(NT):
        b = kt % 2
        K.op(K.pe, lambda kt=kt, b=b: G.matmul(bank(b).rearrange("p (a b) -> p a b", a=8),
                                               lhsT=ckvnT[:, kt * 128:(kt + 1) * 128], rhs=wukv[:, :, 64:128],
                                               start=True, stop=True),
             reads=[R("wukv")], writes=[R("ps", b)])
        if kt % 2:
            K.op(K.dve, lambda kt=kt, b=b: V.tensor_copy(out=vb[:, kt, :, 0:64], in_=bank(b).rearrange("p (a b) -> p a b", a=8)),
                 reads=[R("ps", b)], writes=[R("vb", kt)])
        else:
            K.op(K.act, lambda kt=kt, b=b: A.copy(out=vb[:, kt, :, 0:64], in_=bank(b).rearrange("p (a b) -> p a b", a=8)),
                 reads=[R("ps", b)], writes=[R("vb", kt)])
    dX2 = [K.dsem("r%d" % i) for i in range(2)]
    def stQ1(t):
        sl = t % 2
        K.dma(K.sp, [(rt2[sl], rope_t[t][:, 128:192])], dX2[sl], writes=[R("rt2", sl)])
        bq = 2 + 2 * sl

        def qp(t=t, bq=bq):
            ins = None
            for (b, c0, n) in [(bq, 0, 512), (bq + 1, 512, 256)]:
                for kc in range(2):
                    ins = G.matmul(bank(b)[:, 0:n], lhsT=cqnT[:, kc, t * 128:(t + 1) * 128], rhs=wuq[:, kc, c0:c0 + n],
                                   start=(kc == 0), stop=(kc == 1))
            return ins
        K.op(K.pe, qp, reads=[R("wuq")], writes=[R("ps", bq), R("ps", bq + 1)])
        qf = qbS[sl].rearrange("p a b -> p (a b)")
        K.op(K.act, lambda qf=qf, bq=bq: A.copy(out=qf[:, 0:512], in_=bank(bq)), reads=[R("ps", bq)], writes=[R("qbS", sl)])
        K.op(K.act, lambda qf=qf, bq=bq: A.copy(out=qf[:, 512:768], in_=bank(bq + 1)[:, 0:256]), reads=[R("ps", bq + 1)],
             writes=[R("qbS", sl)])

    def stQ2(t):
        sl = t % 2
        K.op(K.pool, lambda sl=sl: P.tensor_copy(out=qbr[sl][:, :, 0:64], in_=qbS[sl][:, :, 0:64]), reads=[R("qbS", sl)],
             writes=[R("qbr", sl)])
        rope(qbS[sl][:, :, 64:96], rt2[sl][:, 0:32], rt2[sl][:, 32:64], 16, t1b[sl], t2b[sl], qbr[sl][:, :, 64:96],
             [R("qbS", sl), R("rt2", sl)], R("t1b", sl), R("t2b", sl), [R("qbr", sl)], 8)
        b4 = bankb(6 + sl)

        def trq(sl=sl, b4=b4):
            ins = None
            for h in range(8):
                ins = G.transpose(out=b4[0:96, h * 128:(h + 1) * 128], in_=qbr[sl][:, h, :], identity=ident)
            return ins
        K.op(K.pe, trq, reads=[R("qbr", sl), R("ident")], writes=[R("ps", 6 + sl)])
        K.op(K.dve, lambda t=t, b4=b4: V.tensor_copy(out=qTb[:, :, t * 128:(t + 1) * 128],
                                                     in_=b4[0:96, :].rearrange("p (a b) -> p a b", a=8)),
             reads=[R("ps", 6 + sl)], writes=[R("qTb", t)])

    for i in range(NO + 1):
        if i < NO:
            stQ1(i)
        if i >= 1:
            stQ2(i - 1)

    ktg = [list(range(k, min(k + 3, NT))) for k in range(0, NT, 3)]
    cgs = [[0, 1, 2], [3, 4, 5], [6, 7]]

    def setup_steps(h):
        return [("setup", h, cg) for cg in cgs]

    dWbf = K.dsem("wbf")
    K.dma(K.pool, [(wbf_d[c * 512:(c + 1) * 512, m * 2048:(m + 1) * 2048], src[c * 512:(c + 1) * 512, :])
                   for c in range(8) for m, src in enumerate((wg_d, wu_d, wd_d))], dWbf, writes=[R("wbf")])
    steps = setup_steps(0)
    for h in range(8):
        for qg in range(4):
            steps += [("attn", h, qg, gi) for gi in range(len(ktg))]
            if qg == 0 and h < 7:
                steps += setup_steps(h + 1)
    SK = 2
    sc_scale = 96.0 ** -0.5
    for i in range(len(steps) + SK):
        if i < len(steps):
            stp = steps[i]
            b0 = 3 * (i % 2)
            if stp[0] == "setup":
                _, h, cg = stp
                sl = h % 2
                if cg[0] == 0:
                    K.op(K.pool, lambda sl=sl: P.tensor_copy(out=kTb[sl][64:96, :], in_=krT[64:96, :]),
                         writes=[R("kTbr", sl)])

                def su(h=h, cg=cg, b0=b0):
                    ins = None
                    for j, c in enumerate(cg):
                        ins = G.matmul(bank(b0 + j)[0:64, :], lhsT=wukv[:, h, 0:64], rhs=ckvnT[:, c * 512:(c + 1) * 512],
                                       start=True, stop=True)
                    return ins
                K.op(K.pe, su, reads=[R("wukv")], writes=[R("ps", b0 + j) for j in range(3)])
                K.op(K.dve, lambda sl=sl, cg=cg, b0=b0: V.tensor_copy(
                    out=kTb[sl][0:64, cg[0] * 512:(cg[-1] + 1) * 512], in_=bank(b0, len(cg))[0:64, :]),
                    reads=[R("ps", b0 + j) for j in range(3)], writes=[R("kTb", sl)])
            else:
                _, h, qg, gi = stp
                kl = ktg[gi]

                def sc(h=h, qg=qg, kl=kl, b0=b0):
                    ins = None
                    for j, kt in enumerate(kl):
                        ins = G.matmul(bank(b0 + j), lhsT=kTb[h % 2][:, kt * 128:(kt + 1) * 128],
                                       rhs=qTb[:, h, qg * 512:(qg + 1) * 512], start=True, stop=True)
                    return ins
                K.op(K.pe, sc, reads=[R("kTb", h % 2), R("kTbr", h % 2)] + RL("qTb", range(qg * 4, qg * 4 + 4)),
                     writes=[R("ps", b0 + j) for j in range(3)])
                s4 = i % 4
                nk = len(kl)
                K.op(K.act, lambda s4=s4, b0=b0, nk=nk: A.activation(out=pTm[s4].rearrange("p a b -> p (a b)")[:, 0:nk * 512],
                                                                     in_=bank(b0, nk), func=AF.Exp, scale=sc_scale),
                     reads=[R("ps", b0 + j) for j in range(3)], writes=[R("pTm", s4)])
        if i >= SK and steps[i - SK][0] == "attn":
            _, h, qg, gi = steps[i - SK]
            kl = ktg[gi]
            s4 = (i - SK) % 4
            gsl = (h * 4 + qg) % 2
            ob = 6 + gsl

            def pv(h=h, kl=kl, s4=s4, ob=ob):
                ins = None
                for j, kt in enumerate(kl):
                    ins = G.matmul(bank(ob)[0:96, :], lhsT=vb[:, kt, h, :], rhs=pTm[s4][:, j, :],
                                   start=(kt == 0), stop=(kt == NT - 1))
                return ins
            K.op(K.pe, pv, reads=[R("pTm", s4)] + [R("vb", kt) for kt in kl], writes=[R("ps", ob)])
            if kl[-1] == NT - 1:
                finish(ob, gsl, [(mixTb[(h % 2) * 64:(h % 2) * 64 + 64, h // 2, qg * 512:(qg + 1) * 512],
                                 (lambda ap: ap), R("mixTb", h, qg))], False)
    K.barrier()

    PC = 120 * KIB
    acc = view(2 * KIB, [128, NO, D], F32)
    posu = view(66 * KIB, [128, 2, NO], U32)
    w12 = view(66 * KIB + 128, [128, 2, NO], F32)
    widx = view(66 * KIB + 256, [128, 48], U32)
    sidx = view(66 * KIB + 448, [128, 48], U32)
    wo = view(68 * KIB, [128, 8, D], BF16)
    h2tok = view(PC, [128, NO, D], BF16)
    al = Alloc(PC + 32 * KIB, LIM)
    LGa = al([128, NO, 36], F32)
    r_m4 = al([128, NO], F32)
    r_d4 = al([128, NO, 4], F32)
    r_e4 = al([128, NO, 4], F32)
    r_s4 = al([128, NO], F32)
    r_oh = al([128, NO, 4], F32)
    r_t32 = al([128, NO, 32], F32)
    r_el = al([128, NO, 8], F32)
    r_el2 = al([128, NO, 8], F32)
    r_v1 = al([128, NO], F32)
    r_d8 = al([128, NO, 8], F32)
    r_eq = al([128, NO, 8], F32)
    r_v2 = al([128, NO], F32)
    r_mk = al([128, NO, 8], F32)
    r_ex = al([128, NO, 8], F32)
    r_cw = al([128, NO, 8], F32)
    r_den = al([128, NO], F32)
    SORT0 = al.o
    xt = [al([128, D], F32) for i in range(2)]
    tmpB = [al([128, D], F32) for i in range(2)]
    a2B = al([128, D], F32)
    sh2B = al([128, D], F32)
    h2Th = [al([128, 8, 128], BF16) for i in range(2)]
    lo_t = [al([128, D], BF16) for i in range(2)]
    h2Tl = [al([128, 8, 128], BF16) for i in range(2)]
    wrs = al([128, 8, 36], F32)
    wrh = al([128, 8, 36], BF16)
    wrl = al([128, 8, 36], BF16)
    rab = al([128, 32], F32)
    sqs = [al([128, 8, 128], BF16) for i in range(2)]
    junk = al([128, D], BF16)
    gt1B = tmpB[0]
    g2B = tmpB[1]

    K.op(K.dve, lambda: V.memset(st, 0.0), writes=[R("st")])
    dW3 = K.dsem("wC")
    K.dma(K.sp, [(a2B, modB(4)), (sh2B, modB(3)), (gt1B, modB(2)), (g2B, gB_d[:, D:2 * D]),
                 (wrs, wr_d.rearrange("(kc p) n -> p kc n", p=128))], dW3,
          writes=[R("a2B"), R("sh2B"), R("tmpB", 0), R("tmpB", 1), R("wrs")])
    K.op(K.dve, lambda: V.scalar_tensor_tensor(out=a2B, in0=a2B, scalar=1.0, in1=g2B, op0=ALU.add, op1=ALU.mult),
         reads=[R("a2B"), R("tmpB", 1)], writes=[R("a2B"), R("tmpB", 1)])
    K.op(K.act, lambda: A.copy(out=wrh, in_=wrs), reads=[R("wrs")], writes=[R("wrh")])
    K.op(K.pool, lambda: P.tensor_tensor(out=wrl, in0=wrs, in1=wrh, op=ALU.subtract), reads=[R("wrs"), R("wrh")],
         writes=[R("wrl")])
    dWoL = K.dsem("woL")
    K.dma(K.sp, [(wo, wo_d.rearrange("(kc p) n -> p kc n", p=128))], dWoL, writes=[R("wo")])
    for t in range(NO):
        sl = t % 2
        K.op(K.pool, lambda t=t, sl=sl: P.tensor_tensor(out=sqs[sl][:, 0:4, :], in0=mixTa[:, :, t * 128:(t + 1) * 128],
                                                        in1=mixTa[:, :, t * 128:(t + 1) * 128], op=ALU.mult),
             writes=[R("sqs", sl)])
        K.op(K.act, lambda t=t, sl=sl: A.activation(out=sqs[sl][:, 4:8, :], in_=mixTb[:, :, t * 128:(t + 1) * 128], func=AF.Square),
             writes=[R("sqsb", sl)])

        def ssq(t=t, sl=sl):
            ins = None
            for g2 in range(2):
                for j in range(4):
                    ins = G.matmul(bank(7)[:, 2 * t + g2:2 * t + g2 + 1], lhsT=sqs[sl][:, 4 * g2 + j, :], rhs=ones_b[:, 0:1],
                                   start=(j == 0), stop=(j == 3))
            return ins
        K.op(K.pe, ssq, reads=[R("sqs", sl), R("sqsb", sl), R("ones")], writes=[R("ps", 7)])
    K.op(K.act, lambda: A.activation(out=rab, in_=bank(7)[:, 0:32], func=AF.Ln, scale=1.0 / 512, bias=eps_t[:, 0:1]),
         reads=[R("ps", 7), R("eps")], writes=[R("rab")])
    K.op(K.act, lambda: A.activation(out=rab, in_=rab, func=AF.Exp, scale=-0.5), reads=[R("rab")], writes=[R("rab")])

    brB = prm[:, 28:64]
    def stC1(t):
        sl = t % 2
        K.dma(K.sp, [(xt[sl], x_t[t])], dX[sl], writes=[R("xt", sl)])

        def op_(t=t):
            ins = None
            for (mt, b0, k0) in [(mixTa, 0, 0), (mixTb, 2, 4)]:
                for dh in range(2):
                    for kc in range(4):
                        ins = G.matmul(bank(b0 + dh), lhsT=mt[:, kc, t * 128:(t + 1) * 128],
                                       rhs=wo[:, k0 + kc, dh * 512:(dh + 1) * 512], start=(kc == 0), stop=(kc == 3))
            return ins
        K.op(K.pe, op_, reads=[R("wo")], writes=[R("ps", b) for b in range(4)])
        K.op(K.dve, lambda t=t, sl=sl: V.scalar_tensor_tensor(out=acc[:, t, :], in0=bank(0, 2), scalar=rab[:, 2 * t:2 * t + 1],
                                                              in1=xt[sl], op0=ALU.mult, op1=ALU.add),
             reads=[R("ps", 0), R("ps", 1), R("rab"), R("xt", sl)], writes=[R("acc", t)])
        K.op(K.dve, lambda t=t: V.scalar_tensor_tensor(out=acc[:, t, :], in0=bank(2, 2), scalar=rab[:, 2 * t + 1:2 * t + 2],
                                                       in1=acc[:, t, :], op0=ALU.mult, op1=ALU.add),
             reads=[R("ps", 2), R("ps", 3), R("rab"), R("acc", t)], writes=[R("acc", t)])
        K.op(K.act, lambda t=t: A.activation(out=junk, in_=acc[:, t, :], func=AF.Square, accum_out=st[:, t:t + 1]),
             reads=[R("acc", t), R("st")], writes=[R("junk"), R("stx", t)])
        rstd_cols(st[:, t:t + 1], st[:, 32 + t:33 + t], 1.0 / D, [R("stx", t)], [R("strx", t)])

    def stC2(t):
        sl = t % 2
        K.op(K.act, lambda t=t, sl=sl: A.activation(out=tmpB[sl], in_=acc[:, t, :], func=AF.Copy, scale=st[:, 32 + t:33 + t]),
             reads=[R("acc", t), R("strx", t)], writes=[R("tmpB", sl)])
        K.op(K.dve, lambda sl=sl: V.tensor_tensor(out=tmpB[sl], in0=tmpB[sl], in1=a2B, op=ALU.mult),
             reads=[R("tmpB", sl), R("a2B")], writes=[R("tmpB", sl)])
        K.op(K.dve, lambda sl=sl: V.tensor_tensor(out=tmpB[sl], in0=tmpB[sl], in1=sh2B, op=ALU.add),
             reads=[R("tmpB", sl), R("sh2B")], writes=[R("tmpB", sl)])
        K.op(K.act, lambda sl=sl, t=t: A.copy(out=h2tok[:, t, :], in_=tmpB[sl]), reads=[R("tmpB", sl)], writes=[R("h2tok", t)])
        K.op(K.pool, lambda sl=sl, t=t: P.tensor_tensor(out=lo_t[sl], in0=tmpB[sl], in1=h2tok[:, t, :], op=ALU.subtract),
             reads=[R("tmpB", sl), R("h2tok", t)], writes=[R("lo_t", sl)])

    def stC2b(t):
        sl = t % 2

        def tr2(sl=sl, t=t):
            ins = None
            for kc in range(8):
                G.transpose(out=bankb(4)[:, kc * 128:(kc + 1) * 128], in_=h2tok[:, t, kc * 128:(kc + 1) * 128], identity=ident)
                ins = G.transpose(out=bankb(5)[:, kc * 128:(kc + 1) * 128], in_=lo_t[sl][:, kc * 128:(kc + 1) * 128],
                                  identity=ident)
            return ins
        K.op(K.pe, tr2, reads=[R("h2tok", t), R("lo_t", sl), R("ident")], writes=[R("ps", 4), R("ps", 5)])
        K.op(K.act, lambda sl=sl: A.copy(out=h2Th[sl], in_=bankb(4).rearrange("p (a b) -> p a b", a=8)),
             reads=[R("ps", 4)], writes=[R("h2Th", sl)])
        K.op(K.dve, lambda sl=sl: V.tensor_copy(out=h2Tl[sl], in_=bankb(5).rearrange("p (a b) -> p a b", a=8)),
             reads=[R("ps", 5)], writes=[R("h2Tl", sl)])

    def stC3(t):
        sl = t % 2

        def lg(t=t, sl=sl):
            ins = None
            combos = [(h2Th[sl], wrh), (h2Tl[sl], wrh), (h2Th[sl], wrl)]
            for ci, (a_, w_) in enumerate(combos):
                for kc in range(8):
                    ins = G.matmul(bank(6)[:, 0:36], lhsT=a_[:, kc, :], rhs=w_[:, kc, :],
                                   start=(ci == 0 and kc == 0), stop=(ci == 2 and kc == 7))
            return ins
        K.op(K.pe, lg, reads=[R("h2Th", sl), R("h2Tl", sl), R("wrh"), R("wrl")], writes=[R("ps", 6)])
        K.op(K.dve, lambda t=t: V.tensor_tensor(out=LGa[:, t, :], in0=bank(6)[:, 0:36], in1=brB, op=ALU.add),
             reads=[R("ps", 6), R("prm")], writes=[R("rw")])

    for i in range(NO + 3):
        if i < NO:
            stC1(i)
        if 0 <= i - 1 < NO:
            stC2(i - 1)
        if 0 <= i - 2 < NO:
            stC2b(i - 2)
        if 0 <= i - 3 < NO:
            stC3(i - 3)
    RW = [R("rw")]

    def rop(eng, fn):
        K.op(eng, fn, reads=RW, writes=RW)
    T = NO
    Lg = LGa[:, :, 0:4]
    rop(K.dve, lambda: V.tensor_reduce(out=r_m4, in_=Lg, axis=AX.X, op=ALU.max))
    rop(K.dve, lambda: V.tensor_tensor(out=r_d4, in0=Lg, in1=bc(r_m4.unsqueeze(2), [128, T, 4]), op=ALU.subtract))
    rop(K.act, lambda: A.activation(out=r_e4, in_=r_d4, func=AF.Exp))
    rop(K.dve, lambda: V.tensor_reduce(out=r_s4, in_=r_e4, axis=AX.X, op=ALU.add))
    rop(K.dve, lambda: V.tensor_scalar(out=r_oh, in0=r_d4, scalar1=0.0, scalar2=None, op0=ALU.is_ge))
    for g in range(4):
        rop(K.dve, lambda g=g: V.tensor_tensor(out=r_t32[:, :, g * 8:(g + 1) * 8], in0=LGa[:, :, 4 + g * 8:12 + g * 8],
                                               in1=bc(r_oh[:, :, g:g + 1], [128, T, 8]), op=ALU.mult))
    rop(K.dve, lambda: V.tensor_tensor(out=r_el, in0=r_t32[:, :, 0:8], in1=r_t32[:, :, 8:16], op=ALU.add))
    rop(K.dve, lambda: V.tensor_tensor(out=r_el2, in0=r_t32[:, :, 16:24], in1=r_t32[:, :, 24:32], op=ALU.add))
    rop(K.dve, lambda: V.tensor_tensor(out=r_el, in0=r_el, in1=r_el2, op=ALU.add))
    rop(K.dve, lambda: V.tensor_reduce(out=r_v1, in_=r_el, axis=AX.X, op=ALU.max))
    rop(K.dve, lambda: V.tensor_tensor(out=r_d8, in0=r_el, in1=bc(r_v1.unsqueeze(2), [128, T, 8]), op=ALU.subtract))
    rop(K.dve, lambda: V.tensor_scalar(out=r_eq, in0=r_d8, scalar1=0.0, scalar2=None, op0=ALU.is_ge))
    rop(K.dve, lambda: V.scalar_tensor_tensor(out=r_el2, in0=r_eq, scalar=-1e30, in1=r_d8, op0=ALU.mult, op1=ALU.add))
    rop(K.dve, lambda: V.tensor_reduce(out=r_v2, in_=r_el2, axis=AX.X, op=ALU.max))
    rop(K.dve, lambda: V.tensor_tensor(out=r_mk, in0=r_d8, in1=bc(r_v2.unsqueeze(2), [128, T, 8]), op=ALU.is_ge))
    rop(K.act, lambda: A.activation(out=r_ex, in_=r_d8, func=AF.Exp))
    rop(K.dve, lambda: V.tensor_tensor(out=r_cw, in0=r_mk, in1=r_ex, op=ALU.mult))
    rop(K.dve, lambda: V.tensor_reduce(out=r_den, in_=r_cw, axis=AX.X, op=ALU.add))
    rop(K.dve, lambda: V.tensor_tensor(out=r_den, in0=r_den, in1=r_s4, op=ALU.mult))
    rop(K.dve, lambda: V.reciprocal(out=r_den, in_=r_den))
    rop(K.dve, lambda: V.tensor_tensor(out=r_cw, in0=r_cw, in1=bc(r_den.unsqueeze(2), [128, T, 8]), op=ALU.mult))
    K.barrier()

    al = Alloc(SORT0, LIM)
    m2 = al([128, NO, 8], F32)
    A1 = al([128, NO, NE], F32)
    A2 = al([128, NO, NE], F32)
    Mb = al([128, NO, NE], BF16)
    utri = al([128, 128], BF16)
    onesq = al([128, 128], BF16)
    cnt = al([128, NE], F32)
    thr = al([128, 8], F32)
    cmp8 = al([128, NE, 8], F32)
    Tt = al([128, NE], F32)
    sca = al([128, NE], F32)
    scb = al([128, NE], F32)
    off256 = al([128, NE], F32)
    offT = al([128, NE], F32)
    posf = al([128, NO, NE], F32)
    ptmp = al([128, NO, NE], F32)
    posk = al([128, 2, NO], F32)
    jv = al([128, 48], F32)
    ev = al([128, NE], F32)
    pidx = al([128, 1], F32)
    indg = al([128, NE, 48], F32)
    indl = al([128, NE, 48], F32)
    eidf = al([128, 48], F32)
    anyf = al([128, 48], F32)
    real1 = al([128, 48], F32)
    jv256 = al([128, 48], F32)
    sidf = al([128, 48], F32)
    p2 = al([128, 1], F32)
    SR = [R("sort")]

    def sop(eng, fn, extra_r=(), extra_w=()):
        K.op(eng, fn, reads=SR + list(extra_r), writes=SR + list(extra_w))
    T = NO
    sop(K.pool, lambda: P.memset(utri, 1.0))
    sop(K.pool, lambda: P.affine_select(out=utri, in_=utri, pattern=[[1, 128]], compare_op=ALU.is_ge, fill=0.0, base=-1,
                                        channel_multiplier=-1))
    sop(K.pool, lambda: P.memset(onesq, 1.0))
    for m in range(8):
        sop(K.dve, lambda m=m: V.memset(thr[:, m:m + 1], 256.0 * m))
    sop(K.pool, lambda: P.iota(jv, pattern=[[1, 48]], base=0, channel_multiplier=0, allow_small_or_imprecise_dtypes=True))
    sop(K.pool, lambda: P.iota(ev, pattern=[[1, NE]], base=0, channel_multiplier=0, allow_small_or_imprecise_dtypes=True))
    sop(K.pool, lambda: P.iota(pidx, pattern=[[0, 1]], base=0, channel_multiplier=1, allow_small_or_imprecise_dtypes=True))
    sop(K.dve, lambda: V.tensor_tensor(out=m2, in0=r_mk, in1=r_eq, op=ALU.subtract), extra_r=RW)
    for g in range(4):
        sop(K.dve, lambda g=g: V.tensor_tensor(out=A1[:, :, g * 8:(g + 1) * 8], in0=r_eq,
                                               in1=bc(r_oh[:, :, g:g + 1], [128, T, 8]), op=ALU.mult), extra_r=RW)
        sop(K.dve, lambda g=g: V.tensor_tensor(out=A2[:, :, g * 8:(g + 1) * 8], in0=m2,
                                               in1=bc(r_oh[:, :, g:g + 1], [128, T, 8]), op=ALU.mult), extra_r=RW)
    sop(K.dve, lambda: V.tensor_tensor(out=r_ex, in0=r_cw, in1=r_eq, op=ALU.mult), extra_r=RW, extra_w=RW)
    sop(K.dve, lambda: V.tensor_reduce(out=w12[:, 0, :], in_=r_ex, axis=AX.X, op=ALU.add), extra_r=RW, extra_w=[R("w12")])
    sop(K.dve, lambda: V.tensor_tensor(out=r_ex, in0=r_cw, in1=m2, op=ALU.mult), extra_r=RW, extra_w=RW)
    sop(K.dve, lambda: V.tensor_reduce(out=w12[:, 1, :], in_=r_ex, axis=AX.X, op=ALU.add), extra_r=RW, extra_w=[R("w12")])
    sop(K.dve, lambda: V.tensor_tensor(out=Mb, in0=A1, in1=A2, op=ALU.add))
    def rk():
        ins = None
        for i in range(NO):
            for i2 in range(i):
                G.matmul(bank(0)[:, i * 32:(i + 1) * 32], lhsT=onesq, rhs=Mb[:, i2, :], start=(i2 == 0), stop=False)
            ins = G.matmul(bank(0)[:, i * 32:(i + 1) * 32], lhsT=utri, rhs=Mb[:, i, :], start=(i == 0), stop=True)
        for i in range(NO):
            ins = G.matmul(bank(1)[:, 0:32], lhsT=onesq, rhs=Mb[:, i, :], start=(i == 0), stop=(i == NO - 1))
        return ins
    K.op(K.pe, rk, reads=SR, writes=[R("ps", 0), R("ps", 1)])
    sop(K.dve, lambda: V.tensor_copy(out=cnt, in_=bank(1)[:, 0:32]), extra_r=[R("ps", 1)])
    sop(K.dve, lambda: V.tensor_tensor(out=cmp8, in0=bc(cnt.unsqueeze(2), [128, NE, 8]), in1=bc(thr.unsqueeze(1), [128, NE, 8]),
                                       op=ALU.is_gt))
    sop(K.dve, lambda: V.tensor_reduce(out=Tt, in_=cmp8, axis=AX.X, op=ALU.add))
    sop(K.dve, lambda: V.tensor_copy(out=sca, in_=Tt))
    cur, oth = sca, scb
    for sft in (1, 2, 4, 8, 16):
        sop(K.dve, lambda cur=cur, oth=oth: V.tensor_copy(out=oth, in_=cur))
        sop(K.dve, lambda cur=cur, oth=oth, sft=sft: V.tensor_tensor(out=oth[:, sft:NE], in0=cur[:, sft:NE], in1=cur[:, 0:NE - sft],
                                                                      op=ALU.add))
        cur, oth = oth, cur
    incl = cur
    sop(K.dve, lambda: V.tensor_tensor(out=offT, in0=incl, in1=Tt, op=ALU.subtract))
    sop(K.dve, lambda: V.tensor_scalar(out=off256, in0=offT, scalar1=256.0, scalar2=None, op0=ALU.mult))
    sop(K.dve, lambda: V.tensor_tensor(out=posf, in0=bank(0).rearrange("p (a b) -> p a b", a=NO),
                                       in1=bc(off256.unsqueeze(1), [128, T, NE]), op=ALU.add), extra_r=[R("ps", 0)])
    for k, Ak in enumerate((A1, A2)):
        sop(K.dve, lambda Ak=Ak: V.tensor_tensor(out=ptmp, in0=posf, in1=Ak, op=ALU.mult))
        sop(K.dve, lambda k=k: V.tensor_reduce(out=posk[:, k, :], in_=ptmp, axis=AX.X, op=ALU.add))
    sop(K.dve, lambda: V.tensor_copy(out=posu, in_=posk), extra_w=[R("posu")])
    dSc = K.dsem("scat")
    for i in range(NO):
        for k in range(2):
            K._waits(K.pool, [R("posu"), R("h2tok", i)], [R("xs_d")])
            P.indirect_dma_start(out=xs_d[:, :], out_offset=bass.IndirectOffsetOnAxis(posu[:, k, i:i + 1], 0),
                                 in_=h2tok[:, i, :], in_offset=None).then_inc(dSc.sem, 16)
            dSc.cnt += 16
    K._commit((dSc.sem, dSc.cnt), [R("posu")] + RL("h2tok", range(NO)), [R("xs_d")])
    sop(K.dve, lambda: V.tensor_tensor(out=indg, in0=bc(jv.unsqueeze(1), [128, NE, 48]), in1=bc(offT.unsqueeze(2), [128, NE, 48]),
                                       op=ALU.is_ge))
    sop(K.dve, lambda: V.tensor_tensor(out=indl, in0=bc(jv.unsqueeze(1), [128, NE, 48]), in1=bc(incl.unsqueeze(2), [128, NE, 48]),
                                       op=ALU.is_lt))
    sop(K.dve, lambda: V.tensor_tensor(out=indg, in0=indg, in1=indl, op=ALU.mult))
    sop(K.dve, lambda: V.tensor_reduce(out=anyf, in_=indg.rearrange("p e j -> p j e"), axis=AX.X, op=ALU.add))
    sop(K.dve, lambda: V.tensor_tensor(out=indl, in0=indg, in1=bc(ev.unsqueeze(2), [128, NE, 48]), op=ALU.mult))
    sop(K.dve, lambda: V.tensor_reduce(out=eidf, in_=indl.rearrange("p e j -> p j e"), axis=AX.X, op=ALU.add))
    sop(K.dve, lambda: V.tensor_tensor(out=sca, in0=cnt, in1=off256, op=ALU.add))
    sop(K.dve, lambda: V.tensor_scalar(out=jv256, in0=jv, scalar1=256.0, scalar2=None, op0=ALU.mult))
    sop(K.dve, lambda: V.tensor_tensor(out=indl, in0=bc(sca.unsqueeze(2), [128, NE, 48]), in1=bc(jv256.unsqueeze(1), [128, NE, 48]),
                                       op=ALU.subtract))
    sop(K.dve, lambda: V.tensor_tensor(out=indl, in0=indl, in1=indg, op=ALU.mult))
    sop(K.dve, lambda: V.tensor_reduce(out=real1, in_=indl.rearrange("p e j -> p j e"), axis=AX.X, op=ALU.add))
    sop(K.dve, lambda: V.tensor_scalar(out=p2, in0=pidx, scalar1=2.0, scalar2=None, op0=ALU.mult))
    sop(K.dve, lambda: V.tensor_scalar(out=sidf, in0=real1, scalar1=p2[:, 0:1], scalar2=None, op0=ALU.is_gt))
    sop(K.dve, lambda: V.tensor_scalar(out=sidf, in0=sidf, scalar1=-1.0e6, scalar2=1.0e6, op0=ALU.mult, op1=ALU.add))
    sop(K.dve, lambda: V.tensor_scalar(out=jv256, in0=jv, scalar1=128.0, scalar2=pidx[:, 0:1], op0=ALU.mult, op1=ALU.add))
    sop(K.dve, lambda: V.tensor_tensor(out=sidf, in0=sidf, in1=jv256, op=ALU.add))
    sop(K.dve, lambda: V.tensor_copy(out=sidx, in_=sidf), extra_w=[R("sidx")])
    sop(K.dve, lambda: V.tensor_scalar(out=anyf, in0=anyf, scalar1=-32.0, scalar2=32.0, op0=ALU.mult, op1=ALU.add))
    sop(K.dve, lambda: V.tensor_tensor(out=eidf, in0=eidf, in1=anyf, op=ALU.add))
    sop(K.dve, lambda: V.tensor_scalar(out=eidf, in0=eidf, scalar1=128.0, scalar2=pidx[:, 0:1], op0=ALU.mult, op1=ALU.add))
    sop(K.dve, lambda: V.tensor_copy(out=widx, in_=eidf), extra_w=[R("widx")])
    alw = Alloc(68 * KIB, 116 * KIB)
    wall = [alw([128, 3 * 2048], BF16) for i in range(4)]
    wgb = [w[:, 0:2048].rearrange("p (a b) -> p a b", a=8) for w in wall]
    wub = [w[:, 2048:4096].rearrange("p (a b) -> p a b", a=8) for w in wall]
    wdb = [w[:, 4096:6144].rearrange("p (a b) -> p a b", a=2) for w in wall]
    dG = [K.dsem("g%d" % i) for i in range(4)]
    breg = P.to_reg(NE * 128 - 1)

    def gather_w(q, j):
        s4 = q % 4
        K._waits(K.pool, [R("widx"), R("wbf")], [R("wall", s4)])
        P.indirect_dma_start(out=wall[s4], out_offset=None, in_=wbf_d[:, :],
                             in_offset=bass.IndirectOffsetOnAxis(widx[:, j:j + 1], 0),
                             bounds_check=breg, oob_is_err=False).then_inc(dG[s4].sem, 16)
        dG[s4].cnt += 16
        K._commit((dG[s4].sem, dG[s4].cnt), [R("widx"), R("wbf")], [R("wall", s4)])
    order = [0, 1, 2, 3]
    for g3 in range(14):
        order += [4 + 2 * g3, 5 + 2 * g3, 32 + g3]
    order += [46, 47]
    assert sorted(order) == list(range(NTL))

    for q in range(4):
        gather_w(q, order[q])
    K.barrier()

    al = Alloc(116 * KIB, LIM)
    xs = [al([128, 2, D], BF16) for i in range(4)]
    xT = [al([128, 8, 256], BF16) for i in range(2)]
    ssb = [al([128, 512], F32) for i in range(2)]
    hid = [al([128, 2, 256], BF16) for i in range(2)]
    ysb = [al([128, 2, D], F32) for i in range(2)]
    gt2B = al([128, D], F32)
    gfB = al([128, D], F32)
    yg = [al([128, 2, D], F32) for i in range(2)]
    yt = [al([128, D], F32) for i in range(2)]
    junk = al([128, D], BF16)

    dXs = [K.dsem("xs%d" % i) for i in range(4)]
    dYs = [K.dsem("ys%d" % i) for i in range(2)]
    dF = K.dsem("fin")
    K.op(K.dve, lambda: V.memset(st, 0.0), writes=[R("st")])
    K.dma(K.sp, [(gt2B, modB(5)), (gfB, gB_d[:, 2 * D:3 * D])], dF, writes=[R("gt2B"), R("gfB")])
    sreg = P.to_reg(NTL * 128 - 1)
    for i in range(4):
        K.op(K.act, lambda i=i: A.memzero(xs[i].rearrange("p a b -> p (a b)")), writes=[R("xs", i)])
    xs_p = xs_d.rearrange("(n r) d -> n (r d)", r=2)
    ys_p = ys_d.rearrange("(n r) d -> n (r d)", r=2)

    def stM1(q, j):
        K._waits(K.pool, [R("sidx"), R("xs_d")], [R("xs", q % 4)])
        P.indirect_dma_start(out=xs[q % 4].rearrange("p a b -> p (a b)"), out_offset=None, in_=xs_p,
                             in_offset=bass.IndirectOffsetOnAxis(sidx[:, j:j + 1], 0),
                             bounds_check=sreg, oob_is_err=False).then_inc(dXs[q % 4].sem, 16)
        dXs[q % 4].cnt += 16
        K._commit((dXs[q % 4].sem, dXs[q % 4].cnt), [R("sidx"), R("xs_d")], [R("xs", q % 4)])

    def stM2(q, j):
        s3, s2 = q % 4, q % 2

        def tr():
            ins = None
            for s_ in range(2):
                for kc in range(8):
                    ins = G.transpose(out=bankb(s_)[:, kc * 128:(kc + 1) * 128], in_=xs[s3][:, s_, kc * 128:(kc + 1) * 128],
                                      identity=ident)
            return ins
        K.op(K.pe, tr, reads=[R("xs", s3), R("ident")], writes=[R("ps", 0), R("ps", 1)])
        K.op(K.act, lambda: A.copy(out=xT[s2][:, :, 0:128], in_=bankb(0).rearrange("p (a b) -> p a b", a=8)),
             reads=[R("ps", 0)], writes=[R("xT", s2)])
        K.op(K.dve, lambda: V.tensor_copy(out=xT[s2][:, :, 128:256], in_=bankb(1).rearrange("p (a b) -> p a b", a=8)),
             reads=[R("ps", 1)], writes=[R("xT", s2)])

    def stM3(q, j):
        s4, s2 = q % 4, q % 2

        def mm():
            ins = None
            for (wb, b) in ((wgb[s4], 2), (wub[s4], 3)):
                for fc in range(2):
                    for kc in range(8):
                        ins = G.matmul(bank(b)[:, fc * 256:(fc + 1) * 256], lhsT=wb[:, kc, fc * 128:(fc + 1) * 128],
                                       rhs=xT[s2][:, kc, :], start=(kc == 0), stop=(kc == 7))
            return ins
        K.op(K.pe, mm, reads=[R("wall", s4), R("xT", s2)], writes=[R("ps", 2), R("ps", 3)])
        K.op(K.act, lambda: A.activation(out=ssb[s2], in_=bank(2), func=AF.Silu), reads=[R("ps", 2)], writes=[R("ssb", s2)])
        K.op(K.dve, lambda: V.tensor_tensor(out=hid[s2].rearrange("p a b -> p (a b)"), in0=ssb[s2], in1=bank(3), op=ALU.mult),
             reads=[R("ssb", s2), R("ps", 3)], writes=[R("hid", s2)])

    def stM4(q, j):
        s4, s2 = q % 4, q % 2
        for sh in range(2):
            b0 = 4 + 2 * sh

            def mm(sh=sh, b0=b0):
                ins = None
                for dh in range(2):
                    for fc in range(2):
                        ins = G.matmul(bank(b0 + dh), lhsT=hid[s2][:, fc, sh * 128:(sh + 1) * 128],
                                       rhs=wdb[s4][:, fc, dh * 512:(dh + 1) * 512], start=(fc == 0), stop=(fc == 1))
                return ins
            K.op(K.pe, mm, reads=[R("hid", s2), R("wall", s4)], writes=[R("ps", b0), R("ps", b0 + 1)])
            K.op(K.dve, lambda sh=sh, b0=b0: V.tensor_tensor(out=ysb[s2][:, sh, :], in0=bank(b0, 2), in1=gt2B, op=ALU.mult),
                 reads=[R("ps", b0), R("ps", b0 + 1), R("gt2B")], writes=[R("ysb", s2)])

    def stM4b(q, j):
        s2 = q % 2
        K._waits(K.pool, [R("sidx"), R("ysb", s2)], [R("ys_d", j)])
        P.indirect_dma_start(out=ys_p, out_offset=bass.IndirectOffsetOnAxis(sidx[:, j:j + 1], 0),
                             in_=ysb[s2].rearrange("p a b -> p (a b)"), in_offset=None, bounds_check=sreg,
                             oob_is_err=False).then_inc(dYs[s2].sem, 16)
        dYs[s2].cnt += 16
        K._commit((dYs[s2].sem, dYs[s2].cnt), [R("sidx"), R("ysb", s2)], [R("ys_d", j)])

    stM1(0, order[0])
    stM1(1, order[1])
    for i in range(NTL + 3):
        if 0 <= i - 3 < NTL:
            stM4(i - 3, order[i - 3])
            if i + 1 < NTL:
                gather_w(i + 1, order[i + 1])
        if i + 2 < NTL:
            stM1(i + 2, order[i + 2])
        if 0 <= i - 1 < NTL:
            stM2(i - 1, order[i - 1])
        if 0 <= i - 2 < NTL:
            stM3(i - 2, order[i - 2])
        if 0 <= i - 3 < NTL:
            stM4b(i - 3, order[i - 3])

    dO = [K.dsem("o%d" % i) for i in range(2)]
    dYg = [K.dsem("yg%d" % i) for i in range(2)]
    y_t = y_d.rearrange("(t p) d -> t p d", p=128)
    for t in range(NO):
        sl = t % 2
        ysr = [R("ys_d", j) for j in range(NTL)]
        K._waits(K.pool, ysr + [R("posu")], [R("yg", sl)])
        for k in range(2):
            P.indirect_dma_start(out=yg[sl][:, k, :], out_offset=None, in_=ys_d[:, :],
                                 in_offset=bass.IndirectOffsetOnAxis(posu[:, k, t:t + 1], 0)).then_inc(dYg[sl].sem, 16)
            dYg[sl].cnt += 16
        K._commit((dYg[sl].sem, dYg[sl].cnt), [R("posu")], [R("yg", sl)])
        for k in range(2):
            K.op(K.dve, lambda t=t, k=k, sl=sl: V.scalar_tensor_tensor(out=acc[:, t, :], in0=yg[sl][:, k, :],
                                                                       scalar=w12[:, k, t:t + 1], in1=acc[:, t, :],
                                                                       op0=ALU.mult, op1=ALU.add),
                 reads=[R("yg", sl), R("w12"), R("acc", t)], writes=[R("acc", t)])
        K.op(K.act, lambda t=t: A.activation(out=junk, in_=acc[:, t, :], func=AF.Square, accum_out=st[:, t:t + 1]),
             reads=[R("acc", t), R("st")], writes=[R("junk"), R("stx", t)])
        rstd_cols(st[:, t:t + 1], st[:, 32 + t:33 + t], 1.0 / D, [R("stx", t)], [R("strx", t)])
        K.op(K.dve, lambda t=t, sl=sl: V.scalar_tensor_tensor(out=yt[sl], in0=acc[:, t, :], scalar=st[:, 32 + t:33 + t], in1=gfB,
                                                              op0=ALU.mult, op1=ALU.mult),
             reads=[R("acc", t), R("strx", t), R("gfB")], writes=[R("yt", sl)])
        K.dma(K.sp, [(y_t[t], yt[sl])], dO[sl], reads=[R("yt", sl)])
    for d in dO:
        K.sp.h.wait_ge(d.sem, d.cnt)
    es.close()
    return nc


_ROPE_THETA = 10000.0


def _rope_table():
    out = np.zeros((S, 192), np.float32)
    pos = np.arange(S, dtype=np.float32)[:, None]
    for (dim, o) in ((64, 0), (32, 128)):
        inv = (1.0 / (_ROPE_THETA ** (np.arange(0, dim, 2, dtype=np.float32) / dim))).astype(np.float32)
        ang = (pos * inv[None, :]).astype(np.float32)
        c, s_ = np.cos(ang).astype(np.float32), np.sin(ang).astype(np.float32)
        out[:, o:o + dim] = np.concatenate([c, c], axis=1)
        out[:, o + dim:o + 2 * dim] = np.concatenate([-s_, s_], axis=1)
    return out


_NC_CACHE = {}


def kernel(x, c, w_ada, b_ada, g_norm1, w_in, g_q_lora, w_uq, g_kv_lora, w_ukv, sink,
           g_out_swa, g_out_mla, w_out, g_norm2, w_router_group, b_router_group,
           w_router_expert, b_router_expert, w_exp_gate, w_exp_up, w_exp_down, g_final):
    f = lambda a: np.ascontiguousarray(np.asarray(a, dtype=np.float32))
    x = f(x); c = f(c)
    if "nc" not in _NC_CACHE:
        _NC_CACHE["nc"] = build_program()
    nc = _NC_CACHE["nc"]
    rope = _rope_table()
    w_in0 = f(w_in)[0]
    perm = np.concatenate([np.arange(0, 768), np.arange(960, 1120), np.arange(768, 960)])
    w_in_p = np.ascontiguousarray(w_in0[:, perm])
    gB = np.ascontiguousarray(np.broadcast_to(
        np.concatenate([f(g_norm1)[0], f(g_norm2)[0], f(g_final)])[None, :], (128, 3 * D)))
    w_r = np.ascontiguousarray(np.concatenate([f(w_router_group)[0], f(w_router_expert)[0]], axis=1))
    b_r = np.concatenate([f(b_router_group)[0], f(b_router_expert)[0]])
    gcat = np.concatenate([f(g_out_swa)[0], f(g_out_mla)[0]])
    jj = np.arange(128)[:, None]
    rr = np.arange(128)[None, :]
    mprev = (jj >= rr).astype(np.float32)
    mnext = (jj <= rr).astype(np.float32)
    shared = {
        "w_ada": f(w_ada)[0], "b_ada": f(b_ada), "w_in": w_in_p, "w_uq": f(w_uq)[0], "w_ukv": f(w_ukv)[0],
        "w_out": f(w_out)[0], "w_r": w_r, "w_g": np.ascontiguousarray(f(w_exp_gate)[0].reshape(NE, 8, 128, 256).transpose(0, 2, 1, 3).reshape(NE * 128, 2048)),
        "w_u": np.ascontiguousarray(f(w_exp_up)[0].reshape(NE, 8, 128, 256).transpose(0, 2, 1, 3).reshape(NE * 128, 2048)),
        "w_d": np.ascontiguousarray(f(w_exp_down)[0].reshape(NE, 2, 128, D).transpose(0, 2, 1, 3).reshape(NE * 128, 2048)),
        "gB": gB,
    }
    in_maps = []
    for core in range(8):
        b, hf = core // 2, core % 2
        own = slice(hf * 2048, (hf + 1) * 2048)
        oth = slice((1 - hf) * 2048, (2 - hf) * 2048)
        prm = np.zeros((128, 64), np.float32)
        prm[:, 0:8] = c[b].reshape(8, 128).T
        prm[0:96, 8:10] = f(g_q_lora)[0].reshape(2, 96).T
        prm[:, 10] = f(g_kv_lora)[0]
        prm[:, 11:19] = gcat.reshape(8, 128).T
        prm[:, 20:28] = f(sink)[0][None, :]
        prm[:, 28:64] = b_r[None, :]
        msk = np.stack([mprev, mnext, mprev * float(hf == 1), mnext * float(hf == 0)], axis=1).reshape(128, 512)
        m = dict(shared)
        m["x"] = np.ascontiguousarray(np.concatenate([x[b, own], x[b, oth]], axis=0))
        m["rope"] = np.ascontiguousarray(np.concatenate([rope[own], rope[oth]], axis=0))
        m["prm"] = prm
        m["g1p"] = np.ascontiguousarray(f(g_norm1)[0].reshape(8, 128).T)
        m["msk"] = np.ascontiguousarray(msk.astype(np.float32))
        in_maps.append(m)
    res = run_bass_kernel_spmd(nc, in_maps, core_ids=list(range(8)))
    out = np.zeros((4, S, D), np.float32)
    for core in range(8):
        b, hf = core // 2, core % 2
        out[b, hf * 2048:(hf + 1) * 2048] = res.results[core]["y"]
    return out
```

```python
import numpy as np
from contextlib import ExitStack

import concourse.bass as bass
import concourse.mybir as mybir
from concourse.bass_utils import run_bass_kernel_spmd

F32 = mybir.dt.float32
BF16 = mybir.dt.bfloat16
U8 = mybir.dt.uint8
U32 = mybir.dt.uint32
AF = mybir.ActivationFunctionType
ALU = mybir.AluOpType
AX = mybir.AxisListType

D = 1024
S = 4096
NT = 32
NO = 16
EPS = 1e-6
NE = 32
KIB = 1024


class Region:
    __slots__ = ("name", "lw", "rd")

    def __init__(self, name):
        self.name = name
        self.lw = None
        self.rd = []


class Eng:
    def __init__(self, K, name, h):
        self.name = name
        self.h = h
        self.sem = K.es.enter_context(K.nc.semaphore("tl_" + name))
        self.cnt = 0
        self.waited = {}


class DSem:
    def __init__(self, K, name):
        self.sem = K.es.enter_context(K.nc.semaphore("d_" + name))
        self.cnt = 0


class KB:
    def __init__(self, nc, es):
        self.nc = nc
        self.es = es
        self.pe = Eng(self, "pe", nc.tensor)
        self.dve = Eng(self, "dve", nc.vector)
        self.act = Eng(self, "act", nc.scalar)
        self.pool = Eng(self, "pool", nc.gpsimd)
        self.sp = Eng(self, "sp", nc.sync)
        self.engs = [self.pe, self.dve, self.act, self.pool, self.sp]
        self.dsems = []

    def dsem(self, name):
        d = DSem(self, name)
        self.dsems.append(d)
        return d

    def _waits(self, eng, reads, writes):
        need = {}

        def add(t):
            if t is None:
                return
            s, v = t
            k = id(s)
            if k not in need or need[k][1] < v:
                need[k] = (s, v)

        for r in reads:
            add(r.lw)
        for w in writes:
            add(w.lw)
            for t in w.rd:
                add(t)
        for k, (s, v) in need.items():
            if eng.waited.get(k, 0) >= v:
                continue
            if eng.name == "pe" and s is eng.sem:
                continue
            eng.h.wait_ge(s, v)
            eng.waited[k] = v

    def _commit(self, ticket, reads, writes):
        for r in reads:
            r.rd.append(ticket)
            if len(r.rd) > 48:
                best = {}
                for (s, v) in r.rd:
                    if id(s) not in best or best[id(s)][1] < v:
                        best[id(s)] = (s, v)
                r.rd = list(best.values())
        for w in writes:
            w.lw = ticket
            w.rd = []

    def op(self, eng, fn, reads=(), writes=()):
        self._waits(eng, reads, writes)
        ins = fn()
        eng.cnt += 1
        ins.then_inc(eng.sem, 1)
        self._commit((eng.sem, eng.cnt), reads, writes)
        return ins

    def dma(self, q, pairs, dsem, reads=(), writes=(), **kw):
        self._waits(q, reads, writes)
        for (o, i) in pairs:
            q.h.dma_start(out=o, in_=i, **kw).then_inc(dsem.sem, 16)
            dsem.cnt += 16
        self._commit((dsem.sem, dsem.cnt), reads, writes)

    def barrier(self):
        for e in self.engs:
            for o in self.engs:
                if o is e or o.cnt == 0:
                    continue
                if e.waited.get(id(o.sem), 0) < o.cnt:
                    e.h.wait_ge(o.sem, o.cnt)
                    e.waited[id(o.sem)] = o.cnt
            for d in self.dsems:
                if d.cnt and e.waited.get(id(d.sem), 0) < d.cnt:
                    e.h.wait_ge(d.sem, d.cnt)
                    e.waited[id(d.sem)] = d.cnt


_DT_SIZE = {F32: 4, BF16: 2, U32: 4}


def build_program():
    nc = bass.Bass("TRN2", target_bir_lowering=False)
    es = ExitStack()
    K = KB(nc, es)
    V, A, P, G = nc.vector, nc.scalar, nc.gpsimd, nc.tensor

    def din(name, shape):
        return nc.dram_tensor(name, shape, F32, kind="ExternalInput").ap()

    x_d = din("x", [S, D])
    prm_d = din("prm", [128, 64])
    g1p_d = din("g1p", [128, 8])
    gB_d = din("gB", [128, 3 * D])
    rope_d = din("rope", [S, 192])
    msk_d = din("msk", [128, 512])
    wada_d = din("w_ada", [D, 6 * D])
    bada_d = din("b_ada", [1, 6 * D])
    win_d = din("w_in", [D, 1120])
    wuq_d = din("w_uq", [192, 768])
    wukv_d = din("w_ukv", [128, 1024])
    wout_d = din("w_out", [D, D])
    wr_d = din("w_r", [D, 36])
    wg_d = din("w_g", [NE * 128, 2048])
    wu_d = din("w_u", [NE * 128, 2048])
    wd_d = din("w_d", [NE * 128, 2048])
    y_d = nc.dram_tensor("y", [NO * 128, D], F32, kind="ExternalOutput").ap()
    mod_d = nc.dram_tensor("mod_scratch", [1, 6 * D], F32, kind="Internal").ap()
    NTL = 48
    xs_d = nc.dram_tensor("xs_scratch", [NTL * 256, D], BF16, kind="Internal").ap()
    ys_d = nc.dram_tensor("ys_scratch", [NTL * 256, D], F32, kind="Internal").ap()
    wo_d = nc.dram_tensor("wo_scratch", [D, D], BF16, kind="Internal").ap()
    wbf_d = nc.dram_tensor("wbf_all", [NE * 128, 3 * 2048], BF16, kind="Internal").ap()

    arena = nc.alloc_sbuf_tensor("arena", [128, 206 * KIB], U8)
    ps = nc.alloc_psum_tensor("ps", [128, 4096], F32)
    LIM = 206 * KIB

    def view(off, shape, dt, p0=0):
        off = int(off)
        n = 1
        for s_ in shape[1:]:
            n *= s_
        assert off + n * _DT_SIZE[dt] <= LIM, (off, shape)
        ap = arena[p0:p0 + shape[0], off:off + n * _DT_SIZE[dt]].bitcast(dt)
        if len(shape) == 3:
            ap = ap.rearrange("p (a b) -> p a b", a=shape[1])
        elif len(shape) == 4:
            ap = ap.rearrange("p (a b c) -> p a b c", a=shape[1], b=shape[2])
        return ap

    class Alloc:
        def __init__(self, start, end):
            self.o = start
            self.end = end

        def __call__(self, shape, dt, p0=0):
            n = 1
            for s_ in shape[1:]:
                n *= s_
            nb = (n * _DT_SIZE[dt] + 31) // 32 * 32
            v = view(self.o, shape, dt, p0)
            self.o += nb
            assert self.o <= self.end, (self.o, self.end, shape)
            return v

    def bank(b, n=1):
        return ps[:, b * 512:(b + n) * 512]

    def bankb(b):
        return ps[:, b * 512:(b + 1) * 512].bitcast(BF16)

    def bc(ap, shape):
        return ap.to_broadcast(shape)

    def modB(i):
        return mod_d[0, i * D:(i + 1) * D].partition_broadcast(128)

    Rg = {}

    def R(*key):
        if key not in Rg:
            Rg[key] = Region(str(key))
        return Rg[key]

    def RL(name, idxs):
        return [R(name, i) for i in idxs]

    def h3(ap, h):
        return ap.rearrange("p (h d) -> p h d", h=h)

    prm = view(0, [128, 64], F32)
    ident = view(256, [128, 128], BF16)
    ones_b = view(512, [128, 4], BF16)
    sT = view(528, [128, 8], F32)
    eps_t = view(560, [128, 1], F32)
    es8 = view(576, [1, 8], F32)
    st = view(640, [128, 160], F32)
    vsink = view(1280, [1, 96], BF16)
    g1P = view(1472, [128, 8], F32)
    a1P = view(1504, [128, 8], F32)
    sh1P = view(1536, [128, 8], F32)
    sh1Pb = view(1568, [128, 8], BF16)
    onesrow = view(1600, [1, 128], BF16)
    L2 = 2 * KIB
    L3 = 26 * KIB
    L4 = 86 * KIB
    PA = 118 * KIB

    dPrm = K.dsem("prm")
    K.dma(K.sp, [(prm, prm_d[:, :])], dPrm, writes=[R("prm")])
    K.op(K.pool, lambda: P.memset(ident, 1.0), writes=[R("ident")])
    K.op(K.pool, lambda: P.affine_select(out=ident, in_=ident, pattern=[[-1, 128]],
                                         compare_op=ALU.is_equal, fill=0.0, base=0,
                                         channel_multiplier=1),
         reads=[R("ident")], writes=[R("ident")])
    K.op(K.dve, lambda: V.memset(ones_b, 1.0), writes=[R("ones")])
    K.op(K.dve, lambda: V.memset(st, 0.0), writes=[R("st")])
    K.op(K.dve, lambda: V.memset(eps_t, EPS), writes=[R("eps")])
    K.op(K.dve, lambda: V.memset(vsink[0:1, 0:64], 0.0), writes=[R("vsink")])
    K.op(K.dve, lambda: V.memset(vsink[0:1, 64:96], 1.0), writes=[R("vsink")])
    K.op(K.act, lambda: A.activation(out=sT, in_=prm[:, 0:8], func=AF.Silu),
         reads=[R("prm")], writes=[R("sT")])

    def rstd_cols(src, dst, n_inv, rd, wr):
        K.op(K.act, lambda: A.activation(out=dst, in_=src, func=AF.Ln, scale=n_inv, bias=eps_t[:, 0:1]),
             reads=list(rd) + [R("eps")], writes=list(wr))
        K.op(K.act, lambda: A.activation(out=dst, in_=dst, func=AF.Exp, scale=-0.5),
             reads=list(wr), writes=list(wr))

    al = Alloc(4 * KIB, LIM)
    wa = [al([128, 8, 512], F32) for i in range(3)]
    bada = al([1, 6 * D], F32)
    modrow = al([1, 6 * D], F32)
    dWa = [K.dsem("wa%d" % i) for i in range(3)]
    dWin = K.dsem("win")
    win0 = view(PA, [128, 8, 1120], BF16)
    K.dma(K.pool, [(win0, win_d.rearrange("(kc p) n -> p kc n", p=128))], dWin, writes=[R("win")])
    dBa = K.dsem("bada")
    wada_v = wada_d.rearrange("(kc p) n -> p kc n", p=128)
    K.dma(K.sp, [(bada, bada_d[:, :])], dBa, writes=[R("bada")])
    for j in range(12):
        sl = j % 3
        K.dma(K.sp, [(wa[sl], wada_v[:, :, j * 512:(j + 1) * 512])], dWa[sl], writes=[R("wa", sl)])
        pb = bank(j % 2)

        def mm(sl=sl, pb=pb):
            ins = None
            for kc in range(8):
                ins = G.matmul(pb[0:1, :], lhsT=sT[:, kc:kc + 1], rhs=wa[sl][:, kc, :],
                               start=(kc == 0), stop=(kc == 7))
            return ins
        K.op(K.pe, mm, reads=[R("wa", sl), R("sT")], writes=[R("ps", j % 2)])
        K.op(K.dve, lambda j=j, pb=pb: V.tensor_tensor(out=modrow[0:1, j * 512:(j + 1) * 512], in0=pb[0:1, :],
                                                       in1=bada[0:1, j * 512:(j + 1) * 512], op=ALU.add),
             reads=[R("ps", j % 2), R("bada")], writes=[R("modrow")])
    dM = K.dsem("mod")
    K.dma(K.sp, [(mod_d[:, :], modrow)], dM, reads=[R("modrow")], writes=[R("mod_d")])
    K.barrier()

    ckvnT = view(L2, [128, S], BF16)
    krT = view(L2 + 8 * KIB, [96, S], BF16)
    cqnT = view(L2 + 16 * KIB, [96, 2, NO * 128], BF16)
    qTa = view(L3, [64, 8, NO * 128], BF16)
    kTa = view(L3 + 32 * KIB, [64, 2, S], BF16)
    va = view(L3 + 48 * KIB, [128, NT, 2, 96], BF16)

    al = Alloc(PA, LIM)
    win = al([128, 8, 1120], BF16)
    b1row = al([1, 1120], BF16)
    xt = [al([128, D], F32) for i in range(2)]
    hb = [al([128, D], BF16) for i in range(3)]
    hT = [al([128, 8, 128], BF16) for i in range(3)]
    projS = [al([128, 1120], F32) for i in range(3)]
    rt = [al([128, 192], F32) for i in range(4)]
    t1 = [al([128, 640], F32) for i in range(2)]
    t2 = [al([128, 640], F32) for i in range(2)]
    t1r = [al([128, 32], F32) for i in range(2)]
    t2r = [al([128, 32], F32) for i in range(2)]
    qkr = [al([128, 640], BF16) for i in range(2)]
    ckvn = [al([128, 128], BF16) for i in range(2)]
    krs = [al([128, 96], BF16) for i in range(2)]
    cqn = [al([128, 192], BF16) for i in range(2)]
    junk = al([128, D], BF16)
    junk2 = al([128, 192], BF16)
    assert al.o <= 198 * KIB, al.o
    dW = K.dsem("wA")
    K.dma(K.sp, [(g1P, g1p_d[:, :]),
                 (a1P, mod_d[0, D:2 * D].rearrange("(kc p) -> p kc", p=128)),
                 (sh1P, mod_d[0, 0:D].rearrange("(kc p) -> p kc", p=128))], dW,
          reads=[R("mod_d")], writes=[R("a1P"), R("sh1P")], allow_slow_non_contiguous=True)
    K.op(K.dve, lambda: V.scalar_tensor_tensor(out=a1P, in0=a1P, scalar=1.0, in1=g1P, op0=ALU.add, op1=ALU.mult),
         reads=[R("a1P")], writes=[R("a1P")])
    K.op(K.dve, lambda: V.tensor_copy(out=sh1Pb, in_=sh1P), reads=[R("sh1P")], writes=[R("sh1Pb")])
    K.op(K.dve, lambda: V.memset(onesrow, 1.0), writes=[R("onesrow")])

    def b1mm():
        ins = None
        for (b, c0, n) in [(2, 0, 512), (3, 512, 512), (4, 1024, 96)]:
            for kc in range(8):
                ins = G.matmul(bank(b)[0:1, 0:n], lhsT=sh1Pb[:, kc:kc + 1], rhs=win[:, kc, c0:c0 + n],
                               start=(kc == 0), stop=(kc == 7))
        return ins
    K.op(K.pe, b1mm, reads=[R("sh1Pb"), R("win")], writes=[R("ps", 2), R("ps", 3), R("ps", 4)])
    for (b, c0, n) in [(2, 0, 512), (3, 512, 512), (4, 1024, 96)]:
        K.op(K.dve, lambda b=b, c0=c0, n=n: V.tensor_copy(out=b1row[0:1, c0:c0 + n], in_=bank(b)[0:1, 0:n]),
             reads=[R("ps", b)], writes=[R("b1row")])
    for kc in range(8):
        K.op(K.dve, lambda kc=kc: V.tensor_scalar(out=win[:, kc, :], in0=win[:, kc, :], scalar1=a1P[:, kc:kc + 1], scalar2=None,
                                                  op0=ALU.mult),
             reads=[R("win"), R("a1P")], writes=[R("win")])
    K.op(K.pool, lambda: P.memset(va[:, :, :, 64:96], 1.0), writes=RL("va", range(NT)))
    for i in range(2):
        K.op(K.pool, lambda i=i: P.memset(krs[i], 0.0), writes=[R("krs", i)])

    dX = [K.dsem("x%d" % i) for i in range(2)]
    x_t = x_d.rearrange("(t p) d -> t p d", p=128)
    rope_t = rope_d.rearrange("(t p) d -> t p d", p=128)
    CQS = (128.0 / 192.0) ** 0.5

    def rope(src3, cs, sn, half, o1, o2, dst, rd, r1, r2, wr_dst, nh):
        w = 2 * half
        K.op(K.dve, lambda: V.tensor_tensor(out=o1, in0=src3, in1=bc(cs.unsqueeze(1), [128, nh, w]), op=ALU.mult),
             reads=rd, writes=[r1])
        K.op(K.dve, lambda: V.tensor_tensor(out=o2[:, :, 0:half], in0=src3[:, :, half:w],
                                            in1=bc(sn[:, 0:half].unsqueeze(1), [128, nh, half]), op=ALU.mult),
             reads=rd, writes=[r2])
        K.op(K.dve, lambda: V.tensor_tensor(out=o2[:, :, half:w], in0=src3[:, :, 0:half],
                                            in1=bc(sn[:, half:w].unsqueeze(1), [128, nh, half]), op=ALU.mult),
             reads=rd, writes=[r2])
        K.op(K.pool, lambda: P.tensor_tensor(out=dst, in0=o1, in1=o2, op=ALU.add),
             reads=[r1, r2], writes=wr_dst)

    def stA1(t):
        own = t < NO
        sl = t % 2
        K.dma(K.sp, [(xt[sl], x_t[t]), (rt[t % 4], rope_t[t])], dX[sl], writes=[R("xt", sl), R("rt", t % 4)])
        K.op(K.act, lambda sl=sl, t=t: A.activation(out=junk, in_=xt[sl], func=AF.Square, accum_out=st[:, t:t + 1]),
             reads=[R("xt", sl)], writes=[R("junk"), R("stx", t)])
        rstd_cols(st[:, t:t + 1], st[:, 32 + t:33 + t], 1.0 / D, [R("stx", t)], [R("strx", t)])
        K.op(K.act, lambda sl=sl, t=t: A.activation(out=hb[t % 3], in_=xt[sl], func=AF.Copy, scale=st[:, 32 + t:33 + t]),
             reads=[R("xt", sl), R("strx", t)], writes=[R("hb", t % 3)])

    def stA2(t):
        own = t < NO
        sl = t % 2

        def tr(sl=sl):
            ins = None
            for kc in range(8):
                ins = G.transpose(out=bankb(sl)[:, kc * 128:(kc + 1) * 128], in_=hb[t % 3][:, kc * 128:(kc + 1) * 128],
                                  identity=ident)
            return ins
        K.op(K.pe, tr, reads=[R("hb", t % 3), R("ident")], writes=[R("ps", sl)])
        K.op(K.act, lambda sl=sl: A.copy(out=hT[t % 3], in_=bankb(sl).rearrange("p (a b) -> p a b", a=8)),
             reads=[R("ps", sl)], writes=[R("hT", t % 3)])

    def stA2b(t):
        own = t < NO
        sl = t % 2
        chunks = [(2, 0, 512), (3, 512, 416), (4, 928, 192)] if own else [(3, 512, 416)]

        def proj(sl=sl, chunks=chunks):
            ins = None
            for (b, c0, n) in chunks:
                for kc in range(8):
                    G.matmul(bank(b)[:, 0:n], lhsT=hT[t % 3][:, kc, :], rhs=win[:, kc, c0:c0 + n],
                             start=(kc == 0), stop=False)
                ins = G.matmul(bank(b)[:, 0:n], lhsT=onesrow[0:1, :], rhs=b1row[0:1, c0:c0 + n], start=False, stop=True)
            return ins
        K.op(K.pe, proj, reads=[R("hT", t % 3), R("win"), R("b1row"), R("onesrow")], writes=[R("ps", b) for (b, _, _) in chunks])
        for (b, c0, n) in chunks:
            if b == 2:
                K.op(K.dve, lambda b=b, c0=c0, n=n, sl=sl: V.tensor_copy(out=projS[t % 3][:, c0:c0 + n], in_=bank(b)[:, 0:n]),
                     reads=[R("ps", b)], writes=[R("projS", t % 3, b)])
            else:
                K.op(K.act, lambda b=b, c0=c0, n=n, sl=sl: A.copy(out=projS[t % 3][:, c0:c0 + n], in_=bank(b)[:, 0:n]),
                     reads=[R("ps", b)], writes=[R("projS", t % 3, b)])

    def stA3(t):
        own = t < NO
        sl = t % 2
        pS = projS[t % 3]
        if own:
            rope(h3(pS[:, 0:640], 10), rt[t % 4][:, 0:64], rt[t % 4][:, 64:128], 32, h3(t1[sl], 10), h3(t2[sl], 10),
                 h3(qkr[sl], 10), [R("projS", t % 3, 2), R("projS", t % 3, 3), R("rt", t % 4)], R("t1", sl), R("t2", sl),
                 [R("qkr", sl)], 10)
        else:
            rope(h3(pS[:, 512:640], 2), rt[t % 4][:, 0:64], rt[t % 4][:, 64:128], 32, h3(t1[sl][:, 512:640], 2),
                 h3(t2[sl][:, 512:640], 2), h3(qkr[sl][:, 512:640], 2), [R("projS", t % 3, 3), R("rt", t % 4)],
                 R("t1", sl), R("t2", sl), [R("qkr", sl)], 2)
        rope(h3(pS[:, 896:928], 1), rt[t % 4][:, 128:160], rt[t % 4][:, 160:192], 16, h3(t1r[sl], 1), h3(t2r[sl], 1),
             h3(krs[sl][:, 64:96], 1), [R("projS", t % 3, 3), R("rt", t % 4)], R("t1r", sl), R("t2r", sl), [R("krs", sl)], 1)
        K.op(K.pool, lambda t=t, pS=pS: P.tensor_copy(out=va[:, t, :, 0:64], in_=h3(pS[:, 640:768], 2)),
             reads=[R("projS", t % 3, 3)], writes=[R("va", t)])
        K.op(K.act, lambda pS=pS, t=t: A.activation(out=junk2[:, 0:128], in_=pS[:, 768:896], func=AF.Square,
                                                    accum_out=st[:, 64 + 2 * t:65 + 2 * t]),
             reads=[R("projS", t % 3, 3)], writes=[R("junk2"), R("stk", t)])
        if own:
            K.op(K.act, lambda pS=pS, t=t: A.activation(out=junk2, in_=pS[:, 928:1120], func=AF.Square, scale=CQS,
                                                        accum_out=st[:, 65 + 2 * t:66 + 2 * t]),
                 reads=[R("projS", t % 3, 4)], writes=[R("junk2"), R("stk", t)])
        nsc = 2 if own else 1
        rc = 128 + 2 * (t % 16)
        rstd_cols(st[:, 64 + 2 * t:64 + 2 * t + nsc], st[:, rc:rc + nsc], 1.0 / 128, [R("stk", t)], [R("strk", t % 16)])
        K.op(K.dve, lambda pS=pS, sl=sl, rc=rc: V.tensor_scalar(out=ckvn[sl], in0=pS[:, 768:896], scalar1=st[:, rc:rc + 1],
                                                                scalar2=None, op0=ALU.mult),
             reads=[R("projS", t % 3, 3), R("strk", t % 16)], writes=[R("ckvn", sl)])
        if own:
            K.op(K.dve, lambda pS=pS, sl=sl, rc=rc: V.tensor_scalar(out=cqn[sl], in0=pS[:, 928:1120],
                                                                    scalar1=st[:, rc + 1:rc + 2], scalar2=None, op0=ALU.mult),
                 reads=[R("projS", t % 3, 4), R("strk", t % 16)], writes=[R("cqn", sl)])
        bk = 6 + sl
        b6 = bankb(bk)

        def trB(own=own, sl=sl, b6=b6):
            G.transpose(out=b6[0:64, 0:128], in_=qkr[sl][:, 512:576], identity=ident)
            G.transpose(out=b6[0:64, 128:256], in_=qkr[sl][:, 576:640], identity=ident)
            G.transpose(out=b6[:, 256:384], in_=ckvn[sl], identity=ident)
            ins = G.transpose(out=b6[0:96, 384:512], in_=krs[sl], identity=ident)
            if own:
                G.transpose(out=b6[0:96, 512:640], in_=cqn[sl][:, 0:96], identity=ident)
                ins = G.transpose(out=b6[0:96, 640:768], in_=cqn[sl][:, 96:192], identity=ident)
            return ins
        K.op(K.pe, trB, reads=[R("qkr", sl), R("ckvn", sl), R("krs", sl), R("ident")] + ([R("cqn", sl)] if own else []),
             writes=[R("ps", bk)])
        K.op(K.dve, lambda t=t, b6=b6: V.tensor_copy(out=kTa[:, :, t * 128:(t + 1) * 128],
                                                     in_=b6[0:64, 0:256].rearrange("p (a b) -> p a b", a=2)),
             reads=[R("ps", bk)], writes=[R("kTa", t)])
        K.op(K.dve, lambda t=t, b6=b6: V.tensor_copy(out=ckvnT[:, t * 128:(t + 1) * 128], in_=b6[:, 256:384]),
             reads=[R("ps", bk)], writes=[R("ckvnT", t)])
        K.op(K.dve, lambda t=t, b6=b6: V.tensor_copy(out=krT[64:96, t * 128:(t + 1) * 128], in_=b6[64:96, 384:512]),
             reads=[R("ps", bk)], writes=[R("krT", t)])
        if own:
            K.op(K.dve, lambda t=t, b6=b6: V.tensor_copy(out=cqnT[:, :, t * 128:(t + 1) * 128],
                                                         in_=b6[0:96, 512:768].rearrange("p (a b) -> p a b", a=2)),
                 reads=[R("ps", bk)], writes=[R("cqnT", t)])
            b5 = bankb(5)

            def trQ(sl=sl, b5=b5):
                ins = None
                for h in range(8):
                    ins = G.transpose(out=b5[0:64, h * 128:(h + 1) * 128], in_=qkr[sl][:, h * 64:(h + 1) * 64],
                                      identity=ident)
                return ins
            K.op(K.pe, trQ, reads=[R("qkr", sl), R("ident")], writes=[R("ps", 5)])
            K.op(K.act, lambda t=t, b5=b5: A.copy(out=qTa[:, :, t * 128:(t + 1) * 128],
                                                  in_=b5[0:64, :].rearrange("p (a b) -> p a b", a=8)),
                 reads=[R("ps", 5)], writes=[R("qTa", t)])

    gt1Ba = view(86 * KIB, [128, D], F32)
    wstg = [view(90 * KIB + i * 4 * KIB, [128, D], F32) for i in range(2)]
    wobf = [view(98 * KIB + i * 2 * KIB, [128, D], BF16) for i in range(2)]
    dWo = [K.dsem("wo%d" % i) for i in range(2)]
    dWos = [K.dsem("wos%d" % i) for i in range(2)]
    dGt = K.dsem("gt1a")
    wout_v = wout_d.rearrange("(kc p) n -> kc p n", p=128)
    wo_dv = wo_d.rearrange("(kc p) n -> kc p n", p=128)
    K.dma(K.sp, [(gt1Ba, modB(2))], dGt, reads=[R("mod_d")], writes=[R("gt1Ba")])

    def wo_load(kc):
        K.dma(K.sp, [(wstg[kc % 2], wout_v[kc])], dWo[kc % 2], writes=[R("wstg", kc % 2)])

    def wo_fold(kc):
        K.op(K.dve, lambda: V.scalar_tensor_tensor(out=wobf[kc % 2], in0=wstg[kc % 2], scalar=prm[:, 11 + kc:12 + kc],
                                                   in1=gt1Ba, op0=ALU.mult, op1=ALU.mult),
             reads=[R("wstg", kc % 2), R("prm"), R("gt1Ba")], writes=[R("wobf", kc % 2)])

    def wo_store(kc):
        K.dma(K.sp, [(wo_dv[kc], wobf[kc % 2])], dWos[kc % 2], reads=[R("wobf", kc % 2)], writes=[R("wo_d", kc)])

    zsrc = view(198 * KIB, [128, 2048], F32)
    K.op(K.pool, lambda: P.memset(zsrc, 0.0), writes=[R("zsrc")])
    dZf = K.dsem("zf")
    xs_z = xs_d.rearrange("(p r) d -> p r d", p=128)
    ys_z = ys_d.rearrange("(p r) d -> p r d", p=128)
    zjobs = [(xs_z[:, 4 * c:4 * c + 4, :], zsrc.bitcast(BF16).rearrange("p (a b) -> p a b", a=4)) for c in range(24)]
    zjobs += [(ys_z[:, 2 * c:2 * c + 2, :], zsrc.rearrange("p (a b) -> p a b", a=2)) for c in range(48)]

    for i in range(NT + 3):
        for zi in range(3 * i, min(3 * i + 3, len(zjobs))):
            K.dma(K.sp, [zjobs[zi]], dZf, reads=[R("zsrc")], writes=[R("zf", zi)])
        if i >= 6 and (i - 6) % 2 == 0 and (i - 6) // 2 < 8:
            wo_load((i - 6) // 2)
        if i >= 7 and (i - 7) % 2 == 0 and (i - 7) // 2 < 8:
            wo_fold((i - 7) // 2)
        if i >= 8 and (i - 8) % 2 == 0 and (i - 8) // 2 < 8:
            wo_store((i - 8) // 2)
        if i < NT:
            stA1(i)
        if 0 <= i - 1 < NT:
            stA2(i - 1)
        if 0 <= i - 2 < NT:
            stA2b(i - 2)
        if 0 <= i - 3 < NT:
            stA3(i - 3)
    K.barrier()

    mixTa = view(L4, [128, 4, NO * 128], BF16)
    mixTb = view(L4 + 16 * KIB, [128, 4, NO * 128], BF16)
    al = Alloc(PA, LIM)
    pT = [al([128, 3, 512], BF16) for i in range(2)]
    rden = [al([64, 512], F32) for i in range(2)]
    lnt = [al([32, 512], F32) for i in range(2)]
    msk = al([128, 4, 128], BF16)
    esrow = al([1, 8, 128], BF16)
    dMsk = K.dsem("msk")
    K.dma(K.pool, [(msk, msk_d.rearrange("p (a b) -> p a b", a=4))], dMsk, writes=[R("msk")])
    K.op(K.act, lambda: A.activation(out=es8, in_=prm[0:1, 20:28], func=AF.Exp), reads=[R("prm")], writes=[R("es8")])
    K.op(K.dve, lambda: V.tensor_copy(out=esrow, in_=bc(es8.unsqueeze(2), [1, 8, 128])), reads=[R("es8")],
         writes=[R("esrow")])

    def finish(ob, rd_sl, writers, use_act):
        oT = bank(ob)
        rd = rden[rd_sl]
        if use_act:
            K.op(K.act, lambda: A.activation(out=lnt[rd_sl], in_=oT[64:96, :], func=AF.Ln),
                 reads=[R("ps", ob)], writes=[R("lnt", rd_sl)])
            K.op(K.act, lambda: A.activation(out=rd[0:32, :], in_=lnt[rd_sl], func=AF.Exp, scale=-1.0),
                 reads=[R("lnt", rd_sl)], writes=[R("rden", rd_sl)])
            K.op(K.dve, lambda: V.tensor_copy(out=rd[32:64, :], in_=rd[0:32, :]),
                 reads=[R("rden", rd_sl)], writes=[R("rden", rd_sl)])
        else:
            K.op(K.dve, lambda: V.reciprocal(out=rd[0:32, :], in_=oT[64:96, :]),
                 reads=[R("ps", ob)], writes=[R("rden", rd_sl)])
            K.op(K.dve, lambda: V.tensor_copy(out=rd[32:64, :], in_=rd[0:32, :]),
                 reads=[R("rden", rd_sl)], writes=[R("rden", rd_sl)])
        for (out_ap, in_sl, wr) in writers:
            K.op(K.dve, lambda out_ap=out_ap, in_sl=in_sl: V.tensor_tensor(out=out_ap, in0=in_sl(oT[0:64, :]),
                                                                           in1=in_sl(rd), op=ALU.mult),
                 reads=[R("ps", ob), R("rden", rd_sl)], writes=[wr])

    def swa_ctx(it):
        n, kvh = divmod(it, 2)
        kts = [(31 if n == 0 else n - 1, 2 if n == 0 else 0), (n, None), (16 if n == NO - 1 else n + 1, 3 if n == NO - 1 else 1)]
        sl = it % 2
        return n, kvh, kts, sl, 3 * sl, 6 + sl

    def stW1(it):
        n, kvh, kts, sl, b0, ob = swa_ctx(it)

        def sc():
            ins = None
            for i, (kt, _) in enumerate(kts):
                ins = G.matmul(bank(b0 + i).rearrange("p (a b) -> p a b", a=4),
                               lhsT=kTa[:, kvh, kt * 128:(kt + 1) * 128],
                               rhs=qTa[:, kvh * 4:(kvh + 1) * 4, n * 128:(n + 1) * 128], start=True, stop=True)
            return ins
        K.op(K.pe, sc, reads=[], writes=[R("ps", b0 + i) for i in range(3)])
        K.op(K.act, lambda: A.activation(out=pT[sl].rearrange("p a b -> p (a b)"), in_=bank(b0, 3),
                                         func=AF.Exp, scale=0.125),
             reads=[R("ps", b0 + i) for i in range(3)], writes=[R("pT", sl)])
        for i, (kt, m) in enumerate(kts):
            if m is None:
                continue
            K.op(K.dve, lambda i=i, m=m: V.tensor_tensor(
                out=pT[sl][:, i, :].rearrange("p (a b) -> p a b", a=4),
                in0=pT[sl][:, i, :].rearrange("p (a b) -> p a b", a=4),
                in1=bc(msk[:, m, :].unsqueeze(1), [128, 4, 128]), op=ALU.mult),
                reads=[R("pT", sl), R("msk")], writes=[R("pT", sl)])

    def stW2(it):
        n, kvh, kts, sl, b0, ob = swa_ctx(it)

        def pv():
            for i, (kt, _) in enumerate(kts):
                G.matmul(bank(ob)[0:96, :], lhsT=va[:, kt, kvh, :], rhs=pT[sl][:, i, :],
                         start=(i == 0), stop=False)
            return G.matmul(bank(ob)[0:96, :], lhsT=vsink[0:1, :],
                            rhs=esrow[0:1, kvh * 4:(kvh + 1) * 4, :].rearrange("p a b -> p (a b)"),
                            start=False, stop=True)
        K.op(K.pe, pv, reads=[R("pT", sl), R("vsink"), R("esrow")], writes=[R("ps", ob)])
        writers = []
        for par in range(2):
            def in_sl(ap, par=par):
                return ap.rearrange("p (i two b) -> p i two b", two=2, b=128)[:, :, par, :]
            writers.append((mixTa[par * 64:par * 64 + 64, 2 * kvh:2 * kvh + 2, n * 128:(n + 1) * 128], in_sl,
                            R("mixTa", n, kvh, par)))
        finish(ob, sl, writers, True)

    for i in range(2 * NO + 1):
        if i < 2 * NO:
            stW1(i)
        if i >= 1:
            stW2(i - 1)
    K.barrier()

    qTb = view(L3, [96, 8, NO * 128], BF16)
    kTb = [view(L3 + 32 * KIB + i * 8 * KIB, [96, S], BF16) for i in range(2)]
    pTm = [view(L3 + 48 * KIB + i * 3072, [128, 3, 512], BF16) for i in range(4)]
    al = Alloc(PA, LIM)
    vb = al([128, NT, 8, 96], BF16)
    rden = [al([64, 512], F32) for i in range(2)]
    wuqs = al([96, 2, 768], F32)
    wuq = al([96, 2, 768], BF16)
    wukvs = al([128, 1024], F32)
    wukv = al([128, 8, 128], BF16)
    rt2 = [al([128, 64], F32) for i in range(2)]
    qbS = [al([128, 8, 96], F32) for i in range(2)]
    t1b = [al([128, 8, 32], F32) for i in range(2)]
    t2b = [al([128, 8, 32], F32) for i in range(2)]
    qbr = [al([128, 8, 96], BF16) for i in range(2)]

    dW2 = K.dsem("wB")
    K.dma(K.sp, [(wuqs, wuq_d.rearrange("(kc p) n -> p kc n", p=96)), (wukvs, wukv_d[:, :])], dW2,
          writes=[R("wuqs"), R("wukvs")])
    for kc in range(2):
        K.op(K.dve, lambda kc=kc: V.tensor_scalar(out=wuq[:, kc, :], in0=wuqs[:, kc, :], scalar1=prm[0:96, 8 + kc:9 + kc],
                                                  scalar2=None, op0=ALU.mult),
             reads=[R("wuqs"), R("prm")], writes=[R("wuq")])
    K.op(K.dve, lambda: V.tensor_scalar(out=wukv.rearrange("p a b -> p (a b)"), in0=wukvs, scalar1=prm[:, 10:11],
                                        scalar2=None, op0=ALU.mult),
         reads=[R("wukvs"), R("prm")], writes=[R("wukv")])
    K.op(K.pool, lambda: P.memset(vb[:, :, :, 64:96], 1.0), writes=RL("vb", range(NT)))
    for kt in range(NT):
        b = kt % 2
        K.op(K.pe, lambda kt=kt, b=b: G.matmul(bank(b).rearrange("p (a b) -> p a b", a=8),
                                               lhsT=ckvnT[:, kt * 128:(kt + 1) * 128], rhs=wukv[:, :, 64:128],
                                               start=True, stop=True),
             reads=[R("wukv")], writes=[R("ps", b)])
        if kt % 2:
            K.op(K.dve, lambda kt=kt, b=b: V.tensor_copy(out=vb[:, kt, :, 0:64], in_=bank(b).rearrange("p (a b) -> p a b", a=8)),
                 reads=[R("ps", b)], writes=[R("vb", kt)])
        else:
            K.op(K.act, lambda kt=kt, b=b: A.copy(out=vb[:, kt, :, 0:64], in_=bank(b).rearrange("p (a b) -> p a b", a=8)),
                 reads=[R("ps", b)], writes=[R("vb", kt)])
    dX2 = [K.dsem("r%d" % i) for i in range(2)]
    def stQ1(t):
        sl = t % 2
        K.dma(K.sp, [(rt2[sl], rope_t[t][:, 128:192])], dX2[sl], writes=[R("rt2", sl)])
        bq = 2 + 2 * sl

        def qp(t=t, bq=bq):
            ins = None
            for (b, c0, n) in [(bq, 0, 512), (bq + 1, 512, 256)]:
                for kc in range(2):
                    ins = G.matmul(bank(b)[:, 0:n], lhsT=cqnT[:, kc, t * 128:(t + 1) * 128], rhs=wuq[:, kc, c0:c0 + n],
                                   start=(kc == 0), stop=(kc == 1))
            return ins
        K.op(K.pe, qp, reads=[R("wuq")], writes=[R("ps", bq), R("ps", bq + 1)])
        qf = qbS[sl].rearrange("p a b -> p (a b)")
        K.op(K.act, lambda qf=qf, bq=bq: A.copy(out=qf[:, 0:512], in_=bank(bq)), reads=[R("ps", bq)], writes=[R("qbS", sl)])
        K.op(K.act, lambda qf=qf, bq=bq: A.copy(out=qf[:, 512:768], in_=bank(bq + 1)[:, 0:256]), reads=[R("ps", bq + 1)],
             writes=[R("qbS", sl)])

    def stQ2(t):
        sl = t % 2
        K.op(K.pool, lambda sl=sl: P.tensor_copy(out=qbr[sl][:, :, 0:64], in_=qbS[sl][:, :, 0:64]), reads=[R("qbS", sl)],
             writes=[R("qbr", sl)])
        rope(qbS[sl][:, :, 64:96], rt2[sl][:, 0:32], rt2[sl][:, 32:64], 16, t1b[sl], t2b[sl], qbr[sl][:, :, 64:96],
             [R("qbS", sl), R("rt2", sl)], R("t1b", sl), R("t2b", sl), [R("qbr", sl)], 8)
        b4 = bankb(6 + sl)

        def trq(sl=sl, b4=b4):
            ins = None
            for h in range(8):
                ins = G.transpose(out=b4[0:96, h * 128:(h + 1) * 128], in_=qbr[sl][:, h, :], identity=ident)
            return ins
        K.op(K.pe, trq, reads=[R("qbr", sl), R("ident")], writes=[R("ps", 6 + sl)])
        K.op(K.dve, lambda t=t, b4=b4: V.tensor_copy(out=qTb[:, :, t * 128:(t + 1) * 128],
                                                     in_=b4[0:96, :].rearrange("p (a b) -> p a b", a=8)),
             reads=[R("ps", 6 + sl)], writes=[R("qTb", t)])

    for i in range(NO + 1):
        if i < NO:
            stQ1(i)
        if i >= 1:
            stQ2(i - 1)

    ktg = [list(range(k, min(k + 3, NT))) for k in range(0, NT, 3)]
    cgs = [[0, 1, 2], [3, 4, 5], [6, 7]]

    def setup_steps(h):
        return [("setup", h, cg) for cg in cgs]

    dWbf = K.dsem("wbf")
    K.dma(K.pool, [(wbf_d[c * 512:(c + 1) * 512, m * 2048:(m + 1) * 2048], src[c * 512:(c + 1) * 512, :])
                   for c in range(8) for m, src in enumerate((wg_d, wu_d, wd_d))], dWbf, writes=[R("wbf")])
    steps = setup_steps(0)
    for h in range(8):
        for qg in range(4):
            steps += [("attn", h, qg, gi) for gi in range(len(ktg))]
            if qg == 0 and h < 7:
                steps += setup_steps(h + 1)
    SK = 2
    sc_scale = 96.0 ** -0.5
    for i in range(len(steps) + SK):
        if i < len(steps):
            stp = steps[i]
            b0 = 3 * (i % 2)
            if stp[0] == "setup":
                _, h, cg = stp
                sl = h % 2
                if cg[0] == 0:
                    K.op(K.pool, lambda sl=sl: P.tensor_copy(out=kTb[sl][64:96, :], in_=krT[64:96, :]),
                         writes=[R("kTbr", sl)])

                def su(h=h, cg=cg, b0=b0):
                    ins = None
                    for j, c in enumerate(cg):
                        ins = G.matmul(bank(b0 + j)[0:64, :], lhsT=wukv[:, h, 0:64], rhs=ckvnT[:, c * 512:(c + 1) * 512],
                                       start=True, stop=True)
                    return ins
                K.op(K.pe, su, reads=[R("wukv")], writes=[R("ps", b0 + j) for j in range(3)])
                K.op(K.dve, lambda sl=sl, cg=cg, b0=b0: V.tensor_copy(
                    out=kTb[sl][0:64, cg[0] * 512:(cg[-1] + 1) * 512], in_=bank(b0, len(cg))[0:64, :]),
                    reads=[R("ps", b0 + j) for j in range(3)], writes=[R("kTb", sl)])
            else:
                _, h, qg, gi = stp
                kl = ktg[gi]

                def sc(h=h, qg=qg, kl=kl, b0=b0):
                    ins = None
                    for j, kt in enumerate(kl):
                        ins = G.matmul(bank(b0 + j), lhsT=kTb[h % 2][:, kt * 128:(kt + 1) * 128],
                                       rhs=qTb[:, h, qg * 512:(qg + 1) * 512], start=True, stop=True)
                    return ins
                K.op(K.pe, sc, reads=[R("kTb", h % 2), R("kTbr", h % 2)] + RL("qTb", range(qg * 4, qg * 4 + 4)),
                     writes=[R("ps", b0 + j) for j in range(3)])
                s4 = i % 4
                nk = len(kl)
                K.op(K.act, lambda s4=s4, b0=b0, nk=nk: A.activation(out=pTm[s4].rearrange("p a b -> p (a b)")[:, 0:nk * 512],
                                                                     in_=bank(b0, nk), func=AF.Exp, scale=sc_scale),
                     reads=[R("ps", b0 + j) for j in range(3)], writes=[R("pTm", s4)])
        if i >= SK and steps[i - SK][0] == "attn":
            _, h, qg, gi = steps[i - SK]
            kl = ktg[gi]
            s4 = (i - SK) % 4
            gsl = (h * 4 + qg) % 2
            ob = 6 + gsl

            def pv(h=h, kl=kl, s4=s4, ob=ob):
                ins = None
                for j, kt in enumerate(kl):
                    ins = G.matmul(bank(ob)[0:96, :], lhsT=vb[:, kt, h, :], rhs=pTm[s4][:, j, :],
                                   start=(kt == 0), stop=(kt == NT - 1))
                return ins
            K.op(K.pe, pv, reads=[R("pTm", s4)] + [R("vb", kt) for kt in kl], writes=[R("ps", ob)])
            if kl[-1] == NT - 1:
                finish(ob, gsl, [(mixTb[(h % 2) * 64:(h % 2) * 64 + 64, h // 2, qg * 512:(qg + 1) * 512],
                                 (lambda ap: ap), R("mixTb", h, qg))], False)
    K.barrier()

    PC = 120 * KIB
    acc = view(2 * KIB, [128, NO, D], F32)
    posu = view(66 * KIB, [128, 2, NO], U32)
    w12 = view(66 * KIB + 128, [128, 2, NO], F32)
    widx = view(66 * KIB + 256, [128, 48], U32)
    sidx = view(66 * KIB + 448, [128, 48], U32)
    wo = view(68 * KIB, [128, 8, D], BF16)
    h2tok = view(PC, [128, NO, D], BF16)
    al = Alloc(PC + 32 * KIB, LIM)
    LGa = al([128, NO, 36], F32)
    r_m4 = al([128, NO], F32)
    r_d4 = al([128, NO, 4], F32)
    r_e4 = al([128, NO, 4], F32)
    r_s4 = al([128, NO], F32)
    r_oh = al([128, NO, 4], F32)
    r_t32 = al([128, NO, 32], F32)
    r_el = al([128, NO, 8], F32)
    r_el2 = al([128, NO, 8], F32)
    r_v1 = al([128, NO], F32)
    r_d8 = al([128, NO, 8], F32)
    r_eq = al([128, NO, 8], F32)
    r_v2 = al([128, NO], F32)
    r_mk = al([128, NO, 8], F32)
    r_ex = al([128, NO, 8], F32)
    r_cw = al([128, NO, 8], F32)
    r_den = al([128, NO], F32)
    SORT0 = al.o
    xt = [al([128, D], F32) for i in range(2)]
    tmpB = [al([128, D], F32) for i in range(2)]
    a2B = al([128, D], F32)
    sh2B = al([128, D], F32)
    h2Th = [al([128, 8, 128], BF16) for i in range(2)]
    lo_t = [al([128, D], BF16) for i in range(2)]
    h2Tl = [al([128, 8, 128], BF16) for i in range(2)]
    wrs = al([128, 8, 36], F32)
    wrh = al([128, 8, 36], BF16)
    wrl = al([128, 8, 36], BF16)
    rab = al([128, 32], F32)
    sqs = [al([128, 8, 128], BF16) for i in range(2)]
    junk = al([128, D], BF16)
    gt1B = tmpB[0]
    g2B = tmpB[1]

    K.op(K.dve, lambda: V.memset(st, 0.0), writes=[R("st")])
    dW3 = K.dsem("wC")
    K.dma(K.sp, [(a2B, modB(4)), (sh2B, modB(3)), (gt1B, modB(2)), (g2B, gB_d[:, D:2 * D]),
                 (wrs, wr_d.rearrange("(kc p) n -> p kc n", p=128))], dW3,
          writes=[R("a2B"), R("sh2B"), R("tmpB", 0), R("tmpB", 1), R("wrs")])
    K.op(K.dve, lambda: V.scalar_tensor_tensor(out=a2B, in0=a2B, scalar=1.0, in1=g2B, op0=ALU.add, op1=ALU.mult),
         reads=[R("a2B"), R("tmpB", 1)], writes=[R("a2B"), R("tmpB", 1)])
    K.op(K.act, lambda: A.copy(out=wrh, in_=wrs), reads=[R("wrs")], writes=[R("wrh")])
    K.op(K.pool, lambda: P.tensor_tensor(out=wrl, in0=wrs, in1=wrh, op=ALU.subtract), reads=[R("wrs"), R("wrh")],
         writes=[R("wrl")])
    dWoL = K.dsem("woL")
    K.dma(K.sp, [(wo, wo_d.rearrange("(kc p) n -> p kc n", p=128))], dWoL, writes=[R("wo")])
    for t in range(NO):
        sl = t % 2
        K.op(K.pool, lambda t=t, sl=sl: P.tensor_tensor(out=sqs[sl][:, 0:4, :], in0=mixTa[:, :, t * 128:(t + 1) * 128],
                                                        in1=mixTa[:, :, t * 128:(t + 1) * 128], op=ALU.mult),
             writes=[R("sqs", sl)])
        K.op(K.act, lambda t=t, sl=sl: A.activation(out=sqs[sl][:, 4:8, :], in_=mixTb[:, :, t * 128:(t + 1) * 128], func=AF.Square),
             writes=[R("sqsb", sl)])

        def ssq(t=t, sl=sl):
            ins = None
            for g2 in range(2):
                for j in range(4):
                    ins = G.matmul(bank(7)[:, 2 * t + g2:2 * t + g2 + 1], lhsT=sqs[sl][:, 4 * g2 + j, :], rhs=ones_b[:, 0:1],
                                   start=(j == 0), stop=(j == 3))
            return ins
        K.op(K.pe, ssq, reads=[R("sqs", sl), R("sqsb", sl), R("ones")], writes=[R("ps", 7)])
    K.op(K.act, lambda: A.activation(out=rab, in_=bank(7)[:, 0:32], func=AF.Ln, scale=1.0 / 512, bias=eps_t[:, 0:1]),
         reads=[R("ps", 7), R("eps")], writes=[R("rab")])
    K.op(K.act, lambda: A.activation(out=rab, in_=rab, func=AF.Exp, scale=-0.5), reads=[R("rab")], writes=[R("rab")])

    brB = prm[:, 28:64]
    def stC1(t):
        sl = t % 2
        K.dma(K.sp, [(xt[sl], x_t[t])], dX[sl], writes=[R("xt", sl)])

        def op_(t=t):
            ins = None
            for (mt, b0, k0) in [(mixTa, 0, 0), (mixTb, 2, 4)]:
                for dh in range(2):
                    for kc in range(4):
                        ins = G.matmul(bank(b0 + dh), lhsT=mt[:, kc, t * 128:(t + 1) * 128],
                                       rhs=wo[:, k0 + kc, dh * 512:(dh + 1) * 512], start=(kc == 0), stop=(kc == 3))
            return ins
        K.op(K.pe, op_, reads=[R("wo")], writes=[R("ps", b) for b in range(4)])
        K.op(K.dve, lambda t=t, sl=sl: V.scalar_tensor_tensor(out=acc[:, t, :], in0=bank(0, 2), scalar=rab[:, 2 * t:2 * t + 1],
                                                              in1=xt[sl], op0=ALU.mult, op1=ALU.add),
             reads=[R("ps", 0), R("ps", 1), R("rab"), R("xt", sl)], writes=[R("acc", t)])
        K.op(K.dve, lambda t=t: V.scalar_tensor_tensor(out=acc[:, t, :], in0=bank(2, 2), scalar=rab[:, 2 * t + 1:2 * t + 2],
                                                       in1=acc[:, t, :], op0=ALU.mult, op1=ALU.add),
             reads=[R("ps", 2), R("ps", 3), R("rab"), R("acc", t)], writes=[R("acc", t)])
        K.op(K.act, lambda t=t: A.activation(out=junk, in_=acc[:, t, :], func=AF.Square, accum_out=st[:, t:t + 1]),
             reads=[R("acc", t), R("st")], writes=[R("junk"), R("stx", t)])
        rstd_cols(st[:, t:t + 1], st[:, 32 + t:33 + t], 1.0 / D, [R("stx", t)], [R("strx", t)])

    def stC2(t):
        sl = t % 2
        K.op(K.act, lambda t=t, sl=sl: A.activation(out=tmpB[sl], in_=acc[:, t, :], func=AF.Copy, scale=st[:, 32 + t:33 + t]),
             reads=[R("acc", t), R("strx", t)], writes=[R("tmpB", sl)])
        K.op(K.dve, lambda sl=sl: V.tensor_tensor(out=tmpB[sl], in0=tmpB[sl], in1=a2B, op=ALU.mult),
             reads=[R("tmpB", sl), R("a2B")], writes=[R("tmpB", sl)])
        K.op(K.dve, lambda sl=sl: V.tensor_tensor(out=tmpB[sl], in0=tmpB[sl], in1=sh2B, op=ALU.add),
             reads=[R("tmpB", sl), R("sh2B")], writes=[R("tmpB", sl)])
        K.op(K.act, lambda sl=sl, t=t: A.copy(out=h2tok[:, t, :], in_=tmpB[sl]), reads=[R("tmpB", sl)], writes=[R("h2tok", t)])
        K.op(K.pool, lambda sl=sl, t=t: P.tensor_tensor(out=lo_t[sl], in0=tmpB[sl], in1=h2tok[:, t, :], op=ALU.subtract),
             reads=[R("tmpB", sl), R("h2tok", t)], writes=[R("lo_t", sl)])

    def stC2b(t):
        sl = t % 2

        def tr2(sl=sl, t=t):
            ins = None
            for kc in range(8):
                G.transpose(out=bankb(4)[:, kc * 128:(kc + 1) * 128], in_=h2tok[:, t, kc * 128:(kc + 1) * 128], identity=ident)
                ins = G.transpose(out=bankb(5)[:, kc * 128:(kc + 1) * 128], in_=lo_t[sl][:, kc * 128:(kc + 1) * 128],
                                  identity=ident)
            return ins
        K.op(K.pe, tr2, reads=[R("h2tok", t), R("lo_t", sl), R("ident")], writes=[R("ps", 4), R("ps", 5)])
        K.op(K.act, lambda sl=sl: A.copy(out=h2Th[sl], in_=bankb(4).rearrange("p (a b) -> p a b", a=8)),
             reads=[R("ps", 4)], writes=[R("h2Th", sl)])
        K.op(K.dve, lambda sl=sl: V.tensor_copy(out=h2Tl[sl], in_=bankb(5).rearrange("p (a b) -> p a b", a=8)),
             reads=[R("ps", 5)], writes=[R("h2Tl", sl)])

    def stC3(t):
        sl = t % 2

        def lg(t=t, sl=sl):
            ins = None
            combos = [(h2Th[sl], wrh), (h2Tl[sl], wrh), (h2Th[sl], wrl)]
            for ci, (a_, w_) in enumerate(combos):
                for kc in range(8):
                    ins = G.matmul(bank(6)[:, 0:36], lhsT=a_[:, kc, :], rhs=w_[:, kc, :],
                                   start=(ci == 0 and kc == 0), stop=(ci == 2 and kc == 7))
            return ins
        K.op(K.pe, lg, reads=[R("h2Th", sl), R("h2Tl", sl), R("wrh"), R("wrl")], writes=[R("ps", 6)])
        K.op(K.dve, lambda t=t: V.tensor_tensor(out=LGa[:, t, :], in0=bank(6)[:, 0:36], in1=brB, op=ALU.add),
             reads=[R("ps", 6), R("prm")], writes=[R("rw")])

    for i in range(NO + 3):
        if i < NO:
            stC1(i)
        if 0 <= i - 1 < NO:
            stC2(i - 1)
        if 0 <= i - 2 < NO:
            stC2b(i - 2)
        if 0 <= i - 3 < NO:
            stC3(i - 3)
    RW = [R("rw")]

    def rop(eng, fn):
        K.op(eng, fn, reads=RW, writes=RW)
    T = NO
    Lg = LGa[:, :, 0:4]
    rop(K.dve, lambda: V.tensor_reduce(out=r_m4, in_=Lg, axis=AX.X, op=ALU.max))
    rop(K.dve, lambda: V.tensor_tensor(out=r_d4, in0=Lg, in1=bc(r_m4.unsqueeze(2), [128, T, 4]), op=ALU.subtract))
    rop(K.act, lambda: A.activation(out=r_e4, in_=r_d4, func=AF.Exp))
    rop(K.dve, lambda: V.tensor_reduce(out=r_s4, in_=r_e4, axis=AX.X, op=ALU.add))
    rop(K.dve, lambda: V.tensor_scalar(out=r_oh, in0=r_d4, scalar1=0.0, scalar2=None, op0=ALU.is_ge))
    for g in range(4):
        rop(K.dve, lambda g=g: V.tensor_tensor(out=r_t32[:, :, g * 8:(g + 1) * 8], in0=LGa[:, :, 4 + g * 8:12 + g * 8],
                                               in1=bc(r_oh[:, :, g:g + 1], [128, T, 8]), op=ALU.mult))
    rop(K.dve, lambda: V.tensor_tensor(out=r_el, in0=r_t32[:, :, 0:8], in1=r_t32[:, :, 8:16], op=ALU.add))
    rop(K.dve, lambda: V.tensor_tensor(out=r_el2, in0=r_t32[:, :, 16:24], in1=r_t32[:, :, 24:32], op=ALU.add))
    rop(K.dve, lambda: V.tensor_tensor(out=r_el, in0=r_el, in1=r_el2, op=ALU.add))
    rop(K.dve, lambda: V.tensor_reduce(out=r_v1, in_=r_el, axis=AX.X, op=ALU.max))
    rop(K.dve, lambda: V.tensor_tensor(out=r_d8, in0=r_el, in1=bc(r_v1.unsqueeze(2), [128, T, 8]), op=ALU.subtract))
    rop(K.dve, lambda: V.tensor_scalar(out=r_eq, in0=r_d8, scalar1=0.0, scalar2=None, op0=ALU.is_ge))
    rop(K.dve, lambda: V.scalar_tensor_tensor(out=r_el2, in0=r_eq, scalar=-1e30, in1=r_d8, op0=ALU.mult, op1=ALU.add))
    rop(K.dve, lambda: V.tensor_reduce(out=r_v2, in_=r_el2, axis=AX.X, op=ALU.max))
    rop(K.dve, lambda: V.tensor_tensor(out=r_mk, in0=r_d8, in1=bc(r_v2.unsqueeze(2), [128, T, 8]), op=ALU.is_ge))
    rop(K.act, lambda: A.activation(out=r_ex, in_=r_d8, func=AF.Exp))
    rop(K.dve, lambda: V.tensor_tensor(out=r_cw, in0=r_mk, in1=r_ex, op=ALU.mult))
    rop(K.dve, lambda: V.tensor_reduce(out=r_den, in_=r_cw, axis=AX.X, op=ALU.add))
    rop(K.dve, lambda: V.tensor_tensor(out=r_den, in0=r_den, in1=r_s4, op=ALU.mult))
    rop(K.dve, lambda: V.reciprocal(out=r_den, in_=r_den))
    rop(K.dve, lambda: V.tensor_tensor(out=r_cw, in0=r_cw, in1=bc(r_den.unsqueeze(2), [128, T, 8]), op=ALU.mult))
    K.barrier()

    al = Alloc(SORT0, LIM)
    m2 = al([128, NO, 8], F32)
    A1 = al([128, NO, NE], F32)
    A2 = al([128, NO, NE], F32)
    Mb = al([128, NO, NE], BF16)
    utri = al([128, 128], BF16)
    onesq = al([128, 128], BF16)
    cnt = al([128, NE], F32)
    thr = al([128, 8], F32)
    cmp8 = al([128, NE, 8], F32)
    Tt = al([128, NE], F32)
    sca = al([128, NE], F32)
    scb = al([128, NE], F32)
    off256 = al([128, NE], F32)
    offT = al([128, NE], F32)
    posf = al([128, NO, NE], F32)
    ptmp = al([128, NO, NE], F32)
    posk = al([128, 2, NO], F32)
    jv = al([128, 48], F32)
    ev = al([128, NE], F32)
    pidx = al([128, 1], F32)
    indg = al([128, NE, 48], F32)
    indl = al([128, NE, 48], F32)
    eidf = al([128, 48], F32)
    anyf = al([128, 48], F32)
    real1 = al([128, 48], F32)
    jv256 = al([128, 48], F32)
    sidf = al([128, 48], F32)
    p2 = al([128, 1], F32)
    SR = [R("sort")]

    def sop(eng, fn, extra_r=(), extra_w=()):
        K.op(eng, fn, reads=SR + list(extra_r), writes=SR + list(extra_w))
    T = NO
    sop(K.pool, lambda: P.memset(utri, 1.0))
    sop(K.pool, lambda: P.affine_select(out=utri, in_=utri, pattern=[[1, 128]], compare_op=ALU.is_ge, fill=0.0, base=-1,
                                        channel_multiplier=-1))
    sop(K.pool, lambda: P.memset(onesq, 1.0))
    for m in range(8):
        sop(K.dve, lambda m=m: V.memset(thr[:, m:m + 1], 256.0 * m))
    sop(K.pool, lambda: P.iota(jv, pattern=[[1, 48]], base=0, channel_multiplier=0, allow_small_or_imprecise_dtypes=True))
    sop(K.pool, lambda: P.iota(ev, pattern=[[1, NE]], base=0, channel_multiplier=0, allow_small_or_imprecise_dtypes=True))
    sop(K.pool, lambda: P.iota(pidx, pattern=[[0, 1]], base=0, channel_multiplier=1, allow_small_or_imprecise_dtypes=True))
    sop(K.dve, lambda: V.tensor_tensor(out=m2, in0=r_mk, in1=r_eq, op=ALU.subtract), extra_r=RW)
    for g in range(4):
        sop(K.dve, lambda g=g: V.tensor_tensor(out=A1[:, :, g * 8:(g + 1) * 8], in0=r_eq,
                                               in1=bc(r_oh[:, :, g:g + 1], [128, T, 8]), op=ALU.mult), extra_r=RW)
        sop(K.dve, lambda g=g: V.tensor_tensor(out=A2[:, :, g * 8:(g + 1) * 8], in0=m2,
                                               in1=bc(r_oh[:, :, g:g + 1], [128, T, 8]), op=ALU.mult), extra_r=RW)
    sop(K.dve, lambda: V.tensor_tensor(out=r_ex, in0=r_cw, in1=r_eq, op=ALU.mult), extra_r=RW, extra_w=RW)
    sop(K.dve, lambda: V.tensor_reduce(out=w12[:, 0, :], in_=r_ex, axis=AX.X, op=ALU.add), extra_r=RW, extra_w=[R("w12")])
    sop(K.dve, lambda: V.tensor_tensor(out=r_ex, in0=r_cw, in1=m2, op=ALU.mult), extra_r=RW, extra_w=RW)
    sop(K.dve, lambda: V.tensor_reduce(out=w12[:, 1, :], in_=r_ex, axis=AX.X, op=ALU.add), extra_r=RW, extra_w=[R("w12")])
    sop(K.dve, lambda: V.tensor_tensor(out=Mb, in0=A1, in1=A2, op=ALU.add))
    def rk():
        ins = None
        for i in range(NO):
            for i2 in range(i):
                G.matmul(bank(0)[:, i * 32:(i + 1) * 32], lhsT=onesq, rhs=Mb[:, i2, :], start=(i2 == 0), stop=False)
            ins = G.matmul(bank(0)[:, i * 32:(i + 1) * 32], lhsT=utri, rhs=Mb[:, i, :], start=(i == 0), stop=True)
        for i in range(NO):
            ins = G.matmul(bank(1)[:, 0:32], lhsT=onesq, rhs=Mb[:, i, :], start=(i == 0), stop=(i == NO - 1))
        return ins
    K.op(K.pe, rk, reads=SR, writes=[R("ps", 0), R("ps", 1)])
    sop(K.dve, lambda: V.tensor_copy(out=cnt, in_=bank(1)[:, 0:32]), extra_r=[R("ps", 1)])
    sop(K.dve, lambda: V.tensor_tensor(out=cmp8, in0=bc(cnt.unsqueeze(2), [128, NE, 8]), in1=bc(thr.unsqueeze(1), [128, NE, 8]),
                                       op=ALU.is_gt))
    sop(K.dve, lambda: V.tensor_reduce(out=Tt, in_=cmp8, axis=AX.X, op=ALU.add))
    sop(K.dve, lambda: V.tensor_copy(out=sca, in_=Tt))
    cur, oth = sca, scb
    for sft in (1, 2, 4, 8, 16):
        sop(K.dve, lambda cur=cur, oth=oth: V.tensor_copy(out=oth, in_=cur))
        sop(K.dve, lambda cur=cur, oth=oth, sft=sft: V.tensor_tensor(out=oth[:, sft:NE], in0=cur[:, sft:NE], in1=cur[:, 0:NE - sft],
                                                                      op=ALU.add))
        cur, oth = oth, cur
    incl = cur
    sop(K.dve, lambda: V.tensor_tensor(out=offT, in0=incl, in1=Tt, op=ALU.subtract))
    sop(K.dve, lambda: V.tensor_scalar(out=off256, in0=offT, scalar1=256.0, scalar2=None, op0=ALU.mult))
    sop(K.dve, lambda: V.tensor_tensor(out=posf, in0=bank(0).rearrange("p (a b) -> p a b", a=NO),
                                       in1=bc(off256.unsqueeze(1), [128, T, NE]), op=ALU.add), extra_r=[R("ps", 0)])
    for k, Ak in enumerate((A1, A2)):
        sop(K.dve, lambda Ak=Ak: V.tensor_tensor(out=ptmp, in0=posf, in1=Ak, op=ALU.mult))
        sop(K.dve, lambda k=k: V.tensor_reduce(out=posk[:, k, :], in_=ptmp, axis=AX.X, op=ALU.add))
    sop(K.dve, lambda: V.tensor_copy(out=posu, in_=posk), extra_w=[R("posu")])
    dSc = K.dsem("scat")
    for i in range(NO):
        for k in range(2):
            K._waits(K.pool, [R("posu"), R("h2tok", i)], [R("xs_d")])
            P.indirect_dma_start(out=xs_d[:, :], out_offset=bass.IndirectOffsetOnAxis(posu[:, k, i:i + 1], 0),
                                 in_=h2tok[:, i, :], in_offset=None).then_inc(dSc.sem, 16)
            dSc.cnt += 16
    K._commit((dSc.sem, dSc.cnt), [R("posu")] + RL("h2tok", range(NO)), [R("xs_d")])
    sop(K.dve, lambda: V.tensor_tensor(out=indg, in0=bc(jv.unsqueeze(1), [128, NE, 48]), in1=bc(offT.unsqueeze(2), [128, NE, 48]),
                                       op=ALU.is_ge))
    sop(K.dve, lambda: V.tensor_tensor(out=indl, in0=bc(jv.unsqueeze(1), [128, NE, 48]), in1=bc(incl.unsqueeze(2), [128, NE, 48]),
                                       op=ALU.is_lt))
    sop(K.dve, lambda: V.tensor_tensor(out=indg, in0=indg, in1=indl, op=ALU.mult))
    sop(K.dve, lambda: V.tensor_reduce(out=anyf, in_=indg.rearrange("p e j -> p j e"), axis=AX.X, op=ALU.add))
    sop(K.dve, lambda: V.tensor_tensor(out=indl, in0=indg, in1=bc(ev.unsqueeze(2), [128, NE, 48]), op=ALU.mult))
    sop(K.dve, lambda: V.tensor_reduce(out=eidf, in_=indl.rearrange("p e j -> p j e"), axis=AX.X, op=ALU.add))
    sop(K.dve, lambda: V.tensor_tensor(out=sca, in0=cnt, in1=off256, op=ALU.add))
    sop(K.dve, lambda: V.tensor_scalar(out=jv256, in0=jv, scalar1=256.0, scalar2=None, op0=ALU.mult))
    sop(K.dve, lambda: V.tensor_tensor(out=indl, in0=bc(sca.unsqueeze(2), [128, NE, 48]), in1=bc(jv256.unsqueeze(1), [128, NE, 48]),
                                       op=ALU.subtract))
    sop(K.dve, lambda: V.tensor_tensor(out=indl, in0=indl, in1=indg, op=ALU.mult))
    sop(K.dve, lambda: V.tensor_reduce(out=real1, in_=indl.rearrange("p e j -> p j e"), axis=AX.X, op=ALU.add))
    sop(K.dve, lambda: V.tensor_scalar(out=p2, in0=pidx, scalar1=2.0, scalar2=None, op0=ALU.mult))
    sop(K.dve, lambda: V.tensor_scalar(out=sidf, in0=real1, scalar1=p2[:, 0:1], scalar2=None, op0=ALU.is_gt))
    sop(K.dve, lambda: V.tensor_scalar(out=sidf, in0=sidf, scalar1=-1.0e6, scalar2=1.0e6, op0=ALU.mult, op1=ALU.add))
    sop(K.dve, lambda: V.tensor_scalar(out=jv256, in0=jv, scalar1=128.0, scalar2=pidx[:, 0:1], op0=ALU.mult, op1=ALU.add))
    sop(K.dve, lambda: V.tensor_tensor(out=sidf, in0=sidf, in1=jv256, op=ALU.add))
    sop(K.dve, lambda: V.tensor_copy(out=sidx, in_=sidf), extra_w=[R("sidx")])
    sop(K.dve, lambda: V.tensor_scalar(out=anyf, in0=anyf, scalar1=-32.0, scalar2=32.0, op0=ALU.mult, op1=ALU.add))
    sop(K.dve, lambda: V.tensor_tensor(out=eidf, in0=eidf, in1=anyf, op=ALU.add))
    sop(K.dve, lambda: V.tensor_scalar(out=eidf, in0=eidf, scalar1=128.0, scalar2=pidx[:, 0:1], op0=ALU.mult, op1=ALU.add))
    sop(K.dve, lambda: V.tensor_copy(out=widx, in_=eidf), extra_w=[R("widx")])
    alw = Alloc(68 * KIB, 116 * KIB)
    wall = [alw([128, 3 * 2048], BF16) for i in range(4)]
    wgb = [w[:, 0:2048].rearrange("p (a b) -> p a b", a=8) for w in wall]
    wub = [w[:, 2048:4096].rearrange("p (a b) -> p a b", a=8) for w in wall]
    wdb = [w[:, 4096:6144].rearrange("p (a b) -> p a b", a=2) for w in wall]
    dG = [K.dsem("g%d" % i) for i in range(4)]
    breg = P.to_reg(NE * 128 - 1)
    for i in range(4):
        K.op(K.act, lambda i=i: A.memzero(wall[i]), writes=[R("wall", i)])

    def gather_w(q, j):
        s4 = q % 4
        K._waits(K.pool, [R("widx"), R("wbf")], [R("wall", s4)])
        P.indirect_dma_start(out=wall[s4], out_offset=None, in_=wbf_d[:, :],
                             in_offset=bass.IndirectOffsetOnAxis(widx[:, j:j + 1], 0),
                             bounds_check=breg, oob_is_err=False).then_inc(dG[s4].sem, 16)
        dG[s4].cnt += 16
        K._commit((dG[s4].sem, dG[s4].cnt), [R("widx"), R("wbf")], [R("wall", s4)])
    order = []
    for g3 in range(16):
        order += [2 * g3, 2 * g3 + 1, 32 + g3]
    assert sorted(order) == list(range(NTL))

    for q in range(4):
        gather_w(q, order[q])
    K.barrier()

    al = Alloc(116 * KIB, LIM)
    xs = [al([128, 2, D], BF16) for i in range(4)]
    xT = [al([128, 8, 256], BF16) for i in range(2)]
    ssb = [al([128, 512], F32) for i in range(2)]
    hid = [al([128, 2, 256], BF16) for i in range(2)]
    ysb = [al([128, 2, D], F32) for i in range(2)]
    gt2B = al([128, D], F32)
    gfB = al([128, D], F32)
    yg = [al([128, 2, D], F32) for i in range(2)]
    yt = [al([128, D], F32) for i in range(2)]
    junk = al([128, D], BF16)

    dXs = [K.dsem("xs%d" % i) for i in range(4)]
    dYs = [K.dsem("ys%d" % i) for i in range(2)]
    dF = K.dsem("fin")
    K.op(K.dve, lambda: V.memset(st, 0.0), writes=[R("st")])
    K.dma(K.sp, [(gt2B, modB(5)), (gfB, gB_d[:, 2 * D:3 * D])], dF, writes=[R("gt2B"), R("gfB")])
    sreg = P.to_reg(NTL * 128 - 1)
    for i in range(4):
        K.op(K.act, lambda i=i: A.memzero(xs[i].rearrange("p a b -> p (a b)")), writes=[R("xs", i)])
    xs_p = xs_d.rearrange("(n r) d -> n (r d)", r=2)
    ys_p = ys_d.rearrange("(n r) d -> n (r d)", r=2)

    def stM1(q, j):
        K._waits(K.pool, [R("sidx"), R("xs_d")], [R("xs", q % 4)])
        P.indirect_dma_start(out=xs[q % 4].rearrange("p a b -> p (a b)"), out_offset=None, in_=xs_p,
                             in_offset=bass.IndirectOffsetOnAxis(sidx[:, j:j + 1], 0),
                             bounds_check=sreg, oob_is_err=False).then_inc(dXs[q % 4].sem, 16)
        dXs[q % 4].cnt += 16
        K._commit((dXs[q % 4].sem, dXs[q % 4].cnt), [R("sidx"), R("xs_d")], [R("xs", q % 4)])

    def stM2(q, j):
        s3, s2 = q % 4, q % 2

        def tr():
            ins = None
            for s_ in range(2):
                for kc in range(8):
                    ins = G.transpose(out=bankb(s_)[:, kc * 128:(kc + 1) * 128], in_=xs[s3][:, s_, kc * 128:(kc + 1) * 128],
                                      identity=ident)
            return ins
        K.op(K.pe, tr, reads=[R("xs", s3), R("ident")], writes=[R("ps", 0), R("ps", 1)])
        K.op(K.act, lambda: A.copy(out=xT[s2][:, :, 0:128], in_=bankb(0).rearrange("p (a b) -> p a b", a=8)),
             reads=[R("ps", 0)], writes=[R("xT", s2)])
        K.op(K.dve, lambda: V.tensor_copy(out=xT[s2][:, :, 128:256], in_=bankb(1).rearrange("p (a b) -> p a b", a=8)),
             reads=[R("ps", 1)], writes=[R("xT", s2)])

    def stM3(q, j):
        s4, s2 = q % 4, q % 2

        def mm():
            ins = None
            for (wb, b) in ((wgb[s4], 2), (wub[s4], 3)):
                for fc in range(2):
                    for kc in range(8):
                        ins = G.matmul(bank(b)[:, fc * 256:(fc + 1) * 256], lhsT=wb[:, kc, fc * 128:(fc + 1) * 128],
                                       rhs=xT[s2][:, kc, :], start=(kc == 0), stop=(kc == 7))
            return ins
        K.op(K.pe, mm, reads=[R("wall", s4), R("xT", s2)], writes=[R("ps", 2), R("ps", 3)])
        K.op(K.act, lambda: A.activation(out=ssb[s2], in_=bank(2), func=AF.Silu), reads=[R("ps", 2)], writes=[R("ssb", s2)])
        K.op(K.dve, lambda: V.tensor_tensor(out=hid[s2].rearrange("p a b -> p (a b)"), in0=ssb[s2], in1=bank(3), op=ALU.mult),
             reads=[R("ssb", s2), R("ps", 3)], writes=[R("hid", s2)])

    def stM4(q, j):
        s4, s2 = q % 4, q % 2
        for sh in range(2):
            b0 = 4 + 2 * sh

            def mm(sh=sh, b0=b0):
                ins = None
                for dh in range(2):
                    for fc in range(2):
                        ins = G.matmul(bank(b0 + dh), lhsT=hid[s2][:, fc, sh * 128:(sh + 1) * 128],
                                       rhs=wdb[s4][:, fc, dh * 512:(dh + 1) * 512], start=(fc == 0), stop=(fc == 1))
                return ins
            K.op(K.pe, mm, reads=[R("hid", s2), R("wall", s4)], writes=[R("ps", b0), R("ps", b0 + 1)])
            K.op(K.dve, lambda sh=sh, b0=b0: V.tensor_tensor(out=ysb[s2][:, sh, :], in0=bank(b0, 2), in1=gt2B, op=ALU.mult),
                 reads=[R("ps", b0), R("ps", b0 + 1), R("gt2B")], writes=[R("ysb", s2)])

    def stM4b(q, j):
        s2 = q % 2
        K._waits(K.pool, [R("sidx"), R("ysb", s2)], [R("ys_d", j)])
        P.indirect_dma_start(out=ys_p, out_offset=bass.IndirectOffsetOnAxis(sidx[:, j:j + 1], 0),
                             in_=ysb[s2].rearrange("p a b -> p (a b)"), in_offset=None, bounds_check=sreg,
                             oob_is_err=False).then_inc(dYs[s2].sem, 16)
        dYs[s2].cnt += 16
        K._commit((dYs[s2].sem, dYs[s2].cnt), [R("sidx"), R("ysb", s2)], [R("ys_d", j)])

    stM1(0, order[0])
    stM1(1, order[1])
    for i in range(NTL + 3):
        if 0 <= i - 3 < NTL:
            stM4(i - 3, order[i - 3])
            if i + 1 < NTL:
                gather_w(i + 1, order[i + 1])
        if i + 2 < NTL:
            stM1(i + 2, order[i + 2])
        if 0 <= i - 1 < NTL:
            stM2(i - 1, order[i - 1])
        if 0 <= i - 2 < NTL:
            stM3(i - 2, order[i - 2])
        if 0 <= i - 3 < NTL:
            stM4b(i - 3, order[i - 3])

    dO = [K.dsem("o%d" % i) for i in range(2)]
    dYg = [K.dsem("yg%d" % i) for i in range(2)]
    y_t = y_d.rearrange("(t p) d -> t p d", p=128)
    for t in range(NO):
        sl = t % 2
        ysr = [R("ys_d", j) for j in range(NTL)]
        K._waits(K.pool, ysr + [R("posu")], [R("yg", sl)])
        for k in range(2):
            P.indirect_dma_start(out=yg[sl][:, k, :], out_offset=None, in_=ys_d[:, :],
                                 in_offset=bass.IndirectOffsetOnAxis(posu[:, k, t:t + 1], 0)).then_inc(dYg[sl].sem, 16)
            dYg[sl].cnt += 16
        K._commit((dYg[sl].sem, dYg[sl].cnt), [R("posu")], [R("yg", sl)])
        for k in range(2):
            K.op(K.dve, lambda t=t, k=k, sl=sl: V.scalar_tensor_tensor(out=acc[:, t, :], in0=yg[sl][:, k, :],
                                                                       scalar=w12[:, k, t:t + 1], in1=acc[:, t, :],
                                                                       op0=ALU.mult, op1=ALU.add),
                 reads=[R("yg", sl), R("w12"), R("acc", t)], writes=[R("acc", t)])
        K.op(K.act, lambda t=t: A.activation(out=junk, in_=acc[:, t, :], func=AF.Square, accum_out=st[:, t:t + 1]),
             reads=[R("acc", t), R("st")], writes=[R("junk"), R("stx", t)])
        rstd_cols(st[:, t:t + 1], st[:, 32 + t:33 + t], 1.0 / D, [R("stx", t)], [R("strx", t)])
        K.op(K.dve, lambda t=t, sl=sl: V.scalar_tensor_tensor(out=yt[sl], in0=acc[:, t, :], scalar=st[:, 32 + t:33 + t], in1=gfB,
                                                              op0=ALU.mult, op1=ALU.mult),
             reads=[R("acc", t), R("strx", t), R("gfB")], writes=[R("yt", sl)])
        K.dma(K.sp, [(y_t[t], yt[sl])], dO[sl], reads=[R("yt", sl)])
    for d in dO:
        K.sp.h.wait_ge(d.sem, d.cnt)
    es.close()
    return nc


_ROPE_THETA = 10000.0


def _rope_table():
    out = np.zeros((S, 192), np.float32)
    pos = np.arange(S, dtype=np.float32)[:, None]
    for (dim, o) in ((64, 0), (32, 128)):
        inv = (1.0 / (_ROPE_THETA ** (np.arange(0, dim, 2, dtype=np.float32) / dim))).astype(np.float32)
        ang = (pos * inv[None, :]).astype(np.float32)
        c, s_ = np.cos(ang).astype(np.float32), np.sin(ang).astype(np.float32)
        out[:, o:o + dim] = np.concatenate([c, c], axis=1)
        out[:, o + dim:o + 2 * dim] = np.concatenate([-s_, s_], axis=1)
    return out


_NC_CACHE = {}


def kernel(x, c, w_ada, b_ada, g_norm1, w_in, g_q_lora, w_uq, g_kv_lora, w_ukv, sink,
           g_out_swa, g_out_mla, w_out, g_norm2, w_router_group, b_router_group,
           w_router_expert, b_router_expert, w_exp_gate, w_exp_up, w_exp_down, g_final):
    f = lambda a: np.ascontiguousarray(np.asarray(a, dtype=np.float32))
    x = f(x); c = f(c)
    if "nc" not in _NC_CACHE:
        _NC_CACHE["nc"] = build_program()
    nc = _NC_CACHE["nc"]
    rope = _rope_table()
    w_in0 = f(w_in)[0]
    perm = np.concatenate([np.arange(0, 768), np.arange(960, 1120), np.arange(768, 960)])
    w_in_p = np.ascontiguousarray(w_in0[:, perm])
    gB = np.ascontiguousarray(np.broadcast_to(
        np.concatenate([f(g_norm1)[0], f(g_norm2)[0], f(g_final)])[None, :], (128, 3 * D)))
    w_r = np.ascontiguousarray(np.concatenate([f(w_router_group)[0], f(w_router_expert)[0]], axis=1))
    b_r = np.concatenate([f(b_router_group)[0], f(b_router_expert)[0]])
    gcat = np.concatenate([f(g_out_swa)[0], f(g_out_mla)[0]])
    jj = np.arange(128)[:, None]
    rr = np.arange(128)[None, :]
    mprev = (jj >= rr).astype(np.float32)
    mnext = (jj <= rr).astype(np.float32)
    shared = {
        "w_ada": f(w_ada)[0], "b_ada": f(b_ada), "w_in": w_in_p, "w_uq": f(w_uq)[0], "w_ukv": f(w_ukv)[0],
        "w_out": f(w_out)[0], "w_r": w_r, "w_g": np.ascontiguousarray(f(w_exp_gate)[0].reshape(NE, 8, 128, 256).transpose(0, 2, 1, 3).reshape(NE * 128, 2048)),
        "w_u": np.ascontiguousarray(f(w_exp_up)[0].reshape(NE, 8, 128, 256).transpose(0, 2, 1, 3).reshape(NE * 128, 2048)),
        "w_d": np.ascontiguousarray(f(w_exp_down)[0].reshape(NE, 2, 128, D).transpose(0, 2, 1, 3).reshape(NE * 128, 2048)),
        "gB": gB,
    }
    in_maps = []
    for core in range(8):
        b, hf = core // 2, core % 2
        own = slice(hf * 2048, (hf + 1) * 2048)
        oth = slice((1 - hf) * 2048, (2 - hf) * 2048)
        prm = np.zeros((128, 64), np.float32)
        prm[:, 0:8] = c[b].reshape(8, 128).T
        prm[0:96, 8:10] = f(g_q_lora)[0].reshape(2, 96).T
        prm[:, 10] = f(g_kv_lora)[0]
        prm[:, 11:19] = gcat.reshape(8, 128).T
        prm[:, 20:28] = f(sink)[0][None, :]
        prm[:, 28:64] = b_r[None, :]
        msk = np.stack([mprev, mnext, mprev * float(hf == 1), mnext * float(hf == 0)], axis=1).reshape(128, 512)
        m = dict(shared)
        m["x"] = np.ascontiguousarray(np.concatenate([x[b, own], x[b, oth]], axis=0))
        m["rope"] = np.ascontiguousarray(np.concatenate([rope[own], rope[oth]], axis=0))
        m["prm"] = prm
        m["g1p"] = np.ascontiguousarray(f(g_norm1)[0].reshape(8, 128).T)
        m["msk"] = np.ascontiguousarray(msk.astype(np.float32))
        in_maps.append(m)
    res = run_bass_kernel_spmd(nc, in_maps, core_ids=list(range(8)))
    out = np.zeros((4, S, D), np.float32)
    for core in range(8):
        b, hf = core // 2, core % 2
        out[b, hf * 2048:(hf + 1) * 2048] = res.results[core]["y"]
    return out
```

```python
import numpy as np
from contextlib import ExitStack

import concourse.bass as bass
import concourse.mybir as mybir
from concourse.bass_utils import run_bass_kernel_spmd

F32 = mybir.dt.float32
BF16 = mybir.dt.bfloat16
U8 = mybir.dt.uint8
U32 = mybir.dt.uint32
AF = mybir.ActivationFunctionType
ALU = mybir.AluOpType
AX = mybir.AxisListType

D = 1024
S = 4096
NT = 32
NO = 16
EPS = 1e-6
NE = 32
KIB = 1024


class Region:
    __slots__ = ("name", "lw", "rd")

    def __init__(self, name):
        self.name = name
        self.lw = None
        self.rd = []


class Eng:
    def __init__(self, K, name, h):
        self.name = name
        self.h = h
        self.sem = K.es.enter_context(K.nc.semaphore("tl_" + name))
        self.cnt = 0
        self.waited = {}


class DSem:
    def __init__(self, K, name):
        self.sem = K.es.enter_context(K.nc.semaphore("d_" + name))
        self.cnt = 0


class KB:
    def __init__(self, nc, es):
        self.nc = nc
        self.es = es
        self.pe = Eng(self, "pe", nc.tensor)
        self.dve = Eng(self, "dve", nc.vector)
        self.act = Eng(self, "act", nc.scalar)
        self.pool = Eng(self, "pool", nc.gpsimd)
        self.sp = Eng(self, "sp", nc.sync)
        self.engs = [self.pe, self.dve, self.act, self.pool, self.sp]
        self.dsems = []

    def dsem(self, name):
        d = DSem(self, name)
        self.dsems.append(d)
        return d

    def _waits(self, eng, reads, writes):
        need = {}

        def add(t):
            if t is None:
                return
            s, v = t
            k = id(s)
            if k not in need or need[k][1] < v:
                need[k] = (s, v)

        for r in reads:
            add(r.lw)
        for w in writes:
            add(w.lw)
            for t in w.rd:
                add(t)
        for k, (s, v) in need.items():
            if eng.waited.get(k, 0) >= v:
                continue
            if eng.name == "pe" and s is eng.sem:
                continue
            eng.h.wait_ge(s, v)
            eng.waited[k] = v

    def _commit(self, ticket, reads, writes):
        for r in reads:
            r.rd.append(ticket)
            if len(r.rd) > 48:
                best = {}
                for (s, v) in r.rd:
                    if id(s) not in best or best[id(s)][1] < v:
                        best[id(s)] = (s, v)
                r.rd = list(best.values())
        for w in writes:
            w.lw = ticket
            w.rd = []

    def op(self, eng, fn, reads=(), writes=()):
        self._waits(eng, reads, writes)
        ins = fn()
        eng.cnt += 1
        ins.then_inc(eng.sem, 1)
        self._commit((eng.sem, eng.cnt), reads, writes)
        return ins

    def dma(self, q, pairs, dsem, reads=(), writes=(), **kw):
        self._waits(q, reads, writes)
        for (o, i) in pairs:
            q.h.dma_start(out=o, in_=i, **kw).then_inc(dsem.sem, 16)
            dsem.cnt += 16
        self._commit((dsem.sem, dsem.cnt), reads, writes)

    def barrier(self):
        for e in self.engs:
            for o in self.engs:
                if o is e or o.cnt == 0:
                    continue
                if e.waited.get(id(o.sem), 0) < o.cnt:
                    e.h.wait_ge(o.sem, o.cnt)
                    e.waited[id(o.sem)] = o.cnt
            for d in self.dsems:
                if d.cnt and e.waited.get(id(d.sem), 0) < d.cnt:
                    e.h.wait_ge(d.sem, d.cnt)
                    e.waited[id(d.sem)] = d.cnt


_DT_SIZE = {F32: 4, BF16: 2, U32: 4}


def build_program():
    nc = bass.Bass("TRN2", target_bir_lowering=False)
    es = ExitStack()
    K = KB(nc, es)
    V, A, P, G = nc.vector, nc.scalar, nc.gpsimd, nc.tensor

    def din(name, shape):
        return nc.dram_tensor(name, shape, F32, kind="ExternalInput").ap()

    x_d = din("x", [S, D])
    prm_d = din("prm", [128, 64])
    g1p_d = din("g1p", [128, 8])
    gB_d = din("gB", [128, 3 * D])
    rope_d = din("rope", [S, 192])
    msk_d = din("msk", [128, 512])
    wada_d = din("w_ada", [D, 6 * D])
    bada_d = din("b_ada", [1, 6 * D])
    win_d = din("w_in", [D, 1120])
    wuq_d = din("w_uq", [192, 768])
    wukv_d = din("w_ukv", [128, 1024])
    wout_d = din("w_out", [D, D])
    wr_d = din("w_r", [D, 36])
    wg_d = din("w_g", [NE * 128, 2048])
    wu_d = din("w_u", [NE * 128, 2048])
    wd_d = din("w_d", [NE * 128, 2048])
    y_d = nc.dram_tensor("y", [NO * 128, D], F32, kind="ExternalOutput").ap()
    mod_d = nc.dram_tensor("mod_scratch", [1, 6 * D], F32, kind="Internal").ap()
    NTL = 48
    xs_d = nc.dram_tensor("xs_scratch", [NTL * 256, D], BF16, kind="Internal").ap()
    ys_d = nc.dram_tensor("ys_scratch", [NTL * 256, D], F32, kind="Internal").ap()
    wo_d = nc.dram_tensor("wo_scratch", [D, D], BF16, kind="Internal").ap()
    wbf_d = nc.dram_tensor("wbf_all", [NE * 128, 3 * 2048], BF16, kind="Internal").ap()

    arena = nc.alloc_sbuf_tensor("arena", [128, 206 * KIB], U8)
    ps = nc.alloc_psum_tensor("ps", [128, 4096], F32)
    LIM = 206 * KIB

    def view(off, shape, dt, p0=0):
        off = int(off)
        n = 1
        for s_ in shape[1:]:
            n *= s_
        assert off + n * _DT_SIZE[dt] <= LIM, (off, shape)
        ap = arena[p0:p0 + shape[0], off:off + n * _DT_SIZE[dt]].bitcast(dt)
        if len(shape) == 3:
            ap = ap.rearrange("p (a b) -> p a b", a=shape[1])
        elif len(shape) == 4:
            ap = ap.rearrange("p (a b c) -> p a b c", a=shape[1], b=shape[2])
        return ap

    class Alloc:
        def __init__(self, start, end):
            self.o = start
            self.end = end

        def __call__(self, shape, dt, p0=0):
            n = 1
            for s_ in shape[1:]:
                n *= s_
            nb = (n * _DT_SIZE[dt] + 31) // 32 * 32
            v = view(self.o, shape, dt, p0)
            self.o += nb
            assert self.o <= self.end, (self.o, self.end, shape)
            return v

    def bank(b, n=1):
        return ps[:, b * 512:(b + n) * 512]

    def bankb(b):
        return ps[:, b * 512:(b + 1) * 512].bitcast(BF16)

    def bc(ap, shape):
        return ap.to_broadcast(shape)

    def modB(i):
        return mod_d[0, i * D:(i + 1) * D].partition_broadcast(128)

    Rg = {}

    def R(*key):
        if key not in Rg:
            Rg[key] = Region(str(key))
        return Rg[key]

    def RL(name, idxs):
        return [R(name, i) for i in idxs]

    def h3(ap, h):
        return ap.rearrange("p (h d) -> p h d", h=h)

    prm = view(0, [128, 64], F32)
    ident = view(256, [128, 128], BF16)
    ones_b = view(512, [128, 4], BF16)
    sT = view(528, [128, 8], F32)
    eps_t = view(560, [128, 1], F32)
    es8 = view(576, [1, 8], F32)
    st = view(640, [128, 160], F32)
    vsink = view(1280, [1, 96], BF16)
    g1P = view(1472, [128, 8], F32)
    a1P = view(1504, [128, 8], F32)
    sh1P = view(1536, [128, 8], F32)
    sh1Pb = view(1568, [128, 8], BF16)
    onesrow = view(1600, [1, 128], BF16)
    L2 = 2 * KIB
    L3 = 26 * KIB
    L4 = 86 * KIB
    PA = 118 * KIB

    dPrm = K.dsem("prm")
    K.dma(K.sp, [(prm, prm_d[:, :])], dPrm, writes=[R("prm")])
    K.op(K.pool, lambda: P.memset(ident, 1.0), writes=[R("ident")])
    K.op(K.pool, lambda: P.affine_select(out=ident, in_=ident, pattern=[[-1, 128]],
                                         compare_op=ALU.is_equal, fill=0.0, base=0,
                                         channel_multiplier=1),
         reads=[R("ident")], writes=[R("ident")])
    K.op(K.dve, lambda: V.memset(ones_b, 1.0), writes=[R("ones")])
    K.op(K.dve, lambda: V.memset(st, 0.0), writes=[R("st")])
    K.op(K.dve, lambda: V.memset(eps_t, EPS), writes=[R("eps")])
    K.op(K.dve, lambda: V.memset(vsink[0:1, 0:64], 0.0), writes=[R("vsink")])
    K.op(K.dve, lambda: V.memset(vsink[0:1, 64:96], 1.0), writes=[R("vsink")])
    K.op(K.act, lambda: A.activation(out=sT, in_=prm[:, 0:8], func=AF.Silu),
         reads=[R("prm")], writes=[R("sT")])

    def rstd_cols(src, dst, n_inv, rd, wr):
        K.op(K.act, lambda: A.activation(out=dst, in_=src, func=AF.Ln, scale=n_inv, bias=eps_t[:, 0:1]),
             reads=list(rd) + [R("eps")], writes=list(wr))
        K.op(K.act, lambda: A.activation(out=dst, in_=dst, func=AF.Exp, scale=-0.5),
             reads=list(wr), writes=list(wr))

    al = Alloc(4 * KIB, LIM)
    wa = [al([128, 8, 512], F32) for i in range(3)]
    bada = al([1, 6 * D], F32)
    modrow = al([1, 6 * D], F32)
    dWa = [K.dsem("wa%d" % i) for i in range(3)]
    dWin = K.dsem("win")
    win0 = view(PA, [128, 8, 1120], BF16)
    K.dma(K.pool, [(win0, win_d.rearrange("(kc p) n -> p kc n", p=128))], dWin, writes=[R("win")])
    dBa = K.dsem("bada")
    wada_v = wada_d.rearrange("(kc p) n -> p kc n", p=128)
    K.dma(K.sp, [(bada, bada_d[:, :])], dBa, writes=[R("bada")])
    for j in range(12):
        sl = j % 3
        K.dma(K.sp, [(wa[sl], wada_v[:, :, j * 512:(j + 1) * 512])], dWa[sl], writes=[R("wa", sl)])
        pb = bank(j % 2)

        def mm(sl=sl, pb=pb):
            ins = None
            for kc in range(8):
                ins = G.matmul(pb[0:1, :], lhsT=sT[:, kc:kc + 1], rhs=wa[sl][:, kc, :],
                               start=(kc == 0), stop=(kc == 7))
            return ins
        K.op(K.pe, mm, reads=[R("wa", sl), R("sT")], writes=[R("ps", j % 2)])
        K.op(K.dve, lambda j=j, pb=pb: V.tensor_tensor(out=modrow[0:1, j * 512:(j + 1) * 512], in0=pb[0:1, :],
                                                       in1=bada[0:1, j * 512:(j + 1) * 512], op=ALU.add),
             reads=[R("ps", j % 2), R("bada")], writes=[R("modrow")])
    dM = K.dsem("mod")
    K.dma(K.sp, [(mod_d[:, :], modrow)], dM, reads=[R("modrow")], writes=[R("mod_d")])
    K.barrier()

    ckvnT = view(L2, [128, S], BF16)
    krT = view(L2 + 8 * KIB, [96, S], BF16)
    cqnT = view(L2 + 16 * KIB, [96, 2, NO * 128], BF16)
    qTa = view(L3, [64, 8, NO * 128], BF16)
    kTa = view(L3 + 32 * KIB, [64, 2, S], BF16)
    va = view(L3 + 48 * KIB, [128, NT, 2, 96], BF16)

    al = Alloc(PA, LIM)
    win = al([128, 8, 1120], BF16)
    b1row = al([1, 1120], BF16)
    xt = [al([128, D], F32) for i in range(2)]
    hb = [al([128, D], BF16) for i in range(3)]
    hT = [al([128, 8, 128], BF16) for i in range(3)]
    projS = [al([128, 1120], F32) for i in range(3)]
    rt = [al([128, 192], F32) for i in range(4)]
    t1 = [al([128, 640], F32) for i in range(2)]
    t2 = [al([128, 640], F32) for i in range(2)]
    t1r = [al([128, 32], F32) for i in range(2)]
    t2r = [al([128, 32], F32) for i in range(2)]
    qkr = [al([128, 640], BF16) for i in range(2)]
    ckvn = [al([128, 128], BF16) for i in range(2)]
    krs = [al([128, 96], BF16) for i in range(2)]
    cqn = [al([128, 192], BF16) for i in range(2)]
    junk = al([128, D], BF16)
    junk2 = al([128, 192], BF16)
    assert al.o <= 198 * KIB, al.o
    dW = K.dsem("wA")
    K.dma(K.sp, [(g1P, g1p_d[:, :]),
                 (a1P, mod_d[0, D:2 * D].rearrange("(kc p) -> p kc", p=128)),
                 (sh1P, mod_d[0, 0:D].rearrange("(kc p) -> p kc", p=128))], dW,
          reads=[R("mod_d")], writes=[R("a1P"), R("sh1P")], allow_slow_non_contiguous=True)
    K.op(K.dve, lambda: V.scalar_tensor_tensor(out=a1P, in0=a1P, scalar=1.0, in1=g1P, op0=ALU.add, op1=ALU.mult),
         reads=[R("a1P")], writes=[R("a1P")])
    K.op(K.dve, lambda: V.tensor_copy(out=sh1Pb, in_=sh1P), reads=[R("sh1P")], writes=[R("sh1Pb")])
    K.op(K.dve, lambda: V.memset(onesrow, 1.0), writes=[R("onesrow")])

    def b1mm():
        ins = None
        for (b, c0, n) in [(2, 0, 512), (3, 512, 512), (4, 1024, 96)]:
            for kc in range(8):
                ins = G.matmul(bank(b)[0:1, 0:n], lhsT=sh1Pb[:, kc:kc + 1], rhs=win[:, kc, c0:c0 + n],
                               start=(kc == 0), stop=(kc == 7))
        return ins
    K.op(K.pe, b1mm, reads=[R("sh1Pb"), R("win")], writes=[R("ps", 2), R("ps", 3), R("ps", 4)])
    for (b, c0, n) in [(2, 0, 512), (3, 512, 512), (4, 1024, 96)]:
        K.op(K.dve, lambda b=b, c0=c0, n=n: V.tensor_copy(out=b1row[0:1, c0:c0 + n], in_=bank(b)[0:1, 0:n]),
             reads=[R("ps", b)], writes=[R("b1row")])
    for kc in range(8):
        K.op(K.dve, lambda kc=kc: V.tensor_scalar(out=win[:, kc, :], in0=win[:, kc, :], scalar1=a1P[:, kc:kc + 1], scalar2=None,
                                                  op0=ALU.mult),
             reads=[R("win"), R("a1P")], writes=[R("win")])
    K.op(K.pool, lambda: P.memset(va[:, :, :, 64:96], 1.0), writes=RL("va", range(NT)))
    for i in range(2):
        K.op(K.pool, lambda i=i: P.memset(krs[i], 0.0), writes=[R("krs", i)])

    dX = [K.dsem("x%d" % i) for i in range(2)]
    x_t = x_d.rearrange("(t p) d -> t p d", p=128)
    rope_t = rope_d.rearrange("(t p) d -> t p d", p=128)
    CQS = (128.0 / 192.0) ** 0.5

    def rope(src3, cs, sn, half, o1, o2, dst, rd, r1, r2, wr_dst, nh):
        w = 2 * half
        K.op(K.dve, lambda: V.tensor_tensor(out=o1, in0=src3, in1=bc(cs.unsqueeze(1), [128, nh, w]), op=ALU.mult),
             reads=rd, writes=[r1])
        K.op(K.dve, lambda: V.tensor_tensor(out=o2[:, :, 0:half], in0=src3[:, :, half:w],
                                            in1=bc(sn[:, 0:half].unsqueeze(1), [128, nh, half]), op=ALU.mult),
             reads=rd, writes=[r2])
        K.op(K.dve, lambda: V.tensor_tensor(out=o2[:, :, half:w], in0=src3[:, :, 0:half],
                                            in1=bc(sn[:, half:w].unsqueeze(1), [128, nh, half]), op=ALU.mult),
             reads=rd, writes=[r2])
        K.op(K.dve, lambda: V.tensor_tensor(out=dst, in0=o1, in1=o2, op=ALU.add),
             reads=[r1, r2], writes=wr_dst)

    def stA1(t):
        own = t < NO
        sl = t % 2
        K.dma(K.sp, [(xt[sl], x_t[t]), (rt[t % 4], rope_t[t])], dX[sl], writes=[R("xt", sl), R("rt", t % 4)])
        K.op(K.act, lambda sl=sl, t=t: A.activation(out=junk, in_=xt[sl], func=AF.Square, accum_out=st[:, t:t + 1]),
             reads=[R("xt", sl)], writes=[R("junk"), R("stx", t)])
        rstd_cols(st[:, t:t + 1], st[:, 32 + t:33 + t], 1.0 / D, [R("stx", t)], [R("strx", t)])
        K.op(K.act, lambda sl=sl, t=t: A.activation(out=hb[t % 3], in_=xt[sl], func=AF.Copy, scale=st[:, 32 + t:33 + t]),
             reads=[R("xt", sl), R("strx", t)], writes=[R("hb", t % 3)])

    def stA2(t):
        own = t < NO
        sl = t % 2

        def tr(sl=sl):
            ins = None
            for kc in range(8):
                ins = G.transpose(out=bankb(sl)[:, kc * 128:(kc + 1) * 128], in_=hb[t % 3][:, kc * 128:(kc + 1) * 128],
                                  identity=ident)
            return ins
        K.op(K.pe, tr, reads=[R("hb", t % 3), R("ident")], writes=[R("ps", sl)])
        K.op(K.act, lambda sl=sl: A.copy(out=hT[t % 3], in_=bankb(sl).rearrange("p (a b) -> p a b", a=8)),
             reads=[R("ps", sl)], writes=[R("hT", t % 3)])

    def stA2b(t):
        own = t < NO
        sl = t % 2
        chunks = [(2, 0, 512), (3, 512, 416), (4, 928, 192)] if own else [(3, 512, 416)]

        def proj(sl=sl, chunks=chunks):
            ins = None
            for (b, c0, n) in chunks:
                for kc in range(8):
                    G.matmul(bank(b)[:, 0:n], lhsT=hT[t % 3][:, kc, :], rhs=win[:, kc, c0:c0 + n],
                             start=(kc == 0), stop=False)
                ins = G.matmul(bank(b)[:, 0:n], lhsT=onesrow[0:1, :], rhs=b1row[0:1, c0:c0 + n], start=False, stop=True)
            return ins
        K.op(K.pe, proj, reads=[R("hT", t % 3), R("win"), R("b1row"), R("onesrow")], writes=[R("ps", b) for (b, _, _) in chunks])
        for (b, c0, n) in chunks:
            if b == 2:
                K.op(K.dve, lambda b=b, c0=c0, n=n, sl=sl: V.tensor_copy(out=projS[t % 3][:, c0:c0 + n], in_=bank(b)[:, 0:n]),
                     reads=[R("ps", b)], writes=[R("projS", t % 3, b)])
            else:
                K.op(K.act, lambda b=b, c0=c0, n=n, sl=sl: A.copy(out=projS[t % 3][:, c0:c0 + n], in_=bank(b)[:, 0:n]),
                     reads=[R("ps", b)], writes=[R("projS", t % 3, b)])

    def stA3(t):
        own = t < NO
        sl = t % 2
        pS = projS[t % 3]
        if own:
            rope(h3(pS[:, 0:640], 10), rt[t % 4][:, 0:64], rt[t % 4][:, 64:128], 32, h3(t1[sl], 10), h3(t2[sl], 10),
                 h3(qkr[sl], 10), [R("projS", t % 3, 2), R("projS", t % 3, 3), R("rt", t % 4)], R("t1", sl), R("t2", sl),
                 [R("qkr", sl)], 10)
        else:
            rope(h3(pS[:, 512:640], 2), rt[t % 4][:, 0:64], rt[t % 4][:, 64:128], 32, h3(t1[sl][:, 512:640], 2),
                 h3(t2[sl][:, 512:640], 2), h3(qkr[sl][:, 512:640], 2), [R("projS", t % 3, 3), R("rt", t % 4)],
                 R("t1", sl), R("t2", sl), [R("qkr", sl)], 2)
        rope(h3(pS[:, 896:928], 1), rt[t % 4][:, 128:160], rt[t % 4][:, 160:192], 16, h3(t1r[sl], 1), h3(t2r[sl], 1),
             h3(krs[sl][:, 64:96], 1), [R("projS", t % 3, 3), R("rt", t % 4)], R("t1r", sl), R("t2r", sl), [R("krs", sl)], 1)
        K.op(K.dve, lambda t=t, pS=pS: V.tensor_copy(out=va[:, t, :, 0:64], in_=h3(pS[:, 640:768], 2)),
             reads=[R("projS", t % 3, 3)], writes=[R("va", t)])
        K.op(K.act, lambda pS=pS, t=t: A.activation(out=junk2[:, 0:128], in_=pS[:, 768:896], func=AF.Square,
                                                    accum_out=st[:, 64 + 2 * t:65 + 2 * t]),
             reads=[R("projS", t % 3, 3)], writes=[R("junk2"), R("stk", t)])
        if own:
            K.op(K.act, lambda pS=pS, t=t: A.activation(out=junk2, in_=pS[:, 928:1120], func=AF.Square, scale=CQS,
                                                        accum_out=st[:, 65 + 2 * t:66 + 2 * t]),
                 reads=[R("projS", t % 3, 4)], writes=[R("junk2"), R("stk", t)])
        nsc = 2 if own else 1
        rc = 128 + 2 * (t % 16)
        rstd_cols(st[:, 64 + 2 * t:64 + 2 * t + nsc], st[:, rc:rc + nsc], 1.0 / 128, [R("stk", t)], [R("strk", t % 16)])
        K.op(K.dve, lambda pS=pS, sl=sl, rc=rc: V.tensor_scalar(out=ckvn[sl], in0=pS[:, 768:896], scalar1=st[:, rc:rc + 1],
                                                                scalar2=None, op0=ALU.mult),
             reads=[R("projS", t % 3, 3), R("strk", t % 16)], writes=[R("ckvn", sl)])
        if own:
            K.op(K.dve, lambda pS=pS, sl=sl, rc=rc: V.tensor_scalar(out=cqn[sl], in0=pS[:, 928:1120],
                                                                    scalar1=st[:, rc + 1:rc + 2], scalar2=None, op0=ALU.mult),
                 reads=[R("projS", t % 3, 4), R("strk", t % 16)], writes=[R("cqn", sl)])
        bk = 6 + sl
        b6 = bankb(bk)

        def trB(own=own, sl=sl, b6=b6):
            G.transpose(out=b6[0:64, 0:128], in_=qkr[sl][:, 512:576], identity=ident)
            G.transpose(out=b6[0:64, 128:256], in_=qkr[sl][:, 576:640], identity=ident)
            G.transpose(out=b6[:, 256:384], in_=ckvn[sl], identity=ident)
            ins = G.transpose(out=b6[0:96, 384:512], in_=krs[sl], identity=ident)
            if own:
                G.transpose(out=b6[0:96, 512:640], in_=cqn[sl][:, 0:96], identity=ident)
                ins = G.transpose(out=b6[0:96, 640:768], in_=cqn[sl][:, 96:192], identity=ident)
            return ins
        K.op(K.pe, trB, reads=[R("qkr", sl), R("ckvn", sl), R("krs", sl), R("ident")] + ([R("cqn", sl)] if own else []),
             writes=[R("ps", bk)])
        K.op(K.dve, lambda t=t, b6=b6: V.tensor_copy(out=kTa[:, :, t * 128:(t + 1) * 128],
                                                     in_=b6[0:64, 0:256].rearrange("p (a b) -> p a b", a=2)),
             reads=[R("ps", bk)], writes=[R("kTa", t)])
        K.op(K.dve, lambda t=t, b6=b6: V.tensor_copy(out=ckvnT[:, t * 128:(t + 1) * 128], in_=b6[:, 256:384]),
             reads=[R("ps", bk)], writes=[R("ckvnT", t)])
        K.op(K.dve, lambda t=t, b6=b6: V.tensor_copy(out=krT[64:96, t * 128:(t + 1) * 128], in_=b6[64:96, 384:512]),
             reads=[R("ps", bk)], writes=[R("krT", t)])
        if own:
            K.op(K.dve, lambda t=t, b6=b6: V.tensor_copy(out=cqnT[:, :, t * 128:(t + 1) * 128],
                                                         in_=b6[0:96, 512:768].rearrange("p (a b) -> p a b", a=2)),
                 reads=[R("ps", bk)], writes=[R("cqnT", t)])
            b5 = bankb(5)

            def trQ(sl=sl, b5=b5):
                ins = None
                for h in range(8):
                    ins = G.transpose(out=b5[0:64, h * 128:(h + 1) * 128], in_=qkr[sl][:, h * 64:(h + 1) * 64],
                                      identity=ident)
                return ins
            K.op(K.pe, trQ, reads=[R("qkr", sl), R("ident")], writes=[R("ps", 5)])
            K.op(K.act, lambda t=t, b5=b5: A.copy(out=qTa[:, :, t * 128:(t + 1) * 128],
                                                  in_=b5[0:64, :].rearrange("p (a b) -> p a b", a=8)),
                 reads=[R("ps", 5)], writes=[R("qTa", t)])

    gt1Ba = view(86 * KIB, [128, D], F32)
    wstg = [view(90 * KIB + i * 4 * KIB, [128, D], F32) for i in range(2)]
    wobf = [view(98 * KIB + i * 2 * KIB, [128, D], BF16) for i in range(2)]
    dWo = [K.dsem("wo%d" % i) for i in range(2)]
    dWos = [K.dsem("wos%d" % i) for i in range(2)]
    dGt = K.dsem("gt1a")
    wout_v = wout_d.rearrange("(kc p) n -> kc p n", p=128)
    wo_dv = wo_d.rearrange("(kc p) n -> kc p n", p=128)
    K.dma(K.sp, [(gt1Ba, modB(2))], dGt, reads=[R("mod_d")], writes=[R("gt1Ba")])

    def wo_load(kc):
        K.dma(K.sp, [(wstg[kc % 2], wout_v[kc])], dWo[kc % 2], writes=[R("wstg", kc % 2)])

    def wo_fold(kc):
        K.op(K.dve, lambda: V.scalar_tensor_tensor(out=wobf[kc % 2], in0=wstg[kc % 2], scalar=prm[:, 11 + kc:12 + kc],
                                                   in1=gt1Ba, op0=ALU.mult, op1=ALU.mult),
             reads=[R("wstg", kc % 2), R("prm"), R("gt1Ba")], writes=[R("wobf", kc % 2)])

    def wo_store(kc):
        K.dma(K.sp, [(wo_dv[kc], wobf[kc % 2])], dWos[kc % 2], reads=[R("wobf", kc % 2)], writes=[R("wo_d", kc)])

    zsrc = view(198 * KIB, [128, 2048], F32)
    K.op(K.pool, lambda: P.memset(zsrc, 0.0), writes=[R("zsrc")])
    dZf = K.dsem("zf")
    xs_z = xs_d.rearrange("(p r) d -> p r d", p=128)
    ys_z = ys_d.rearrange("(p r) d -> p r d", p=128)
    zjobs = [(xs_z[:, 4 * c:4 * c + 4, :], zsrc.bitcast(BF16).rearrange("p (a b) -> p a b", a=4)) for c in range(24)]
    zjobs += [(ys_z[:, 2 * c:2 * c + 2, :], zsrc.rearrange("p (a b) -> p a b", a=2)) for c in range(48)]

    for i in range(NT + 3):
        for zi in range(3 * i, min(3 * i + 3, len(zjobs))):
            K.dma(K.sp, [zjobs[zi]], dZf, reads=[R("zsrc")], writes=[R("zf", zi)])
        if i >= 6 and (i - 6) % 2 == 0 and (i - 6) // 2 < 8:
            wo_load((i - 6) // 2)
        if i >= 7 and (i - 7) % 2 == 0 and (i - 7) // 2 < 8:
            wo_fold((i - 7) // 2)
        if i >= 8 and (i - 8) % 2 == 0 and (i - 8) // 2 < 8:
            wo_store((i - 8) // 2)
        if i < NT:
            stA1(i)
        if 0 <= i - 1 < NT:
            stA2(i - 1)
        if 0 <= i - 2 < NT:
            stA2b(i - 2)
        if 0 <= i - 3 < NT:
            stA3(i - 3)
    K.barrier()

    mixTa = view(L4, [128, 4, NO * 128], BF16)
    mixTb = view(L4 + 16 * KIB, [128, 4, NO * 128], BF16)
    al = Alloc(PA, LIM)
    pT = [al([128, 3, 512], BF16) for i in range(2)]
    rden = [al([64, 512], F32) for i in range(2)]
    lnt = [al([32, 512], F32) for i in range(2)]
    msk = al([128, 4, 128], BF16)
    esrow = al([1, 8, 128], BF16)
    dMsk = K.dsem("msk")
    K.dma(K.pool, [(msk, msk_d.rearrange("p (a b) -> p a b", a=4))], dMsk, writes=[R("msk")])
    K.op(K.act, lambda: A.activation(out=es8, in_=prm[0:1, 20:28], func=AF.Exp), reads=[R("prm")], writes=[R("es8")])
    K.op(K.dve, lambda: V.tensor_copy(out=esrow, in_=bc(es8.unsqueeze(2), [1, 8, 128])), reads=[R("es8")],
         writes=[R("esrow")])

    def finish(ob, rd_sl, writers, use_act):
        oT = bank(ob)
        rd = rden[rd_sl]
        if use_act:
            K.op(K.act, lambda: A.activation(out=lnt[rd_sl], in_=oT[64:96, :], func=AF.Ln),
                 reads=[R("ps", ob)], writes=[R("lnt", rd_sl)])
            K.op(K.act, lambda: A.activation(out=rd[0:32, :], in_=lnt[rd_sl], func=AF.Exp, scale=-1.0),
                 reads=[R("lnt", rd_sl)], writes=[R("rden", rd_sl)])
            K.op(K.dve, lambda: V.tensor_copy(out=rd[32:64, :], in_=rd[0:32, :]),
                 reads=[R("rden", rd_sl)], writes=[R("rden", rd_sl)])
        else:
            K.op(K.dve, lambda: V.reciprocal(out=rd[0:32, :], in_=oT[64:96, :]),
                 reads=[R("ps", ob)], writes=[R("rden", rd_sl)])
            K.op(K.dve, lambda: V.tensor_copy(out=rd[32:64, :], in_=rd[0:32, :]),
                 reads=[R("rden", rd_sl)], writes=[R("rden", rd_sl)])
        for (out_ap, in_sl, wr) in writers:
            K.op(K.dve, lambda out_ap=out_ap, in_sl=in_sl: V.tensor_tensor(out=out_ap, in0=in_sl(oT[0:64, :]),
                                                                           in1=in_sl(rd), op=ALU.mult),
                 reads=[R("ps", ob), R("rden", rd_sl)], writes=[wr])

    def swa_ctx(it):
        n, kvh = divmod(it, 2)
        kts = [(31 if n == 0 else n - 1, 2 if n == 0 else 0), (n, None), (16 if n == NO - 1 else n + 1, 3 if n == NO - 1 else 1)]
        sl = it % 2
        return n, kvh, kts, sl, 3 * sl, 6 + sl

    def stW1(it):
        n, kvh, kts, sl, b0, ob = swa_ctx(it)

        def sc():
            ins = None
            for i, (kt, _) in enumerate(kts):
                ins = G.matmul(bank(b0 + i).rearrange("p (a b) -> p a b", a=4),
                               lhsT=kTa[:, kvh, kt * 128:(kt + 1) * 128],
                               rhs=qTa[:, kvh * 4:(kvh + 1) * 4, n * 128:(n + 1) * 128], start=True, stop=True)
            return ins
        K.op(K.pe, sc, reads=[], writes=[R("ps", b0 + i) for i in range(3)])
        K.op(K.act, lambda: A.activation(out=pT[sl].rearrange("p a b -> p (a b)"), in_=bank(b0, 3),
                                         func=AF.Exp, scale=0.125),
             reads=[R("ps", b0 + i) for i in range(3)], writes=[R("pT", sl)])
        for i, (kt, m) in enumerate(kts):
            if m is None:
                continue
            K.op(K.dve, lambda i=i, m=m: V.tensor_tensor(
                out=pT[sl][:, i, :].rearrange("p (a b) -> p a b", a=4),
                in0=pT[sl][:, i, :].rearrange("p (a b) -> p a b", a=4),
                in1=bc(msk[:, m, :].unsqueeze(1), [128, 4, 128]), op=ALU.mult),
                reads=[R("pT", sl), R("msk")], writes=[R("pT", sl)])

    def stW2(it):
        n, kvh, kts, sl, b0, ob = swa_ctx(it)

        def pv():
            for i, (kt, _) in enumerate(kts):
                G.matmul(bank(ob)[0:96, :], lhsT=va[:, kt, kvh, :], rhs=pT[sl][:, i, :],
                         start=(i == 0), stop=False)
            return G.matmul(bank(ob)[0:96, :], lhsT=vsink[0:1, :],
                            rhs=esrow[0:1, kvh * 4:(kvh + 1) * 4, :].rearrange("p a b -> p (a b)"),
                            start=False, stop=True)
        K.op(K.pe, pv, reads=[R("pT", sl), R("vsink"), R("esrow")], writes=[R("ps", ob)])
        writers = []
        for par in range(2):
            def in_sl(ap, par=par):
                return ap.rearrange("p (i two b) -> p i two b", two=2, b=128)[:, :, par, :]
            writers.append((mixTa[par * 64:par * 64 + 64, 2 * kvh:2 * kvh + 2, n * 128:(n + 1) * 128], in_sl,
                            R("mixTa", n, kvh, par)))
        finish(ob, sl, writers, True)

    for i in range(2 * NO + 1):
        if i < 2 * NO:
            stW1(i)
        if i >= 1:
            stW2(i - 1)
    K.barrier()

    qTb = view(L3, [96, 8, NO * 128], BF16)
    kTb = [view(L3 + 32 * KIB + i * 8 * KIB, [96, S], BF16) for i in range(2)]
    pTm = [view(L3 + 48 * KIB + i * 3072, [128, 3, 512], BF16) for i in range(4)]
    al = Alloc(PA, LIM)
    vb = al([128, NT, 8, 96], BF16)
    rden = [al([64, 512], F32) for i in range(2)]
    wuqs = al([96, 2, 768], F32)
    wuq = al([96, 2, 768], BF16)
    wukvs = al([128, 1024], F32)
    wukv = al([128, 8, 128], BF16)
    rt2 = [al([128, 64], F32) for i in range(2)]
    qbS = [al([128, 8, 96], F32) for i in range(2)]
    t1b = [al([128, 8, 32], F32) for i in range(2)]
    t2b = [al([128, 8, 32], F32) for i in range(2)]
    qbr = [al([128, 8, 96], BF16) for i in range(2)]

    dW2 = K.dsem("wB")
    K.dma(K.sp, [(wuqs, wuq_d.rearrange("(kc p) n -> p kc n", p=96)), (wukvs, wukv_d[:, :])], dW2,
          writes=[R("wuqs"), R("wukvs")])
    for kc in range(2):
        K.op(K.dve, lambda kc=kc: V.tensor_scalar(out=wuq[:, kc, :], in0=wuqs[:, kc, :], scalar1=prm[0:96, 8 + kc:9 + kc],
                                                  scalar2=None, op0=ALU.mult),
             reads=[R("wuqs"), R("prm")], writes=[R("wuq")])
    K.op(K.dve, lambda: V.tensor_scalar(out=wukv.rearrange("p a b -> p (a b)"), in0=wukvs, scalar1=prm[:, 10:11],
                                        scalar2=None, op0=ALU.mult),
         reads=[R("wukvs"), R("prm")], writes=[R("wukv")])
    K.op(K.pool, lambda: P.memset(vb[:, :, :, 64:96], 1.0), writes=RL("vb", range(NT)))
    for kt in range(NT):
        b = kt % 2
        K.op(K.pe, lambda kt=kt, b=b: G.matmul(bank(b).rearrange("p (a b) -> p a b", a=8),
                                               lhsT=ckvnT[:, kt * 128:(kt + 1) * 128], rhs=wukv[:, :, 64:128],
                                               start=True, stop=True),
             reads=[R("wukv")], writes=[R("ps", b)])
        if kt % 2:
            K.op(K.dve, lambda kt=kt, b=b: V.tensor_copy(out=vb[:, kt, :, 0:64], in_=bank(b).rearrange("p (a b) -> p a b", a=8)),
                 reads=[R("ps", b)], writes=[R("vb", kt)])
        else:
            K.op(K.act, lambda kt=kt, b=b: A.copy(out=vb[:, kt, :, 0:64], in_=bank(b).rearrange("p (a b) -> p a b", a=8)),
                 reads=[R("ps", b)], writes=[R("vb", kt)])
    dX2 = [K.dsem("r%d" % i) for i in range(2)]
    def stQ1(t):
        sl = t % 2
        K.dma(K.sp, [(rt2[sl], rope_t[t][:, 128:192])], dX2[sl], writes=[R("rt2", sl)])
        bq = 2 + 2 * sl

        def qp(t=t, bq=bq):
            ins = None
            for (b, c0, n) in [(bq, 0, 512), (bq + 1, 512, 256)]:
                for kc in range(2):
                    ins = G.matmul(bank(b)[:, 0:n], lhsT=cqnT[:, kc, t * 128:(t + 1) * 128], rhs=wuq[:, kc, c0:c0 + n],
                                   start=(kc == 0), stop=(kc == 1))
            return ins
        K.op(K.pe, qp, reads=[R("wuq")], writes=[R("ps", bq), R("ps", bq + 1)])
        qf = qbS[sl].rearrange("p a b -> p (a b)")
        K.op(K.act, lambda qf=qf, bq=bq: A.copy(out=qf[:, 0:512], in_=bank(bq)), reads=[R("ps", bq)], writes=[R("qbS", sl)])
        K.op(K.act, lambda qf=qf, bq=bq: A.copy(out=qf[:, 512:768], in_=bank(bq + 1)[:, 0:256]), reads=[R("ps", bq + 1)],
             writes=[R("qbS", sl)])

    def stQ2(t):
        sl = t % 2
        K.op(K.pool, lambda sl=sl: P.tensor_copy(out=qbr[sl][:, :, 0:64], in_=qbS[sl][:, :, 0:64]), reads=[R("qbS", sl)],
             writes=[R("qbr", sl)])
        rope(qbS[sl][:, :, 64:96], rt2[sl][:, 0:32], rt2[sl][:, 32:64], 16, t1b[sl], t2b[sl], qbr[sl][:, :, 64:96],
             [R("qbS", sl), R("rt2", sl)], R("t1b", sl), R("t2b", sl), [R("qbr", sl)], 8)
        b4 = bankb(6 + sl)

        def trq(sl=sl, b4=b4):
            ins = None
            for h in range(8):
                ins = G.transpose(out=b4[0:96, h * 128:(h + 1) * 128], in_=qbr[sl][:, h, :], identity=ident)
            return ins
        K.op(K.pe, trq, reads=[R("qbr", sl), R("ident")], writes=[R("ps", 6 + sl)])
        K.op(K.dve, lambda t=t, b4=b4: V.tensor_copy(out=qTb[:, :, t * 128:(t + 1) * 128],
                                                     in_=b4[0:96, :].rearrange("p (a b) -> p a b", a=8)),
             reads=[R("ps", 6 + sl)], writes=[R("qTb", t)])

    for i in range(NO + 1):
        if i < NO:
            stQ1(i)
        if i >= 1:
            stQ2(i - 1)

    ktg = [list(range(k, min(k + 3, NT))) for k in range(0, NT, 3)]
    cgs = [[0, 1, 2], [3, 4, 5], [6, 7]]

    def setup_steps(h):
        return [("setup", h, cg) for cg in cgs]

    dWbf = K.dsem("wbf")
    K.dma(K.pool, [(wbf_d[c * 512:(c + 1) * 512, m * 2048:(m + 1) * 2048], src[c * 512:(c + 1) * 512, :])
                   for c in range(8) for m, src in enumerate((wg_d, wu_d, wd_d))], dWbf, writes=[R("wbf")])
    steps = setup_steps(0)
    for h in range(8):
        for qg in range(4):
            steps += [("attn", h, qg, gi) for gi in range(len(ktg))]
            if qg == 0 and h < 7:
                steps += setup_steps(h + 1)
    SK = 2
    sc_scale = 96.0 ** -0.5
    for i in range(len(steps) + SK):
        if i < len(steps):
            stp = steps[i]
            b0 = 3 * (i % 2)
            if stp[0] == "setup":
                _, h, cg = stp
                sl = h % 2
                if cg[0] == 0:
                    K.op(K.pool, lambda sl=sl: P.tensor_copy(out=kTb[sl][64:96, :], in_=krT[64:96, :]),
                         writes=[R("kTbr", sl)])

                def su(h=h, cg=cg, b0=b0):
                    ins = None
                    for j, c in enumerate(cg):
                        ins = G.matmul(bank(b0 + j)[0:64, :], lhsT=wukv[:, h, 0:64], rhs=ckvnT[:, c * 512:(c + 1) * 512],
                                       start=True, stop=True)
                    return ins
                K.op(K.pe, su, reads=[R("wukv")], writes=[R("ps", b0 + j) for j in range(3)])
                K.op(K.dve, lambda sl=sl, cg=cg, b0=b0: V.tensor_copy(
                    out=kTb[sl][0:64, cg[0] * 512:(cg[-1] + 1) * 512], in_=bank(b0, len(cg))[0:64, :]),
                    reads=[R("ps", b0 + j) for j in range(3)], writes=[R("kTb", sl)])
            else:
                _, h, qg, gi = stp
                kl = ktg[gi]

                def sc(h=h, qg=qg, kl=kl, b0=b0):
                    ins = None
                    for j, kt in enumerate(kl):
                        ins = G.matmul(bank(b0 + j), lhsT=kTb[h % 2][:, kt * 128:(kt + 1) * 128],
                                       rhs=qTb[:, h, qg * 512:(qg + 1) * 512], start=True, stop=True)
                    return ins
                K.op(K.pe, sc, reads=[R("kTb", h % 2), R("kTbr", h % 2)] + RL("qTb", range(qg * 4, qg * 4 + 4)),
                     writes=[R("ps", b0 + j) for j in range(3)])
                s4 = i % 4
                nk = len(kl)
                K.op(K.act, lambda s4=s4, b0=b0, nk=nk: A.activation(out=pTm[s4].rearrange("p a b -> p (a b)")[:, 0:nk * 512],
                                                                     in_=bank(b0, nk), func=AF.Exp, scale=sc_scale),
                     reads=[R("ps", b0 + j) for j in range(3)], writes=[R("pTm", s4)])
        if i >= SK and steps[i - SK][0] == "attn":
            _, h, qg, gi = steps[i - SK]
            kl = ktg[gi]
            s4 = (i - SK) % 4
            gsl = (h * 4 + qg) % 2
            ob = 6 + gsl

            def pv(h=h, kl=kl, s4=s4, ob=ob):
                ins = None
                for j, kt in enumerate(kl):
                    ins = G.matmul(bank(ob)[0:96, :], lhsT=vb[:, kt, h, :], rhs=pTm[s4][:, j, :],
                                   start=(kt == 0), stop=(kt == NT - 1))
                return ins
            K.op(K.pe, pv, reads=[R("pTm", s4)] + [R("vb", kt) for kt in kl], writes=[R("ps", ob)])
            if kl[-1] == NT - 1:
                finish(ob, gsl, [(mixTb[(h % 2) * 64:(h % 2) * 64 + 64, h // 2, qg * 512:(qg + 1) * 512],
                                 (lambda ap: ap), R("mixTb", h, qg))], False)
    K.barrier()

    PC = 120 * KIB
    acc = view(2 * KIB, [128, NO, D], F32)
    posu = view(66 * KIB, [128, 2, NO], U32)
    w12 = view(66 * KIB + 128, [128, 2, NO], F32)
    widx = view(66 * KIB + 256, [128, 48], U32)
    sidx = view(66 * KIB + 448, [128, 48], U32)
    wo = view(68 * KIB, [128, 8, D], BF16)
    h2tok = view(PC, [128, NO, D], BF16)
    al = Alloc(PC + 32 * KIB, LIM)
    LGa = al([128, NO, 36], F32)
    r_m4 = al([128, NO], F32)
    r_d4 = al([128, NO, 4], F32)
    r_e4 = al([128, NO, 4], F32)
    r_s4 = al([128, NO], F32)
    r_oh = al([128, NO, 4], F32)
    r_t32 = al([128, NO, 32], F32)
    r_el = al([128, NO, 8], F32)
    r_el2 = al([128, NO, 8], F32)
    r_v1 = al([128, NO], F32)
    r_d8 = al([128, NO, 8], F32)
    r_eq = al([128, NO, 8], F32)
    r_v2 = al([128, NO], F32)
    r_mk = al([128, NO, 8], F32)
    r_ex = al([128, NO, 8], F32)
    r_cw = al([128, NO, 8], F32)
    r_den = al([128, NO], F32)
    SORT0 = al.o
    xt = [al([128, D], F32) for i in range(2)]
    tmpB = [al([128, D], F32) for i in range(2)]
    a2B = al([128, D], F32)
    sh2B = al([128, D], F32)
    h2Th = [al([128, 8, 128], BF16) for i in range(2)]
    lo_t = [al([128, D], BF16) for i in range(2)]
    h2Tl = [al([128, 8, 128], BF16) for i in range(2)]
    wrs = al([128, 8, 36], F32)
    wrh = al([128, 8, 36], BF16)
    wrl = al([128, 8, 36], BF16)
    rab = al([128, 32], F32)
    sqs = [al([128, 8, 128], BF16) for i in range(2)]
    junk = al([128, D], BF16)
    gt1B = tmpB[0]
    g2B = tmpB[1]

    K.op(K.dve, lambda: V.memset(st, 0.0), writes=[R("st")])
    dW3 = K.dsem("wC")
    K.dma(K.sp, [(a2B, modB(4)), (sh2B, modB(3)), (gt1B, modB(2)), (g2B, gB_d[:, D:2 * D]),
                 (wrs, wr_d.rearrange("(kc p) n -> p kc n", p=128))], dW3,
          writes=[R("a2B"), R("sh2B"), R("tmpB", 0), R("tmpB", 1), R("wrs")])
    K.op(K.dve, lambda: V.scalar_tensor_tensor(out=a2B, in0=a2B, scalar=1.0, in1=g2B, op0=ALU.add, op1=ALU.mult),
         reads=[R("a2B"), R("tmpB", 1)], writes=[R("a2B"), R("tmpB", 1)])
    K.op(K.act, lambda: A.copy(out=wrh, in_=wrs), reads=[R("wrs")], writes=[R("wrh")])
    K.op(K.pool, lambda: P.tensor_tensor(out=wrl, in0=wrs, in1=wrh, op=ALU.subtract), reads=[R("wrs"), R("wrh")],
         writes=[R("wrl")])
    dWoL = K.dsem("woL")
    K.dma(K.sp, [(wo, wo_d.rearrange("(kc p) n -> p kc n", p=128))], dWoL, writes=[R("wo")])
    for t in range(NO):
        sl = t % 2
        if t % 2:
            K.op(K.dve, lambda t=t, sl=sl: V.tensor_tensor(out=sqs[sl][:, 0:4, :], in0=mixTa[:, :, t * 128:(t + 1) * 128],
                                                           in1=mixTa[:, :, t * 128:(t + 1) * 128], op=ALU.mult),
                 writes=[R("sqs", sl)])
        else:
            K.op(K.pool, lambda t=t, sl=sl: P.tensor_tensor(out=sqs[sl][:, 0:4, :], in0=mixTa[:, :, t * 128:(t + 1) * 128],
                                                            in1=mixTa[:, :, t * 128:(t + 1) * 128], op=ALU.mult),
                 writes=[R("sqs", sl)])
        K.op(K.act, lambda t=t, sl=sl: A.activation(out=sqs[sl][:, 4:8, :], in_=mixTb[:, :, t * 128:(t + 1) * 128], func=AF.Square),
             writes=[R("sqsb", sl)])

        def ssq(t=t, sl=sl):
            ins = None
            for g2 in range(2):
                for j in range(4):
                    ins = G.matmul(bank(7)[:, 2 * t + g2:2 * t + g2 + 1], lhsT=sqs[sl][:, 4 * g2 + j, :], rhs=ones_b[:, 0:1],
                                   start=(j == 0), stop=(j == 3))
            return ins
        K.op(K.pe, ssq, reads=[R("sqs", sl), R("sqsb", sl), R("ones")], writes=[R("ps", 7)])
    K.op(K.act, lambda: A.activation(out=rab, in_=bank(7)[:, 0:32], func=AF.Ln, scale=1.0 / 512, bias=eps_t[:, 0:1]),
         reads=[R("ps", 7), R("eps")], writes=[R("rab")])
    K.op(K.act, lambda: A.activation(out=rab, in_=rab, func=AF.Exp, scale=-0.5), reads=[R("rab")], writes=[R("rab")])

    brB = prm[:, 28:64]
    def stC1(t):
        sl = t % 2
        K.dma(K.sp, [(xt[sl], x_t[t])], dX[sl], writes=[R("xt", sl)])

        def op_(t=t):
            ins = None
            for (mt, b0, k0) in [(mixTa, 0, 0), (mixTb, 2, 4)]:
                for dh in range(2):
                    for kc in range(4):
                        ins = G.matmul(bank(b0 + dh), lhsT=mt[:, kc, t * 128:(t + 1) * 128],
                                       rhs=wo[:, k0 + kc, dh * 512:(dh + 1) * 512], start=(kc == 0), stop=(kc == 3))
            return ins
        K.op(K.pe, op_, reads=[R("wo")], writes=[R("ps", b) for b in range(4)])
        K.op(K.dve, lambda t=t, sl=sl: V.scalar_tensor_tensor(out=acc[:, t, :], in0=bank(0, 2), scalar=rab[:, 2 * t:2 * t + 1],
                                                              in1=xt[sl], op0=ALU.mult, op1=ALU.add),
             reads=[R("ps", 0), R("ps", 1), R("rab"), R("xt", sl)], writes=[R("acc", t)])
        K.op(K.dve, lambda t=t: V.scalar_tensor_tensor(out=acc[:, t, :], in0=bank(2, 2), scalar=rab[:, 2 * t + 1:2 * t + 2],
                                                       in1=acc[:, t, :], op0=ALU.mult, op1=ALU.add),
             reads=[R("ps", 2), R("ps", 3), R("rab"), R("acc", t)], writes=[R("acc", t)])
        K.op(K.act, lambda t=t: A.activation(out=junk, in_=acc[:, t, :], func=AF.Square, accum_out=st[:, t:t + 1]),
             reads=[R("acc", t), R("st")], writes=[R("junk"), R("stx", t)])
        rstd_cols(st[:, t:t + 1], st[:, 32 + t:33 + t], 1.0 / D, [R("stx", t)], [R("strx", t)])

    def stC2(t):
        sl = t % 2
        K.op(K.act, lambda t=t, sl=sl: A.activation(out=tmpB[sl], in_=acc[:, t, :], func=AF.Copy, scale=st[:, 32 + t:33 + t]),
             reads=[R("acc", t), R("strx", t)], writes=[R("tmpB", sl)])
        K.op(K.dve, lambda sl=sl: V.tensor_tensor(out=tmpB[sl], in0=tmpB[sl], in1=a2B, op=ALU.mult),
             reads=[R("tmpB", sl), R("a2B")], writes=[R("tmpB", sl)])
        K.op(K.dve, lambda sl=sl: V.tensor_tensor(out=tmpB[sl], in0=tmpB[sl], in1=sh2B, op=ALU.add),
             reads=[R("tmpB", sl), R("sh2B")], writes=[R("tmpB", sl)])
        K.op(K.act, lambda sl=sl, t=t: A.copy(out=h2tok[:, t, :], in_=tmpB[sl]), reads=[R("tmpB", sl)], writes=[R("h2tok", t)])
        K.op(K.pool, lambda sl=sl, t=t: P.tensor_tensor(out=lo_t[sl], in0=tmpB[sl], in1=h2tok[:, t, :], op=ALU.subtract),
             reads=[R("tmpB", sl), R("h2tok", t)], writes=[R("lo_t", sl)])

    def stC2b(t):
        sl = t % 2

        def tr2(sl=sl, t=t):
            ins = None
            for kc in range(8):
                G.transpose(out=bankb(4)[:, kc * 128:(kc + 1) * 128], in_=h2tok[:, t, kc * 128:(kc + 1) * 128], identity=ident)
                ins = G.transpose(out=bankb(5)[:, kc * 128:(kc + 1) * 128], in_=lo_t[sl][:, kc * 128:(kc + 1) * 128],
                                  identity=ident)
            return ins
        K.op(K.pe, tr2, reads=[R("h2tok", t), R("lo_t", sl), R("ident")], writes=[R("ps", 4), R("ps", 5)])
        K.op(K.act, lambda sl=sl: A.copy(out=h2Th[sl], in_=bankb(4).rearrange("p (a b) -> p a b", a=8)),
             reads=[R("ps", 4)], writes=[R("h2Th", sl)])
        K.op(K.dve, lambda sl=sl: V.tensor_copy(out=h2Tl[sl], in_=bankb(5).rearrange("p (a b) -> p a b", a=8)),
             reads=[R("ps", 5)], writes=[R("h2Tl", sl)])

    def stC3(t):
        sl = t % 2

        def lg(t=t, sl=sl):
            ins = None
            combos = [(h2Th[sl], wrh), (h2Tl[sl], wrh), (h2Th[sl], wrl)]
            for ci, (a_, w_) in enumerate(combos):
                for kc in range(8):
                    ins = G.matmul(bank(6)[:, 0:36], lhsT=a_[:, kc, :], rhs=w_[:, kc, :],
                                   start=(ci == 0 and kc == 0), stop=(ci == 2 and kc == 7))
            return ins
        K.op(K.pe, lg, reads=[R("h2Th", sl), R("h2Tl", sl), R("wrh"), R("wrl")], writes=[R("ps", 6)])
        K.op(K.dve, lambda t=t: V.tensor_tensor(out=LGa[:, t, :], in0=bank(6)[:, 0:36], in1=brB, op=ALU.add),
             reads=[R("ps", 6), R("prm")], writes=[R("rw")])

    for i in range(NO + 3):
        if i < NO:
            stC1(i)
        if 0 <= i - 1 < NO:
            stC2(i - 1)
        if 0 <= i - 2 < NO:
            stC2b(i - 2)
        if 0 <= i - 3 < NO:
            stC3(i - 3)
    RW = [R("rw")]

    def rop(eng, fn):
        K.op(eng, fn, reads=RW, writes=RW)
    T = NO
    Lg = LGa[:, :, 0:4]
    rop(K.dve, lambda: V.tensor_reduce(out=r_m4, in_=Lg, axis=AX.X, op=ALU.max))
    rop(K.dve, lambda: V.tensor_tensor(out=r_d4, in0=Lg, in1=bc(r_m4.unsqueeze(2), [128, T, 4]), op=ALU.subtract))
    rop(K.act, lambda: A.activation(out=r_e4, in_=r_d4, func=AF.Exp))
    rop(K.dve, lambda: V.tensor_reduce(out=r_s4, in_=r_e4, axis=AX.X, op=ALU.add))
    rop(K.dve, lambda: V.tensor_scalar(out=r_oh, in0=r_d4, scalar1=0.0, scalar2=None, op0=ALU.is_ge))
    for g in range(4):
        rop(K.dve, lambda g=g: V.tensor_tensor(out=r_t32[:, :, g * 8:(g + 1) * 8], in0=LGa[:, :, 4 + g * 8:12 + g * 8],
                                               in1=bc(r_oh[:, :, g:g + 1], [128, T, 8]), op=ALU.mult))
    rop(K.dve, lambda: V.tensor_tensor(out=r_el, in0=r_t32[:, :, 0:8], in1=r_t32[:, :, 8:16], op=ALU.add))
    rop(K.dve, lambda: V.tensor_tensor(out=r_el2, in0=r_t32[:, :, 16:24], in1=r_t32[:, :, 24:32], op=ALU.add))
    rop(K.dve, lambda: V.tensor_tensor(out=r_el, in0=r_el, in1=r_el2, op=ALU.add))
    rop(K.dve, lambda: V.tensor_reduce(out=r_v1, in_=r_el, axis=AX.X, op=ALU.max))
    rop(K.dve, lambda: V.tensor_tensor(out=r_d8, in0=r_el, in1=bc(r_v1.unsqueeze(2), [128, T, 8]), op=ALU.subtract))
    rop(K.dve, lambda: V.tensor_scalar(out=r_eq, in0=r_d8, scalar1=0.0, scalar2=None, op0=ALU.is_ge))
    rop(K.dve, lambda: V.scalar_tensor_tensor(out=r_el2, in0=r_eq, scalar=-1e30, in1=r_d8, op0=ALU.mult, op1=ALU.add))
    rop(K.dve, lambda: V.tensor_reduce(out=r_v2, in_=r_el2, axis=AX.X, op=ALU.max))
    rop(K.dve, lambda: V.tensor_tensor(out=r_mk, in0=r_d8, in1=bc(r_v2.unsqueeze(2), [128, T, 8]), op=ALU.is_ge))
    rop(K.act, lambda: A.activation(out=r_ex, in_=r_d8, func=AF.Exp))
    rop(K.dve, lambda: V.tensor_tensor(out=r_cw, in0=r_mk, in1=r_ex, op=ALU.mult))
    rop(K.dve, lambda: V.tensor_reduce(out=r_den, in_=r_cw, axis=AX.X, op=ALU.add))
    rop(K.dve, lambda: V.tensor_tensor(out=r_den, in0=r_den, in1=r_s4, op=ALU.mult))
    rop(K.dve, lambda: V.reciprocal(out=r_den, in_=r_den))
    rop(K.dve, lambda: V.tensor_tensor(out=r_cw, in0=r_cw, in1=bc(r_den.unsqueeze(2), [128, T, 8]), op=ALU.mult))
    K.barrier()

    al = Alloc(SORT0, LIM)
    m2 = al([128, NO, 8], F32)
    A1 = al([128, NO, NE], F32)
    A2 = al([128, NO, NE], F32)
    Mb = al([128, NO, NE], BF16)
    utri = al([128, 128], BF16)
    onesq = al([128, 128], BF16)
    cnt = al([128, NE], F32)
    thr = al([128, 8], F32)
    cmp8 = al([128, NE, 8], F32)
    Tt = al([128, NE], F32)
    sca = al([128, NE], F32)
    scb = al([128, NE], F32)
    off256 = al([128, NE], F32)
    offT = al([128, NE], F32)
    posf = al([128, NO, NE], F32)
    ptmp = al([128, NO, NE], F32)
    posk = al([128, 2, NO], F32)
    jv = al([128, 48], F32)
    ev = al([128, NE], F32)
    pidx = al([128, 1], F32)
    indg = al([128, NE, 48], F32)
    indl = al([128, NE, 48], F32)
    eidf = al([128, 48], F32)
    anyf = al([128, 48], F32)
    real1 = al([128, 48], F32)
    jv256 = al([128, 48], F32)
    sidf = al([128, 48], F32)
    p2 = al([128, 1], F32)
    SR = [R("sort")]

    def sop(eng, fn, extra_r=(), extra_w=()):
        K.op(eng, fn, reads=SR + list(extra_r), writes=SR + list(extra_w))
    T = NO
    sop(K.pool, lambda: P.memset(utri, 1.0))
    sop(K.pool, lambda: P.affine_select(out=utri, in_=utri, pattern=[[1, 128]], compare_op=ALU.is_ge, fill=0.0, base=-1,
                                        channel_multiplier=-1))
    sop(K.pool, lambda: P.memset(onesq, 1.0))
    for m in range(8):
        sop(K.dve, lambda m=m: V.memset(thr[:, m:m + 1], 256.0 * m))
    sop(K.pool, lambda: P.iota(jv, pattern=[[1, 48]], base=0, channel_multiplier=0, allow_small_or_imprecise_dtypes=True))
    sop(K.pool, lambda: P.iota(ev, pattern=[[1, NE]], base=0, channel_multiplier=0, allow_small_or_imprecise_dtypes=True))
    sop(K.pool, lambda: P.iota(pidx, pattern=[[0, 1]], base=0, channel_multiplier=1, allow_small_or_imprecise_dtypes=True))
    sop(K.dve, lambda: V.tensor_tensor(out=m2, in0=r_mk, in1=r_eq, op=ALU.subtract), extra_r=RW)
    for g in range(4):
        sop(K.dve, lambda g=g: V.tensor_tensor(out=A1[:, :, g * 8:(g + 1) * 8], in0=r_eq,
                                               in1=bc(r_oh[:, :, g:g + 1], [128, T, 8]), op=ALU.mult), extra_r=RW)
        sop(K.dve, lambda g=g: V.tensor_tensor(out=A2[:, :, g * 8:(g + 1) * 8], in0=m2,
                                               in1=bc(r_oh[:, :, g:g + 1], [128, T, 8]), op=ALU.mult), extra_r=RW)
    sop(K.dve, lambda: V.tensor_tensor(out=r_ex, in0=r_cw, in1=r_eq, op=ALU.mult), extra_r=RW, extra_w=RW)
    sop(K.dve, lambda: V.tensor_reduce(out=w12[:, 0, :], in_=r_ex, axis=AX.X, op=ALU.add), extra_r=RW, extra_w=[R("w12")])
    sop(K.dve, lambda: V.tensor_tensor(out=r_ex, in0=r_cw, in1=m2, op=ALU.mult), extra_r=RW, extra_w=RW)
    sop(K.dve, lambda: V.tensor_reduce(out=w12[:, 1, :], in_=r_ex, axis=AX.X, op=ALU.add), extra_r=RW, extra_w=[R("w12")])
    sop(K.dve, lambda: V.tensor_tensor(out=Mb, in0=A1, in1=A2, op=ALU.add))
    def rk():
        ins = None
        for i in range(NO):
            for i2 in range(i):
                G.matmul(bank(0)[:, i * 32:(i + 1) * 32], lhsT=onesq, rhs=Mb[:, i2, :], start=(i2 == 0), stop=False)
            ins = G.matmul(bank(0)[:, i * 32:(i + 1) * 32], lhsT=utri, rhs=Mb[:, i, :], start=(i == 0), stop=True)
        for i in range(NO):
            ins = G.matmul(bank(1)[:, 0:32], lhsT=onesq, rhs=Mb[:, i, :], start=(i == 0), stop=(i == NO - 1))
        return ins
    K.op(K.pe, rk, reads=SR, writes=[R("ps", 0), R("ps", 1)])
    sop(K.dve, lambda: V.tensor_copy(out=cnt, in_=bank(1)[:, 0:32]), extra_r=[R("ps", 1)])
    sop(K.dve, lambda: V.tensor_tensor(out=cmp8, in0=bc(cnt.unsqueeze(2), [128, NE, 8]), in1=bc(thr.unsqueeze(1), [128, NE, 8]),
                                       op=ALU.is_gt))
    sop(K.dve, lambda: V.tensor_reduce(out=Tt, in_=cmp8, axis=AX.X, op=ALU.add))
    sop(K.dve, lambda: V.tensor_copy(out=sca, in_=Tt))
    cur, oth = sca, scb
    for sft in (1, 2, 4, 8, 16):
        sop(K.dve, lambda cur=cur, oth=oth: V.tensor_copy(out=oth, in_=cur))
        sop(K.dve, lambda cur=cur, oth=oth, sft=sft: V.tensor_tensor(out=oth[:, sft:NE], in0=cur[:, sft:NE], in1=cur[:, 0:NE - sft],
                                                                      op=ALU.add))
        cur, oth = oth, cur
    incl = cur
    sop(K.dve, lambda: V.tensor_tensor(out=offT, in0=incl, in1=Tt, op=ALU.subtract))
    sop(K.dve, lambda: V.tensor_scalar(out=off256, in0=offT, scalar1=256.0, scalar2=None, op0=ALU.mult))
    sop(K.dve, lambda: V.tensor_tensor(out=posf, in0=bank(0).rearrange("p (a b) -> p a b", a=NO),
                                       in1=bc(off256.unsqueeze(1), [128, T, NE]), op=ALU.add), extra_r=[R("ps", 0)])
    for k, Ak in enumerate((A1, A2)):
        sop(K.dve, lambda Ak=Ak: V.tensor_tensor(out=ptmp, in0=posf, in1=Ak, op=ALU.mult))
        sop(K.dve, lambda k=k: V.tensor_reduce(out=posk[:, k, :], in_=ptmp, axis=AX.X, op=ALU.add))
    sop(K.dve, lambda: V.tensor_copy(out=posu, in_=posk), extra_w=[R("posu")])
    dSc = K.dsem("scat")
    for i in range(NO):
        for k in range(2):
            K._waits(K.pool, [R("posu"), R("h2tok", i)], [R("xs_d")])
            P.indirect_dma_start(out=xs_d[:, :], out_offset=bass.IndirectOffsetOnAxis(posu[:, k, i:i + 1], 0),
                                 in_=h2tok[:, i, :], in_offset=None).then_inc(dSc.sem, 16)
            dSc.cnt += 16
    K._commit((dSc.sem, dSc.cnt), [R("posu")] + RL("h2tok", range(NO)), [R("xs_d")])
    sop(K.dve, lambda: V.tensor_tensor(out=indg, in0=bc(jv.unsqueeze(1), [128, NE, 48]), in1=bc(offT.unsqueeze(2), [128, NE, 48]),
                                       op=ALU.is_ge))
    sop(K.dve, lambda: V.tensor_tensor(out=indl, in0=bc(jv.unsqueeze(1), [128, NE, 48]), in1=bc(incl.unsqueeze(2), [128, NE, 48]),
                                       op=ALU.is_lt))
    sop(K.dve, lambda: V.tensor_tensor(out=indg, in0=indg, in1=indl, op=ALU.mult))
    sop(K.dve, lambda: V.tensor_reduce(out=anyf, in_=indg.rearrange("p e j -> p j e"), axis=AX.X, op=ALU.add))
    sop(K.dve, lambda: V.tensor_tensor(out=indl, in0=indg, in1=bc(ev.unsqueeze(2), [128, NE, 48]), op=ALU.mult))
    sop(K.dve, lambda: V.tensor_reduce(out=eidf, in_=indl.rearrange("p e j -> p j e"), axis=AX.X, op=ALU.add))
    sop(K.dve, lambda: V.tensor_tensor(out=sca, in0=cnt, in1=off256, op=ALU.add))
    sop(K.dve, lambda: V.tensor_scalar(out=jv256, in0=jv, scalar1=256.0, scalar2=None, op0=ALU.mult))
    sop(K.dve, lambda: V.tensor_tensor(out=indl, in0=bc(sca.unsqueeze(2), [128, NE, 48]), in1=bc(jv256.unsqueeze(1), [128, NE, 48]),
                                       op=ALU.subtract))
    sop(K.dve, lambda: V.tensor_tensor(out=indl, in0=indl, in1=indg, op=ALU.mult))
    sop(K.dve, lambda: V.tensor_reduce(out=real1, in_=indl.rearrange("p e j -> p j e"), axis=AX.X, op=ALU.add))
    sop(K.dve, lambda: V.tensor_scalar(out=p2, in0=pidx, scalar1=2.0, scalar2=None, op0=ALU.mult))
    sop(K.dve, lambda: V.tensor_scalar(out=sidf, in0=real1, scalar1=p2[:, 0:1], scalar2=None, op0=ALU.is_gt))
    sop(K.dve, lambda: V.tensor_scalar(out=sidf, in0=sidf, scalar1=-1.0e6, scalar2=1.0e6, op0=ALU.mult, op1=ALU.add))
    sop(K.dve, lambda: V.tensor_scalar(out=jv256, in0=jv, scalar1=128.0, scalar2=pidx[:, 0:1], op0=ALU.mult, op1=ALU.add))
    sop(K.dve, lambda: V.tensor_tensor(out=sidf, in0=sidf, in1=jv256, op=ALU.add))
    sop(K.dve, lambda: V.tensor_copy(out=sidx, in_=sidf), extra_w=[R("sidx")])
    sop(K.dve, lambda: V.tensor_scalar(out=anyf, in0=anyf, scalar1=-32.0, scalar2=32.0, op0=ALU.mult, op1=ALU.add))
    sop(K.dve, lambda: V.tensor_tensor(out=eidf, in0=eidf, in1=anyf, op=ALU.add))
    sop(K.dve, lambda: V.tensor_scalar(out=eidf, in0=eidf, scalar1=128.0, scalar2=pidx[:, 0:1], op0=ALU.mult, op1=ALU.add))
    sop(K.dve, lambda: V.tensor_copy(out=widx, in_=eidf), extra_w=[R("widx")])
    alw = Alloc(68 * KIB, 116 * KIB)
    wall = [alw([128, 3 * 2048], BF16) for i in range(4)]
    wgb = [w[:, 0:2048].rearrange("p (a b) -> p a b", a=8) for w in wall]
    wub = [w[:, 2048:4096].rearrange("p (a b) -> p a b", a=8) for w in wall]
    wdb = [w[:, 4096:6144].rearrange("p (a b) -> p a b", a=2) for w in wall]
    dG = [K.dsem("g%d" % i) for i in range(4)]
    breg = P.to_reg(NE * 128 - 1)
    for i in range(4):
        K.op(K.act, lambda i=i: A.memzero(wall[i]), writes=[R("wall", i)])

    def gather_w(q, j):
        s4 = q % 4
        K._waits(K.pool, [R("widx"), R("wbf")], [R("wall", s4)])
        P.indirect_dma_start(out=wall[s4], out_offset=None, in_=wbf_d[:, :],
                             in_offset=bass.IndirectOffsetOnAxis(widx[:, j:j + 1], 0),
                             bounds_check=breg, oob_is_err=False).then_inc(dG[s4].sem, 16)
        dG[s4].cnt += 16
        K._commit((dG[s4].sem, dG[s4].cnt), [R("widx"), R("wbf")], [R("wall", s4)])
    order = []
    for g3 in range(16):
        order += [2 * g3, 2 * g3 + 1, 32 + g3]
    assert sorted(order) == list(range(NTL))

    for q in range(4):
        gather_w(q, order[q])
    K.barrier()

    al = Alloc(116 * KIB, LIM)
    xs = [al([128, 2, D], BF16) for i in range(4)]
    xT = [al([128, 8, 256], BF16) for i in range(2)]
    ssb = [al([128, 512], F32) for i in range(2)]
    hid = [al([128, 2, 256], BF16) for i in range(2)]
    ysb = [al([128, 2, D], F32) for i in range(2)]
    gt2B = al([128, D], F32)
    gfB = al([128, D], F32)
    yg = [al([128, 2, D], F32) for i in range(2)]
    yt = [al([128, D], F32) for i in range(2)]
    junk = al([128, D], BF16)

    dXs = [K.dsem("xs%d" % i) for i in range(4)]
    dYs = [K.dsem("ys%d" % i) for i in range(2)]
    dF = K.dsem("fin")
    K.op(K.dve, lambda: V.memset(st, 0.0), writes=[R("st")])
    K.dma(K.sp, [(gt2B, modB(5)), (gfB, gB_d[:, 2 * D:3 * D])], dF, writes=[R("gt2B"), R("gfB")])
    sreg = P.to_reg(NTL * 128 - 1)
    for i in range(4):
        K.op(K.act, lambda i=i: A.memzero(xs[i].rearrange("p a b -> p (a b)")), writes=[R("xs", i)])
    xs_p = xs_d.rearrange("(n r) d -> n (r d)", r=2)
    ys_p = ys_d.rearrange("(n r) d -> n (r d)", r=2)

    def stM1(q, j):
        K._waits(K.pool, [R("sidx"), R("xs_d")], [R("xs", q % 4)])
        P.indirect_dma_start(out=xs[q % 4].rearrange("p a b -> p (a b)"), out_offset=None, in_=xs_p,
                             in_offset=bass.IndirectOffsetOnAxis(sidx[:, j:j + 1], 0),
                             bounds_check=sreg, oob_is_err=False).then_inc(dXs[q % 4].sem, 16)
        dXs[q % 4].cnt += 16
        K._commit((dXs[q % 4].sem, dXs[q % 4].cnt), [R("sidx"), R("xs_d")], [R("xs", q % 4)])

    def stM2(q, j):
        s3, s2 = q % 4, q % 2

        def tr():
            ins = None
            for s_ in range(2):
                for kc in range(8):
                    ins = G.transpose(out=bankb(s_)[:, kc * 128:(kc + 1) * 128], in_=xs[s3][:, s_, kc * 128:(kc + 1) * 128],
                                      identity=ident)
            return ins
        K.op(K.pe, tr, reads=[R("xs", s3), R("ident")], writes=[R("ps", 0), R("ps", 1)])
        K.op(K.act, lambda: A.copy(out=xT[s2][:, :, 0:128], in_=bankb(0).rearrange("p (a b) -> p a b", a=8)),
             reads=[R("ps", 0)], writes=[R("xT", s2)])
        K.op(K.dve, lambda: V.tensor_copy(out=xT[s2][:, :, 128:256], in_=bankb(1).rearrange("p (a b) -> p a b", a=8)),
             reads=[R("ps", 1)], writes=[R("xT", s2)])

    def stM3(q, j):
        s4, s2 = q % 4, q % 2

        def mm():
            ins = None
            for (wb, b) in ((wgb[s4], 2), (wub[s4], 3)):
                for fc in range(2):
                    for kc in range(8):
                        ins = G.matmul(bank(b)[:, fc * 256:(fc + 1) * 256], lhsT=wb[:, kc, fc * 128:(fc + 1) * 128],
                                       rhs=xT[s2][:, kc, :], start=(kc == 0), stop=(kc == 7))
            return ins
        K.op(K.pe, mm, reads=[R("wall", s4), R("xT", s2)], writes=[R("ps", 2), R("ps", 3)])
        K.op(K.act, lambda: A.activation(out=ssb[s2], in_=bank(2), func=AF.Silu), reads=[R("ps", 2)], writes=[R("ssb", s2)])
        K.op(K.dve, lambda: V.tensor_tensor(out=hid[s2].rearrange("p a b -> p (a b)"), in0=ssb[s2], in1=bank(3), op=ALU.mult),
             reads=[R("ssb", s2), R("ps", 3)], writes=[R("hid", s2)])

    def stM4(q, j):
        s4, s2 = q % 4, q % 2
        for sh in range(2):
            b0 = 4 + 2 * sh

            def mm(sh=sh, b0=b0):
                ins = None
                for dh in range(2):
                    for fc in range(2):
                        ins = G.matmul(bank(b0 + dh), lhsT=hid[s2][:, fc, sh * 128:(sh + 1) * 128],
                                       rhs=wdb[s4][:, fc, dh * 512:(dh + 1) * 512], start=(fc == 0), stop=(fc == 1))
                return ins
            K.op(K.pe, mm, reads=[R("hid", s2), R("wall", s4)], writes=[R("ps", b0), R("ps", b0 + 1)])
            K.op(K.dve, lambda sh=sh, b0=b0: V.tensor_tensor(out=ysb[s2][:, sh, :], in0=bank(b0, 2), in1=gt2B, op=ALU.mult),
                 reads=[R("ps", b0), R("ps", b0 + 1), R("gt2B")], writes=[R("ysb", s2)])

    def stM4b(q, j):
        s2 = q % 2
        K._waits(K.pool, [R("sidx"), R("ysb", s2)], [R("ys_d", j)])
        P.indirect_dma_start(out=ys_p, out_offset=bass.IndirectOffsetOnAxis(sidx[:, j:j + 1], 0),
                             in_=ysb[s2].rearrange("p a b -> p (a b)"), in_offset=None, bounds_check=sreg,
                             oob_is_err=False).then_inc(dYs[s2].sem, 16)
        dYs[s2].cnt += 16
        K._commit((dYs[s2].sem, dYs[s2].cnt), [R("sidx"), R("ysb", s2)], [R("ys_d", j)])

    stM1(0, order[0])
    stM1(1, order[1])
    for i in range(NTL + 3):
        if 0 <= i - 3 < NTL:
            stM4(i - 3, order[i - 3])
            if i + 1 < NTL:
                gather_w(i + 1, order[i + 1])
        if i + 2 < NTL:
            stM1(i + 2, order[i + 2])
        if 0 <= i - 1 < NTL:
            stM2(i - 1, order[i - 1])
        if 0 <= i - 2 < NTL:
            stM3(i - 2, order[i - 2])
        if 0 <= i - 3 < NTL:
            stM4b(i - 3, order[i - 3])

    dO = [K.dsem("o%d" % i) for i in range(2)]
    dYg = [K.dsem("yg%d" % i) for i in range(2)]
    y_t = y_d.rearrange("(t p) d -> t p d", p=128)
    for t in range(NO):
        sl = t % 2
        ysr = [R("ys_d", j) for j in range(NTL)]
        K._waits(K.pool, ysr + [R("posu")], [R("yg", sl)])
        for k in range(2):
            P.indirect_dma_start(out=yg[sl][:, k, :], out_offset=None, in_=ys_d[:, :],
                                 in_offset=bass.IndirectOffsetOnAxis(posu[:, k, t:t + 1], 0)).then_inc(dYg[sl].sem, 16)
            dYg[sl].cnt += 16
        K._commit((dYg[sl].sem, dYg[sl].cnt), [R("posu")], [R("yg", sl)])
        for k in range(2):
            K.op(K.dve, lambda t=t, k=k, sl=sl: V.scalar_tensor_tensor(out=acc[:, t, :], in0=yg[sl][:, k, :],
                                                                       scalar=w12[:, k, t:t + 1], in1=acc[:, t, :],
                                                                       op0=ALU.mult, op1=ALU.add),
                 reads=[R("yg", sl), R("w12"), R("acc", t)], writes=[R("acc", t)])
        K.op(K.act, lambda t=t: A.activation(out=junk, in_=acc[:, t, :], func=AF.Square, accum_out=st[:, t:t + 1]),
             reads=[R("acc", t), R("st")], writes=[R("junk"), R("stx", t)])
        rstd_cols(st[:, t:t + 1], st[:, 32 + t:33 + t], 1.0 / D, [R("stx", t)], [R("strx", t)])
        K.op(K.dve, lambda t=t, sl=sl: V.scalar_tensor_tensor(out=yt[sl], in0=acc[:, t, :], scalar=st[:, 32 + t:33 + t], in1=gfB,
                                                              op0=ALU.mult, op1=ALU.mult),
             reads=[R("acc", t), R("strx", t), R("gfB")], writes=[R("yt", sl)])
        K.dma(K.sp, [(y_t[t], yt[sl])], dO[sl], reads=[R("yt", sl)])
    for d in dO:
        K.sp.h.wait_ge(d.sem, d.cnt)
    es.close()
    return nc


_ROPE_THETA = 10000.0


def _rope_table():
    out = np.zeros((S, 192), np.float32)
    pos = np.arange(S, dtype=np.float32)[:, None]
    for (dim, o) in ((64, 0), (32, 128)):
        inv = (1.0 / (_ROPE_THETA ** (np.arange(0, dim, 2, dtype=np.float32) / dim))).astype(np.float32)
        ang = (pos * inv[None, :]).astype(np.float32)
        c, s_ = np.cos(ang).astype(np.float32), np.sin(ang).astype(np.float32)
        out[:, o:o + dim] = np.concatenate([c, c], axis=1)
        out[:, o + dim:o + 2 * dim] = np.concatenate([-s_, s_], axis=1)
    return out


_NC_CACHE = {}


def kernel(x, c, w_ada, b_ada, g_norm1, w_in, g_q_lora, w_uq, g_kv_lora, w_ukv, sink,
           g_out_swa, g_out_mla, w_out, g_norm2, w_router_group, b_router_group,
           w_router_expert, b_router_expert, w_exp_gate, w_exp_up, w_exp_down, g_final):
    f = lambda a: np.ascontiguousarray(np.asarray(a, dtype=np.float32))
    x = f(x); c = f(c)
    if "nc" not in _NC_CACHE:
        _NC_CACHE["nc"] = build_program()
    nc = _NC_CACHE["nc"]
    rope = _rope_table()
    w_in0 = f(w_in)[0]
    perm = np.concatenate([np.arange(0, 768), np.arange(960, 1120), np.arange(768, 960)])
    w_in_p = np.ascontiguousarray(w_in0[:, perm])
    gB = np.ascontiguousarray(np.broadcast_to(
        np.concatenate([f(g_norm1)[0], f(g_norm2)[0], f(g_final)])[None, :], (128, 3 * D)))
    w_r = np.ascontiguousarray(np.concatenate([f(w_router_group)[0], f(w_router_expert)[0]], axis=1))
    b_r = np.concatenate([f(b_router_group)[0], f(b_router_expert)[0]])
    gcat = np.concatenate([f(g_out_swa)[0], f(g_out_mla)[0]])
    jj = np.arange(128)[:, None]
    rr = np.arange(128)[None, :]
    mprev = (jj >= rr).astype(np.float32)
    mnext = (jj <= rr).astype(np.float32)
    shared = {
        "w_ada": f(w_ada)[0], "b_ada": f(b_ada), "w_in": w_in_p, "w_uq": f(w_uq)[0], "w_ukv": f(w_ukv)[0],
        "w_out": f(w_out)[0], "w_r": w_r, "w_g": np.ascontiguousarray(f(w_exp_gate)[0].reshape(NE, 8, 128, 256).transpose(0, 2, 1, 3).reshape(NE * 128, 2048)),
        "w_u": np.ascontiguousarray(f(w_exp_up)[0].reshape(NE, 8, 128, 256).transpose(0, 2, 1, 3).reshape(NE * 128, 2048)),
        "w_d": np.ascontiguousarray(f(w_exp_down)[0].reshape(NE, 2, 128, D).transpose(0, 2, 1, 3).reshape(NE * 128, 2048)),
        "gB": gB,
    }
    in_maps = []
    for core in range(8):
        b, hf = core // 2, core % 2
        own = slice(hf * 2048, (hf + 1) * 2048)
        oth = slice((1 - hf) * 2048, (2 - hf) * 2048)
        prm = np.zeros((128, 64), np.float32)
        prm[:, 0:8] = c[b].reshape(8, 128).T
        prm[0:96, 8:10] = f(g_q_lora)[0].reshape(2, 96).T
        prm[:, 10] = f(g_kv_lora)[0]
        prm[:, 11:19] = gcat.reshape(8, 128).T
        prm[:, 20:28] = f(sink)[0][None, :]
        prm[:, 28:64] = b_r[None, :]
        msk = np.stack([mprev, mnext, mprev * float(hf == 1), mnext * float(hf == 0)], axis=1).reshape(128, 512)
        m = dict(shared)
        m["x"] = np.ascontiguousarray(np.concatenate([x[b, own], x[b, oth]], axis=0))
        m["rope"] = np.ascontiguousarray(np.concatenate([rope[own], rope[oth]], axis=0))
        m["prm"] = prm
        m["g1p"] = np.ascontiguousarray(f(g_norm1)[0].reshape(8, 128).T)
        m["msk"] = np.ascontiguousarray(msk.astype(np.float32))
        in_maps.append(m)
    res = run_bass_kernel_spmd(nc, in_maps, core_ids=list(range(8)))
    out = np.zeros((4, S, D), np.float32)
    for core in range(8):
        b, hf = core // 2, core % 2
        out[b, hf * 2048:(hf + 1) * 2048] = res.results[core]["y"]
    return out
```
